# Optimizing a Trainium2 kernel written in Bass

```python
import math
import jax
import jax.numpy as jnp
from jax import lax
import numpy as np

D_MODEL = 1024
BATCH = 16
SEQ = 2048
DEPTH = 1

CHUNK = 64
Q_BLOCK = 128
N_MEM = 256
EPS = 1e-5
NEG_INF = -1e30

D_MIX = D_MODEL
MLA_HEADS = 8
MLA_NOPE = 64
MLA_ROPE = 32
MLA_V = 64
MLA_Q_RANK = 256
MLA_KV_RANK = 128
ROPE_THETA = 10000.0

SSD_HEADS = 8
SSD_HEADDIM = 64
SSD_INNER = SSD_HEADS * SSD_HEADDIM
SSD_GROUPS = 2
SSD_STATE = 128
SSD_CONV = 4
SSD_CONV_DIM = SSD_INNER + 2 * SSD_GROUPS * SSD_STATE
DT_MIN = 0.001
DT_MAX = 0.1

IN_COLS = MLA_Q_RANK + MLA_KV_RANK + MLA_ROPE + SSD_INNER + SSD_CONV_DIM + SSD_HEADS

XA_HEADS = 4
XA_HEAD_DIM = D_MODEL // XA_HEADS

N_EXPERT_GROUPS = 4
EXPERTS_PER_GROUP = 8
N_EXPERTS = N_EXPERT_GROUPS * EXPERTS_PER_GROUP
TOP_K = 2
D_EXPERT = 256

DEEPNORM_ALPHA = (2.0 * DEPTH) ** 0.25
DEEPNORM_BETA = (8.0 * DEPTH) ** -0.25

kernel_name = "hybrid_mla_ssd_hmoe_deepnorm"


def layer_norm(x, g, b):
    xf = x.astype(jnp.float32)
    mu = jnp.mean(xf, axis=-1, keepdims=True)
    var = jnp.mean(jnp.square(xf - mu), axis=-1, keepdims=True)
    return ((xf - mu) * lax.rsqrt(var + EPS) * g + b).astype(x.dtype)


def rms_norm(x, g):
    xf = x.astype(jnp.float32)
    return (xf * lax.rsqrt(jnp.mean(jnp.square(xf), axis=-1, keepdims=True) + EPS) * g).astype(x.dtype)


def rotary_tables(seq, dim, dtype):
    pos = jnp.arange(seq, dtype=jnp.float32)
    inv_freq = ROPE_THETA ** (-jnp.arange(0, dim, 2, dtype=jnp.float32) / dim)
    ang = pos[:, None] * inv_freq[None, :]
    return jnp.cos(ang).astype(dtype), jnp.sin(ang).astype(dtype)


def apply_rope(u, cos, sin):
    u1, u2 = jnp.split(u, 2, axis=-1)
    return jnp.concatenate([u1 * cos - u2 * sin, u2 * cos + u1 * sin], axis=-1)


def chunk_causal_attention(q, k, v):
    b, s, h, dqk = q.shape
    nb = s // Q_BLOCK
    scale = dqk ** -0.5
    q_blocks = q.reshape(b, nb, Q_BLOCK, h, dqk).transpose(1, 0, 2, 3, 4)
    k_chunk = jnp.arange(s) // CHUNK

    def one_block(args):
        q_blk, blk = args
        q_chunk = (blk * Q_BLOCK + jnp.arange(Q_BLOCK)) // CHUNK
        scores = jnp.einsum("bqhd,bkhd->bhqk", q_blk, k).astype(jnp.float32) * scale
        allowed = k_chunk[None, :] <= q_chunk[:, None]
        scores = jnp.where(allowed, scores, NEG_INF)
        p = jax.nn.softmax(scores, axis=-1).astype(v.dtype)
        return jnp.einsum("bhqk,bkhd->bqhd", p, v)

    out = lax.map(one_block, (q_blocks, jnp.arange(nb)))
    return out.transpose(1, 0, 2, 3, 4).reshape(b, s, h, v.shape[-1])


def causal_depthwise_conv(u, w, bias):
    out = lax.conv_general_dilated(
        u, w[:, None, :], window_strides=(1,), padding=((SSD_CONV - 1, 0),),
        dimension_numbers=("NWC", "WIO", "NWC"), feature_group_count=u.shape[-1])
    return out + bias


def ssd_chunked_scan(xh, dt, a, bm, cm):
    b, s, h, p = xh.shape
    g, n = bm.shape[2], bm.shape[3]
    r = h // g
    nc = s // CHUNK
    x = (xh * dt[..., None]).reshape(b, nc, CHUNK, g, r, p)
    adt = (dt * a).reshape(b, nc, CHUNK, g, r)
    bm = bm.reshape(b, nc, CHUNK, g, n)
    cm = cm.reshape(b, nc, CHUNK, g, n)
    acs = jnp.cumsum(adt, axis=2)
    acs_t = acs.transpose(0, 1, 3, 4, 2)
    seg = acs_t[..., :, None] - acs_t[..., None, :]
    causal = jnp.tril(jnp.ones((CHUNK, CHUNK), dtype=bool))
    decay_in = jnp.exp(jnp.where(causal, seg, -jnp.inf))
    cb = jnp.einsum("bclgn,bcsgn->bcgls", cm, bm)
    y_diag = jnp.einsum("bcgls,bcgrls,bcsgrp->bclgrp", cb, decay_in, x)
    decay_to_end = jnp.exp(acs[:, :, -1:] - acs)
    states = jnp.einsum("bclgn,bclgr,bclgrp->bcgrpn", bm, decay_to_end, x)
    chunk_decay = jnp.exp(acs[:, :, -1])

    def step(state, inp):
        s_c, d_c = inp
        return d_c[..., None, None] * state + s_c, state

    h0 = jnp.zeros((b, g, r, p, n), x.dtype)
    _, prev = lax.scan(step, h0, (states.transpose(1, 0, 2, 3, 4, 5),
                                  chunk_decay.transpose(1, 0, 2, 3)))
    prev = prev.transpose(1, 0, 2, 3, 4, 5)
    y_off = jnp.einsum("bclgn,bcgrpn,bclgr->bclgrp", cm, prev, jnp.exp(acs))
    return (y_diag + y_off).reshape(b, s, h, p)


def hybrid_mixer(x, cos, sin, w_in, mla_q_norm, w_q_up, mla_kv_norm, w_kv_up,
                 ssd_conv_w, ssd_conv_b, ssd_dt_bias, ssd_a_log, ssd_d, ssd_norm, w_out):
    b, s, _ = x.shape
    proj = x @ w_in
    o1 = MLA_Q_RANK
    o2 = o1 + MLA_KV_RANK
    o3 = o2 + MLA_ROPE
    o4 = o3 + SSD_INNER
    o5 = o4 + SSD_CONV_DIM
    c_q, c_kv, k_rope, z, xbc, dt_raw = jnp.split(proj, [o1, o2, o3, o4, o5], axis=-1)

    q = (rms_norm(c_q, mla_q_norm) @ w_q_up).reshape(b, s, MLA_HEADS, MLA_NOPE + MLA_ROPE)
    q_nope, q_pe = q[..., :MLA_NOPE], q[..., MLA_NOPE:]
    kv = (rms_norm(c_kv, mla_kv_norm) @ w_kv_up).reshape(b, s, MLA_HEADS, MLA_NOPE + MLA_V)
    k_nope, v = kv[..., :MLA_NOPE], kv[..., MLA_NOPE:]
    q_pe = apply_rope(q_pe, cos[None, :, None, :], sin[None, :, None, :])
    k_pe = apply_rope(k_rope, cos[None], sin[None])
    q_full = jnp.concatenate([q_nope, q_pe], axis=-1)
    k_full = jnp.concatenate(
        [k_nope, jnp.broadcast_to(k_pe[:, :, None, :], (b, s, MLA_HEADS, MLA_ROPE))], axis=-1)
    attn = chunk_causal_attention(q_full, k_full, v).reshape(b, s, MLA_HEADS * MLA_V)

    xbc = jax.nn.silu(causal_depthwise_conv(xbc, ssd_conv_w, ssd_conv_b))
    xs, bm, cm = jnp.split(xbc, [SSD_INNER, SSD_INNER + SSD_GROUPS * SSD_STATE], axis=-1)
    xh = xs.reshape(b, s, SSD_HEADS, SSD_HEADDIM).astype(jnp.float32)
    dt = jax.nn.softplus(dt_raw.astype(jnp.float32) + ssd_dt_bias)
    a = -jnp.exp(ssd_a_log.astype(jnp.float32))
    y = ssd_chunked_scan(xh, dt, a,
                         bm.reshape(b, s, SSD_GROUPS, SSD_STATE).astype(jnp.float32),
                         cm.reshape(b, s, SSD_GROUPS, SSD_STATE).astype(jnp.float32))
    y = y + ssd_d.astype(jnp.float32)[None, None, :, None] * xh
    yz = (y.reshape(b, s, SSD_INNER) * jax.nn.silu(z.astype(jnp.float32)))
    yz = yz.reshape(b, s, SSD_GROUPS, SSD_INNER // SSD_GROUPS)
    yz = yz * lax.rsqrt(jnp.mean(jnp.square(yz), axis=-1, keepdims=True) + EPS)
    ssd_out = (yz.reshape(b, s, SSD_INNER) * ssd_norm).astype(x.dtype)

    return jnp.concatenate([attn, ssd_out], axis=-1) @ w_out


def memory_cross_attention(x, mem, wq, wk, wv, wo):
    b, s, _ = x.shape
    m = mem.shape[1]
    q = (x @ wq).reshape(b, s, XA_HEADS, XA_HEAD_DIM)
    k = (mem @ wk).reshape(b, m, XA_HEADS, XA_HEAD_DIM)
    v = (mem @ wv).reshape(b, m, XA_HEADS, XA_HEAD_DIM)
    scores = jnp.einsum("bshd,bmhd->bhsm", q, k).astype(jnp.float32) * (XA_HEAD_DIM ** -0.5)
    p = jax.nn.softmax(scores, axis=-1).astype(x.dtype)
    o = jnp.einsum("bhsm,bmhd->bshd", p, v).reshape(b, s, D_MODEL)
    return o @ wo


def hierarchical_moe(x, group_w, group_b, expert_w, expert_b, w_gate, w_up, w_down):
    b, s, d = x.shape
    t = x.reshape(b * s, d)
    n_tok = t.shape[0]
    group_logits = (t @ group_w).astype(jnp.float32) + group_b
    group_probs = jax.nn.softmax(group_logits, axis=-1)
    g_idx = jnp.argmax(group_logits, axis=-1)
    g_gate = jnp.take_along_axis(group_probs, g_idx[:, None], axis=-1)
    e_logits = ((t @ expert_w).astype(jnp.float32) + expert_b).reshape(
        n_tok, N_EXPERT_GROUPS, EXPERTS_PER_GROUP)
    in_group = jnp.take_along_axis(e_logits, g_idx[:, None, None], axis=1)[:, 0]
    top_vals, top_idx = lax.top_k(in_group, TOP_K)
    gates = jax.nn.softmax(top_vals, axis=-1) * g_gate
    expert_id = g_idx[:, None] * EXPERTS_PER_GROUP + top_idx
    combine = jnp.sum(jax.nn.one_hot(expert_id, N_EXPERTS, dtype=jnp.float32)
                      * gates[..., None], axis=1).astype(t.dtype)
    acc = jnp.zeros_like(t)
    for e in range(N_EXPERTS):
        hdn = jax.nn.silu(t @ w_gate[e]) * (t @ w_up[e])
        acc = acc + (hdn @ w_down[e]) * combine[:, e:e + 1]
    return acc.reshape(b, s, d)


def setup_inputs(seed: int = 0) -> dict:
    key = jax.random.key(seed)
    ks = jax.random.split(key, 40)
    L = DEPTH

    def nrm(k, shape, fan_in, scale=1.0):
        return jax.random.normal(k, shape, jnp.float32) * (scale * fan_in ** -0.5)

    def gain(k, n):
        return 1.0 + 0.02 * jax.random.normal(k, (L, n), jnp.float32)

    def small(k, shape):
        return 0.01 * jax.random.normal(k, shape, jnp.float32)

    dt0 = jnp.exp(jax.random.uniform(ks[9], (L, SSD_HEADS), jnp.float32,
                                     minval=math.log(DT_MIN), maxval=math.log(DT_MAX)))
    return {
        "x": jax.random.normal(ks[0], (BATCH, SEQ, D_MODEL), jnp.float32),
        "mem": jax.random.normal(ks[1], (BATCH, N_MEM, D_MODEL), jnp.float32),
        "w_in": nrm(ks[2], (L, D_MODEL, IN_COLS), D_MODEL),
        "mla_q_norm": gain(ks[3], MLA_Q_RANK),
        "w_q_up": nrm(ks[4], (L, MLA_Q_RANK, MLA_HEADS * (MLA_NOPE + MLA_ROPE)), MLA_Q_RANK),
        "mla_kv_norm": gain(ks[5], MLA_KV_RANK),
        "w_kv_up": nrm(ks[6], (L, MLA_KV_RANK, MLA_HEADS * (MLA_NOPE + MLA_V)), MLA_KV_RANK),
        "ssd_conv_w": nrm(ks[7], (L, SSD_CONV, SSD_CONV_DIM), SSD_CONV),
        "ssd_conv_b": small(ks[8], (L, SSD_CONV_DIM)),
        "ssd_dt_bias": dt0 + jnp.log(-jnp.expm1(-dt0)),
        "ssd_a_log": jnp.log(jax.random.uniform(ks[10], (L, SSD_HEADS), jnp.float32,
                                                minval=1.0, maxval=16.0)),
        "ssd_d": 1.0 + 0.1 * jax.random.normal(ks[11], (L, SSD_HEADS), jnp.float32),
        "ssd_norm": gain(ks[12], SSD_INNER),
        "w_out": nrm(ks[13], (L, D_MIX, D_MODEL), D_MIX, DEEPNORM_BETA),
        "ln1_g": gain(ks[14], D_MODEL),
        "ln1_b": small(ks[15], (L, D_MODEL)),
        "xa_wq": nrm(ks[16], (L, D_MODEL, D_MODEL), D_MODEL),
        "xa_wk": nrm(ks[17], (L, D_MODEL, D_MODEL), D_MODEL),
        "xa_wv": nrm(ks[18], (L, D_MODEL, D_MODEL), D_MODEL, DEEPNORM_BETA),
        "xa_wo": nrm(ks[19], (L, D_MODEL, D_MODEL), D_MODEL, DEEPNORM_BETA),
        "ln2_g": gain(ks[20], D_MODEL),
        "ln2_b": small(ks[21], (L, D_MODEL)),
        "router_group_w": nrm(ks[22], (L, D_MODEL, N_EXPERT_GROUPS), D_MODEL),
        "router_group_b": small(ks[23], (L, N_EXPERT_GROUPS)),
        "router_expert_w": nrm(ks[24], (L, D_MODEL, N_EXPERTS), D_MODEL),
        "router_expert_b": small(ks[25], (L, N_EXPERTS)),
        "expert_w_gate": nrm(ks[26], (L, N_EXPERTS, D_MODEL, D_EXPERT), D_MODEL),
        "expert_w_up": nrm(ks[27], (L, N_EXPERTS, D_MODEL, D_EXPERT), D_MODEL),
        "expert_w_down": nrm(ks[28], (L, N_EXPERTS, D_EXPERT, D_MODEL), D_EXPERT, DEEPNORM_BETA),
        "ln3_g": gain(ks[29], D_MODEL),
        "ln3_b": small(ks[30], (L, D_MODEL)),
    }


def reference(x, mem, w_in, mla_q_norm, w_q_up, mla_kv_norm, w_kv_up,
              ssd_conv_w, ssd_conv_b, ssd_dt_bias, ssd_a_log, ssd_d, ssd_norm, w_out,
              ln1_g, ln1_b, xa_wq, xa_wk, xa_wv, xa_wo, ln2_g, ln2_b,
              router_group_w, router_group_b, router_expert_w, router_expert_b,
              expert_w_gate, expert_w_up, expert_w_down, ln3_g, ln3_b):
    cos, sin = rotary_tables(x.shape[1], MLA_ROPE, x.dtype)
    h = x
    for l in range(DEPTH):
        mix = hybrid_mixer(h, cos, sin, w_in[l], mla_q_norm[l], w_q_up[l], mla_kv_norm[l],
                           w_kv_up[l], ssd_conv_w[l], ssd_conv_b[l], ssd_dt_bias[l],
                           ssd_a_log[l], ssd_d[l], ssd_norm[l], w_out[l])
        h = layer_norm(DEEPNORM_ALPHA * h + mix, ln1_g[l], ln1_b[l])
        xa = memory_cross_attention(h, mem, xa_wq[l], xa_wk[l], xa_wv[l], xa_wo[l])
        h = layer_norm(DEEPNORM_ALPHA * h + xa, ln2_g[l], ln2_b[l])
        ffn = hierarchical_moe(h, router_group_w[l], router_group_b[l], router_expert_w[l],
                               router_expert_b[l], expert_w_gate[l], expert_w_up[l],
                               expert_w_down[l])
        h = layer_norm(DEEPNORM_ALPHA * h + ffn, ln3_g[l], ln3_b[l])
    return h
```

```python
import numpy as np
from contextlib import ExitStack
import concourse.bass as bass
import concourse.mybir as mybir
from concourse.bass_utils import run_bass_kernel_spmd

F32 = mybir.dt.float32
BF16 = mybir.dt.bfloat16
AF = mybir.ActivationFunctionType
ALU = mybir.AluOpType

N_CORES = 8
SEQ = 2048
D = 1024
NT = SEQ // 128
NG = SEQ // 512
ALPHA = 2.0 ** 0.25
EPS = 1e-5
NEXP = 32


class T:
    __slots__ = ("w", "r")

    def __init__(self):
        self.w = None
        self.r = {}


class Sched:
    def __init__(self, nc, es, n_dma=80):
        self.nc = nc
        self.eng = {"pe": nc.tensor, "act": nc.scalar, "dve": nc.vector, "pool": nc.gpsimd, "sp": nc.sync}
        self.sem = {k: es.enter_context(nc.semaphore("s_" + k)) for k in ("pe", "act", "dve", "pool")}
        self.cnt = {k: 0 for k in self.sem}
        self.dsem = [es.enter_context(nc.semaphore("d%d" % i)) for i in range(n_dma)]
        self.dcnt = [0] * n_dma
        n_cast = 16
        self.qslots = {"sp": list(range(0, (n_dma - n_cast) // 2)), "pool": list(range((n_dma - n_cast) // 2, n_dma - n_cast)),
                       "cast": list(range(n_dma - n_cast, n_dma))}
        self.qnext = {"sp": 0, "pool": 0, "cast": 0}
        self.seen = {}
        self.bregs = {}

    def _semof(self, key):
        return self.sem[key] if isinstance(key, str) else self.dsem[key]

    def _need(self, eng, reads, writes):
        need = {}

        def add(k, v):
            if k == eng:
                if eng == "pe":
                    return
                if self.cnt[eng] - v >= 4:
                    return
            if self.seen.get((eng, k), 0) >= v:
                return
            if need.get(k, 0) < v:
                need[k] = v

        for t in reads:
            if t.w is not None:
                add(*t.w)
        for t in writes:
            if t.w is not None:
                add(*t.w)
            for k, v in t.r.items():
                add(k, v)
        return need

    def _wait(self, eng, key, val):
        if self.seen.get((eng, key), 0) >= val:
            return
        self.eng[eng].wait_ge(self._semof(key), val)
        self.seen[(eng, key)] = val

    def _waits(self, eng, reads, writes):
        for k, v in self._need(eng, reads, writes).items():
            self._wait(eng, k, v)

    def _commit(self, ticket, reads, writes):
        k, v = ticket
        for t in reads:
            if t.r.get(k, 0) < v:
                t.r[k] = v
        for t in writes:
            t.w = ticket
            t.r = {}

    def op(self, eng, reads, writes, emit):
        need = self._need(eng, reads, writes)
        keys = list(need)
        attach = keys[-1] if keys else None
        for k in keys[:-1]:
            self._wait(eng, k, need[k])
        r = emit()
        first, last = (r[0], r[-1]) if isinstance(r, list) else (r, r)
        if attach is not None:
            first._wait_ge(self._semof(attach), need[attach])
            self.seen[(eng, attach)] = need[attach]
        self.cnt[eng] += 1
        last.then_inc(self.sem[eng], 1)
        self._commit((eng, self.cnt[eng]), reads, writes)

    def dma(self, q, out, in_, reads=(), writes=(), out_off=None, in_off=None, bound=None, slots=None):
        self._waits(q, reads, writes)
        sp_ = slots or q
        sl = self.qslots[sp_]
        slot = sl[self.qnext[sp_]]
        self.qnext[sp_] = (self.qnext[sp_] + 1) % len(sl)
        if self.dcnt[slot] > 0:
            self._wait(q, slot, self.dcnt[slot])
        if out_off is None and in_off is None:
            inst = self.eng[q].dma_start(out=out, in_=in_)
        else:
            if bound not in self.bregs:
                self.bregs[bound] = self.nc.gpsimd.to_reg(bound)
            bound = self.bregs[bound]
            inst = self.nc.gpsimd.indirect_dma_start(
                out=out, out_offset=None if out_off is None else bass.IndirectOffsetOnAxis(ap=out_off, axis=0),
                in_=in_, in_offset=None if in_off is None else bass.IndirectOffsetOnAxis(ap=in_off, axis=0),
                bounds_check=bound, oob_is_err=False)
        inst.then_inc(self.dsem[slot], 16)
        self.dcnt[slot] += 16
        self._commit((slot, self.dcnt[slot]), reads, writes)

    def barrier(self, engines=("pe", "act", "dve", "pool", "sp"), full=False):
        skip = () if full else set(self.qslots["cast"])
        for e in engines:
            for k in self.sem:
                if self.cnt[k] > 0:
                    self._wait(e, k, self.cnt[k])
            for s in range(len(self.dsem)):
                if self.dcnt[s] > 0 and s not in skip:
                    self._wait(e, s, self.dcnt[s])


def v3(ap, b):
    return ap.rearrange("p (a b) -> p a b", b=b)


def build(n_seq=2, stop_after=None, dumps=()):
    nc = bass.Bass("TRN2", target_bir_lowering=False)

    def din(name, shape):
        return nc.dram_tensor(name, list(shape), F32, kind="ExternalInput").ap()

    x = din("x", [n_seq, SEQ, D])
    mem = din("mem", [n_seq, 256, D])
    w_in = din("w_in", [D, 1960])
    wkr_sw = din("wkr_sw", [D, 96])
    wq_d = din("wq", [256, 768])
    wqsw_d = din("wq_sw", [256, 768])
    qn_d = din("qn", [128, 2])
    wkv_d = din("wkv", [128, 1024])
    kvn_d = din("kvn", [128, 1])
    convw_d = din("convw", [128, 32])
    convb_d = din("convb", [128, 8])
    dtb_d = din("dtb", [128, 128])
    alog_d = din("alog", [128, 8])
    dcol_d = din("dcol", [128, 4])
    ncol_d = din("ncol", [128, 4])
    wout_d = din("wout", [D, D])
    xwq_d = din("xwq", [D, D])
    xwk_d = din("xwk", [D, D])
    xwv_d = din("xwv", [D, D])
    xwo_d = din("xwo", [D, D])
    lnp_d = din("lnp", [6, 128, D])
    rw_d = din("rw", [D, 36])
    rb_d = din("rb", [128, 36])
    wg_d = din("wg", [NEXP, D, 256])
    wu_d = din("wu", [NEXP, D, 256])
    wd_d = din("wd", [NEXP, 256, D])
    ident_d = din("ident", [128, 128])
    tri_d = din("tri", [128, 128])
    negmask_d = din("negmask", [128, 512])
    cc_d = din("cc", [128, SEQ])
    ss_d = din("ss", [128, SEQ])
    su_d = din("su", [128, 128])
    thr_d = din("thr", [128, 32])
    pcol_d = din("pcol", [128, 1])
    GT = n_seq * NT
    NTOK = n_seq * SEQ
    TB = 3
    TSZ = 128 * TB
    NTI = -(-(2 * NTOK) // TSZ) + 31
    tstart_d = din("tstart", [128, 64])
    out_d = nc.dram_tensor("out", [n_seq, SEQ, D], F32, kind="ExternalOutput").ap()
    XB = nc.dram_tensor("XB", [NTOK, D], BF16, kind="Internal").ap()
    H2D = nc.dram_tensor("H2D", [NTOK, D], F32, kind="Internal").ap()
    XS = nc.dram_tensor("XS", [NTI * TSZ, D], BF16, kind="Internal").ap()
    YS = nc.dram_tensor("YS", [NTI * TSZ, D], BF16, kind="Internal").ap()
    DWB = {n: nc.dram_tensor("DWB_" + n, [D, D], BF16, kind="Internal").ap() for n in ("wout", "xwq", "xwk", "xwv", "xwo")}
    WGB = nc.dram_tensor("WGB", [NEXP * 128, 2048], BF16, kind="Internal").ap()
    WUB = nc.dram_tensor("WUB", [NEXP * 128, 2048], BF16, kind="Internal").ap()
    WDB = nc.dram_tensor("WDB", [NEXP * 128, 2048], BF16, kind="Internal").ap()
    dump_d = {}
    for name, shape in dumps:
        dump_d[name] = nc.dram_tensor("dbg_" + name, list(shape), F32, kind="ExternalOutput").ap()

    es = ExitStack()
    with es:
        S = Sched(nc, es)

        def sb(name, shape, dt):
            return es.enter_context(nc.sbuf_tensor(name, list(shape), dt))

        ident_bf = sb("ident_bf", [128, 128], BF16)
        ones_bf = sb("ones_bf", [128, 128], BF16)
        ones_f = sb("ones_f", [128, 128], F32)
        tri_f = sb("tri_f", [128, 128], F32)
        ident_f = sb("ident_f", [128, 128], F32)
        negmask = sb("negmask_s", [128, 512], F32)
        cc = sb("cc_s", [128, SEQ], BF16)
        ss = sb("ss_s", [128, SEQ], BF16)
        lng = sb("lng", [128, D], F32)
        lnb = sb("lnb", [128, D], F32)
        qn = sb("qn_s", [128, 2], F32)
        kvn = sb("kvn_s", [128, 1], F32)
        convw = sb("convw_s", [128, 32], F32)
        convb = sb("convb_s", [128, 8], F32)
        dtb = sb("dtb_s", [128, 128], F32)
        a_bc = sb("a_bc", [128, 8], F32)
        dcol = sb("dcol_s", [128, 4], F32)
        ncol = sb("ncol_s", [128, 4], F32)
        rb = sb("rb_s", [128, 36], F32)
        rw = sb("rw_s", [128, 8 * 36], BF16)
        small = sb("small", [128, 512], F32)
        su_bf = sb("su_bf", [128, 128], BF16)
        thr = sb("thr_s", [128, 32], F32)
        pcol = sb("pcol_s", [128, 1], F32)
        tstart = sb("tstart_s", [128, 64], F32)
        ONE = sb("ONE", [128, GT * 64], BF16)
        RK = sb("RK", [128, GT * 2], F32)
        G12 = sb("G12", [128, GT * 2], F32)
        Crun = sb("Crun", [128, 32], F32)
        mo_f = sb("mo_f", [128, 128], F32)
        mo_i = sb("mo_i", [128, 128], mybir.dt.int32)
        tRk = T()
        rdx = cc[:].bitcast(F32)
        dt_tok = sb("dt_tok", [128, 128], F32)
        tC = T()
        tLN = T()
        tSmall = T()

        A = sb("arenaA", [128, 16384], BF16)
        H = sb("arenaH", [128, 16384], F32)
        K = sb("arenaK", [128, 16384], BF16)
        W = sb("arenaW", [128, 24576], BF16)
        lnst = sb("lnst", [128, 64], F32)
        tLNS = [T() for _ in range(4)]
        lnstate = {"i": 0}
        PS = [es.enter_context(nc.psum_tensor("ps%d" % i, [128, 1024], F32)) for i in range(4)]
        PT_ = [T() for _ in range(8)]
        pstate = {"b": 0}

        class Bank:
            def __init__(self, ap, toks):
                self.ap = ap
                self.toks = toks

            @property
            def bf(self):
                return self.ap.bitcast(BF16)

        busy = set()

        def ps1(hold=False):
            b = pstate["b"]
            while b in busy:
                b = (b + 1) % 8
            pstate["b"] = (b + 1) % 8
            bk_ = Bank(PS[b // 2][:, (b % 2) * 512:(b % 2) * 512 + 512], [PT_[b]])
            bk_.idx = [b]
            if hold:
                busy.add(b)
            return bk_

        def ps2(hold=False):
            b = pstate["b"]
            if b % 2:
                b = (b + 1) % 8
            while b in busy or (b + 1) in busy:
                b = (b + 2) % 8
            pstate["b"] = (b + 2) % 8
            bk_ = Bank(PS[b // 2][:, :], [PT_[b], PT_[b + 1]])
            bk_.idx = [b, b + 1]
            if hold:
                busy.update(bk_.idx)
            return bk_

        def psrel(bk_):
            for b in bk_.idx:
                busy.discard(b)

        def Wf(lo, hi):
            return W[:, lo:hi].bitcast(F32)

        def Kf(lo, hi):
            return K[:, lo:hi].bitcast(F32)

        def Hb(lo, hi):
            return H[:, lo:hi].bitcast(BF16)

        def dump(name, ap, toks):
            if name in dump_d:
                S.dma("pool", out=dump_d[name], in_=ap, reads=toks)

        def ld(q, dst, src):
            S.dma(q, out=dst, in_=src, writes=[tC])

        ld("pool", ident_bf[:], ident_d[:, :])
        ld("sp", ident_f[:], ident_d[:, :])
        ld("sp", tri_f[:], tri_d[:, :])
        ld("sp", negmask[:], negmask_d[:, :])
        ld("pool", su_bf[:], su_d[:, :])
        ld("sp", thr[:], thr_d[:, :])
        ld("sp", pcol[:], pcol_d[:, :])
        ld("sp", tstart[:], tstart_d[:, :])
        S.op("dve", [], [tRk], lambda: nc.vector.memset(Crun[:], 0.0))
        ld("pool", cc[:], cc_d[:, :])
        ld("pool", ss[:], ss_d[:, :])
        for dst, src in ((qn, qn_d), (kvn, kvn_d), (convw, convw_d), (convb, convb_d), (dtb, dtb_d),
                         (a_bc, alog_d), (dcol, dcol_d), (ncol, ncol_d), (rb, rb_d)):
            ld("sp", dst[:], src[:, :])
        ld("pool", v3(rw[:], 36), rw_d.rearrange("(k p) c -> p k c", p=128))
        S.op("dve", [], [tC], lambda: nc.vector.memset(ones_f[:], 1.0))
        S.op("dve", [], [tC], lambda: nc.vector.memset(ones_bf[:], 1.0))
        S.op("act", [tC], [tC], lambda: nc.scalar.activation(out=a_bc[:], in_=a_bc[:], func=AF.Exp))
        S.op("dve", [tC], [tC], lambda: nc.vector.tensor_scalar_mul(out=a_bc[:], in0=a_bc[:], scalar1=-1.0))
        S.barrier()

        def rstd_from(dst, src, scale, reads, writes):
            S.op("act", reads, writes, lambda: nc.scalar.activation(out=dst, in_=src, func=AF.Ln, scale=scale, bias=EPS))
            S.op("act", [], writes, lambda: nc.scalar.activation(out=dst, in_=dst, func=AF.Exp, scale=-0.5))

        def layer_norm_tile(hap, tH, li, s):
            j = lnstate["i"] % 4
            lnstate["i"] += 1
            tS = tLNS[j]
            base = j * 16
            st = lnst[:, base:base + 12]
            mv = lnst[:, base + 12:base + 14]
            rs = lnst[:, base + 14:base + 15]
            nm = lnst[:, base + 15:base + 16]
            S.op("dve", [tH], [tS], lambda: nc.vector.bn_stats(out=st[:, 0:6], in_=hap[:, 0:512]))
            S.op("dve", [tH], [tS], lambda: nc.vector.bn_stats(out=st[:, 6:12], in_=hap[:, 512:1024]))
            S.op("dve", [], [tS], lambda: nc.vector.bn_aggr(out=mv, in_=st))
            rstd_from(rs, mv[:, 1:2], 1.0, [tS], [tS])
            S.op("dve", [tS], [tS], lambda: nc.vector.scalar_tensor_tensor(
                out=nm, in0=mv[:, 0:1], scalar=-1.0, in1=rs, op0=ALU.mult, op1=ALU.mult))
            S.op("act", [tS], [tH], lambda: nc.scalar.activation(out=hap, in_=hap, func=AF.Identity, scale=rs, bias=nm))
            S.op("dve", [tLN], [tH], lambda: nc.vector.tensor_tensor(out=hap, in0=hap, in1=lng[:], op=ALU.mult))
            S.op("dve", [tLN], [tH], lambda: nc.vector.tensor_tensor(out=hap, in0=hap, in1=lnb[:], op=ALU.add))

        def load_ln(li):
            S.dma("sp", out=lng[:], in_=lnp_d[2 * li, :, :], writes=[tLN])
            S.dma("sp", out=lnb[:], in_=lnp_d[2 * li + 1, :, :], writes=[tLN])

        def transpose_to(hb, tHB, dstT, tt, tDst):
            pb = ps1()
            S.op("pe", [tHB, tC], pb.toks, lambda: [nc.tensor.transpose(
                pb.bf[:, k * 128:(k + 1) * 128], hb[:, k * 128:(k + 1) * 128], ident_bf[:]) for k in range(8)])
            S.op("act", pb.toks, [tDst], lambda: nc.scalar.copy(
                out=dstT[:, :, tt * 128:(tt + 1) * 128], in_=v3(pb.bf[:, 0:1024], 128)))

        actT = v3(A[:, :], SEQ)
        catT = v3(K[:, :], SEQ)
        hview = v3(H[:, :], D)

        for s in range(n_seq):
            tXT = [T() for _ in range(NT)]
            if s > 0:
                S.dma("pool", out=cc[:], in_=cc_d[:, :], writes=[tC])
            NXB = 4
            xin = [K[:, 0:1024], K[:, 1024:2048], K[:, 8192:9216], K[:, 9216:10240]]
            txin = [T() for _ in range(NXB)]
            win = v3(W[:, 0:15680], 1960)
            wkr = v3(W[:, 15680:16448], 96)
            wq_s = v3(W[:, 16448:17984], 768)
            wq_sw = v3(W[:, 17984:19520], 768)
            wkv_s = W[:, 19520:20544]
            tWinL, tWs = [T() for _ in range(9)], T()
            for tt in range(NXB):
                S.dma("pool", out=xin[tt], in_=x[s, tt * 128:(tt + 1) * 128, :], writes=[txin[tt]])
            WCG = [(0, 416), (416, 928), (928, 1440), (1440, 1960)]
            w_in3 = w_in.rearrange("(k p) c -> p k c", p=128)
            S.dma("pool", out=win[:, :, 0:416], in_=w_in3[:, :, 0:416], writes=[tWinL[0]])
            S.dma("pool", out=wkr, in_=wkr_sw.rearrange("(k p) c -> p k c", p=128), writes=[tWinL[8]])
            for gi in range(1, 4):
                S.dma("pool", out=win[:, :, WCG[gi][0]:WCG[gi][1]], in_=w_in3[:, :, WCG[gi][0]:WCG[gi][1]], writes=[tWinL[gi]])
            S.dma("pool", out=wq_s, in_=wq_d.rearrange("(k p) c -> p k c", p=128), writes=[tWs])
            S.dma("pool", out=wq_sw, in_=wqsw_d.rearrange("(k p) c -> p k c", p=128), writes=[tWs])
            S.dma("pool", out=wkv_s, in_=wkv_d[:, :], writes=[tWs])
            for r in range(2):
                S.op("dve", [tWs, tC], [tWs], lambda r=r: nc.vector.tensor_scalar_mul(out=wq_s[:, r, :], in0=wq_s[:, r, :], scalar1=qn[:, r:r + 1]))
                S.op("dve", [tWs, tC], [tWs], lambda r=r: nc.vector.tensor_scalar_mul(out=wq_sw[:, r, :], in0=wq_sw[:, r, :], scalar1=qn[:, r:r + 1]))
            S.op("dve", [tWs, tC], [tWs], lambda: nc.vector.tensor_scalar_mul(out=wkv_s, in0=wkv_s, scalar1=kvn[:, 0:1]))
            for tt in range(NT):
                transpose_to(xin[tt % NXB], txin[tt % NXB], actT, tt, tXT[tt])
                if tt + NXB < NT:
                    S.dma("pool", out=xin[tt % NXB], in_=x[s, (tt + NXB) * 128:(tt + NXB + 1) * 128, :], writes=[txin[tt % NXB]])
            if s == 0:
                tDW = {n: [T() for _ in range(8)] for n in DWB}
                for n, src in (("wout", wout_d), ("xwk", xwk_d), ("xwv", xwv_d), ("xwq", xwq_d), ("xwo", xwo_d)):
                    for k in range(8):
                        S.dma("pool", out=DWB[n][k * 128:(k + 1) * 128, :], in_=src[k * 128:(k + 1) * 128, :], writes=[tDW[n][k]], slots="cast")
            if s == 0 and stop_after is None:
                for e in range(NEXP):
                    rows = slice(e * 128, (e + 1) * 128)
                    S.dma("pool", out=WGB[rows, :].rearrange("p (k f) -> p k f", k=8), in_=wg_d[e].rearrange("(k p) f -> p k f", p=128), writes=[T()], slots="cast")
                    S.dma("pool", out=WUB[rows, :].rearrange("p (k f) -> p k f", k=8), in_=wu_d[e].rearrange("(k p) f -> p k f", p=128), writes=[T()], slots="cast")
                    S.dma("pool", out=WDB[rows, :].rearrange("p (k f) -> p k f", k=2), in_=wd_d[e].rearrange("(k p) f -> p k f", p=128), writes=[T()], slots="cast")
            if stop_after == "P1":
                dump("xT", actT[:, 0, :], tXT)
                break

            zs = v3(Hb(0, 4096), SEQ)
            xsT = v3(Hb(4096, 8192), SEQ)
            BT = v3(Hb(8192, 10240), SEQ)
            CT = v3(Hb(10240, 12288), SEQ)
            cqT = v3(Hb(12288, 14336), SEQ)
            ckvT = Hb(14336, 15360)
            kpe = Hb(15360, 16384)
            tZ, tXs, tB, tCt, tCq, tCkv, tKpe = T(), T(), T(), T(), T(), T(), T()
            pre = [Kf(2048 + i * 1040, 2048 + i * 1040 + 1030) for i in range(2)]
            acc = [Kf(4128 + i * 1024, 4128 + (i + 1) * 1024) for i in range(2)]
            sqb = K[:, 6176:6688]
            rsb = Kf(6688, 7712)
            tPre, tAcc, tSq, tRs = [T(), T()], [T(), T()], T(), T()
            tDt = T()

            def proj_mm(bank, M, lhs_fn, tg, wt=0):
                S.op("pe", list(tXT[tg * 4:tg * 4 + 4]) + [tWinL[wt]], bank.toks, lambda: [nc.tensor.matmul(
                    bank.ap[0:M, :], lhs_fn(k), actT[:, k, tg * 512:(tg + 1) * 512], start=(k == 0), stop=(k == 7)) for k in range(8)])

            for tg in range(NG):
                cols = slice(tg * 512, (tg + 1) * 512)
                bq = [ps1(), ps1()]
                for r in range(2):
                    proj_mm(bq[r], 128, lambda k, r=r: win[:, k, r * 128:(r + 1) * 128], tg)
                bs = ps1()
                for r in range(2):
                    S.op("act", bq[r].toks, [tSq], lambda r=r: nc.scalar.activation(out=sqb, in_=bq[r].ap, func=AF.Square))
                    S.op("pe", [tSq, tC], bs.toks, lambda r=r: nc.tensor.matmul(bs.ap, ones_bf[:], sqb, start=(r == 0), stop=(r == 1)))
                rstd_from(rsb, bs.ap, 1.0 / 256, bs.toks, [tRs])
                for r in range(2):
                    S.op("dve", bq[r].toks + [tRs], [tCq], lambda r=r: nc.vector.tensor_tensor(out=cqT[:, r, cols], in0=bq[r].ap, in1=rsb, op=ALU.mult))
                bk = ps1()
                proj_mm(bk, 128, lambda k: win[:, k, 256:384], tg)
                bs = ps1()
                S.op("act", bk.toks, [tSq], lambda: nc.scalar.activation(out=sqb, in_=bk.ap, func=AF.Square))
                S.op("pe", [tSq, tC], bs.toks, lambda: nc.tensor.matmul(bs.ap, ones_bf[:], sqb, start=True, stop=True))
                rstd_from(rsb, bs.ap, 1.0 / 128, bs.toks, [tRs])
                S.op("dve", bk.toks + [tRs], [tCkv], lambda: nc.vector.tensor_tensor(out=ckvT[:, cols], in0=bk.ap, in1=rsb, op=ALU.mult))
                ba, bb = ps1(), ps1()
                proj_mm(ba, 96, lambda k: win[:, k, 320:416], tg)
                proj_mm(bb, 96, lambda k: wkr[:, k, :], tg, 8)
                t1 = acc[0]
                t2 = acc[1]
                S.op("dve", ba.toks + [tC], [tAcc[0]], lambda: nc.vector.tensor_tensor(out=t1[64:96, :], in0=ba.ap[64:96, :], in1=cc[64:96, cols], op=ALU.mult))
                S.op("dve", bb.toks + [tC], [tAcc[1]], lambda: nc.vector.tensor_tensor(out=t2[64:96, :], in0=bb.ap[64:96, :], in1=ss[64:96, cols], op=ALU.mult))
                S.op("dve", [tAcc[0], tAcc[1]], [tKpe], lambda: nc.vector.tensor_tensor(out=kpe[64:96, cols], in0=t1[64:96, :], in1=t2[64:96, :], op=ALU.add))
                for j in range(4):
                    bz = ps1()
                    proj_mm(bz, 128, lambda k, j=j: win[:, k, 416 + j * 128:416 + (j + 1) * 128], tg, 1)
                    S.op("act", bz.toks, [tZ], lambda j=j, bz=bz: nc.scalar.activation(out=zs[:, j, cols], in_=bz.ap, func=AF.Silu))
            for c in range(8):
                if c < 4:
                    dst, tdst = xsT[:, c, :], tXs
                elif c < 6:
                    dst, tdst = BT[:, c - 4, :], tB
                else:
                    dst, tdst = CT[:, c - 6, :], tCt
                S.op("dve", [], [tPre[0]], lambda: nc.vector.memset(pre[0][:, 0:3], 0.0))
                for tg in range(NG):
                    p_, a_ = pre[tg % 2], acc[tg % 2]
                    tp, ta = tPre[tg % 2], tAcc[tg % 2]
                    bx = ps1()
                    proj_mm(bx, 128, lambda k, c=c: win[:, k, 928 + c * 128:928 + (c + 1) * 128], tg, 2 if c < 4 else 3)
                    S.op("act", bx.toks, [tp], lambda: nc.scalar.copy(out=p_[:, 3:515], in_=bx.ap))
                    if tg < NG - 1:
                        S.op("act", [tp], [tPre[(tg + 1) % 2]], lambda: nc.scalar.copy(out=pre[(tg + 1) % 2][:, 0:3], in_=p_[:, 512:515]))
                    S.op("dve", [tp, tC], [ta], lambda: nc.vector.tensor_scalar_mul(out=a_, in0=p_[:, 0:512], scalar1=convw[:, c * 4:c * 4 + 1]))
                    for j in range(1, 4):
                        S.op("dve", [tp, tC], [ta], lambda j=j: nc.vector.scalar_tensor_tensor(
                            out=a_, in0=p_[:, j:j + 512], scalar=convw[:, c * 4 + j:c * 4 + j + 1], in1=a_, op0=ALU.mult, op1=ALU.add))
                    S.op("act", [ta, tC], [tdst], lambda: nc.scalar.activation(
                        out=dst[:, tg * 512:(tg + 1) * 512], in_=a_, func=AF.Silu, bias=convb[:, c:c + 1]))
            bd = ps1()
            for tt in range(NT):
                S.op("pe", [tXT[tt], tWinL[3]], bd.toks, lambda tt=tt: [nc.tensor.matmul(
                    bd.ap[:, tt * 8:(tt + 1) * 8], actT[:, k, tt * 128:(tt + 1) * 128], win[:, k, 1952:1960],
                    start=(k == 0), stop=(k == 7)) for k in range(8)])
            S.op("dve", bd.toks + [tC], [tDt], lambda: nc.vector.tensor_tensor(out=dt_tok[:], in0=bd.ap[:, 0:128], in1=dtb[:], op=ALU.add))
            S.op("act", [tDt], [tDt], lambda: nc.scalar.activation(out=dt_tok[:], in_=dt_tok[:], func=AF.Exp))
            S.op("act", [tDt], [tDt], lambda: nc.scalar.activation(out=dt_tok[:], in_=dt_tok[:], func=AF.Ln, bias=1.0))
            if stop_after == "P2":
                dump("cqT", cqT[:, 0, :], [tCq])
                dump("ckvT", ckvT, [tCkv])
                dump("kpe", kpe, [tKpe])
                dump("zs", zs[:, 0, :], [tZ])
                dump("xsT", xsT[:, 0, :], [tXs])
                dump("BT", BT[:, 0, :], [tB])
                dump("dt", dt_tok[:], [tDt])
                break
            S.barrier(engines=("act", "dve", "pool", "sp"))
            qT = [A[:, i * 2048:(i + 1) * 2048] for i in range(2)]
            kT = [A[:, (2 + i) * 2048:(3 + i) * 2048] for i in range(2)]
            Vaug = [v3(A[:, (4 + i) * 2048:(5 + i) * 2048], 128) for i in range(2)]
            PTb = [A[:, 12288 + i * 512:12288 + (i + 1) * 512] for i in range(3)]
            rdb = A[:, 13824:14848].bitcast(F32)
            t1 = [Wf(0, 1024), A[:, 14848:15872].bitcast(F32)]
            t2 = [Wf(1024, 2048), Wf(13568, 14592)]
            tQ, tK, tV, tPT = [T(), T()], [T(), T()], [T(), T()], [T(), T(), T()]
            tT1, tT2, tRd = [T(), T()], [T(), T()], T()
            tCat = [T() for _ in range(8)]
            S.op("dve", [], [tV[0]], lambda: nc.vector.memset(Vaug[0][:, :, 64:128], 1.0))
            S.op("dve", [], [tV[1]], lambda: nc.vector.memset(Vaug[1][:, :, 0:64], 1.0))
            if s == 0 and stop_after is None:
                ztile = W[:, 14592:15616]
                tZ = T()
                S.op("dve", [], [tZ], lambda: nc.vector.memset(ztile, 0.0))
                for j in range(NTI * TB):
                    S.dma("sp", out=XS[j * 128:(j + 1) * 128, :], in_=ztile, reads=[tZ])
            SCALE = 96.0 ** -0.5
            pstate_pt = {"i": 0}

            def qkv_head(h):
                hp = h % 2
                q_h, k_h, V_h = qT[hp], kT[hp], Vaug[hp]
                tq, tk, tv = tQ[hp], tK[hp], tV[hp]
                for tg in range(NG):
                    cols = slice(tg * 512, (tg + 1) * 512)
                    S.op("dve", [tKpe], [tk], lambda: nc.vector.tensor_copy(out=k_h[64:96, cols], in_=kpe[64:96, cols]))
                    yield
                    ba, bb = ps1(), ps1()
                    S.op("pe", [tWs, tCq], ba.toks, lambda: [nc.tensor.matmul(
                        ba.ap[0:96, :], wq_s[:, r, h * 96:(h + 1) * 96], cqT[:, r, cols], start=(r == 0), stop=(r == 1)) for r in range(2)])
                    S.op("pe", [tWs, tCq], bb.toks, lambda: [nc.tensor.matmul(
                        bb.ap[0:96, :], wq_sw[:, r, h * 96:(h + 1) * 96], cqT[:, r, cols], start=(r == 0), stop=(r == 1)) for r in range(2)])
                    bk = ps1()
                    S.op("pe", [tWs, tCkv], bk.toks, lambda: nc.tensor.matmul(
                        bk.ap[0:64, :], wkv_s[:, h * 128:h * 128 + 64], ckvT[:, cols], start=True, stop=True))
                    tt1, tt2 = tT1[tg % 2], tT2[tg % 2]
                    u1, u2 = t1[tg % 2], t2[tg % 2]
                    S.op("dve", ba.toks + [tC], [tt1], lambda: nc.vector.tensor_tensor(out=u1[0:96, :], in0=ba.ap[0:96, :], in1=cc[0:96, cols], op=ALU.mult))
                    yield
                    S.op("dve", bb.toks + [tC], [tt2], lambda: nc.vector.tensor_tensor(out=u2[0:96, :], in0=bb.ap[0:96, :], in1=ss[0:96, cols], op=ALU.mult))
                    yield
                    S.op("dve", [tt1, tt2], [tq], lambda: nc.vector.tensor_tensor(out=q_h[0:96, cols], in0=u1[0:96, :], in1=u2[0:96, :], op=ALU.add))
                    yield
                    S.op("dve", bk.toks, [tk], lambda: nc.vector.tensor_copy(out=k_h[0:64, cols], in_=bk.ap[0:64, :]))
                    yield
                vo = 0 if hp == 0 else 64
                for half in range(2):
                    bv = ps1()
                    S.op("pe", [tWs, tCkv], bv.toks, lambda: [nc.tensor.matmul(
                        bv.ap[:, j * 64:(j + 1) * 64], ckvT[:, (half * 8 + j) * 128:(half * 8 + j + 1) * 128],
                        wkv_s[:, h * 128 + 64:(h + 1) * 128], start=True, stop=True) for j in range(8)])
                    S.op("dve", bv.toks, [tv], lambda: nc.vector.tensor_copy(out=V_h[:, half * 8:(half + 1) * 8, vo:vo + 64], in_=v3(bv.ap, 64)))
                    yield

            def attn_head(h):
                hp = h % 2
                q_h, k_h, V_h = qT[hp], kT[hp], Vaug[hp]
                tq, tk, tv = tQ[hp], tK[hp], tV[hp]
                orow = slice(0, 64) if hp == 0 else slice(64, 128)
                drow = slice(64, 128) if hp == 0 else slice(0, 64)
                items = [(g, kj) for g in range(NG) for kj in range(4 * g + 4)]
                sbank = {}
                bo_of = {}

                def emit_S(i):
                    g, kj = items[i]
                    c0 = max(0, kj - 4 * g) * 128
                    if g not in bo_of:
                        bo_of[g] = ps1(hold=True)
                    b_ = ps1()
                    sbank[i] = b_
                    S.op("pe", [tq, tk], b_.toks, lambda: nc.tensor.matmul(
                        b_.ap[:, c0:512], k_h[0:96, kj * 128:(kj + 1) * 128], q_h[0:96, g * 512 + c0:(g + 1) * 512], start=True, stop=True))

                LOOK = 2
                for i in range(min(LOOK, len(items))):
                    emit_S(i)
                for i, (g, kj) in enumerate(items):
                    nk = 4 * g + 4
                    c0 = max(0, kj - 4 * g) * 128
                    b_ = sbank.pop(i)
                    pt, tpt = PTb[pstate_pt["i"] % 3], tPT[pstate_pt["i"] % 3]
                    pstate_pt["i"] += 1
                    S.op("act", b_.toks, [tpt], lambda: nc.scalar.activation(out=pt[:, c0:512], in_=b_.ap[:, c0:512], func=AF.Exp, scale=SCALE))
                    if kj >= 4 * g:
                        S.op("dve", [], [tpt], lambda: nc.vector.memset(pt[64:128, c0:c0 + 64], 0.0))
                    if i + LOOK < len(items):
                        emit_S(i + LOOK)
                    bo = bo_of[g]
                    S.op("pe", [tpt, tv], bo.toks, lambda: nc.tensor.matmul(
                        bo.ap[:, c0:512], V_h[:, kj, :], pt[:, c0:512], start=(kj == 0), stop=(kj == nk - 1)))
                    if kj == nk - 1:
                        S.op("dve", bo.toks, [tRd], lambda: nc.vector.reciprocal(out=rdb[drow, :], in_=bo.ap[drow, :]))
                        S.op("dve", bo.toks + [tRd], [tCat[h // 2]], lambda: nc.vector.tensor_tensor(
                            out=catT[orow, h // 2, g * 512:(g + 1) * 512], in0=bo.ap[orow, :], in1=rdb[drow, :], op=ALU.mult))
                        psrel(bo)
                    yield

            def p4_gen():
                for _ in qkv_head(0):
                    pass
                for h in range(8):
                    nxt = qkv_head(h + 1) if h + 1 < 8 else None
                    for n_, _ in enumerate(attn_head(h)):
                        if nxt is not None and n_ % 2 == 1:
                            if next(nxt, "done") == "done":
                                nxt = None
                        yield
                    if nxt is not None:
                        for _ in nxt:
                            pass

            if stop_after == "P4":
                for _ in p4_gen():
                    pass
                dump("cat_attn", catT[:, 0:4, :], tCat)
                break
            def ssd_set(i):
                b0 = 2048 if i == 0 else 15616
                so = 16 if i == 0 else 64
                d = dict(
                    adt=small[:, so:so + 8], acs_sb=small[:, so + 8:so + 16], dte=small[:, so + 16:so + 24],
                    cdk=small[:, so + 24:so + 32], dd=small[:, so + 32:so + 40], nacs=small[:, so + 40:so + 48],
                    R=Wf(b0, b0 + 2048), E=Wf(b0 + 2048, b0 + 3072), DEC=Wf(b0 + 3072, b0 + 4096),
                    Mfin=[W[:, b0 + 4096:b0 + 4608], W[:, b0 + 4608:b0 + 5120]],
                    CexpT=[W[:, b0 + 5120:b0 + 5632], W[:, b0 + 5632:b0 + 6144]],
                    xdt=W[:, b0 + 6144:b0 + 6656], xdte=W[:, b0 + 6656:b0 + 7168], Btok=W[:, b0 + 7168:b0 + 7424],
                    cbm=[Wf(b0 + 7424, b0 + 7680), Wf(b0 + 7680, b0 + 7936)],
                    tAdt=T(), tSm2=T(), tR=T(), tE=T(), tDec=T(), tXdt=T(), tXdte=T(), tBtok=T(), tCbm=T(),
                    tMf=[T(), T()], tCe=[T(), T()])
                return d

            SB = [ssd_set(0), ssd_set(1)]
            prev_f = Wf(9984, 11008)
            prev_bf = W[:, 11008:11520]
            yz = Wf(11520, 12544)
            sq = W[:, 12544:13056]
            rstd_g = Wf(13056, 13312)
            yv = Wf(13312, 13568)
            tPrevF, tPrevB, tYv, tYz, tSq2, tRg = T(), T(), T(), T(), T(), T()
            S.op("dve", [], [tWs], lambda: nc.vector.memset(small[:, 250:251], 0.0))
            S.op("act", [], [tWs], lambda: nc.scalar.copy(out=small[:, 251:252], in_=small[:, 250:251]))
            S.op("dve", [], [tPrevF], lambda: nc.vector.memset(prev_f, 0.0))
            S.op("dve", [], [tPrevB], lambda: nc.vector.memset(prev_bf, 0.0))

            def ssd_stage1(c):
                B_ = SB[c % 2]
                adt, acs_sb, dte, cdk, dd, nacs = B_["adt"], B_["acs_sb"], B_["dte"], B_["cdk"], B_["dd"], B_["nacs"]
                R, E, DEC, Mfin, CexpT, xdt, xdte, Btok, cbm = (B_[k] for k in ("R", "E", "DEC", "Mfin", "CexpT", "xdt", "xdte", "Btok", "cbm"))
                tAdt, tSm2, tR, tE, tDec, tXdt, tXdte, tBtok, tCbm, tMf, tCe = (B_[k] for k in (
                    "tAdt", "tSm2", "tR", "tE", "tDec", "tXdt", "tXdte", "tBtok", "tCbm", "tMf", "tCe"))
                tsl = slice(c * 128, (c + 1) * 128)
                dtc = dt_tok[:, c * 8:(c + 1) * 8]
                S.op("dve", [tDt, tC], [tAdt], lambda: nc.vector.tensor_tensor(out=adt, in0=dtc, in1=a_bc[:], op=ALU.mult))
                bx = ps1()
                S.op("pe", [tXs, tC], bx.toks, lambda: [nc.tensor.transpose(
                    bx.bf[:, j * 128:(j + 1) * 128], xsT[:, j, tsl], ident_bf[:]) for j in range(4)])
                S.op("dve", bx.toks + [tDt], [tXdt], lambda: nc.vector.tensor_tensor(
                    out=v3(xdt, 64), in0=v3(bx.bf[:, 0:512], 64), in1=dtc.unsqueeze(2).to_broadcast([128, 8, 64]), op=ALU.mult))
                bb_ = ps1()
                S.op("pe", [tB, tC], bb_.toks, lambda: [nc.tensor.transpose(
                    bb_.bf[:, g * 128:(g + 1) * 128], BT[:, g, tsl], ident_bf[:]) for g in range(2)])
                S.op("act", bb_.toks, [tBtok], lambda: nc.scalar.copy(out=Btok, in_=bb_.bf[:, 0:256]))
                yield
                ba_ = ps1()
                S.op("pe", [tAdt, tC], ba_.toks, lambda: [
                    nc.tensor.matmul(ba_.ap[:, 0:8], tri_f[:], adt, start=True, stop=True),
                    nc.tensor.matmul(ba_.ap[:, 8:16], ones_f[:], adt, start=True, stop=True)])
                S.op("act", ba_.toks, [tSm2], lambda: nc.scalar.copy(out=acs_sb, in_=ba_.ap[:, 0:8]))
                S.op("dve", ba_.toks + [tSm2], [tSm2], lambda: nc.vector.tensor_tensor(out=dd, in0=ba_.ap[:, 8:16], in1=acs_sb, op=ALU.subtract))
                S.op("act", [tSm2], [tSm2], lambda: nc.scalar.activation(out=dte, in_=dd, func=AF.Exp))
                S.op("act", ba_.toks, [tSm2], lambda: nc.scalar.activation(out=cdk, in_=ba_.ap[:, 8:16], func=AF.Exp))
                S.op("dve", [tSm2], [tSm2], lambda: nc.vector.tensor_scalar_mul(out=nacs, in0=acs_sb, scalar1=-1.0))
                yield
                S.op("dve", [tXdt, tSm2], [tXdte], lambda: nc.vector.tensor_tensor(
                    out=v3(xdte, 64), in0=v3(xdt, 64), in1=dte.unsqueeze(2).to_broadcast([128, 8, 64]), op=ALU.mult))
                S.op("dve", [tAdt, tC], [tR], lambda: nc.vector.tensor_tensor(
                    out=v3(R, 128), in0=tri_f[:].unsqueeze(1).to_broadcast([128, 8, 128]),
                    in1=adt.unsqueeze(2).to_broadcast([128, 8, 128]), op=ALU.mult))
                yield
                for g in range(2):
                    bc = ps1()
                    S.op("pe", [tB, tCt], bc.toks, lambda: nc.tensor.matmul(bc.ap[:, 0:128], BT[:, g, tsl], CT[:, g, tsl], start=True, stop=True))
                    S.op("act", bc.toks, [tCbm], lambda: nc.scalar.copy(out=cbm[g], in_=bc.ap[:, 0:128]))
                    bA = ps1()
                    S.op("pe", [tR, tC], bA.toks, lambda: nc.tensor.matmul(bA.ap, ones_f[:], R[:, g * 512:(g + 1) * 512], start=True, stop=True))
                    yield
                    S.op("act", bA.toks, [tDec], lambda: nc.scalar.activation(out=DEC, in_=bA.ap, func=AF.Exp))
                    S.op("dve", bA.toks + [tC, tDec], [tE], lambda: nc.vector.tensor_tensor(out=E, in0=bA.ap, in1=negmask[:], op=ALU.add))
                    for hh in range(4):
                        S.op("act", [tE, tSm2], [tE], lambda: nc.scalar.activation(
                            out=E[:, hh * 128:(hh + 1) * 128], in_=E[:, hh * 128:(hh + 1) * 128], func=AF.Exp,
                            bias=nacs[:, g * 4 + hh:g * 4 + hh + 1]))
                    yield
                    S.op("dve", [tE, tCbm], [tMf[g]], lambda: nc.vector.tensor_tensor(
                        out=v3(Mfin[g], 128), in0=v3(E, 128), in1=cbm[g].unsqueeze(1).to_broadcast([128, 4, 128]), op=ALU.mult))
                    S.op("dve", [tDec, tCt], [tCe[g]], lambda: nc.vector.tensor_tensor(
                        out=v3(CexpT[g], 128), in0=v3(DEC, 128), in1=CT[:, g, tsl].unsqueeze(1).to_broadcast([128, 4, 128]), op=ALU.mult))
                    yield

            def ssd_stage2(c):
                B_ = SB[c % 2]
                cdk, Mfin, CexpT, xdt, xdte, Btok = (B_[k] for k in ("cdk", "Mfin", "CexpT", "xdt", "xdte", "Btok"))
                tSm2, tXdt, tXdte, tBtok, tMf, tCe = (B_[k] for k in ("tSm2", "tXdt", "tXdte", "tBtok", "tMf", "tCe"))
                tsl = slice(c * 128, (c + 1) * 128)
                for pair in range(4):
                    g = pair // 2
                    by = ps1()
                    for hp in range(2):
                        h = pair * 2 + hp
                        hh = h % 4
                        rows = slice(hp * 64, hp * 64 + 64)
                        tp_ = None if hp == 0 else (0, 64)
                        S.op("pe", [tXdt, tMf[g], tPrevB, tCe[g]], by.toks, lambda: [
                            nc.tensor.matmul(by.ap[rows, 0:128], xdt[:, h * 64:(h + 1) * 64], Mfin[g][:, hh * 128:(hh + 1) * 128],
                                             start=True, stop=False, tile_position=tp_),
                            nc.tensor.matmul(by.ap[rows, 0:128], prev_bf[:, h * 64:(h + 1) * 64], CexpT[g][:, hh * 128:(hh + 1) * 128],
                                             start=False, stop=True, tile_position=tp_)])
                    S.op("dve", by.toks + [tXs, tC], [tYv], lambda: nc.vector.scalar_tensor_tensor(
                        out=yv, in0=xsT[:, pair, tsl], scalar=dcol[:, pair:pair + 1], in1=by.ap[:, 0:128], op0=ALU.mult, op1=ALU.add))
                    S.op("dve", [tYv, tZ], [tYz], lambda: nc.vector.tensor_tensor(
                        out=yz[:, pair * 128:(pair + 1) * 128], in0=yv, in1=zs[:, pair, tsl], op=ALU.mult))
                    S.op("act", [tYz], [tSq2], lambda: nc.scalar.activation(
                        out=sq[:, pair * 128:(pair + 1) * 128], in_=yz[:, pair * 128:(pair + 1) * 128], func=AF.Square))
                    yield
                    if pair % 2 == 1:
                        bn_ = ps1()
                        S.op("pe", [tSq2, tC], bn_.toks, lambda: [nc.tensor.matmul(
                            bn_.ap[:, 0:128], ones_bf[:], sq[:, (pair - 1 + i) * 128:(pair + i) * 128], start=(i == 0), stop=(i == 1)) for i in range(2)])
                        rstd_from(rstd_g, bn_.ap[:, 0:128], 1.0 / 256, bn_.toks, [tRg])
                        for pp in (pair - 1, pair):
                            S.op("dve", [tYz, tRg, tC], [tCat[4 + pp]], lambda: nc.vector.scalar_tensor_tensor(
                                out=catT[:, 4 + pp, tsl], in0=yz[:, pp * 128:(pp + 1) * 128], scalar=ncol[:, pp:pp + 1], in1=rstd_g,
                                op0=ALU.mult, op1=ALU.mult))
                        yield
                bst = ps1()
                S.op("pe", [tBtok, tXdte], bst.toks, lambda: [nc.tensor.matmul(
                    bst.ap[:, g * 256:(g + 1) * 256], Btok[:, g * 128:(g + 1) * 128], xdte[:, g * 256:(g + 1) * 256],
                    start=True, stop=True) for g in range(2)])
                S.op("dve", [tSm2], [tPrevF], lambda: nc.vector.tensor_tensor(
                    out=v3(prev_f, 64), in0=v3(prev_f, 64), in1=cdk.unsqueeze(2).to_broadcast([128, 8, 64]), op=ALU.mult))
                S.op("dve", bst.toks, [tPrevF], lambda: nc.vector.tensor_tensor(out=prev_f, in0=prev_f, in1=bst.ap, op=ALU.add))
                S.op("act", [tPrevF], [tPrevB], lambda: nc.scalar.copy(out=prev_bf, in_=prev_f))
                yield

            def p5_gen():
                yield from ssd_stage1(0)
                for c in range(NT):
                    ga = ssd_stage1(c + 1) if c + 1 < NT else iter(())
                    gb = ssd_stage2(c)
                    a_alive = b_alive = True
                    while a_alive or b_alive:
                        if a_alive and next(ga, "done") == "done":
                            a_alive = False
                        if b_alive and next(gb, "done") == "done":
                            b_alive = False
                        yield

            g4, g5 = p4_gen(), p5_gen()
            alive = {"4": True, "5": True}

            def step(gen, key, n):
                for _ in range(n):
                    if alive[key]:
                        try:
                            next(gen)
                        except StopIteration:
                            alive[key] = False

            while alive["4"]:
                step(g4, "4", 64)
            while alive["5"]:
                step(g5, "5", 64)

            if stop_after == "P5":
                dump("cat_ssd", catT[:, 4:8, :], tCat)
                break
            S.barrier(engines=("act", "dve", "pool", "sp"))
            wout = v3(W[:, 0:8192], 1024)
            tWoL = [T() for _ in range(8)]
            for k in range(8):
                S.dma("sp", out=wout[:, k, :], in_=DWB["wout"][k * 128:(k + 1) * 128, :], reads=[tDW["wout"][k]], writes=[tWoL[k]])
            load_ln(0)
            xres = [Wf(8192, 10240), Wf(10240, 12288)]
            hbW = [W[:, 12288:13312], W[:, 13312:14336]]
            tXr, tHb = [T(), T()], [T(), T()]
            tH = [T() for _ in range(NT)]
            tHT = [T() for _ in range(NT)]
            def p6_mm(tt):
                tok = slice(tt * 128, (tt + 1) * 128)
                xr = xres[tt % 2]
                S.dma("sp", out=xr, in_=x[s, tok, :], writes=[tXr[tt % 2]])
                bm = ps2()
                S.op("pe", tCat + tWoL, bm.toks, lambda: [nc.tensor.matmul(
                    bm.ap[:, hf * 512:(hf + 1) * 512], catT[:, c, tok], wout[:, c, hf * 512:(hf + 1) * 512],
                    start=(c == 0), stop=(c == 7)) for hf in range(2) for c in range(8)])
                return bm

            bms = {0: p6_mm(0)}
            for tt in range(NT):
                if tt + 1 < NT:
                    bms[tt + 1] = p6_mm(tt + 1)
                bm = bms.pop(tt)
                xr = xres[tt % 2]
                hap = hview[:, tt, :]
                S.op("dve", bm.toks + [tXr[tt % 2]], [tH[tt]], lambda: nc.vector.scalar_tensor_tensor(
                    out=hap, in0=xr, scalar=ALPHA, in1=bm.ap, op0=ALU.mult, op1=ALU.add))
                layer_norm_tile(hap, tH[tt], 0, s)
                S.op("act", [tH[tt]], [tHb[tt % 2]], lambda: nc.scalar.copy(out=hbW[tt % 2], in_=hap))
                transpose_to(hbW[tt % 2], tHb[tt % 2], actT, tt, tHT[tt])
            if stop_after == "P6":
                dump("h1", hview, tH)
                break
            S.barrier(engines=("act", "dve", "pool", "sp"))
            xo = v3(K[:, 0:4096], 512)
            KxT = v3(K[:, 4096:6144], 256)
            Vx = v3(K[:, 6144:8192], 1024)
            memT = v3(K[:, 8192:10240], 256)
            membf = v3(K[:, 10240:12288], 1024)
            hbK = [K[:, 12288:13312], K[:, 13312:14336]]
            QxT = [K[:, 14336:14848], K[:, 14848:15360]]
            PTx = [K[:, 15360:15872], K[:, 15872:16384]]
            wk = v3(W[:, 0:8192], 1024)
            wv = v3(W[:, 8192:16384], 1024)
            wq = v3(W[:, 16384:24576], 1024)
            tMem, tMemT, tKx, tVx, tXo, tRdx = T(), T(), T(), T(), T(), T()
            tWk, tWv, tWq = [T() for _ in range(8)], [T() for _ in range(8)], [T() for _ in range(8)]
            tQx, tPx, tRt = [T(), T()], [T(), T()], T()
            for k in range(8):
                S.dma("sp", out=wk[:, k, :], in_=DWB["xwk"][k * 128:(k + 1) * 128, :], reads=[tDW["xwk"][k]], writes=[tWk[k]])
            S.dma("pool", out=membf, in_=mem[s].rearrange("(t p) d -> p t d", p=128), writes=[tMem])
            for k in range(8):
                S.dma("sp", out=wv[:, k, :], in_=DWB["xwv"][k * 128:(k + 1) * 128, :], reads=[tDW["xwv"][k]], writes=[tWv[k]])
            for k in range(8):
                S.dma("sp", out=wq[:, k, :], in_=DWB["xwq"][k * 128:(k + 1) * 128, :], reads=[tDW["xwq"][k]], writes=[tWq[k]])
            load_ln(1)
            for mt in range(2):
                pb = ps1()
                S.op("pe", [tMem, tC], pb.toks, lambda: [nc.tensor.transpose(
                    pb.bf[:, k * 128:(k + 1) * 128], membf[:, mt, k * 128:(k + 1) * 128], ident_bf[:]) for k in range(8)])
                S.op("act", pb.toks, [tMemT], lambda: nc.scalar.copy(out=memT[:, :, mt * 128:(mt + 1) * 128], in_=v3(pb.bf[:, 0:1024], 128)))
            for c in range(8):
                b_ = ps1()
                S.op("pe", [tMemT] + tWk, b_.toks, lambda: [nc.tensor.matmul(
                    b_.ap[:, 0:256], wk[:, k, c * 128:(c + 1) * 128], memT[:, k, :], start=(k == 0), stop=(k == 7)) for k in range(8)])
                S.op("act", b_.toks, [tKx], lambda: nc.scalar.copy(out=KxT[:, c, :], in_=b_.ap[:, 0:256]))
            for mt in range(2):
                b2 = ps2()
                S.op("pe", [tMemT] + tWv, b2.toks, lambda: [nc.tensor.matmul(
                    b2.ap[:, hf * 512:(hf + 1) * 512], memT[:, k, mt * 128:(mt + 1) * 128], wv[:, k, hf * 512:(hf + 1) * 512],
                    start=(k == 0), stop=(k == 7)) for hf in range(2) for k in range(8)])
                S.op("act", b2.toks, [tVx], lambda: nc.scalar.copy(out=Vx[:, mt, :], in_=b2.ap))
            wo = wk
            for k in range(8):
                S.dma("sp", out=wo[:, k, :], in_=DWB["xwo"][k * 128:(k + 1) * 128, :], reads=[tDW["xwo"][k]], writes=[tWk[k]])
            XSC = 256.0 ** -0.5
            lg = small[:, 64:100]
            gmax, ngmax, gsum, ggate = small[:, 100:101], small[:, 101:102], small[:, 102:103], small[:, 103:104]
            gone, ge, pen = small[:, 104:108], small[:, 108:112], small[:, 112:116]
            me, one1, me2, one2 = small[:, 116:148], small[:, 148:180], small[:, 180:212], small[:, 212:244]
            m1, m2, d21, e21, g1, g2 = (small[:, 244 + i:245 + i] for i in range(6))
            QxTg = [[K[:, 14336 + (i * 2 + dc) * 512:14336 + (i * 2 + dc + 1) * 512] for dc in range(2)] for i in range(2)]
            PTxg = [[K[:, 10240 + (i * 2 + mt) * 512:10240 + (i * 2 + mt + 1) * 512] for mt in range(2)] for i in range(2)]
            tQxg = [[T(), T()], [T(), T()]]
            tPxg = [[T(), T()], [T(), T()]]
            tRdxg = [T(), T()]
            units = [(tg, h) for tg in range(NG) for h in range(4)]

            def xa_A(i):
                tg, h = units[i]
                cols = slice(tg * 512, (tg + 1) * 512)
                for dc in range(2):
                    c = h * 2 + dc
                    bq_ = ps1()
                    S.op("pe", tHT[tg * 4:tg * 4 + 4] + tWq, bq_.toks, lambda: [nc.tensor.matmul(
                        bq_.ap, wq[:, k, c * 128:(c + 1) * 128], actT[:, k, cols], start=(k == 0), stop=(k == 7)) for k in range(8)])
                    S.op("act", bq_.toks, [tQxg[i % 2][dc]], lambda: nc.scalar.copy(out=QxTg[i % 2][dc], in_=bq_.ap))

            def xa_B(i):
                tg, h = units[i]
                for mt in range(2):
                    bs_ = ps1()
                    S.op("pe", tQxg[i % 2] + [tKx, tMem], bs_.toks, lambda: [nc.tensor.matmul(
                        bs_.ap, KxT[:, h * 2 + dc, mt * 128:(mt + 1) * 128], QxTg[i % 2][dc], start=(dc == 0), stop=(dc == 1)) for dc in range(2)])
                    S.op("act", bs_.toks, [tPxg[i % 2][mt]], lambda: nc.scalar.activation(out=PTxg[i % 2][mt], in_=bs_.ap, func=AF.Exp, scale=XSC))

            def xa_C(i):
                tg, h = units[i]
                ptx = PTxg[i % 2]
                rd_ = rdx[:, (i % 2) * 512:(i % 2 + 1) * 512]
                bd_ = ps1()
                S.op("pe", tPxg[i % 2] + [tC], bd_.toks, lambda: [nc.tensor.matmul(
                    bd_.ap, ones_bf[:], ptx[mt], start=(mt == 0), stop=(mt == 1)) for mt in range(2)])
                S.op("dve", bd_.toks, [tRdxg[i % 2]], lambda: nc.vector.reciprocal(out=rd_, in_=bd_.ap))
                for dc in range(2):
                    c = h * 2 + dc
                    bo_ = ps1()
                    S.op("pe", tPxg[i % 2] + [tVx], bo_.toks, lambda: [nc.tensor.matmul(
                        bo_.ap, Vx[:, mt, c * 128:(c + 1) * 128], ptx[mt], start=(mt == 0), stop=(mt == 1)) for mt in range(2)])
                    S.op("dve", bo_.toks + [tRdxg[i % 2]], [tXo], lambda: nc.vector.tensor_tensor(out=xo[:, c, :], in0=bo_.ap, in1=rd_, op=ALU.mult))

            def xa_mm(tt):
                j = tt % 4
                bm = ps2()
                S.op("pe", [tXo] + tWk, bm.toks, lambda: [nc.tensor.matmul(
                    bm.ap[:, hf * 512:(hf + 1) * 512], xo[:, c, j * 128:(j + 1) * 128], wo[:, c, hf * 512:(hf + 1) * 512],
                    start=(c == 0), stop=(c == 7)) for hf in range(2) for c in range(8)])
                return bm

            V_ = nc.vector
            AXX_ = mybir.AxisListType.X
            tRtS = [T(), T()]

            def xa_tail(tt, bm, si):
                rb0 = 64 + si * 256
                lg = small[:, rb0:rb0 + 36]
                gmax, ngmax, gsum, ggate = (small[:, rb0 + 36 + i:rb0 + 37 + i] for i in range(4))
                gone, ge, pen = small[:, rb0 + 40:rb0 + 44], small[:, rb0 + 44:rb0 + 48], small[:, rb0 + 48:rb0 + 52]
                me, me2 = small[:, rb0 + 52:rb0 + 84], small[:, rb0 + 84:rb0 + 116]
                m1, m2, d21, e21, g1 = (small[:, rb0 + 116 + i:rb0 + 117 + i] for i in range(5))
                tRt = tRtS[si]
                rt = lambda f: S.op("dve", [tRt], [tRt], f)
                hap = hview[:, tt, :]
                tHt = tH[tt]
                gt = s * NT + tt
                S.op("dve", bm.toks, [tHt], lambda: nc.vector.scalar_tensor_tensor(
                    out=hap, in0=hap, scalar=ALPHA, in1=bm.ap, op0=ALU.mult, op1=ALU.add))
                j_ = lnstate["i"] % 4
                lnstate["i"] += 1
                tS = tLNS[j_]
                base = j_ * 16
                st, mv = lnst[:, base:base + 12], lnst[:, base + 12:base + 14]
                rs, nm = lnst[:, base + 14:base + 15], lnst[:, base + 15:base + 16]
                S.op("dve", [tHt], [tS], lambda: nc.vector.bn_stats(out=st[:, 0:6], in_=hap[:, 0:512]))
                S.op("dve", [tHt], [tS], lambda: nc.vector.bn_stats(out=st[:, 6:12], in_=hap[:, 512:1024]))
                S.op("dve", [], [tS], lambda: nc.vector.bn_aggr(out=mv, in_=st))
                yield
                rstd_from(rs, mv[:, 1:2], 1.0, [tS], [tS])
                yield
                S.op("dve", [tS], [tS], lambda: nc.vector.scalar_tensor_tensor(
                    out=nm, in0=mv[:, 0:1], scalar=-1.0, in1=rs, op0=ALU.mult, op1=ALU.mult))
                yield
                S.op("act", [tS], [tHt], lambda: nc.scalar.activation(out=hap, in_=hap, func=AF.Identity, scale=rs, bias=nm))
                yield
                S.op("dve", [tLN], [tHt], lambda: nc.vector.tensor_tensor(out=hap, in0=hap, in1=lng[:], op=ALU.mult))
                yield
                S.op("dve", [tLN], [tHt], lambda: nc.vector.tensor_tensor(out=hap, in0=hap, in1=lnb[:], op=ALU.add))
                yield
                S.op("act", [tHt], [tHb[tt % 2]], lambda: nc.scalar.copy(out=hbK[tt % 2], in_=hap))
                yield
                transpose_to(hbK[tt % 2], tHb[tt % 2], actT, tt, tHT[tt])
                yield
                bl = ps1()
                S.op("pe", [tHT[tt], tC], bl.toks, lambda: [nc.tensor.matmul(
                    bl.ap[:, 0:36], actT[:, k, tt * 128:(tt + 1) * 128], v3(rw[:], 36)[:, k, :], start=(k == 0), stop=(k == 7)) for k in range(8)])
                yield
                S.op("dve", bl.toks + [tC], [tRt], lambda: V_.tensor_tensor(out=lg, in0=bl.ap[:, 0:36], in1=rb[:], op=ALU.add))
                rt(lambda: V_.reduce_max(out=gmax, in_=lg[:, 0:4], axis=AXX_))
                rt(lambda: V_.tensor_scalar(out=gone, in0=lg[:, 0:4], scalar1=gmax, scalar2=None, op0=ALU.is_equal))
                rt(lambda: V_.tensor_scalar_mul(out=ngmax, in0=gmax, scalar1=-1.0))
                yield
                S.op("act", [tRt], [tRt], lambda: nc.scalar.activation(out=ge, in_=lg[:, 0:4], func=AF.Exp, bias=ngmax))
                yield
                rt(lambda: V_.reduce_sum(out=gsum, in_=ge, axis=AXX_))
                rt(lambda: V_.reciprocal(out=ggate, in_=gsum))
                rt(lambda: V_.tensor_scalar(out=pen, in0=gone, scalar1=-1.0, scalar2=1e9, op0=ALU.add, op1=ALU.mult))
                rt(lambda: V_.tensor_tensor(out=v3(me, 8), in0=v3(lg[:, 4:36], 8), in1=pen.unsqueeze(2).to_broadcast([128, 4, 8]), op=ALU.add))
                rt(lambda: V_.reduce_max(out=m1, in_=me, axis=AXX_))
                o1 = ONE[:, (gt * 2) * 32:(gt * 2 + 1) * 32]
                o2 = ONE[:, (gt * 2 + 1) * 32:(gt * 2 + 2) * 32]
                S.op("dve", [tRt], [tRt, tRk], lambda: V_.tensor_scalar(out=o1, in0=me, scalar1=m1, scalar2=None, op0=ALU.is_equal))
                rt(lambda: V_.scalar_tensor_tensor(out=me2, in0=o1, scalar=-1e9, in1=me, op0=ALU.mult, op1=ALU.add))
                rt(lambda: V_.reduce_max(out=m2, in_=me2, axis=AXX_))
                S.op("dve", [tRt], [tRt, tRk], lambda: V_.tensor_scalar(out=o2, in0=me2, scalar1=m2, scalar2=None, op0=ALU.is_equal))
                rt(lambda: V_.tensor_tensor(out=d21, in0=m2, in1=m1, op=ALU.subtract))
                yield
                S.op("act", [tRt], [tRt], lambda: nc.scalar.activation(out=e21, in_=d21, func=AF.Exp))
                yield
                rt(lambda: V_.tensor_scalar_add(out=g1, in0=e21, scalar1=1.0))
                rt(lambda: V_.reciprocal(out=g1, in_=g1))
                S.op("dve", [tRt], [tRt, tRk], lambda: V_.tensor_tensor(out=G12[:, gt * 2:gt * 2 + 1], in0=g1, in1=ggate, op=ALU.mult))
                S.op("dve", [tRt], [tRt, tRk], lambda: V_.tensor_tensor(out=G12[:, gt * 2 + 1:gt * 2 + 2], in0=G12[:, gt * 2:gt * 2 + 1], in1=e21, op=ALU.mult))
                bR = ps1()
                S.op("pe", [tRk, tC], bR.toks, lambda: [
                    nc.tensor.matmul(bR.ap[:, 0:64], su_bf[:], ONE[:, gt * 64:(gt + 1) * 64], start=True, stop=True),
                    nc.tensor.matmul(bR.ap[:, 64:128], ones_bf[:], ONE[:, gt * 64:(gt + 1) * 64], start=True, stop=True)])
                yield
                ta, tb = me, me2
                S.op("dve", bR.toks + [tRk, tRt], [tRt], lambda: V_.tensor_tensor(out=ta, in0=bR.ap[:, 0:32], in1=Crun[:], op=ALU.add))
                rt(lambda: V_.tensor_tensor(out=tb, in0=ta, in1=o1, op=ALU.mult))
                S.op("dve", [tRt], [tRt, tRk], lambda: V_.reduce_sum(out=RK[:, gt * 2:gt * 2 + 1], in_=tb, axis=AXX_))
                S.op("dve", bR.toks + [tRk, tRt], [tRt], lambda: V_.tensor_tensor(out=ta, in0=bR.ap[:, 32:64], in1=Crun[:], op=ALU.add))
                S.op("dve", bR.toks + [tRt], [tRt], lambda: V_.tensor_tensor(out=ta, in0=bR.ap[:, 64:96], in1=ta, op=ALU.add))
                rt(lambda: V_.tensor_tensor(out=tb, in0=ta, in1=o2, op=ALU.mult))
                S.op("dve", [tRt], [tRt, tRk], lambda: V_.reduce_sum(out=RK[:, gt * 2 + 1:gt * 2 + 2], in_=tb, axis=AXX_))
                S.op("dve", bR.toks + [tRt], [tRk], lambda: V_.tensor_tensor(out=Crun[:], in0=bR.ap[:, 64:96], in1=Crun[:], op=ALU.add))
                S.op("dve", bR.toks + [tRt], [tRk], lambda: V_.tensor_tensor(out=Crun[:], in0=bR.ap[:, 96:128], in1=Crun[:], op=ALU.add))
                S.dma("sp", out=XB[gt * 128:(gt + 1) * 128, :], in_=hbK[tt % 2], reads=[tHb[tt % 2]])
                S.dma("sp", out=H2D[gt * 128:(gt + 1) * 128, :], in_=hap, reads=[tHt])
                yield

            xa_A(0)
            for ui in range(len(units)):
                xa_B(ui)
                if ui + 1 < len(units):
                    xa_A(ui + 1)
                xa_C(ui)
                tg, h = units[ui]
                if h != 3:
                    continue
                for pr in ((0, 1), (2, 3)):
                    gens = []
                    for si, j in enumerate(pr):
                        tt = tg * 4 + j
                        gens.append(xa_tail(tt, xa_mm(tt), si))
                    alive_ = [True, True]
                    while any(alive_):
                        for gi in range(2):
                            if alive_[gi] and next(gens[gi], "done") == "done":
                                alive_[gi] = False
            if stop_after == "P7":
                dump("h2", hview, tH)
                break
            S.barrier(engines=("act", "dve", "pool", "sp"))
        if stop_after is None:
            S.barrier(full=True)
            V_ = nc.vector
            AXX = mybir.AxisListType.X
            tM = T()
            padc, pA, pB, basev = small[:, 116:148], small[:, 148:180], small[:, 180:212], small[:, 212:244]
            cmpb = H[:, 0:1024]
            mo = lambda f, extra=(): S.op("dve", [tM, tRk, tC] + list(extra), [tM], f)
            mo(lambda: V_.tensor_tensor(out=v3(cmpb, 32), in0=Crun[:].unsqueeze(2).to_broadcast([128, 32, 32]),
                                        in1=thr[:].unsqueeze(1).to_broadcast([128, 32, 32]), op=ALU.is_gt))
            mo(lambda: V_.reduce_sum(out=padc, in_=v3(cmpb, 32), axis=AXX))
            mo(lambda: V_.tensor_scalar_mul(out=padc, in0=padc, scalar1=float(TSZ)))
            cur, nxt = padc, pA
            for sft in (1, 2, 4, 8, 16):
                mo(lambda: V_.tensor_copy(out=nxt[:, 0:sft], in_=cur[:, 0:sft]))
                mo(lambda: V_.tensor_tensor(out=nxt[:, sft:32], in0=cur[:, sft:32], in1=cur[:, 0:32 - sft], op=ALU.add))
                cur, nxt = nxt, (pB if nxt is pA else pA)
            endv = cur
            mo(lambda: V_.tensor_tensor(out=basev, in0=endv, in1=padc, op=ALU.subtract))
            cmpt = H[:, 2048:2048 + NTI * 32]
            tef = mo_f[:, 0:NTI]
            widx = mo_i[:, 0:NTI]
            mo(lambda: V_.tensor_tensor(out=v3(cmpt, 32), in0=endv.unsqueeze(1).to_broadcast([128, NTI, 32]),
                                        in1=tstart[:, 0:NTI].unsqueeze(2).to_broadcast([128, NTI, 32]), op=ALU.is_le))
            mo(lambda: V_.reduce_sum(out=tef, in_=v3(cmpt, 32), axis=AXX))
            mo(lambda: V_.tensor_scalar(out=tef, in0=tef, scalar1=128.0, scalar2=pcol[:, 0:1], op0=ALU.mult, op1=ALU.add))
            mo(lambda: V_.tensor_copy(out=widx, in_=tef))
            tmp3 = H[:, 4096:4096 + GT * 64]
            posf = mo_f[:, 64:64 + GT * 2]
            posI = mo_i[:, 64:64 + GT * 2]
            mo(lambda: V_.tensor_tensor(out=v3(tmp3, 32), in0=v3(ONE[:], 32), in1=basev.unsqueeze(1).to_broadcast([128, GT * 2, 32]), op=ALU.mult))
            mo(lambda: V_.reduce_sum(out=posf, in_=v3(tmp3, 32), axis=AXX))
            mo(lambda: V_.tensor_tensor(out=posf, in0=posf, in1=RK[:], op=ALU.add))
            mo(lambda: V_.tensor_copy(out=posI, in_=posf))
            xbt = [A[:, i * 1024:(i + 1) * 1024] for i in range(2)]
            tXb = [T(), T()]
            for gt in range(GT):
                S.dma("sp", out=xbt[gt % 2], in_=XB[gt * 128:(gt + 1) * 128, :], writes=[tXb[gt % 2]])
                for k in range(2):
                    S.dma("pool", out=XS[:, :], in_=xbt[gt % 2], out_off=posI[:, gt * 2 + k:gt * 2 + k + 1], bound=NTI * TSZ - 1,
                          reads=[tXb[gt % 2], tM])
            S.barrier()
            xsb = [v3(A[:, 2048 + i * 4096:2048 + i * 4096 + TB * 1024], 1024) for i in range(2)]
            xst = [v3(K[:, 4096 + i * 4096:4096 + i * 4096 + 8 * TSZ], TSZ) for i in range(2)]
            Ssb = [Kf(0, 2 * TSZ), Kf(1024, 1024 + 2 * TSZ)]
            HD = [v3(K[:, 2048:2048 + 2 * TSZ], TSZ), v3(K[:, 3072:3072 + 2 * TSZ], TSZ)]
            Ysb = [K[:, 12288:13312], K[:, 13312:14336]]
            tXsb, tXst, tSs, tHD, tY = [T(), T()], [T(), T()], [T(), T()], [T(), T()], [T(), T()]
            NWB = 3
            Eb = [W[:, i * 6144:(i + 1) * 6144] for i in range(NWB)]
            tEw = [[T(), T(), T()] for _ in range(NWB)]

            def moe_prefetch_w(ti):
                eb = Eb[ti % NWB]
                for part, src in enumerate((WGB, WUB, WDB)):
                    S.dma("pool", out=eb[:, part * 2048:(part + 1) * 2048], in_=src[:, :], in_off=widx[:, ti:ti + 1], bound=NEXP * 128 - 1,
                          reads=[tM], writes=[tEw[ti % NWB][part]])

            def moe_prefetch_x(ti):
                S.dma("sp", out=xsb[ti % 2], in_=XS[ti * TSZ:(ti + 1) * TSZ, :].rearrange("(j p) d -> p j d", p=128), writes=[tXsb[ti % 2]])

            def moe_TR(ti):
                xs_, txs = xsb[ti % 2], tXsb[ti % 2]
                xt_, txt = xst[ti % 2], tXst[ti % 2]
                for j in range(TB):
                    pb = ps1()
                    S.op("pe", [txs, tC], pb.toks, lambda: [nc.tensor.transpose(
                        pb.bf[:, k * 128:(k + 1) * 128], xs_[:, j, k * 128:(k + 1) * 128], ident_bf[:]) for k in range(8)])
                    if j % 2 == 0:
                        S.op("act", pb.toks, [txt], lambda: nc.scalar.copy(out=xt_[:, :, j * 128:(j + 1) * 128], in_=v3(pb.bf[:, 0:1024], 128)))
                    else:
                        S.op("dve", pb.toks, [txt], lambda: nc.vector.tensor_copy(out=xt_[:, :, j * 128:(j + 1) * 128], in_=v3(pb.bf[:, 0:1024], 128)))

            def moe_GU(ti):
                eb = Eb[ti % NWB]
                tE3 = tEw[ti % NWB]
                wg = v3(eb[:, 0:2048], 256)
                wu = v3(eb[:, 2048:4096], 256)
                xt_, txt = xst[ti % 2], tXst[ti % 2]
                hd, thd = HD[ti % 2], tHD[ti % 2]
                for fc in range(2):
                    bg, bu = ps1(), ps1()
                    S.op("pe", [txt, tE3[0]], bg.toks, lambda: [nc.tensor.matmul(
                        bg.ap[:, 0:TSZ], wg[:, k, fc * 128:(fc + 1) * 128], xt_[:, k, :], start=(k == 0), stop=(k == 7)) for k in range(8)])
                    S.op("pe", [txt, tE3[1]], bu.toks, lambda: [nc.tensor.matmul(
                        bu.ap[:, 0:TSZ], wu[:, k, fc * 128:(fc + 1) * 128], xt_[:, k, :], start=(k == 0), stop=(k == 7)) for k in range(8)])
                    S.op("act", bg.toks, [tSs[fc]], lambda: nc.scalar.activation(out=Ssb[fc], in_=bg.ap[:, 0:TSZ], func=AF.Silu))
                    S.op("dve", bu.toks + [tSs[fc]], [thd], lambda: nc.vector.tensor_tensor(out=hd[:, fc, :], in0=Ssb[fc], in1=bu.ap[:, 0:TSZ], op=ALU.mult))

            def moe_D(ti):
                eb = Eb[ti % NWB]
                tE3 = tEw[ti % NWB]
                wd = v3(eb[:, 4096:6144], 1024)
                hd, thd = HD[ti % 2], tHD[ti % 2]
                for j in range(TB):
                    bd2 = ps2()
                    S.op("pe", [thd, tE3[2]], bd2.toks, lambda: [nc.tensor.matmul(
                        bd2.ap[:, hf * 512:(hf + 1) * 512], hd[:, fc, j * 128:(j + 1) * 128], wd[:, fc, hf * 512:(hf + 1) * 512],
                        start=(fc == 0), stop=(fc == 1)) for hf in range(2) for fc in range(2)])
                    yb, ty = Ysb[moe_state["yi"] % 2], tY[moe_state["yi"] % 2]
                    moe_state["yi"] += 1
                    S.op("act", bd2.toks, [ty], lambda: nc.scalar.copy(out=yb, in_=bd2.ap))
                    S.dma("sp", out=YS[ti * TSZ + j * 128:ti * TSZ + (j + 1) * 128, :], in_=yb, reads=[ty])

            moe_state = {"yi": 0}
            moe_prefetch_w(0)
            moe_prefetch_x(0)
            if NTI > 1:
                moe_prefetch_w(1)
                moe_prefetch_x(1)
            moe_TR(0)
            moe_GU(0)
            for ti in range(NTI):
                if ti + 2 < NTI:
                    moe_prefetch_w(ti + 2)
                if ti + 1 < NTI:
                    moe_TR(ti + 1)
                if ti + 2 < NTI:
                    moe_prefetch_x(ti + 2)
                moe_D(ti)
                if ti + 1 < NTI:
                    moe_GU(ti + 1)
            S.barrier()
            load_ln(2)
            NYB = 4
            ybuf = [[H[:, i * 3072:i * 3072 + 512].bitcast(BF16), H[:, i * 3072 + 512:i * 3072 + 1024].bitcast(BF16),
                     H[:, i * 3072 + 1024:i * 3072 + 2048], H[:, i * 3072 + 2048:i * 3072 + 3072]] for i in range(NYB)]
            tYb = [[T(), T(), T(), T()] for _ in range(NYB)]

            def comb_prefetch(gt):
                y1, y2, ytmp, hh = ybuf[gt % NYB]
                t1_, t2_, tt_, th = tYb[gt % NYB]
                S.dma("pool", out=y1, in_=YS[:, :], in_off=posI[:, gt * 2:gt * 2 + 1], bound=NTI * TSZ - 1, reads=[tM], writes=[t1_])
                S.dma("pool", out=y2, in_=YS[:, :], in_off=posI[:, gt * 2 + 1:gt * 2 + 2], bound=NTI * TSZ - 1, reads=[tM], writes=[t2_])
                S.dma("sp", out=hh, in_=H2D[gt * 128:(gt + 1) * 128, :], writes=[th])

            def comb_gen(gt):
                y1, y2, ytmp, hh = ybuf[gt % NYB]
                t1_, t2_, tt_, th = tYb[gt % NYB]
                S.op("act", [t1_, tRk], [tt_], lambda: nc.scalar.activation(out=ytmp, in_=y1, func=AF.Copy, scale=G12[:, gt * 2:gt * 2 + 1]))
                yield
                S.op("dve", [t2_, tRk, tt_], [tt_], lambda: V_.scalar_tensor_tensor(
                    out=ytmp, in0=y2, scalar=G12[:, gt * 2 + 1:gt * 2 + 2], in1=ytmp, op0=ALU.mult, op1=ALU.add))
                S.op("dve", [tt_], [th], lambda: V_.scalar_tensor_tensor(out=hh, in0=hh, scalar=ALPHA, in1=ytmp, op0=ALU.mult, op1=ALU.add))
                j_ = lnstate["i"] % 4
                lnstate["i"] += 1
                tS = tLNS[j_]
                base = j_ * 16
                st, mv = lnst[:, base:base + 12], lnst[:, base + 12:base + 14]
                rs, nm = lnst[:, base + 14:base + 15], lnst[:, base + 15:base + 16]
                S.op("dve", [th], [tS], lambda: nc.vector.bn_stats(out=st[:, 0:6], in_=hh[:, 0:512]))
                S.op("dve", [th], [tS], lambda: nc.vector.bn_stats(out=st[:, 6:12], in_=hh[:, 512:1024]))
                S.op("dve", [], [tS], lambda: nc.vector.bn_aggr(out=mv, in_=st))
                yield
                rstd_from(rs, mv[:, 1:2], 1.0, [tS], [tS])
                yield
                S.op("dve", [tS], [tS], lambda: nc.vector.scalar_tensor_tensor(
                    out=nm, in0=mv[:, 0:1], scalar=-1.0, in1=rs, op0=ALU.mult, op1=ALU.mult))
                yield
                S.op("act", [tS], [th], lambda: nc.scalar.activation(out=hh, in_=hh, func=AF.Identity, scale=rs, bias=nm))
                yield
                S.op("dve", [tLN], [th], lambda: nc.vector.tensor_tensor(out=hh, in0=hh, in1=lng[:], op=ALU.mult))
                yield
                S.op("dve", [tLN], [th], lambda: nc.vector.tensor_tensor(out=hh, in0=hh, in1=lnb[:], op=ALU.add))
                sq_, tq_ = gt // NT, gt % NT
                S.dma("sp", out=out_d[sq_, tq_ * 128:(tq_ + 1) * 128, :], in_=hh, reads=[th])
                yield

            comb_prefetch(0)
            comb_prefetch(1)
            for g0 in range(0, GT, 2):
                for gn in (g0 + 2, g0 + 3):
                    if gn < GT:
                        comb_prefetch(gn)
                gens = [comb_gen(g0), comb_gen(g0 + 1)]
                alive_ = [True, True]
                while any(alive_):
                    for gi in range(2):
                        if alive_[gi] and next(gens[gi], "done") == "done":
                            alive_[gi] = False
        S.barrier(engines=("sp",), full=True)
    return nc


def _rope_tables():
    pos = np.arange(SEQ, dtype=np.float32)
    inv_freq = (np.float32(10000.0) ** (-(np.arange(0, 32, 2, dtype=np.float32)) / np.float32(32))).astype(np.float32)
    ang = (pos[:, None] * inv_freq[None, :]).astype(np.float32)
    cos = np.cos(ang).astype(np.float32)
    sin = np.sin(ang).astype(np.float32)
    cc = np.zeros((128, SEQ), np.float32)
    ss = np.zeros((128, SEQ), np.float32)
    cc[0:64] = 1.0
    cc[64:80] = cos.T
    cc[80:96] = cos.T
    ss[64:80] = -sin.T
    ss[80:96] = sin.T
    return cc, ss


def prep_shared(inp):
    f = np.float32
    g = lambda k: np.asarray(inp[k], dtype=f)[0]
    w_in = g("w_in")
    wkr = np.zeros((D, 96), f)
    wkr[:, 64:80] = w_in[:, 400:416]
    wkr[:, 80:96] = w_in[:, 384:400]
    wq = g("w_q_up")
    perm = np.arange(768)
    for h in range(8):
        for j in range(32):
            perm[h * 96 + 64 + j] = h * 96 + 64 + (j + 16) % 32
    wq_sw = wq[:, perm]
    convw = g("ssd_conv_w").reshape(4, 8, 128).transpose(2, 1, 0).reshape(128, 32)
    convb = g("ssd_conv_b").reshape(8, 128).T
    dtb = np.broadcast_to(np.tile(g("ssd_dt_bias"), 16)[None, :], (128, 128))
    alog = np.broadcast_to(g("ssd_a_log")[None, :], (128, 8))
    sd = g("ssd_d")
    dcol = np.stack([sd[pair * 2 + (np.arange(128) // 64)] for pair in range(4)], axis=1)
    ncol = g("ssd_norm").reshape(4, 128).T
    lnp = np.stack([np.broadcast_to(g(k)[None, :], (128, D)) for k in ("ln1_g", "ln1_b", "ln2_g", "ln2_b", "ln3_g", "ln3_b")])
    rw = np.concatenate([g("router_group_w"), g("router_expert_w")], axis=1)
    rb = np.broadcast_to(np.concatenate([g("router_group_b"), g("router_expert_b")])[None, :], (128, 36))
    tri = np.triu(np.ones((128, 128), f))
    nm = np.where(np.arange(128)[None, :] >= np.arange(128)[:, None], 0.0, -30000.0).astype(f)
    cc, ss = _rope_tables()
    su = np.triu(np.ones((128, 128), f), k=1)
    thr = np.broadcast_to((np.arange(32, dtype=f) * 384.0)[None, :], (128, 32))
    tstart = np.broadcast_to((np.arange(64, dtype=f) * 384.0)[None, :], (128, 64))
    pcol = np.arange(128, dtype=f).reshape(128, 1)
    sh = {
        "su": su, "thr": thr, "tstart": tstart, "pcol": pcol,
        "w_in": w_in, "wkr_sw": wkr, "wq": wq, "wq_sw": wq_sw, "qn": g("mla_q_norm").reshape(2, 128).T,
        "wkv": g("w_kv_up"), "kvn": g("mla_kv_norm").reshape(128, 1), "convw": convw, "convb": convb,
        "dtb": dtb, "alog": alog, "dcol": dcol, "ncol": ncol, "wout": g("w_out"), "xwq": g("xa_wq"),
        "xwk": g("xa_wk"), "xwv": g("xa_wv"), "xwo": g("xa_wo"), "lnp": lnp, "rw": rw, "rb": rb,
        "wg": g("expert_w_gate"), "wu": g("expert_w_up"), "wd": g("expert_w_down"),
        "ident": np.eye(128, dtype=f), "tri": tri, "negmask": np.tile(nm, (1, 4)), "cc": cc, "ss": ss,
    }
    return {k: np.ascontiguousarray(v, dtype=f) for k, v in sh.items()}


def kernel(**inputs):
    sh = prep_shared(inputs)
    x = np.asarray(inputs["x"], dtype=np.float32)
    mem = np.asarray(inputs["mem"], dtype=np.float32)
    nc = build(n_seq=2)
    in_maps = []
    for c in range(N_CORES):
        m = dict(sh)
        m["x"] = np.ascontiguousarray(x[2 * c:2 * c + 2])
        m["mem"] = np.ascontiguousarray(mem[2 * c:2 * c + 2])
        in_maps.append(m)
    res = run_bass_kernel_spmd(nc, in_maps, core_ids=list(range(N_CORES)))
    return np.concatenate([r["out"] for r in res.results], axis=0)
```

```python
import numpy as np
from contextlib import ExitStack
import concourse.bass as bass
import concourse.mybir as mybir
from concourse.bass_utils import run_bass_kernel_spmd

F32 = mybir.dt.float32
BF16 = mybir.dt.bfloat16
AF = mybir.ActivationFunctionType
ALU = mybir.AluOpType

N_CORES = 8
SEQ = 2048
D = 1024
NT = SEQ // 128
NG = SEQ // 512
ALPHA = 2.0 ** 0.25
EPS = 1e-5
NEXP = 32


class T:
    __slots__ = ("w", "r")

    def __init__(self):
        self.w = None
        self.r = {}


class Sched:
    def __init__(self, nc, es, n_dma=80):
        self.nc = nc
        self.eng = {"pe": nc.tensor, "act": nc.scalar, "dve": nc.vector, "pool": nc.gpsimd, "sp": nc.sync}
        self.sem = {k: es.enter_context(nc.semaphore("s_" + k)) for k in ("pe", "act", "dve", "pool")}
        self.cnt = {k: 0 for k in self.sem}
        self.dsem = [es.enter_context(nc.semaphore("d%d" % i)) for i in range(n_dma)]
        self.dcnt = [0] * n_dma
        n_cast = 16
        self.qslots = {"sp": list(range(0, (n_dma - n_cast) // 2)), "pool": list(range((n_dma - n_cast) // 2, n_dma - n_cast)),
                       "cast": list(range(n_dma - n_cast, n_dma))}
        self.qnext = {"sp": 0, "pool": 0, "cast": 0}
        self.seen = {}
        self.bregs = {}

    def _semof(self, key):
        return self.sem[key] if isinstance(key, str) else self.dsem[key]

    def _need(self, eng, reads, writes):
        need = {}

        def add(k, v):
            if k == eng:
                if eng == "pe":
                    return
                if self.cnt[eng] - v >= 4:
                    return
            if self.seen.get((eng, k), 0) >= v:
                return
            if need.get(k, 0) < v:
                need[k] = v

        for t in reads:
            if t.w is not None:
                add(*t.w)
        for t in writes:
            if t.w is not None:
                add(*t.w)
            for k, v in t.r.items():
                add(k, v)
        return need

    def _wait(self, eng, key, val):
        if self.seen.get((eng, key), 0) >= val:
            return
        self.eng[eng].wait_ge(self._semof(key), val)
        self.seen[(eng, key)] = val

    def _waits(self, eng, reads, writes):
        for k, v in self._need(eng, reads, writes).items():
            self._wait(eng, k, v)

    def _commit(self, ticket, reads, writes):
        k, v = ticket
        for t in reads:
            if t.r.get(k, 0) < v:
                t.r[k] = v
        for t in writes:
            t.w = ticket
            t.r = {}

    def op(self, eng, reads, writes, emit):
        need = self._need(eng, reads, writes)
        keys = list(need)
        attach = keys[-1] if keys else None
        for k in keys[:-1]:
            self._wait(eng, k, need[k])
        r = emit()
        first, last = (r[0], r[-1]) if isinstance(r, list) else (r, r)
        if attach is not None:
            first._wait_ge(self._semof(attach), need[attach])
            self.seen[(eng, attach)] = need[attach]
        self.cnt[eng] += 1
        last.then_inc(self.sem[eng], 1)
        self._commit((eng, self.cnt[eng]), reads, writes)

    def dma(self, q, out, in_, reads=(), writes=(), out_off=None, in_off=None, bound=None, slots=None):
        self._waits(q, reads, writes)
        sp_ = slots or q
        sl = self.qslots[sp_]
        slot = sl[self.qnext[sp_]]
        self.qnext[sp_] = (self.qnext[sp_] + 1) % len(sl)
        if self.dcnt[slot] > 0:
            self._wait(q, slot, self.dcnt[slot])
        if out_off is None and in_off is None:
            inst = self.eng[q].dma_start(out=out, in_=in_)
        else:
            if bound not in self.bregs:
                self.bregs[bound] = self.nc.gpsimd.to_reg(bound)
            bound = self.bregs[bound]
            inst = self.nc.gpsimd.indirect_dma_start(
                out=out, out_offset=None if out_off is None else bass.IndirectOffsetOnAxis(ap=out_off, axis=0),
                in_=in_, in_offset=None if in_off is None else bass.IndirectOffsetOnAxis(ap=in_off, axis=0),
                bounds_check=bound, oob_is_err=False)
        inst.then_inc(self.dsem[slot], 16)
        self.dcnt[slot] += 16
        self._commit((slot, self.dcnt[slot]), reads, writes)

    def barrier(self, engines=("pe", "act", "dve", "pool", "sp"), full=False):
        skip = () if full else set(self.qslots["cast"])
        for e in engines:
            for k in self.sem:
                if self.cnt[k] > 0:
                    self._wait(e, k, self.cnt[k])
            for s in range(len(self.dsem)):
                if self.dcnt[s] > 0 and s not in skip:
                    self._wait(e, s, self.dcnt[s])


def v3(ap, b):
    return ap.rearrange("p (a b) -> p a b", b=b)


def build(n_seq=2, stop_after=None, dumps=()):
    nc = bass.Bass("TRN2", target_bir_lowering=False)

    def din(name, shape):
        return nc.dram_tensor(name, list(shape), F32, kind="ExternalInput").ap()

    x = din("x", [n_seq, SEQ, D])
    mem = din("mem", [n_seq, 256, D])
    w_in = din("w_in", [D, 1960])
    wkr_sw = din("wkr_sw", [D, 96])
    wq_d = din("wq", [256, 768])
    wqsw_d = din("wq_sw", [256, 768])
    qn_d = din("qn", [128, 2])
    wkv_d = din("wkv", [128, 1024])
    kvn_d = din("kvn", [128, 1])
    convw_d = din("convw", [128, 32])
    convb_d = din("convb", [128, 8])
    dtb_d = din("dtb", [128, 128])
    alog_d = din("alog", [128, 8])
    dcol_d = din("dcol", [128, 4])
    ncol_d = din("ncol", [128, 4])
    wout_d = din("wout", [D, D])
    xwq_d = din("xwq", [D, D])
    xwk_d = din("xwk", [D, D])
    xwv_d = din("xwv", [D, D])
    xwo_d = din("xwo", [D, D])
    lnp_d = din("lnp", [6, 128, D])
    rw_d = din("rw", [D, 36])
    rb_d = din("rb", [128, 36])
    wg_d = din("wg", [NEXP, D, 256])
    wu_d = din("wu", [NEXP, D, 256])
    wd_d = din("wd", [NEXP, 256, D])
    ident_d = din("ident", [128, 128])
    tri_d = din("tri", [128, 128])
    negmask_d = din("negmask", [128, 512])
    cc_d = din("cc", [128, SEQ])
    ss_d = din("ss", [128, SEQ])
    su_d = din("su", [128, 128])
    thr_d = din("thr", [128, 32])
    pcol_d = din("pcol", [128, 1])
    GT = n_seq * NT
    NTOK = n_seq * SEQ
    TB = 3
    TSZ = 128 * TB
    NTI = -(-(2 * NTOK) // TSZ) + 31
    tstart_d = din("tstart", [128, 64])
    out_d = nc.dram_tensor("out", [n_seq, SEQ, D], F32, kind="ExternalOutput").ap()
    XB = nc.dram_tensor("XB", [NTOK, D], BF16, kind="Internal").ap()
    H2D = nc.dram_tensor("H2D", [NTOK, D], F32, kind="Internal").ap()
    XS = nc.dram_tensor("XS", [NTI * TSZ, D], BF16, kind="Internal").ap()
    YS = nc.dram_tensor("YS", [NTI * TSZ, D], BF16, kind="Internal").ap()
    DWB = {n: nc.dram_tensor("DWB_" + n, [D, D], BF16, kind="Internal").ap() for n in ("wout", "xwq", "xwk", "xwv", "xwo")}
    WGB = nc.dram_tensor("WGB", [NEXP * 128, 2048], BF16, kind="Internal").ap()
    WUB = nc.dram_tensor("WUB", [NEXP * 128, 2048], BF16, kind="Internal").ap()
    WDB = nc.dram_tensor("WDB", [NEXP * 128, 2048], BF16, kind="Internal").ap()
    dump_d = {}
    for name, shape in dumps:
        dump_d[name] = nc.dram_tensor("dbg_" + name, list(shape), F32, kind="ExternalOutput").ap()

    es = ExitStack()
    with es:
        S = Sched(nc, es)

        def sb(name, shape, dt):
            return es.enter_context(nc.sbuf_tensor(name, list(shape), dt))

        ident_bf = sb("ident_bf", [128, 128], BF16)
        ones_bf = sb("ones_bf", [128, 128], BF16)
        ones_f = sb("ones_f", [128, 128], F32)
        tri_f = sb("tri_f", [128, 128], F32)
        ident_f = sb("ident_f", [128, 128], F32)
        negmask = sb("negmask_s", [128, 512], F32)
        cc = sb("cc_s", [128, SEQ], BF16)
        ss = sb("ss_s", [128, SEQ], BF16)
        lng = sb("lng", [128, D], F32)
        lnb = sb("lnb", [128, D], F32)
        qn = sb("qn_s", [128, 2], F32)
        kvn = sb("kvn_s", [128, 1], F32)
        convw = sb("convw_s", [128, 32], F32)
        convb = sb("convb_s", [128, 8], F32)
        dtb = sb("dtb_s", [128, 128], F32)
        a_bc = sb("a_bc", [128, 8], F32)
        dcol = sb("dcol_s", [128, 4], F32)
        ncol = sb("ncol_s", [128, 4], F32)
        rb = sb("rb_s", [128, 36], F32)
        rw = sb("rw_s", [128, 8 * 36], BF16)
        small = sb("small", [128, 512], F32)
        su_bf = sb("su_bf", [128, 128], BF16)
        thr = sb("thr_s", [128, 32], F32)
        pcol = sb("pcol_s", [128, 1], F32)
        tstart = sb("tstart_s", [128, 64], F32)
        ONE = sb("ONE", [128, GT * 64], BF16)
        RK = sb("RK", [128, GT * 2], F32)
        G12 = sb("G12", [128, GT * 2], F32)
        Crun = sb("Crun", [128, 32], F32)
        mo_f = sb("mo_f", [128, 128], F32)
        mo_i = sb("mo_i", [128, 128], mybir.dt.int32)
        tRk = T()
        rdx = cc[:].bitcast(F32)
        dt_tok = sb("dt_tok", [128, 128], F32)
        tC = T()
        tLN = T()
        tSmall = T()

        A = sb("arenaA", [128, 16384], BF16)
        H = sb("arenaH", [128, 16384], F32)
        K = sb("arenaK", [128, 16384], BF16)
        W = sb("arenaW", [128, 24576], BF16)
        lnst = sb("lnst", [128, 64], F32)
        tLNS = [T() for _ in range(4)]
        lnstate = {"i": 0}
        PS = [es.enter_context(nc.psum_tensor("ps%d" % i, [128, 1024], F32)) for i in range(4)]
        PT_ = [T() for _ in range(8)]
        pstate = {"b": 0}

        class Bank:
            def __init__(self, ap, toks):
                self.ap = ap
                self.toks = toks

            @property
            def bf(self):
                return self.ap.bitcast(BF16)

        busy = set()

        def ps1(hold=False):
            b = pstate["b"]
            while b in busy:
                b = (b + 1) % 8
            pstate["b"] = (b + 1) % 8
            bk_ = Bank(PS[b // 2][:, (b % 2) * 512:(b % 2) * 512 + 512], [PT_[b]])
            bk_.idx = [b]
            if hold:
                busy.add(b)
            return bk_

        def ps2(hold=False):
            b = pstate["b"]
            if b % 2:
                b = (b + 1) % 8
            while b in busy or (b + 1) in busy:
                b = (b + 2) % 8
            pstate["b"] = (b + 2) % 8
            bk_ = Bank(PS[b // 2][:, :], [PT_[b], PT_[b + 1]])
            bk_.idx = [b, b + 1]
            if hold:
                busy.update(bk_.idx)
            return bk_

        def psrel(bk_):
            for b in bk_.idx:
                busy.discard(b)

        def Wf(lo, hi):
            return W[:, lo:hi].bitcast(F32)

        def Kf(lo, hi):
            return K[:, lo:hi].bitcast(F32)

        def Hb(lo, hi):
            return H[:, lo:hi].bitcast(BF16)

        def dump(name, ap, toks):
            if name in dump_d:
                S.dma("pool", out=dump_d[name], in_=ap, reads=toks)

        def ld(q, dst, src):
            S.dma(q, out=dst, in_=src, writes=[tC])

        ld("pool", ident_bf[:], ident_d[:, :])
        ld("sp", ident_f[:], ident_d[:, :])
        ld("sp", tri_f[:], tri_d[:, :])
        ld("sp", negmask[:], negmask_d[:, :])
        ld("pool", su_bf[:], su_d[:, :])
        ld("sp", thr[:], thr_d[:, :])
        ld("sp", pcol[:], pcol_d[:, :])
        ld("sp", tstart[:], tstart_d[:, :])
        S.op("dve", [], [tRk], lambda: nc.vector.memset(Crun[:], 0.0))
        ld("pool", cc[:], cc_d[:, :])
        ld("pool", ss[:], ss_d[:, :])
        for dst, src in ((qn, qn_d), (kvn, kvn_d), (convw, convw_d), (convb, convb_d), (dtb, dtb_d),
                         (a_bc, alog_d), (dcol, dcol_d), (ncol, ncol_d), (rb, rb_d)):
            ld("sp", dst[:], src[:, :])
        ld("pool", v3(rw[:], 36), rw_d.rearrange("(k p) c -> p k c", p=128))
        S.op("dve", [], [tC], lambda: nc.vector.memset(ones_f[:], 1.0))
        S.op("dve", [], [tC], lambda: nc.vector.memset(ones_bf[:], 1.0))
        S.op("act", [tC], [tC], lambda: nc.scalar.activation(out=a_bc[:], in_=a_bc[:], func=AF.Exp))
        S.op("dve", [tC], [tC], lambda: nc.vector.tensor_scalar_mul(out=a_bc[:], in0=a_bc[:], scalar1=-1.0))
        S.barrier()

        def rstd_from(dst, src, scale, reads, writes):
            S.op("act", reads, writes, lambda: nc.scalar.activation(out=dst, in_=src, func=AF.Ln, scale=scale, bias=EPS))
            S.op("act", [], writes, lambda: nc.scalar.activation(out=dst, in_=dst, func=AF.Exp, scale=-0.5))

        def layer_norm_tile(hap, tH, li, s):
            j = lnstate["i"] % 4
            lnstate["i"] += 1
            tS = tLNS[j]
            base = j * 16
            st = lnst[:, base:base + 12]
            mv = lnst[:, base + 12:base + 14]
            rs = lnst[:, base + 14:base + 15]
            nm = lnst[:, base + 15:base + 16]
            S.op("dve", [tH], [tS], lambda: nc.vector.bn_stats(out=st[:, 0:6], in_=hap[:, 0:512]))
            S.op("dve", [tH], [tS], lambda: nc.vector.bn_stats(out=st[:, 6:12], in_=hap[:, 512:1024]))
            S.op("dve", [], [tS], lambda: nc.vector.bn_aggr(out=mv, in_=st))
            rstd_from(rs, mv[:, 1:2], 1.0, [tS], [tS])
            S.op("dve", [tS], [tS], lambda: nc.vector.scalar_tensor_tensor(
                out=nm, in0=mv[:, 0:1], scalar=-1.0, in1=rs, op0=ALU.mult, op1=ALU.mult))
            S.op("act", [tS], [tH], lambda: nc.scalar.activation(out=hap, in_=hap, func=AF.Identity, scale=rs, bias=nm))
            S.op("dve", [tLN], [tH], lambda: nc.vector.tensor_tensor(out=hap, in0=hap, in1=lng[:], op=ALU.mult))
            S.op("dve", [tLN], [tH], lambda: nc.vector.tensor_tensor(out=hap, in0=hap, in1=lnb[:], op=ALU.add))

        def load_ln(li):
            S.dma("sp", out=lng[:], in_=lnp_d[2 * li, :, :], writes=[tLN])
            S.dma("sp", out=lnb[:], in_=lnp_d[2 * li + 1, :, :], writes=[tLN])

        def transpose_to(hb, tHB, dstT, tt, tDst):
            pb = ps1()
            S.op("pe", [tHB, tC], pb.toks, lambda: [nc.tensor.transpose(
                pb.bf[:, k * 128:(k + 1) * 128], hb[:, k * 128:(k + 1) * 128], ident_bf[:]) for k in range(8)])
            S.op("act", pb.toks, [tDst], lambda: nc.scalar.copy(
                out=dstT[:, :, tt * 128:(tt + 1) * 128], in_=v3(pb.bf[:, 0:1024], 128)))

        actT = v3(A[:, :], SEQ)
        catT = v3(K[:, :], SEQ)
        hview = v3(H[:, :], D)

        for s in range(n_seq):
            tXT = [T() for _ in range(NT)]
            if s > 0:
                S.dma("pool", out=cc[:], in_=cc_d[:, :], writes=[tC])
            NXB = 4
            xin = [K[:, 0:1024], K[:, 1024:2048], K[:, 8192:9216], K[:, 9216:10240]]
            txin = [T() for _ in range(NXB)]
            win = v3(W[:, 0:15680], 1960)
            wkr = v3(W[:, 15680:16448], 96)
            wq_s = v3(W[:, 16448:17984], 768)
            wq_sw = v3(W[:, 17984:19520], 768)
            wkv_s = W[:, 19520:20544]
            tWinL, tWs = [T() for _ in range(9)], T()
            for tt in range(NXB):
                S.dma("pool", out=xin[tt], in_=x[s, tt * 128:(tt + 1) * 128, :], writes=[txin[tt]])
            WCG = [(0, 416), (416, 928), (928, 1440), (1440, 1960)]
            w_in3 = w_in.rearrange("(k p) c -> p k c", p=128)
            S.dma("pool", out=win[:, :, 0:416], in_=w_in3[:, :, 0:416], writes=[tWinL[0]])
            S.dma("pool", out=wkr, in_=wkr_sw.rearrange("(k p) c -> p k c", p=128), writes=[tWinL[8]])
            for gi in range(1, 4):
                S.dma("pool", out=win[:, :, WCG[gi][0]:WCG[gi][1]], in_=w_in3[:, :, WCG[gi][0]:WCG[gi][1]], writes=[tWinL[gi]])
            S.dma("pool", out=wq_s, in_=wq_d.rearrange("(k p) c -> p k c", p=128), writes=[tWs])
            S.dma("pool", out=wq_sw, in_=wqsw_d.rearrange("(k p) c -> p k c", p=128), writes=[tWs])
            S.dma("pool", out=wkv_s, in_=wkv_d[:, :], writes=[tWs])
            for r in range(2):
                S.op("dve", [tWs, tC], [tWs], lambda r=r: nc.vector.tensor_scalar_mul(out=wq_s[:, r, :], in0=wq_s[:, r, :], scalar1=qn[:, r:r + 1]))
                S.op("dve", [tWs, tC], [tWs], lambda r=r: nc.vector.tensor_scalar_mul(out=wq_sw[:, r, :], in0=wq_sw[:, r, :], scalar1=qn[:, r:r + 1]))
            S.op("dve", [tWs, tC], [tWs], lambda: nc.vector.tensor_scalar_mul(out=wkv_s, in0=wkv_s, scalar1=kvn[:, 0:1]))
            for tt in range(NT):
                transpose_to(xin[tt % NXB], txin[tt % NXB], actT, tt, tXT[tt])
                if tt + NXB < NT:
                    S.dma("pool", out=xin[tt % NXB], in_=x[s, (tt + NXB) * 128:(tt + NXB + 1) * 128, :], writes=[txin[tt % NXB]])
            if s == 0:
                tDW = {n: [T() for _ in range(8)] for n in DWB}
                for n, src in (("wout", wout_d), ("xwk", xwk_d), ("xwv", xwv_d), ("xwq", xwq_d), ("xwo", xwo_d)):
                    for k in range(8):
                        S.dma("pool", out=DWB[n][k * 128:(k + 1) * 128, :], in_=src[k * 128:(k + 1) * 128, :], writes=[tDW[n][k]], slots="cast")
            if s == 0 and stop_after is None:
                for e in range(NEXP):
                    rows = slice(e * 128, (e + 1) * 128)
                    S.dma("pool", out=WGB[rows, :].rearrange("p (k f) -> p k f", k=8), in_=wg_d[e].rearrange("(k p) f -> p k f", p=128), writes=[T()], slots="cast")
                    S.dma("pool", out=WUB[rows, :].rearrange("p (k f) -> p k f", k=8), in_=wu_d[e].rearrange("(k p) f -> p k f", p=128), writes=[T()], slots="cast")
                    S.dma("pool", out=WDB[rows, :].rearrange("p (k f) -> p k f", k=2), in_=wd_d[e].rearrange("(k p) f -> p k f", p=128), writes=[T()], slots="cast")
            if stop_after == "P1":
                dump("xT", actT[:, 0, :], tXT)
                break

            zs = v3(Hb(0, 4096), SEQ)
            xsT = v3(Hb(4096, 8192), SEQ)
            BT = v3(Hb(8192, 10240), SEQ)
            CT = v3(Hb(10240, 12288), SEQ)
            cqT = v3(Hb(12288, 14336), SEQ)
            ckvT = Hb(14336, 15360)
            kpe = Hb(15360, 16384)
            tZ, tXs, tB, tCt, tCq, tCkv, tKpe = T(), T(), T(), T(), T(), T(), T()
            pre = [Kf(2048 + i * 1040, 2048 + i * 1040 + 1030) for i in range(2)]
            acc = [Kf(4128 + i * 1024, 4128 + (i + 1) * 1024) for i in range(2)]
            sqb = K[:, 6176:6688]
            rsb = Kf(6688, 7712)
            tPre, tAcc, tSq, tRs = [T(), T()], [T(), T()], T(), T()
            tDt = T()

            def proj_mm(bank, M, lhs_fn, tg, wt=0):
                S.op("pe", list(tXT[tg * 4:tg * 4 + 4]) + [tWinL[wt]], bank.toks, lambda: [nc.tensor.matmul(
                    bank.ap[0:M, :], lhs_fn(k), actT[:, k, tg * 512:(tg + 1) * 512], start=(k == 0), stop=(k == 7)) for k in range(8)])

            for tg in range(NG):
                cols = slice(tg * 512, (tg + 1) * 512)
                bq = [ps1(), ps1()]
                for r in range(2):
                    proj_mm(bq[r], 128, lambda k, r=r: win[:, k, r * 128:(r + 1) * 128], tg)
                bs = ps1()
                for r in range(2):
                    S.op("act", bq[r].toks, [tSq], lambda r=r: nc.scalar.activation(out=sqb, in_=bq[r].ap, func=AF.Square))
                    S.op("pe", [tSq, tC], bs.toks, lambda r=r: nc.tensor.matmul(bs.ap, ones_bf[:], sqb, start=(r == 0), stop=(r == 1)))
                rstd_from(rsb, bs.ap, 1.0 / 256, bs.toks, [tRs])
                for r in range(2):
                    S.op("dve", bq[r].toks + [tRs], [tCq], lambda r=r: nc.vector.tensor_tensor(out=cqT[:, r, cols], in0=bq[r].ap, in1=rsb, op=ALU.mult))
                bk = ps1()
                proj_mm(bk, 128, lambda k: win[:, k, 256:384], tg)
                bs = ps1()
                S.op("act", bk.toks, [tSq], lambda: nc.scalar.activation(out=sqb, in_=bk.ap, func=AF.Square))
                S.op("pe", [tSq, tC], bs.toks, lambda: nc.tensor.matmul(bs.ap, ones_bf[:], sqb, start=True, stop=True))
                rstd_from(rsb, bs.ap, 1.0 / 128, bs.toks, [tRs])
                S.op("dve", bk.toks + [tRs], [tCkv], lambda: nc.vector.tensor_tensor(out=ckvT[:, cols], in0=bk.ap, in1=rsb, op=ALU.mult))
                ba, bb = ps1(), ps1()
                proj_mm(ba, 96, lambda k: win[:, k, 320:416], tg)
                proj_mm(bb, 96, lambda k: wkr[:, k, :], tg, 8)
                t1 = acc[0]
                t2 = acc[1]
                S.op("dve", ba.toks + [tC], [tAcc[0]], lambda: nc.vector.tensor_tensor(out=t1[64:96, :], in0=ba.ap[64:96, :], in1=cc[64:96, cols], op=ALU.mult))
                S.op("dve", bb.toks + [tC], [tAcc[1]], lambda: nc.vector.tensor_tensor(out=t2[64:96, :], in0=bb.ap[64:96, :], in1=ss[64:96, cols], op=ALU.mult))
                S.op("dve", [tAcc[0], tAcc[1]], [tKpe], lambda: nc.vector.tensor_tensor(out=kpe[64:96, cols], in0=t1[64:96, :], in1=t2[64:96, :], op=ALU.add))
                for j in range(4):
                    bz = ps1()
                    proj_mm(bz, 128, lambda k, j=j: win[:, k, 416 + j * 128:416 + (j + 1) * 128], tg, 1)
                    S.op("act", bz.toks, [tZ], lambda j=j, bz=bz: nc.scalar.activation(out=zs[:, j, cols], in_=bz.ap, func=AF.Silu))
            for c in range(8):
                if c < 4:
                    dst, tdst = xsT[:, c, :], tXs
                elif c < 6:
                    dst, tdst = BT[:, c - 4, :], tB
                else:
                    dst, tdst = CT[:, c - 6, :], tCt
                S.op("dve", [], [tPre[0]], lambda: nc.vector.memset(pre[0][:, 0:3], 0.0))
                for tg in range(NG):
                    p_, a_ = pre[tg % 2], acc[tg % 2]
                    tp, ta = tPre[tg % 2], tAcc[tg % 2]
                    bx = ps1()
                    proj_mm(bx, 128, lambda k, c=c: win[:, k, 928 + c * 128:928 + (c + 1) * 128], tg, 2 if c < 4 else 3)
                    S.op("act", bx.toks, [tp], lambda: nc.scalar.copy(out=p_[:, 3:515], in_=bx.ap))
                    if tg < NG - 1:
                        S.op("act", [tp], [tPre[(tg + 1) % 2]], lambda: nc.scalar.copy(out=pre[(tg + 1) % 2][:, 0:3], in_=p_[:, 512:515]))
                    S.op("dve", [tp, tC], [ta], lambda: nc.vector.tensor_scalar_mul(out=a_, in0=p_[:, 0:512], scalar1=convw[:, c * 4:c * 4 + 1]))
                    for j in range(1, 4):
                        S.op("dve", [tp, tC], [ta], lambda j=j: nc.vector.scalar_tensor_tensor(
                            out=a_, in0=p_[:, j:j + 512], scalar=convw[:, c * 4 + j:c * 4 + j + 1], in1=a_, op0=ALU.mult, op1=ALU.add))
                    S.op("act", [ta, tC], [tdst], lambda: nc.scalar.activation(
                        out=dst[:, tg * 512:(tg + 1) * 512], in_=a_, func=AF.Silu, bias=convb[:, c:c + 1]))
            bd = ps1()
            for tt in range(NT):
                S.op("pe", [tXT[tt], tWinL[3]], bd.toks, lambda tt=tt: [nc.tensor.matmul(
                    bd.ap[:, tt * 8:(tt + 1) * 8], actT[:, k, tt * 128:(tt + 1) * 128], win[:, k, 1952:1960],
                    start=(k == 0), stop=(k == 7)) for k in range(8)])
            S.op("dve", bd.toks + [tC], [tDt], lambda: nc.vector.tensor_tensor(out=dt_tok[:], in0=bd.ap[:, 0:128], in1=dtb[:], op=ALU.add))
            S.op("act", [tDt], [tDt], lambda: nc.scalar.activation(out=dt_tok[:], in_=dt_tok[:], func=AF.Exp))
            S.op("act", [tDt], [tDt], lambda: nc.scalar.activation(out=dt_tok[:], in_=dt_tok[:], func=AF.Ln, bias=1.0))
            if stop_after == "P2":
                dump("cqT", cqT[:, 0, :], [tCq])
                dump("ckvT", ckvT, [tCkv])
                dump("kpe", kpe, [tKpe])
                dump("zs", zs[:, 0, :], [tZ])
                dump("xsT", xsT[:, 0, :], [tXs])
                dump("BT", BT[:, 0, :], [tB])
                dump("dt", dt_tok[:], [tDt])
                break
            S.barrier(engines=("act", "dve", "pool", "sp"))
            qT = [A[:, i * 2048:(i + 1) * 2048] for i in range(2)]
            kT = [A[:, (2 + i) * 2048:(3 + i) * 2048] for i in range(2)]
            Vaug = [v3(A[:, (4 + i) * 2048:(5 + i) * 2048], 128) for i in range(2)]
            PTb = [A[:, 12288 + i * 512:12288 + (i + 1) * 512] for i in range(3)]
            rdb = A[:, 13824:14848].bitcast(F32)
            t1 = [Wf(0, 1024), A[:, 14848:15872].bitcast(F32)]
            t2 = [Wf(1024, 2048), Wf(13568, 14592)]
            tQ, tK, tV, tPT = [T(), T()], [T(), T()], [T(), T()], [T(), T(), T()]
            tT1, tT2, tRd = [T(), T()], [T(), T()], T()
            tCat = [T() for _ in range(8)]
            S.op("dve", [], [tV[0]], lambda: nc.vector.memset(Vaug[0][:, :, 64:128], 1.0))
            S.op("dve", [], [tV[1]], lambda: nc.vector.memset(Vaug[1][:, :, 0:64], 1.0))
            if s == 0 and stop_after is None:
                ztile = W[:, 14592:15616]
                tZ = T()
                S.op("dve", [], [tZ], lambda: nc.vector.memset(ztile, 0.0))
                for j in range(NTI * TB):
                    S.dma("sp", out=XS[j * 128:(j + 1) * 128, :], in_=ztile, reads=[tZ])
            SCALE = 96.0 ** -0.5
            pstate_pt = {"i": 0}

            def qkv_head(h):
                hp = h % 2
                q_h, k_h, V_h = qT[hp], kT[hp], Vaug[hp]
                tq, tk, tv = tQ[hp], tK[hp], tV[hp]
                for tg in range(NG):
                    cols = slice(tg * 512, (tg + 1) * 512)
                    S.op("dve", [tKpe], [tk], lambda: nc.vector.tensor_copy(out=k_h[64:96, cols], in_=kpe[64:96, cols]))
                    yield
                    ba, bb = ps1(), ps1()
                    S.op("pe", [tWs, tCq], ba.toks, lambda: [nc.tensor.matmul(
                        ba.ap[0:96, :], wq_s[:, r, h * 96:(h + 1) * 96], cqT[:, r, cols], start=(r == 0), stop=(r == 1)) for r in range(2)])
                    S.op("pe", [tWs, tCq], bb.toks, lambda: [nc.tensor.matmul(
                        bb.ap[0:96, :], wq_sw[:, r, h * 96:(h + 1) * 96], cqT[:, r, cols], start=(r == 0), stop=(r == 1)) for r in range(2)])
                    bk = ps1()
                    S.op("pe", [tWs, tCkv], bk.toks, lambda: nc.tensor.matmul(
                        bk.ap[0:64, :], wkv_s[:, h * 128:h * 128 + 64], ckvT[:, cols], start=True, stop=True))
                    tt1, tt2 = tT1[tg % 2], tT2[tg % 2]
                    u1, u2 = t1[tg % 2], t2[tg % 2]
                    S.op("dve", ba.toks + [tC], [tt1], lambda: nc.vector.tensor_tensor(out=u1[0:96, :], in0=ba.ap[0:96, :], in1=cc[0:96, cols], op=ALU.mult))
                    yield
                    S.op("dve", bb.toks + [tC], [tt2], lambda: nc.vector.tensor_tensor(out=u2[0:96, :], in0=bb.ap[0:96, :], in1=ss[0:96, cols], op=ALU.mult))
                    yield
                    S.op("dve", [tt1, tt2], [tq], lambda: nc.vector.tensor_tensor(out=q_h[0:96, cols], in0=u1[0:96, :], in1=u2[0:96, :], op=ALU.add))
                    yield
                    S.op("dve", bk.toks, [tk], lambda: nc.vector.tensor_copy(out=k_h[0:64, cols], in_=bk.ap[0:64, :]))
                    yield
                vo = 0 if hp == 0 else 64
                for half in range(2):
                    bv = ps1()
                    S.op("pe", [tWs, tCkv], bv.toks, lambda: [nc.tensor.matmul(
                        bv.ap[:, j * 64:(j + 1) * 64], ckvT[:, (half * 8 + j) * 128:(half * 8 + j + 1) * 128],
                        wkv_s[:, h * 128 + 64:(h + 1) * 128], start=True, stop=True) for j in range(8)])
                    S.op("dve", bv.toks, [tv], lambda: nc.vector.tensor_copy(out=V_h[:, half * 8:(half + 1) * 8, vo:vo + 64], in_=v3(bv.ap, 64)))
                    yield

            def attn_head(h):
                hp = h % 2
                q_h, k_h, V_h = qT[hp], kT[hp], Vaug[hp]
                tq, tk, tv = tQ[hp], tK[hp], tV[hp]
                orow = slice(0, 64) if hp == 0 else slice(64, 128)
                drow = slice(64, 128) if hp == 0 else slice(0, 64)
                items = [(g, kj) for g in range(NG) for kj in range(4 * g + 4)]
                sbank = {}
                bo_of = {}

                def emit_S(i):
                    g, kj = items[i]
                    c0 = max(0, kj - 4 * g) * 128
                    if g not in bo_of:
                        bo_of[g] = ps1(hold=True)
                    b_ = ps1()
                    sbank[i] = b_
                    S.op("pe", [tq, tk], b_.toks, lambda: nc.tensor.matmul(
                        b_.ap[:, c0:512], k_h[0:96, kj * 128:(kj + 1) * 128], q_h[0:96, g * 512 + c0:(g + 1) * 512], start=True, stop=True))

                LOOK = 2
                for i in range(min(LOOK, len(items))):
                    emit_S(i)
                for i, (g, kj) in enumerate(items):
                    nk = 4 * g + 4
                    c0 = max(0, kj - 4 * g) * 128
                    b_ = sbank.pop(i)
                    pt, tpt = PTb[pstate_pt["i"] % 3], tPT[pstate_pt["i"] % 3]
                    pstate_pt["i"] += 1
                    S.op("act", b_.toks, [tpt], lambda: nc.scalar.activation(out=pt[:, c0:512], in_=b_.ap[:, c0:512], func=AF.Exp, scale=SCALE))
                    if kj >= 4 * g:
                        S.op("dve", [], [tpt], lambda: nc.vector.memset(pt[64:128, c0:c0 + 64], 0.0))
                    if i + LOOK < len(items):
                        emit_S(i + LOOK)
                    bo = bo_of[g]
                    S.op("pe", [tpt, tv], bo.toks, lambda: nc.tensor.matmul(
                        bo.ap[:, c0:512], V_h[:, kj, :], pt[:, c0:512], start=(kj == 0), stop=(kj == nk - 1)))
                    if kj == nk - 1:
                        S.op("dve", bo.toks, [tRd], lambda: nc.vector.reciprocal(out=rdb[drow, :], in_=bo.ap[drow, :]))
                        S.op("dve", bo.toks + [tRd], [tCat[h // 2]], lambda: nc.vector.tensor_tensor(
                            out=catT[orow, h // 2, g * 512:(g + 1) * 512], in0=bo.ap[orow, :], in1=rdb[drow, :], op=ALU.mult))
                        psrel(bo)
                    yield

            def p4_gen():
                for _ in qkv_head(0):
                    pass
                for h in range(8):
                    nxt = qkv_head(h + 1) if h + 1 < 8 else None
                    for n_, _ in enumerate(attn_head(h)):
                        if nxt is not None and n_ % 2 == 1:
                            if next(nxt, "done") == "done":
                                nxt = None
                        yield
                    if nxt is not None:
                        for _ in nxt:
                            pass

            if stop_after == "P4":
                for _ in p4_gen():
                    pass
                dump("cat_attn", catT[:, 0:4, :], tCat)
                break
            def ssd_set(i):
                b0 = 2048 if i == 0 else 15616
                so = 16 if i == 0 else 64
                d = dict(
                    adt=small[:, so:so + 8], acs_sb=small[:, so + 8:so + 16], dte=small[:, so + 16:so + 24],
                    cdk=small[:, so + 24:so + 32], dd=small[:, so + 32:so + 40], nacs=small[:, so + 40:so + 48],
                    R=Wf(b0, b0 + 2048), E=Wf(b0 + 2048, b0 + 3072), DEC=Wf(b0 + 3072, b0 + 4096),
                    Mfin=[W[:, b0 + 4096:b0 + 4608], W[:, b0 + 4608:b0 + 5120]],
                    CexpT=[W[:, b0 + 5120:b0 + 5632], W[:, b0 + 5632:b0 + 6144]],
                    xdt=W[:, b0 + 6144:b0 + 6656], xdte=W[:, b0 + 6656:b0 + 7168], Btok=W[:, b0 + 7168:b0 + 7424],
                    cbm=[Wf(b0 + 7424, b0 + 7680), Wf(b0 + 7680, b0 + 7936)],
                    tAdt=T(), tSm2=T(), tR=T(), tE=T(), tDec=T(), tXdt=T(), tXdte=T(), tBtok=T(), tCbm=T(),
                    tMf=[T(), T()], tCe=[T(), T()])
                return d

            SB = [ssd_set(0), ssd_set(1)]
            prev_f = Wf(9984, 11008)
            prev_bf = W[:, 11008:11520]
            yz = Wf(11520, 12544)
            sq = W[:, 12544:13056]
            rstd_g = Wf(13056, 13312)
            yv = Wf(13312, 13568)
            tPrevF, tPrevB, tYv, tYz, tSq2, tRg = T(), T(), T(), T(), T(), T()
            S.op("dve", [], [tWs], lambda: nc.vector.memset(small[:, 250:251], 0.0))
            S.op("act", [], [tWs], lambda: nc.scalar.copy(out=small[:, 251:252], in_=small[:, 250:251]))
            S.op("dve", [], [tPrevF], lambda: nc.vector.memset(prev_f, 0.0))
            S.op("dve", [], [tPrevB], lambda: nc.vector.memset(prev_bf, 0.0))

            def ssd_stage1(c):
                B_ = SB[c % 2]
                adt, acs_sb, dte, cdk, dd, nacs = B_["adt"], B_["acs_sb"], B_["dte"], B_["cdk"], B_["dd"], B_["nacs"]
                R, E, DEC, Mfin, CexpT, xdt, xdte, Btok, cbm = (B_[k] for k in ("R", "E", "DEC", "Mfin", "CexpT", "xdt", "xdte", "Btok", "cbm"))
                tAdt, tSm2, tR, tE, tDec, tXdt, tXdte, tBtok, tCbm, tMf, tCe = (B_[k] for k in (
                    "tAdt", "tSm2", "tR", "tE", "tDec", "tXdt", "tXdte", "tBtok", "tCbm", "tMf", "tCe"))
                tsl = slice(c * 128, (c + 1) * 128)
                dtc = dt_tok[:, c * 8:(c + 1) * 8]
                S.op("dve", [tDt, tC], [tAdt], lambda: nc.vector.tensor_tensor(out=adt, in0=dtc, in1=a_bc[:], op=ALU.mult))
                bx = ps1()
                S.op("pe", [tXs, tC], bx.toks, lambda: [nc.tensor.transpose(
                    bx.bf[:, j * 128:(j + 1) * 128], xsT[:, j, tsl], ident_bf[:]) for j in range(4)])
                S.op("dve", bx.toks + [tDt], [tXdt], lambda: nc.vector.tensor_tensor(
                    out=v3(xdt, 64), in0=v3(bx.bf[:, 0:512], 64), in1=dtc.unsqueeze(2).to_broadcast([128, 8, 64]), op=ALU.mult))
                bb_ = ps1()
                S.op("pe", [tB, tC], bb_.toks, lambda: [nc.tensor.transpose(
                    bb_.bf[:, g * 128:(g + 1) * 128], BT[:, g, tsl], ident_bf[:]) for g in range(2)])
                S.op("act", bb_.toks, [tBtok], lambda: nc.scalar.copy(out=Btok, in_=bb_.bf[:, 0:256]))
                yield
                ba_ = ps1()
                S.op("pe", [tAdt, tC], ba_.toks, lambda: [
                    nc.tensor.matmul(ba_.ap[:, 0:8], tri_f[:], adt, start=True, stop=True),
                    nc.tensor.matmul(ba_.ap[:, 8:16], ones_f[:], adt, start=True, stop=True)])
                S.op("act", ba_.toks, [tSm2], lambda: nc.scalar.copy(out=acs_sb, in_=ba_.ap[:, 0:8]))
                S.op("dve", ba_.toks + [tSm2], [tSm2], lambda: nc.vector.tensor_tensor(out=dd, in0=ba_.ap[:, 8:16], in1=acs_sb, op=ALU.subtract))
                S.op("act", [tSm2], [tSm2], lambda: nc.scalar.activation(out=dte, in_=dd, func=AF.Exp))
                S.op("act", ba_.toks, [tSm2], lambda: nc.scalar.activation(out=cdk, in_=ba_.ap[:, 8:16], func=AF.Exp))
                S.op("dve", [tSm2], [tSm2], lambda: nc.vector.tensor_scalar_mul(out=nacs, in0=acs_sb, scalar1=-1.0))
                yield
                S.op("dve", [tXdt, tSm2], [tXdte], lambda: nc.vector.tensor_tensor(
                    out=v3(xdte, 64), in0=v3(xdt, 64), in1=dte.unsqueeze(2).to_broadcast([128, 8, 64]), op=ALU.mult))
                S.op("dve", [tAdt, tC], [tR], lambda: nc.vector.tensor_tensor(
                    out=v3(R, 128), in0=tri_f[:].unsqueeze(1).to_broadcast([128, 8, 128]),
                    in1=adt.unsqueeze(2).to_broadcast([128, 8, 128]), op=ALU.mult))
                yield
                for g in range(2):
                    bc = ps1()
                    S.op("pe", [tB, tCt], bc.toks, lambda: nc.tensor.matmul(bc.ap[:, 0:128], BT[:, g, tsl], CT[:, g, tsl], start=True, stop=True))
                    S.op("act", bc.toks, [tCbm], lambda: nc.scalar.copy(out=cbm[g], in_=bc.ap[:, 0:128]))
                    bA = ps1()
                    S.op("pe", [tR, tC], bA.toks, lambda: nc.tensor.matmul(bA.ap, ones_f[:], R[:, g * 512:(g + 1) * 512], start=True, stop=True))
                    yield
                    S.op("act", bA.toks, [tDec], lambda: nc.scalar.activation(out=DEC, in_=bA.ap, func=AF.Exp))
                    for hh in range(4):
                        S.op("dve", bA.toks + [tC, tDec, tSm2], [tE], lambda: nc.vector.scalar_tensor_tensor(
                            out=E[:, hh * 128:(hh + 1) * 128], in0=bA.ap[:, hh * 128:(hh + 1) * 128],
                            scalar=nacs[:, g * 4 + hh:g * 4 + hh + 1], in1=negmask[:, 0:128], op0=ALU.add, op1=ALU.add))
                    S.op("act", [tE], [tE], lambda: nc.scalar.activation(out=E, in_=E, func=AF.Exp))
                    yield
                    S.op("dve", [tE, tCbm], [tMf[g]], lambda: nc.vector.tensor_tensor(
                        out=v3(Mfin[g], 128), in0=v3(E, 128), in1=cbm[g].unsqueeze(1).to_broadcast([128, 4, 128]), op=ALU.mult))
                    S.op("dve", [tDec, tCt], [tCe[g]], lambda: nc.vector.tensor_tensor(
                        out=v3(CexpT[g], 128), in0=v3(DEC, 128), in1=CT[:, g, tsl].unsqueeze(1).to_broadcast([128, 4, 128]), op=ALU.mult))
                    yield

            def ssd_stage2(c):
                B_ = SB[c % 2]
                cdk, Mfin, CexpT, xdt, xdte, Btok = (B_[k] for k in ("cdk", "Mfin", "CexpT", "xdt", "xdte", "Btok"))
                tSm2, tXdt, tXdte, tBtok, tMf, tCe = (B_[k] for k in ("tSm2", "tXdt", "tXdte", "tBtok", "tMf", "tCe"))
                tsl = slice(c * 128, (c + 1) * 128)
                for pair in range(4):
                    g = pair // 2
                    by = ps1()
                    for hp in range(2):
                        h = pair * 2 + hp
                        hh = h % 4
                        rows = slice(hp * 64, hp * 64 + 64)
                        tp_ = None if hp == 0 else (0, 64)
                        S.op("pe", [tXdt, tMf[g], tPrevB, tCe[g]], by.toks, lambda: [
                            nc.tensor.matmul(by.ap[rows, 0:128], xdt[:, h * 64:(h + 1) * 64], Mfin[g][:, hh * 128:(hh + 1) * 128],
                                             start=True, stop=False, tile_position=tp_),
                            nc.tensor.matmul(by.ap[rows, 0:128], prev_bf[:, h * 64:(h + 1) * 64], CexpT[g][:, hh * 128:(hh + 1) * 128],
                                             start=False, stop=True, tile_position=tp_)])
                    S.op("dve", by.toks + [tXs, tC], [tYv], lambda: nc.vector.scalar_tensor_tensor(
                        out=yv, in0=xsT[:, pair, tsl], scalar=dcol[:, pair:pair + 1], in1=by.ap[:, 0:128], op0=ALU.mult, op1=ALU.add))
                    S.op("dve", [tYv, tZ], [tYz], lambda: nc.vector.tensor_tensor(
                        out=yz[:, pair * 128:(pair + 1) * 128], in0=yv, in1=zs[:, pair, tsl], op=ALU.mult))
                    S.op("act", [tYz], [tSq2], lambda: nc.scalar.activation(
                        out=sq[:, pair * 128:(pair + 1) * 128], in_=yz[:, pair * 128:(pair + 1) * 128], func=AF.Square))
                    yield
                    if pair % 2 == 1:
                        bn_ = ps1()
                        S.op("pe", [tSq2, tC], bn_.toks, lambda: [nc.tensor.matmul(
                            bn_.ap[:, 0:128], ones_bf[:], sq[:, (pair - 1 + i) * 128:(pair + i) * 128], start=(i == 0), stop=(i == 1)) for i in range(2)])
                        rstd_from(rstd_g, bn_.ap[:, 0:128], 1.0 / 256, bn_.toks, [tRg])
                        for pp in (pair - 1, pair):
                            S.op("dve", [tYz, tRg, tC], [tCat[4 + pp]], lambda: nc.vector.scalar_tensor_tensor(
                                out=catT[:, 4 + pp, tsl], in0=yz[:, pp * 128:(pp + 1) * 128], scalar=ncol[:, pp:pp + 1], in1=rstd_g,
                                op0=ALU.mult, op1=ALU.mult))
                        yield
                bst = ps1()
                S.op("pe", [tBtok, tXdte], bst.toks, lambda: [nc.tensor.matmul(
                    bst.ap[:, g * 256:(g + 1) * 256], Btok[:, g * 128:(g + 1) * 128], xdte[:, g * 256:(g + 1) * 256],
                    start=True, stop=True) for g in range(2)])
                S.op("dve", [tSm2], [tPrevF], lambda: nc.vector.tensor_tensor(
                    out=v3(prev_f, 64), in0=v3(prev_f, 64), in1=cdk.unsqueeze(2).to_broadcast([128, 8, 64]), op=ALU.mult))
                S.op("dve", bst.toks, [tPrevF], lambda: nc.vector.tensor_tensor(out=prev_f, in0=prev_f, in1=bst.ap, op=ALU.add))
                S.op("act", [tPrevF], [tPrevB], lambda: nc.scalar.copy(out=prev_bf, in_=prev_f))
                yield

            def p5_gen():
                yield from ssd_stage1(0)
                for c in range(NT):
                    ga = ssd_stage1(c + 1) if c + 1 < NT else iter(())
                    gb = ssd_stage2(c)
                    a_alive = b_alive = True
                    while a_alive or b_alive:
                        if a_alive and next(ga, "done") == "done":
                            a_alive = False
                        if b_alive and next(gb, "done") == "done":
                            b_alive = False
                        yield

            g4, g5 = p4_gen(), p5_gen()
            alive = {"4": True, "5": True}

            def step(gen, key, n):
                for _ in range(n):
                    if alive[key]:
                        try:
                            next(gen)
                        except StopIteration:
                            alive[key] = False

            while alive["4"]:
                step(g4, "4", 64)
            while alive["5"]:
                step(g5, "5", 64)

            if stop_after == "P5":
                dump("cat_ssd", catT[:, 4:8, :], tCat)
                break
            S.barrier(engines=("act", "dve", "pool", "sp"))
            wout = v3(W[:, 0:8192], 1024)
            tWoL = [T() for _ in range(8)]
            for k in range(8):
                S.dma("sp", out=wout[:, k, :], in_=DWB["wout"][k * 128:(k + 1) * 128, :], reads=[tDW["wout"][k]], writes=[tWoL[k]])
            load_ln(0)
            xres = [Wf(8192, 10240), Wf(10240, 12288)]
            hbW = [W[:, 12288:13312], W[:, 13312:14336]]
            tXr, tHb = [T(), T()], [T(), T()]
            tH = [T() for _ in range(NT)]
            tHT = [T() for _ in range(NT)]
            def p6_mm(tt):
                tok = slice(tt * 128, (tt + 1) * 128)
                xr = xres[tt % 2]
                S.dma("sp", out=xr, in_=x[s, tok, :], writes=[tXr[tt % 2]])
                bm = ps2()
                S.op("pe", tCat + tWoL, bm.toks, lambda: [nc.tensor.matmul(
                    bm.ap[:, hf * 512:(hf + 1) * 512], catT[:, c, tok], wout[:, c, hf * 512:(hf + 1) * 512],
                    start=(c == 0), stop=(c == 7)) for hf in range(2) for c in range(8)])
                return bm

            bms = {0: p6_mm(0)}
            for tt in range(NT):
                if tt + 1 < NT:
                    bms[tt + 1] = p6_mm(tt + 1)
                bm = bms.pop(tt)
                xr = xres[tt % 2]
                hap = hview[:, tt, :]
                S.op("dve", bm.toks + [tXr[tt % 2]], [tH[tt]], lambda: nc.vector.scalar_tensor_tensor(
                    out=hap, in0=xr, scalar=ALPHA, in1=bm.ap, op0=ALU.mult, op1=ALU.add))
                layer_norm_tile(hap, tH[tt], 0, s)
                S.op("act", [tH[tt]], [tHb[tt % 2]], lambda: nc.scalar.copy(out=hbW[tt % 2], in_=hap))
                transpose_to(hbW[tt % 2], tHb[tt % 2], actT, tt, tHT[tt])
            if stop_after == "P6":
                dump("h1", hview, tH)
                break
            S.barrier(engines=("act", "dve", "pool", "sp"))
            xo = v3(K[:, 0:4096], 512)
            KxT = v3(K[:, 4096:6144], 256)
            Vx = v3(K[:, 6144:8192], 1024)
            memT = v3(K[:, 8192:10240], 256)
            membf = v3(K[:, 10240:12288], 1024)
            hbK = [K[:, 12288:13312], K[:, 13312:14336]]
            QxT = [K[:, 14336:14848], K[:, 14848:15360]]
            PTx = [K[:, 15360:15872], K[:, 15872:16384]]
            wk = v3(W[:, 0:8192], 1024)
            wv = v3(W[:, 8192:16384], 1024)
            wq = v3(W[:, 16384:24576], 1024)
            tMem, tMemT, tKx, tVx, tXo, tRdx = T(), T(), T(), T(), T(), T()
            tWk, tWv, tWq = [T() for _ in range(8)], [T() for _ in range(8)], [T() for _ in range(8)]
            tQx, tPx, tRt = [T(), T()], [T(), T()], T()
            for k in range(8):
                S.dma("sp", out=wk[:, k, :], in_=DWB["xwk"][k * 128:(k + 1) * 128, :], reads=[tDW["xwk"][k]], writes=[tWk[k]])
            S.dma("pool", out=membf, in_=mem[s].rearrange("(t p) d -> p t d", p=128), writes=[tMem])
            for k in range(8):
                S.dma("sp", out=wv[:, k, :], in_=DWB["xwv"][k * 128:(k + 1) * 128, :], reads=[tDW["xwv"][k]], writes=[tWv[k]])
            for k in range(8):
                S.dma("sp", out=wq[:, k, :], in_=DWB["xwq"][k * 128:(k + 1) * 128, :], reads=[tDW["xwq"][k]], writes=[tWq[k]])
            load_ln(1)
            for mt in range(2):
                pb = ps1()
                S.op("pe", [tMem, tC], pb.toks, lambda: [nc.tensor.transpose(
                    pb.bf[:, k * 128:(k + 1) * 128], membf[:, mt, k * 128:(k + 1) * 128], ident_bf[:]) for k in range(8)])
                S.op("act", pb.toks, [tMemT], lambda: nc.scalar.copy(out=memT[:, :, mt * 128:(mt + 1) * 128], in_=v3(pb.bf[:, 0:1024], 128)))
            for c in range(8):
                b_ = ps1()
                S.op("pe", [tMemT] + tWk, b_.toks, lambda: [nc.tensor.matmul(
                    b_.ap[:, 0:256], wk[:, k, c * 128:(c + 1) * 128], memT[:, k, :], start=(k == 0), stop=(k == 7)) for k in range(8)])
                S.op("act", b_.toks, [tKx], lambda: nc.scalar.copy(out=KxT[:, c, :], in_=b_.ap[:, 0:256]))
            for mt in range(2):
                b2 = ps2()
                S.op("pe", [tMemT] + tWv, b2.toks, lambda: [nc.tensor.matmul(
                    b2.ap[:, hf * 512:(hf + 1) * 512], memT[:, k, mt * 128:(mt + 1) * 128], wv[:, k, hf * 512:(hf + 1) * 512],
                    start=(k == 0), stop=(k == 7)) for hf in range(2) for k in range(8)])
                S.op("act", b2.toks, [tVx], lambda: nc.scalar.copy(out=Vx[:, mt, :], in_=b2.ap))
            wo = wk
            for k in range(8):
                S.dma("sp", out=wo[:, k, :], in_=DWB["xwo"][k * 128:(k + 1) * 128, :], reads=[tDW["xwo"][k]], writes=[tWk[k]])
            XSC = 256.0 ** -0.5
            lg = small[:, 64:100]
            gmax, ngmax, gsum, ggate = small[:, 100:101], small[:, 101:102], small[:, 102:103], small[:, 103:104]
            gone, ge, pen = small[:, 104:108], small[:, 108:112], small[:, 112:116]
            me, one1, me2, one2 = small[:, 116:148], small[:, 148:180], small[:, 180:212], small[:, 212:244]
            m1, m2, d21, e21, g1, g2 = (small[:, 244 + i:245 + i] for i in range(6))
            QxTg = [[K[:, 14336 + (i * 2 + dc) * 512:14336 + (i * 2 + dc + 1) * 512] for dc in range(2)] for i in range(2)]
            PTxg = [[K[:, 10240 + (i * 2 + mt) * 512:10240 + (i * 2 + mt + 1) * 512] for mt in range(2)] for i in range(2)]
            tQxg = [[T(), T()], [T(), T()]]
            tPxg = [[T(), T()], [T(), T()]]
            tRdxg = [T(), T()]
            units = [(tg, h) for tg in range(NG) for h in range(4)]

            def xa_A(i):
                tg, h = units[i]
                cols = slice(tg * 512, (tg + 1) * 512)
                for dc in range(2):
                    c = h * 2 + dc
                    bq_ = ps1()
                    S.op("pe", tHT[tg * 4:tg * 4 + 4] + tWq, bq_.toks, lambda: [nc.tensor.matmul(
                        bq_.ap, wq[:, k, c * 128:(c + 1) * 128], actT[:, k, cols], start=(k == 0), stop=(k == 7)) for k in range(8)])
                    S.op("act", bq_.toks, [tQxg[i % 2][dc]], lambda: nc.scalar.copy(out=QxTg[i % 2][dc], in_=bq_.ap))

            def xa_B(i):
                tg, h = units[i]
                for mt in range(2):
                    bs_ = ps1()
                    S.op("pe", tQxg[i % 2] + [tKx, tMem], bs_.toks, lambda: [nc.tensor.matmul(
                        bs_.ap, KxT[:, h * 2 + dc, mt * 128:(mt + 1) * 128], QxTg[i % 2][dc], start=(dc == 0), stop=(dc == 1)) for dc in range(2)])
                    S.op("act", bs_.toks, [tPxg[i % 2][mt]], lambda: nc.scalar.activation(out=PTxg[i % 2][mt], in_=bs_.ap, func=AF.Exp, scale=XSC))

            def xa_C(i):
                tg, h = units[i]
                ptx = PTxg[i % 2]
                rd_ = rdx[:, (i % 2) * 512:(i % 2 + 1) * 512]
                bd_ = ps1()
                S.op("pe", tPxg[i % 2] + [tC], bd_.toks, lambda: [nc.tensor.matmul(
                    bd_.ap, ones_bf[:], ptx[mt], start=(mt == 0), stop=(mt == 1)) for mt in range(2)])
                S.op("dve", bd_.toks, [tRdxg[i % 2]], lambda: nc.vector.reciprocal(out=rd_, in_=bd_.ap))
                for dc in range(2):
                    c = h * 2 + dc
                    bo_ = ps1()
                    S.op("pe", tPxg[i % 2] + [tVx], bo_.toks, lambda: [nc.tensor.matmul(
                        bo_.ap, Vx[:, mt, c * 128:(c + 1) * 128], ptx[mt], start=(mt == 0), stop=(mt == 1)) for mt in range(2)])
                    S.op("dve", bo_.toks + [tRdxg[i % 2]], [tXo], lambda: nc.vector.tensor_tensor(out=xo[:, c, :], in0=bo_.ap, in1=rd_, op=ALU.mult))

            def xa_mm(tt):
                j = tt % 4
                bm = ps2()
                S.op("pe", [tXo] + tWk, bm.toks, lambda: [nc.tensor.matmul(
                    bm.ap[:, hf * 512:(hf + 1) * 512], xo[:, c, j * 128:(j + 1) * 128], wo[:, c, hf * 512:(hf + 1) * 512],
                    start=(c == 0), stop=(c == 7)) for hf in range(2) for c in range(8)])
                return bm

            V_ = nc.vector
            AXX_ = mybir.AxisListType.X
            tRtS = [T(), T()]

            def xa_tail(tt, bm, si):
                rb0 = 64 + si * 256
                lg = small[:, rb0:rb0 + 36]
                gmax, ngmax, gsum, ggate = (small[:, rb0 + 36 + i:rb0 + 37 + i] for i in range(4))
                gone, ge, pen = small[:, rb0 + 40:rb0 + 44], small[:, rb0 + 44:rb0 + 48], small[:, rb0 + 48:rb0 + 52]
                me, me2 = small[:, rb0 + 52:rb0 + 84], small[:, rb0 + 84:rb0 + 116]
                m1, m2, d21, e21, g1 = (small[:, rb0 + 116 + i:rb0 + 117 + i] for i in range(5))
                tRt = tRtS[si]
                rt = lambda f: S.op("dve", [tRt], [tRt], f)
                hap = hview[:, tt, :]
                tHt = tH[tt]
                gt = s * NT + tt
                S.op("dve", bm.toks, [tHt], lambda: nc.vector.scalar_tensor_tensor(
                    out=hap, in0=hap, scalar=ALPHA, in1=bm.ap, op0=ALU.mult, op1=ALU.add))
                j_ = lnstate["i"] % 4
                lnstate["i"] += 1
                tS = tLNS[j_]
                base = j_ * 16
                st, mv = lnst[:, base:base + 12], lnst[:, base + 12:base + 14]
                rs, nm = lnst[:, base + 14:base + 15], lnst[:, base + 15:base + 16]
                S.op("dve", [tHt], [tS], lambda: nc.vector.bn_stats(out=st[:, 0:6], in_=hap[:, 0:512]))
                S.op("dve", [tHt], [tS], lambda: nc.vector.bn_stats(out=st[:, 6:12], in_=hap[:, 512:1024]))
                S.op("dve", [], [tS], lambda: nc.vector.bn_aggr(out=mv, in_=st))
                yield
                rstd_from(rs, mv[:, 1:2], 1.0, [tS], [tS])
                yield
                S.op("dve", [tS], [tS], lambda: nc.vector.scalar_tensor_tensor(
                    out=nm, in0=mv[:, 0:1], scalar=-1.0, in1=rs, op0=ALU.mult, op1=ALU.mult))
                yield
                S.op("act", [tS], [tHt], lambda: nc.scalar.activation(out=hap, in_=hap, func=AF.Identity, scale=rs, bias=nm))
                yield
                S.op("dve", [tLN], [tHt], lambda: nc.vector.tensor_tensor(out=hap, in0=hap, in1=lng[:], op=ALU.mult))
                yield
                S.op("dve", [tLN], [tHt], lambda: nc.vector.tensor_tensor(out=hap, in0=hap, in1=lnb[:], op=ALU.add))
                yield
                S.op("act", [tHt], [tHb[tt % 2]], lambda: nc.scalar.copy(out=hbK[tt % 2], in_=hap))
                yield
                transpose_to(hbK[tt % 2], tHb[tt % 2], actT, tt, tHT[tt])
                yield
                bl = ps1()
                S.op("pe", [tHT[tt], tC], bl.toks, lambda: [nc.tensor.matmul(
                    bl.ap[:, 0:36], actT[:, k, tt * 128:(tt + 1) * 128], v3(rw[:], 36)[:, k, :], start=(k == 0), stop=(k == 7)) for k in range(8)])
                yield
                S.op("dve", bl.toks + [tC], [tRt], lambda: V_.tensor_tensor(out=lg, in0=bl.ap[:, 0:36], in1=rb[:], op=ALU.add))
                rt(lambda: V_.reduce_max(out=gmax, in_=lg[:, 0:4], axis=AXX_))
                rt(lambda: V_.tensor_scalar(out=gone, in0=lg[:, 0:4], scalar1=gmax, scalar2=None, op0=ALU.is_equal))
                rt(lambda: V_.tensor_scalar_mul(out=ngmax, in0=gmax, scalar1=-1.0))
                yield
                S.op("act", [tRt], [tRt], lambda: nc.scalar.activation(out=ge, in_=lg[:, 0:4], func=AF.Exp, bias=ngmax))
                yield
                rt(lambda: V_.reduce_sum(out=gsum, in_=ge, axis=AXX_))
                rt(lambda: V_.reciprocal(out=ggate, in_=gsum))
                rt(lambda: V_.tensor_scalar(out=pen, in0=gone, scalar1=-1.0, scalar2=1e9, op0=ALU.add, op1=ALU.mult))
                rt(lambda: V_.tensor_tensor(out=v3(me, 8), in0=v3(lg[:, 4:36], 8), in1=pen.unsqueeze(2).to_broadcast([128, 4, 8]), op=ALU.add))
                rt(lambda: V_.reduce_max(out=m1, in_=me, axis=AXX_))
                o1 = ONE[:, (gt * 2) * 32:(gt * 2 + 1) * 32]
                o2 = ONE[:, (gt * 2 + 1) * 32:(gt * 2 + 2) * 32]
                S.op("dve", [tRt], [tRt, tRk], lambda: V_.tensor_scalar(out=o1, in0=me, scalar1=m1, scalar2=None, op0=ALU.is_equal))
                rt(lambda: V_.scalar_tensor_tensor(out=me2, in0=o1, scalar=-1e9, in1=me, op0=ALU.mult, op1=ALU.add))
                rt(lambda: V_.reduce_max(out=m2, in_=me2, axis=AXX_))
                S.op("dve", [tRt], [tRt, tRk], lambda: V_.tensor_scalar(out=o2, in0=me2, scalar1=m2, scalar2=None, op0=ALU.is_equal))
                rt(lambda: V_.tensor_tensor(out=d21, in0=m2, in1=m1, op=ALU.subtract))
                yield
                S.op("act", [tRt], [tRt], lambda: nc.scalar.activation(out=e21, in_=d21, func=AF.Exp))
                yield
                rt(lambda: V_.tensor_scalar_add(out=g1, in0=e21, scalar1=1.0))
                rt(lambda: V_.reciprocal(out=g1, in_=g1))
                S.op("dve", [tRt], [tRt, tRk], lambda: V_.tensor_tensor(out=G12[:, gt * 2:gt * 2 + 1], in0=g1, in1=ggate, op=ALU.mult))
                S.op("dve", [tRt], [tRt, tRk], lambda: V_.tensor_tensor(out=G12[:, gt * 2 + 1:gt * 2 + 2], in0=G12[:, gt * 2:gt * 2 + 1], in1=e21, op=ALU.mult))
                bR = ps1()
                S.op("pe", [tRk, tC], bR.toks, lambda: [
                    nc.tensor.matmul(bR.ap[:, 0:64], su_bf[:], ONE[:, gt * 64:(gt + 1) * 64], start=True, stop=True),
                    nc.tensor.matmul(bR.ap[:, 64:128], ones_bf[:], ONE[:, gt * 64:(gt + 1) * 64], start=True, stop=True)])
                yield
                ta, tb = me, me2
                S.op("dve", bR.toks + [tRk, tRt], [tRt], lambda: V_.tensor_tensor(out=ta, in0=bR.ap[:, 0:32], in1=Crun[:], op=ALU.add))
                rt(lambda: V_.tensor_tensor(out=tb, in0=ta, in1=o1, op=ALU.mult))
                S.op("dve", [tRt], [tRt, tRk], lambda: V_.reduce_sum(out=RK[:, gt * 2:gt * 2 + 1], in_=tb, axis=AXX_))
                S.op("dve", bR.toks + [tRk, tRt], [tRt], lambda: V_.tensor_tensor(out=ta, in0=bR.ap[:, 32:64], in1=Crun[:], op=ALU.add))
                S.op("dve", bR.toks + [tRt], [tRt], lambda: V_.tensor_tensor(out=ta, in0=bR.ap[:, 64:96], in1=ta, op=ALU.add))
                rt(lambda: V_.tensor_tensor(out=tb, in0=ta, in1=o2, op=ALU.mult))
                S.op("dve", [tRt], [tRt, tRk], lambda: V_.reduce_sum(out=RK[:, gt * 2 + 1:gt * 2 + 2], in_=tb, axis=AXX_))
                S.op("dve", bR.toks + [tRt], [tRk], lambda: V_.tensor_tensor(out=Crun[:], in0=bR.ap[:, 64:96], in1=Crun[:], op=ALU.add))
                S.op("dve", bR.toks + [tRt], [tRk], lambda: V_.tensor_tensor(out=Crun[:], in0=bR.ap[:, 96:128], in1=Crun[:], op=ALU.add))
                S.dma("sp", out=XB[gt * 128:(gt + 1) * 128, :], in_=hbK[tt % 2], reads=[tHb[tt % 2]])
                S.dma("sp", out=H2D[gt * 128:(gt + 1) * 128, :], in_=hap, reads=[tHt])
                yield

            xa_A(0)
            for ui in range(len(units)):
                xa_B(ui)
                if ui + 1 < len(units):
                    xa_A(ui + 1)
                xa_C(ui)
                tg, h = units[ui]
                if h != 3:
                    continue
                for pr in ((0, 1), (2, 3)):
                    gens = []
                    for si, j in enumerate(pr):
                        tt = tg * 4 + j
                        gens.append(xa_tail(tt, xa_mm(tt), si))
                    alive_ = [True, True]
                    while any(alive_):
                        for gi in range(2):
                            if alive_[gi] and next(gens[gi], "done") == "done":
                                alive_[gi] = False
            if stop_after == "P7":
                dump("h2", hview, tH)
                break
            S.barrier(engines=("act", "dve", "pool", "sp"))
        if stop_after is None:
            S.barrier(full=True)
            V_ = nc.vector
            AXX = mybir.AxisListType.X
            tM = T()
            padc, pA, pB, basev = small[:, 116:148], small[:, 148:180], small[:, 180:212], small[:, 212:244]
            cmpb = H[:, 0:1024]
            mo = lambda f, extra=(): S.op("dve", [tM, tRk, tC] + list(extra), [tM], f)
            mo(lambda: V_.tensor_tensor(out=v3(cmpb, 32), in0=Crun[:].unsqueeze(2).to_broadcast([128, 32, 32]),
                                        in1=thr[:].unsqueeze(1).to_broadcast([128, 32, 32]), op=ALU.is_gt))
            mo(lambda: V_.reduce_sum(out=padc, in_=v3(cmpb, 32), axis=AXX))
            mo(lambda: V_.tensor_scalar_mul(out=padc, in0=padc, scalar1=float(TSZ)))
            cur, nxt = padc, pA
            for sft in (1, 2, 4, 8, 16):
                mo(lambda: V_.tensor_copy(out=nxt[:, 0:sft], in_=cur[:, 0:sft]))
                mo(lambda: V_.tensor_tensor(out=nxt[:, sft:32], in0=cur[:, sft:32], in1=cur[:, 0:32 - sft], op=ALU.add))
                cur, nxt = nxt, (pB if nxt is pA else pA)
            endv = cur
            mo(lambda: V_.tensor_tensor(out=basev, in0=endv, in1=padc, op=ALU.subtract))
            cmpt = H[:, 2048:2048 + NTI * 32]
            tef = mo_f[:, 0:NTI]
            widx = mo_i[:, 0:NTI]
            mo(lambda: V_.tensor_tensor(out=v3(cmpt, 32), in0=endv.unsqueeze(1).to_broadcast([128, NTI, 32]),
                                        in1=tstart[:, 0:NTI].unsqueeze(2).to_broadcast([128, NTI, 32]), op=ALU.is_le))
            mo(lambda: V_.reduce_sum(out=tef, in_=v3(cmpt, 32), axis=AXX))
            mo(lambda: V_.tensor_scalar(out=tef, in0=tef, scalar1=128.0, scalar2=pcol[:, 0:1], op0=ALU.mult, op1=ALU.add))
            mo(lambda: V_.tensor_copy(out=widx, in_=tef))
            tmp3 = H[:, 4096:4096 + GT * 64]
            posf = mo_f[:, 64:64 + GT * 2]
            posI = mo_i[:, 64:64 + GT * 2]
            mo(lambda: V_.tensor_tensor(out=v3(tmp3, 32), in0=v3(ONE[:], 32), in1=basev.unsqueeze(1).to_broadcast([128, GT * 2, 32]), op=ALU.mult))
            mo(lambda: V_.reduce_sum(out=posf, in_=v3(tmp3, 32), axis=AXX))
            mo(lambda: V_.tensor_tensor(out=posf, in0=posf, in1=RK[:], op=ALU.add))
            mo(lambda: V_.tensor_copy(out=posI, in_=posf))
            xbt = [A[:, i * 1024:(i + 1) * 1024] for i in range(2)]
            tXb = [T(), T()]
            for gt in range(GT):
                S.dma("sp", out=xbt[gt % 2], in_=XB[gt * 128:(gt + 1) * 128, :], writes=[tXb[gt % 2]])
                for k in range(2):
                    S.dma("pool", out=XS[:, :], in_=xbt[gt % 2], out_off=posI[:, gt * 2 + k:gt * 2 + k + 1], bound=NTI * TSZ - 1,
                          reads=[tXb[gt % 2], tM])
            S.barrier()
            xsb = [v3(A[:, 2048 + i * 4096:2048 + i * 4096 + TB * 1024], 1024) for i in range(2)]
            xst = [v3(K[:, 4096 + i * 4096:4096 + i * 4096 + 8 * TSZ], TSZ) for i in range(2)]
            Ssb = [Kf(0, 2 * TSZ), Kf(1024, 1024 + 2 * TSZ)]
            HD = [v3(K[:, 2048:2048 + 2 * TSZ], TSZ), v3(K[:, 3072:3072 + 2 * TSZ], TSZ)]
            Ysb = [K[:, 12288:13312], K[:, 13312:14336]]
            tXsb, tXst, tSs, tHD, tY = [T(), T()], [T(), T()], [T(), T()], [T(), T()], [T(), T()]
            NWB = 3
            Eb = [W[:, i * 6144:(i + 1) * 6144] for i in range(NWB)]
            tEw = [[T(), T(), T()] for _ in range(NWB)]

            def moe_prefetch_w(ti):
                eb = Eb[ti % NWB]
                for part, src in enumerate((WGB, WUB, WDB)):
                    S.dma("pool", out=eb[:, part * 2048:(part + 1) * 2048], in_=src[:, :], in_off=widx[:, ti:ti + 1], bound=NEXP * 128 - 1,
                          reads=[tM], writes=[tEw[ti % NWB][part]])

            def moe_prefetch_x(ti):
                S.dma("sp", out=xsb[ti % 2], in_=XS[ti * TSZ:(ti + 1) * TSZ, :].rearrange("(j p) d -> p j d", p=128), writes=[tXsb[ti % 2]])

            def moe_TR(ti):
                xs_, txs = xsb[ti % 2], tXsb[ti % 2]
                xt_, txt = xst[ti % 2], tXst[ti % 2]
                for j in range(TB):
                    pb = ps1()
                    S.op("pe", [txs, tC], pb.toks, lambda: [nc.tensor.transpose(
                        pb.bf[:, k * 128:(k + 1) * 128], xs_[:, j, k * 128:(k + 1) * 128], ident_bf[:]) for k in range(8)])
                    if j % 2 == 0:
                        S.op("act", pb.toks, [txt], lambda: nc.scalar.copy(out=xt_[:, :, j * 128:(j + 1) * 128], in_=v3(pb.bf[:, 0:1024], 128)))
                    else:
                        S.op("dve", pb.toks, [txt], lambda: nc.vector.tensor_copy(out=xt_[:, :, j * 128:(j + 1) * 128], in_=v3(pb.bf[:, 0:1024], 128)))

            def moe_GU(ti):
                eb = Eb[ti % NWB]
                tE3 = tEw[ti % NWB]
                wg = v3(eb[:, 0:2048], 256)
                wu = v3(eb[:, 2048:4096], 256)
                xt_, txt = xst[ti % 2], tXst[ti % 2]
                hd, thd = HD[ti % 2], tHD[ti % 2]
                for fc in range(2):
                    bg, bu = ps1(), ps1()
                    S.op("pe", [txt, tE3[0]], bg.toks, lambda: [nc.tensor.matmul(
                        bg.ap[:, 0:TSZ], wg[:, k, fc * 128:(fc + 1) * 128], xt_[:, k, :], start=(k == 0), stop=(k == 7)) for k in range(8)])
                    S.op("pe", [txt, tE3[1]], bu.toks, lambda: [nc.tensor.matmul(
                        bu.ap[:, 0:TSZ], wu[:, k, fc * 128:(fc + 1) * 128], xt_[:, k, :], start=(k == 0), stop=(k == 7)) for k in range(8)])
                    S.op("act", bg.toks, [tSs[fc]], lambda: nc.scalar.activation(out=Ssb[fc], in_=bg.ap[:, 0:TSZ], func=AF.Silu))
                    S.op("dve", bu.toks + [tSs[fc]], [thd], lambda: nc.vector.tensor_tensor(out=hd[:, fc, :], in0=Ssb[fc], in1=bu.ap[:, 0:TSZ], op=ALU.mult))

            def moe_D(ti):
                eb = Eb[ti % NWB]
                tE3 = tEw[ti % NWB]
                wd = v3(eb[:, 4096:6144], 1024)
                hd, thd = HD[ti % 2], tHD[ti % 2]
                for j in range(TB):
                    bd2 = ps2()
                    S.op("pe", [thd, tE3[2]], bd2.toks, lambda: [nc.tensor.matmul(
                        bd2.ap[:, hf * 512:(hf + 1) * 512], hd[:, fc, j * 128:(j + 1) * 128], wd[:, fc, hf * 512:(hf + 1) * 512],
                        start=(fc == 0), stop=(fc == 1)) for hf in range(2) for fc in range(2)])
                    yb, ty = Ysb[moe_state["yi"] % 2], tY[moe_state["yi"] % 2]
                    moe_state["yi"] += 1
                    S.op("act", bd2.toks, [ty], lambda: nc.scalar.copy(out=yb, in_=bd2.ap))
                    S.dma("sp", out=YS[ti * TSZ + j * 128:ti * TSZ + (j + 1) * 128, :], in_=yb, reads=[ty])

            moe_state = {"yi": 0}
            moe_prefetch_w(0)
            moe_prefetch_x(0)
            if NTI > 1:
                moe_prefetch_w(1)
                moe_prefetch_x(1)
            moe_TR(0)
            moe_GU(0)
            for ti in range(NTI):
                if ti + 2 < NTI:
                    moe_prefetch_w(ti + 2)
                if ti + 1 < NTI:
                    moe_TR(ti + 1)
                if ti + 2 < NTI:
                    moe_prefetch_x(ti + 2)
                moe_D(ti)
                if ti + 1 < NTI:
                    moe_GU(ti + 1)
            S.barrier()
            load_ln(2)
            NYB = 4
            ybuf = [[H[:, i * 3072:i * 3072 + 512].bitcast(BF16), H[:, i * 3072 + 512:i * 3072 + 1024].bitcast(BF16),
                     H[:, i * 3072 + 1024:i * 3072 + 2048], H[:, i * 3072 + 2048:i * 3072 + 3072]] for i in range(NYB)]
            tYb = [[T(), T(), T(), T()] for _ in range(NYB)]

            def comb_prefetch(gt):
                y1, y2, ytmp, hh = ybuf[gt % NYB]
                t1_, t2_, tt_, th = tYb[gt % NYB]
                S.dma("pool", out=y1, in_=YS[:, :], in_off=posI[:, gt * 2:gt * 2 + 1], bound=NTI * TSZ - 1, reads=[tM], writes=[t1_])
                S.dma("pool", out=y2, in_=YS[:, :], in_off=posI[:, gt * 2 + 1:gt * 2 + 2], bound=NTI * TSZ - 1, reads=[tM], writes=[t2_])
                S.dma("sp", out=hh, in_=H2D[gt * 128:(gt + 1) * 128, :], writes=[th])

            def comb_gen(gt):
                y1, y2, ytmp, hh = ybuf[gt % NYB]
                t1_, t2_, tt_, th = tYb[gt % NYB]
                S.op("act", [t1_, tRk], [tt_], lambda: nc.scalar.activation(out=ytmp, in_=y1, func=AF.Copy, scale=G12[:, gt * 2:gt * 2 + 1]))
                yield
                S.op("dve", [t2_, tRk, tt_], [tt_], lambda: V_.scalar_tensor_tensor(
                    out=ytmp, in0=y2, scalar=G12[:, gt * 2 + 1:gt * 2 + 2], in1=ytmp, op0=ALU.mult, op1=ALU.add))
                S.op("dve", [tt_], [th], lambda: V_.scalar_tensor_tensor(out=hh, in0=hh, scalar=ALPHA, in1=ytmp, op0=ALU.mult, op1=ALU.add))
                j_ = lnstate["i"] % 4
                lnstate["i"] += 1
                tS = tLNS[j_]
                base = j_ * 16
                st, mv = lnst[:, base:base + 12], lnst[:, base + 12:base + 14]
                rs, nm = lnst[:, base + 14:base + 15], lnst[:, base + 15:base + 16]
                S.op("dve", [th], [tS], lambda: nc.vector.bn_stats(out=st[:, 0:6], in_=hh[:, 0:512]))
                S.op("dve", [th], [tS], lambda: nc.vector.bn_stats(out=st[:, 6:12], in_=hh[:, 512:1024]))
                S.op("dve", [], [tS], lambda: nc.vector.bn_aggr(out=mv, in_=st))
                yield
                rstd_from(rs, mv[:, 1:2], 1.0, [tS], [tS])
                yield
                S.op("dve", [tS], [tS], lambda: nc.vector.scalar_tensor_tensor(
                    out=nm, in0=mv[:, 0:1], scalar=-1.0, in1=rs, op0=ALU.mult, op1=ALU.mult))
                yield
                S.op("act", [tS], [th], lambda: nc.scalar.activation(out=hh, in_=hh, func=AF.Identity, scale=rs, bias=nm))
                yield
                S.op("dve", [tLN], [th], lambda: nc.vector.tensor_tensor(out=hh, in0=hh, in1=lng[:], op=ALU.mult))
                yield
                S.op("dve", [tLN], [th], lambda: nc.vector.tensor_tensor(out=hh, in0=hh, in1=lnb[:], op=ALU.add))
                sq_, tq_ = gt // NT, gt % NT
                S.dma("sp", out=out_d[sq_, tq_ * 128:(tq_ + 1) * 128, :], in_=hh, reads=[th])
                yield

            comb_prefetch(0)
            comb_prefetch(1)
            for g0 in range(0, GT, 2):
                for gn in (g0 + 2, g0 + 3):
                    if gn < GT:
                        comb_prefetch(gn)
                gens = [comb_gen(g0), comb_gen(g0 + 1)]
                alive_ = [True, True]
                while any(alive_):
                    for gi in range(2):
                        if alive_[gi] and next(gens[gi], "done") == "done":
                            alive_[gi] = False
        S.barrier(engines=("sp",), full=True)
    return nc


def _rope_tables():
    pos = np.arange(SEQ, dtype=np.float32)
    inv_freq = (np.float32(10000.0) ** (-(np.arange(0, 32, 2, dtype=np.float32)) / np.float32(32))).astype(np.float32)
    ang = (pos[:, None] * inv_freq[None, :]).astype(np.float32)
    cos = np.cos(ang).astype(np.float32)
    sin = np.sin(ang).astype(np.float32)
    cc = np.zeros((128, SEQ), np.float32)
    ss = np.zeros((128, SEQ), np.float32)
    cc[0:64] = 1.0
    cc[64:80] = cos.T
    cc[80:96] = cos.T
    ss[64:80] = -sin.T
    ss[80:96] = sin.T
    return cc, ss


def prep_shared(inp):
    f = np.float32
    g = lambda k: np.asarray(inp[k], dtype=f)[0]
    w_in = g("w_in")
    wkr = np.zeros((D, 96), f)
    wkr[:, 64:80] = w_in[:, 400:416]
    wkr[:, 80:96] = w_in[:, 384:400]
    wq = g("w_q_up")
    perm = np.arange(768)
    for h in range(8):
        for j in range(32):
            perm[h * 96 + 64 + j] = h * 96 + 64 + (j + 16) % 32
    wq_sw = wq[:, perm]
    convw = g("ssd_conv_w").reshape(4, 8, 128).transpose(2, 1, 0).reshape(128, 32)
    convb = g("ssd_conv_b").reshape(8, 128).T
    dtb = np.broadcast_to(np.tile(g("ssd_dt_bias"), 16)[None, :], (128, 128))
    alog = np.broadcast_to(g("ssd_a_log")[None, :], (128, 8))
    sd = g("ssd_d")
    dcol = np.stack([sd[pair * 2 + (np.arange(128) // 64)] for pair in range(4)], axis=1)
    ncol = g("ssd_norm").reshape(4, 128).T
    lnp = np.stack([np.broadcast_to(g(k)[None, :], (128, D)) for k in ("ln1_g", "ln1_b", "ln2_g", "ln2_b", "ln3_g", "ln3_b")])
    rw = np.concatenate([g("router_group_w"), g("router_expert_w")], axis=1)
    rb = np.broadcast_to(np.concatenate([g("router_group_b"), g("router_expert_b")])[None, :], (128, 36))
    tri = np.triu(np.ones((128, 128), f))
    nm = np.where(np.arange(128)[None, :] >= np.arange(128)[:, None], 0.0, -30000.0).astype(f)
    cc, ss = _rope_tables()
    su = np.triu(np.ones((128, 128), f), k=1)
    thr = np.broadcast_to((np.arange(32, dtype=f) * 384.0)[None, :], (128, 32))
    tstart = np.broadcast_to((np.arange(64, dtype=f) * 384.0)[None, :], (128, 64))
    pcol = np.arange(128, dtype=f).reshape(128, 1)
    sh = {
        "su": su, "thr": thr, "tstart": tstart, "pcol": pcol,
        "w_in": w_in, "wkr_sw": wkr, "wq": wq, "wq_sw": wq_sw, "qn": g("mla_q_norm").reshape(2, 128).T,
        "wkv": g("w_kv_up"), "kvn": g("mla_kv_norm").reshape(128, 1), "convw": convw, "convb": convb,
        "dtb": dtb, "alog": alog, "dcol": dcol, "ncol": ncol, "wout": g("w_out"), "xwq": g("xa_wq"),
        "xwk": g("xa_wk"), "xwv": g("xa_wv"), "xwo": g("xa_wo"), "lnp": lnp, "rw": rw, "rb": rb,
        "wg": g("expert_w_gate"), "wu": g("expert_w_up"), "wd": g("expert_w_down"),
        "ident": np.eye(128, dtype=f), "tri": tri, "negmask": np.tile(nm, (1, 4)), "cc": cc, "ss": ss,
    }
    return {k: np.ascontiguousarray(v, dtype=f) for k, v in sh.items()}


def kernel(**inputs):
    sh = prep_shared(inputs)
    x = np.asarray(inputs["x"], dtype=np.float32)
    mem = np.asarray(inputs["mem"], dtype=np.float32)
    nc = build(n_seq=2)
    in_maps = []
    for c in range(N_CORES):
        m = dict(sh)
        m["x"] = np.ascontiguousarray(x[2 * c:2 * c + 2])
        m["mem"] = np.ascontiguousarray(mem[2 * c:2 * c + 2])
        in_maps.append(m)
    res = run_bass_kernel_spmd(nc, in_maps, core_ids=list(range(N_CORES)))
    return np.concatenate([r["out"] for r in res.results], axis=0)
```

```python
import numpy as np
from contextlib import ExitStack
import concourse.bass as bass
import concourse.mybir as mybir
from concourse.bass_utils import run_bass_kernel_spmd

F32 = mybir.dt.float32
BF16 = mybir.dt.bfloat16
AF = mybir.ActivationFunctionType
ALU = mybir.AluOpType

N_CORES = 8
SEQ = 2048
D = 1024
NT = SEQ // 128
NG = SEQ // 512
ALPHA = 2.0 ** 0.25
EPS = 1e-5
NEXP = 32


class T:
    __slots__ = ("w", "r")

    def __init__(self):
        self.w = None
        self.r = {}


class Sched:
    def __init__(self, nc, es, n_dma=80):
        self.nc = nc
        self.eng = {"pe": nc.tensor, "act": nc.scalar, "dve": nc.vector, "pool": nc.gpsimd, "sp": nc.sync}
        self.sem = {k: es.enter_context(nc.semaphore("s_" + k)) for k in ("pe", "act", "dve", "pool")}
        self.cnt = {k: 0 for k in self.sem}
        self.dsem = [es.enter_context(nc.semaphore("d%d" % i)) for i in range(n_dma)]
        self.dcnt = [0] * n_dma
        n_cast = 16
        self.qslots = {"sp": list(range(0, (n_dma - n_cast) // 2)), "pool": list(range((n_dma - n_cast) // 2, n_dma - n_cast)),
                       "cast": list(range(n_dma - n_cast, n_dma))}
        self.qnext = {"sp": 0, "pool": 0, "cast": 0}
        self.seen = {}
        self.bregs = {}

    def _semof(self, key):
        return self.sem[key] if isinstance(key, str) else self.dsem[key]

    def _need(self, eng, reads, writes):
        need = {}

        def add(k, v):
            if k == eng:
                if eng == "pe":
                    return
                if self.cnt[eng] - v >= 4:
                    return
            if self.seen.get((eng, k), 0) >= v:
                return
            if need.get(k, 0) < v:
                need[k] = v

        for t in reads:
            if t.w is not None:
                add(*t.w)
        for t in writes:
            if t.w is not None:
                add(*t.w)
            for k, v in t.r.items():
                add(k, v)
        return need

    def _wait(self, eng, key, val):
        if self.seen.get((eng, key), 0) >= val:
            return
        self.eng[eng].wait_ge(self._semof(key), val)
        self.seen[(eng, key)] = val

    def _waits(self, eng, reads, writes):
        for k, v in self._need(eng, reads, writes).items():
            self._wait(eng, k, v)

    def _commit(self, ticket, reads, writes):
        k, v = ticket
        for t in reads:
            if t.r.get(k, 0) < v:
                t.r[k] = v
        for t in writes:
            t.w = ticket
            t.r = {}

    def op(self, eng, reads, writes, emit):
        need = self._need(eng, reads, writes)
        keys = list(need)
        attach = keys[-1] if keys else None
        for k in keys[:-1]:
            self._wait(eng, k, need[k])
        r = emit()
        first, last = (r[0], r[-1]) if isinstance(r, list) else (r, r)
        if attach is not None:
            first._wait_ge(self._semof(attach), need[attach])
            self.seen[(eng, attach)] = need[attach]
        self.cnt[eng] += 1
        last.then_inc(self.sem[eng], 1)
        self._commit((eng, self.cnt[eng]), reads, writes)

    def dma(self, q, out, in_, reads=(), writes=(), out_off=None, in_off=None, bound=None, slots=None):
        self._waits(q, reads, writes)
        sp_ = slots or q
        sl = self.qslots[sp_]
        slot = sl[self.qnext[sp_]]
        self.qnext[sp_] = (self.qnext[sp_] + 1) % len(sl)
        if self.dcnt[slot] > 0:
            self._wait(q, slot, self.dcnt[slot])
        if out_off is None and in_off is None:
            inst = self.eng[q].dma_start(out=out, in_=in_)
        else:
            if bound not in self.bregs:
                self.bregs[bound] = self.nc.gpsimd.to_reg(bound)
            bound = self.bregs[bound]
            inst = self.nc.gpsimd.indirect_dma_start(
                out=out, out_offset=None if out_off is None else bass.IndirectOffsetOnAxis(ap=out_off, axis=0),
                in_=in_, in_offset=None if in_off is None else bass.IndirectOffsetOnAxis(ap=in_off, axis=0),
                bounds_check=bound, oob_is_err=False)
        inst.then_inc(self.dsem[slot], 16)
        self.dcnt[slot] += 16
        self._commit((slot, self.dcnt[slot]), reads, writes)

    def barrier(self, engines=("pe", "act", "dve", "pool", "sp"), full=False):
        skip = () if full else set(self.qslots["cast"])
        for e in engines:
            for k in self.sem:
                if self.cnt[k] > 0:
                    self._wait(e, k, self.cnt[k])
            for s in range(len(self.dsem)):
                if self.dcnt[s] > 0 and s not in skip:
                    self._wait(e, s, self.dcnt[s])


def v3(ap, b):
    return ap.rearrange("p (a b) -> p a b", b=b)


def build(n_seq=2, stop_after=None, dumps=()):
    nc = bass.Bass("TRN2", target_bir_lowering=False)

    def din(name, shape):
        return nc.dram_tensor(name, list(shape), F32, kind="ExternalInput").ap()

    x = din("x", [n_seq, SEQ, D])
    mem = din("mem", [n_seq, 256, D])
    w_in = din("w_in", [D, 1960])
    wkr_sw = din("wkr_sw", [D, 96])
    wq_d = din("wq", [256, 768])
    wqsw_d = din("wq_sw", [256, 768])
    qn_d = din("qn", [128, 2])
    wkv_d = din("wkv", [128, 1024])
    kvn_d = din("kvn", [128, 1])
    convw_d = din("convw", [128, 32])
    convb_d = din("convb", [128, 8])
    dtb_d = din("dtb", [128, 128])
    alog_d = din("alog", [128, 8])
    dcol_d = din("dcol", [128, 4])
    ncol_d = din("ncol", [128, 4])
    wout_d = din("wout", [D, D])
    xwq_d = din("xwq", [D, D])
    xwk_d = din("xwk", [D, D])
    xwv_d = din("xwv", [D, D])
    xwo_d = din("xwo", [D, D])
    lnp_d = din("lnp", [6, 128, D])
    rw_d = din("rw", [D, 36])
    rb_d = din("rb", [128, 36])
    wg_d = din("wg", [NEXP, D, 256])
    wu_d = din("wu", [NEXP, D, 256])
    wd_d = din("wd", [NEXP, 256, D])
    ident_d = din("ident", [128, 128])
    tri_d = din("tri", [128, 128])
    negmask_d = din("negmask", [128, 512])
    cc_d = din("cc", [128, SEQ])
    ss_d = din("ss", [128, SEQ])
    su_d = din("su", [128, 128])
    thr_d = din("thr", [128, 32])
    pcol_d = din("pcol", [128, 1])
    GT = n_seq * NT
    NTOK = n_seq * SEQ
    TB = 3
    TSZ = 128 * TB
    NTI = -(-(2 * NTOK) // TSZ) + 31
    tstart_d = din("tstart", [128, 64])
    out_d = nc.dram_tensor("out", [n_seq, SEQ, D], F32, kind="ExternalOutput").ap()
    XB = nc.dram_tensor("XB", [NTOK, D], BF16, kind="Internal").ap()
    H2D = nc.dram_tensor("H2D", [NTOK, D], F32, kind="Internal").ap()
    XS = nc.dram_tensor("XS", [NTI * TSZ, D], BF16, kind="Internal").ap()
    YS = nc.dram_tensor("YS", [NTI * TSZ, D], BF16, kind="Internal").ap()
    DWB = {n: nc.dram_tensor("DWB_" + n, [D, D], BF16, kind="Internal").ap() for n in ("wout", "xwq", "xwk", "xwv", "xwo")}
    XBF = nc.dram_tensor("XBF", [max(n_seq - 1, 1), SEQ, D], BF16, kind="Internal").ap()
    WINB = nc.dram_tensor("WINB", [D, 1960], BF16, kind="Internal").ap()
    WGB = nc.dram_tensor("WGB", [NEXP * 128, 2048], BF16, kind="Internal").ap()
    WUB = nc.dram_tensor("WUB", [NEXP * 128, 2048], BF16, kind="Internal").ap()
    WDB = nc.dram_tensor("WDB", [NEXP * 128, 2048], BF16, kind="Internal").ap()
    dump_d = {}
    for name, shape in dumps:
        dump_d[name] = nc.dram_tensor("dbg_" + name, list(shape), F32, kind="ExternalOutput").ap()

    es = ExitStack()
    with es:
        S = Sched(nc, es)

        def sb(name, shape, dt):
            return es.enter_context(nc.sbuf_tensor(name, list(shape), dt))

        ident_bf = sb("ident_bf", [128, 128], BF16)
        ones_bf = sb("ones_bf", [128, 128], BF16)
        ones_f = sb("ones_f", [128, 128], F32)
        tri_f = sb("tri_f", [128, 128], F32)
        ident_f = sb("ident_f", [128, 128], F32)
        negmask = sb("negmask_s", [128, 512], F32)
        cc = sb("cc_s", [128, SEQ], BF16)
        ss = sb("ss_s", [128, SEQ], BF16)
        lng = sb("lng", [128, D], F32)
        lnb = sb("lnb", [128, D], F32)
        qn = sb("qn_s", [128, 2], F32)
        kvn = sb("kvn_s", [128, 1], F32)
        convw = sb("convw_s", [128, 32], F32)
        convb = sb("convb_s", [128, 8], F32)
        dtb = sb("dtb_s", [128, 128], F32)
        a_bc = sb("a_bc", [128, 8], F32)
        dcol = sb("dcol_s", [128, 4], F32)
        ncol = sb("ncol_s", [128, 4], F32)
        rb = sb("rb_s", [128, 36], F32)
        rw = sb("rw_s", [128, 8 * 36], BF16)
        small = sb("small", [128, 512], F32)
        su_bf = sb("su_bf", [128, 128], BF16)
        thr = sb("thr_s", [128, 32], F32)
        pcol = sb("pcol_s", [128, 1], F32)
        tstart = sb("tstart_s", [128, 64], F32)
        ONE = sb("ONE", [128, GT * 64], BF16)
        RK = sb("RK", [128, GT * 2], F32)
        G12 = sb("G12", [128, GT * 2], F32)
        Crun = sb("Crun", [128, 32], F32)
        mo_f = sb("mo_f", [128, 128], F32)
        mo_i = sb("mo_i", [128, 128], mybir.dt.int32)
        tRk = T()
        rdx = cc[:].bitcast(F32)
        dt_tok = sb("dt_tok", [128, 128], F32)
        tC = T()
        tLN = T()
        tSmall = T()

        A = sb("arenaA", [128, 16384], BF16)
        H = sb("arenaH", [128, 16384], F32)
        K = sb("arenaK", [128, 16384], BF16)
        W = sb("arenaW", [128, 24576], BF16)
        lnst = sb("lnst", [128, 64], F32)
        tLNS = [T() for _ in range(4)]
        lnstate = {"i": 0}
        PS = [es.enter_context(nc.psum_tensor("ps%d" % i, [128, 1024], F32)) for i in range(4)]
        PT_ = [T() for _ in range(8)]
        pstate = {"b": 0}

        class Bank:
            def __init__(self, ap, toks):
                self.ap = ap
                self.toks = toks

            @property
            def bf(self):
                return self.ap.bitcast(BF16)

        busy = set()

        def ps1(hold=False):
            b = pstate["b"]
            while b in busy:
                b = (b + 1) % 8
            pstate["b"] = (b + 1) % 8
            bk_ = Bank(PS[b // 2][:, (b % 2) * 512:(b % 2) * 512 + 512], [PT_[b]])
            bk_.idx = [b]
            if hold:
                busy.add(b)
            return bk_

        def ps2(hold=False):
            b = pstate["b"]
            if b % 2:
                b = (b + 1) % 8
            while b in busy or (b + 1) in busy:
                b = (b + 2) % 8
            pstate["b"] = (b + 2) % 8
            bk_ = Bank(PS[b // 2][:, :], [PT_[b], PT_[b + 1]])
            bk_.idx = [b, b + 1]
            if hold:
                busy.update(bk_.idx)
            return bk_

        def psrel(bk_):
            for b in bk_.idx:
                busy.discard(b)

        def Wf(lo, hi):
            return W[:, lo:hi].bitcast(F32)

        def Kf(lo, hi):
            return K[:, lo:hi].bitcast(F32)

        def Hb(lo, hi):
            return H[:, lo:hi].bitcast(BF16)

        def dump(name, ap, toks):
            if name in dump_d:
                S.dma("pool", out=dump_d[name], in_=ap, reads=toks)

        def ld(q, dst, src):
            S.dma(q, out=dst, in_=src, writes=[tC])

        ld("pool", ident_bf[:], ident_d[:, :])
        ld("sp", ident_f[:], ident_d[:, :])
        ld("sp", tri_f[:], tri_d[:, :])
        ld("sp", negmask[:], negmask_d[:, :])
        ld("pool", su_bf[:], su_d[:, :])
        ld("sp", thr[:], thr_d[:, :])
        ld("sp", pcol[:], pcol_d[:, :])
        ld("sp", tstart[:], tstart_d[:, :])
        S.op("dve", [], [tRk], lambda: nc.vector.memset(Crun[:], 0.0))
        ld("pool", cc[:], cc_d[:, :])
        ld("pool", ss[:], ss_d[:, :])
        for dst, src in ((qn, qn_d), (kvn, kvn_d), (convw, convw_d), (convb, convb_d), (dtb, dtb_d),
                         (a_bc, alog_d), (dcol, dcol_d), (ncol, ncol_d), (rb, rb_d)):
            ld("sp", dst[:], src[:, :])
        ld("pool", v3(rw[:], 36), rw_d.rearrange("(k p) c -> p k c", p=128))
        S.op("dve", [], [tC], lambda: nc.vector.memset(ones_f[:], 1.0))
        S.op("dve", [], [tC], lambda: nc.vector.memset(ones_bf[:], 1.0))
        S.op("act", [tC], [tC], lambda: nc.scalar.activation(out=a_bc[:], in_=a_bc[:], func=AF.Exp))
        S.op("dve", [tC], [tC], lambda: nc.vector.tensor_scalar_mul(out=a_bc[:], in0=a_bc[:], scalar1=-1.0))
        S.barrier()

        def rstd_from(dst, src, scale, reads, writes):
            S.op("act", reads, writes, lambda: nc.scalar.activation(out=dst, in_=src, func=AF.Ln, scale=scale, bias=EPS))
            S.op("act", [], writes, lambda: nc.scalar.activation(out=dst, in_=dst, func=AF.Exp, scale=-0.5))

        def layer_norm_tile(hap, tH, li, s):
            j = lnstate["i"] % 4
            lnstate["i"] += 1
            tS = tLNS[j]
            base = j * 16
            st = lnst[:, base:base + 12]
            mv = lnst[:, base + 12:base + 14]
            rs = lnst[:, base + 14:base + 15]
            nm = lnst[:, base + 15:base + 16]
            S.op("dve", [tH], [tS], lambda: nc.vector.bn_stats(out=st[:, 0:6], in_=hap[:, 0:512]))
            S.op("dve", [tH], [tS], lambda: nc.vector.bn_stats(out=st[:, 6:12], in_=hap[:, 512:1024]))
            S.op("dve", [], [tS], lambda: nc.vector.bn_aggr(out=mv, in_=st))
            rstd_from(rs, mv[:, 1:2], 1.0, [tS], [tS])
            S.op("dve", [tS], [tS], lambda: nc.vector.scalar_tensor_tensor(
                out=nm, in0=mv[:, 0:1], scalar=-1.0, in1=rs, op0=ALU.mult, op1=ALU.mult))
            S.op("act", [tS], [tH], lambda: nc.scalar.activation(out=hap, in_=hap, func=AF.Identity, scale=rs, bias=nm))
            S.op("dve", [tLN], [tH], lambda: nc.vector.tensor_tensor(out=hap, in0=hap, in1=lng[:], op=ALU.mult))
            S.op("dve", [tLN], [tH], lambda: nc.vector.tensor_tensor(out=hap, in0=hap, in1=lnb[:], op=ALU.add))

        def load_ln(li):
            S.dma("sp", out=lng[:], in_=lnp_d[2 * li, :, :], writes=[tLN])
            S.dma("sp", out=lnb[:], in_=lnp_d[2 * li + 1, :, :], writes=[tLN])

        def transpose_to(hb, tHB, dstT, tt, tDst):
            pb = ps1()
            S.op("pe", [tHB, tC], pb.toks, lambda: [nc.tensor.transpose(
                pb.bf[:, k * 128:(k + 1) * 128], hb[:, k * 128:(k + 1) * 128], ident_bf[:]) for k in range(8)])
            S.op("act", pb.toks, [tDst], lambda: nc.scalar.copy(
                out=dstT[:, :, tt * 128:(tt + 1) * 128], in_=v3(pb.bf[:, 0:1024], 128)))

        actT = v3(A[:, :], SEQ)
        catT = v3(K[:, :], SEQ)
        hview = v3(H[:, :], D)

        for s in range(n_seq):
            tXT = [T() for _ in range(NT)]
            if s > 0:
                S.dma("pool", out=cc[:], in_=cc_d[:, :], writes=[tC])
            NXB = 4
            xin = [K[:, 0:1024], K[:, 1024:2048], K[:, 8192:9216], K[:, 9216:10240]]
            txin = [T() for _ in range(NXB)]
            win = v3(W[:, 0:15680], 1960)
            wkr = v3(W[:, 15680:16448], 96)
            wq_s = v3(W[:, 16448:17984], 768)
            wq_sw = v3(W[:, 17984:19520], 768)
            wkv_s = W[:, 19520:20544]
            tWinL, tWs = [T() for _ in range(9)], T()
            def load_x_tile(tt):
                if s == 0:
                    S.dma("pool", out=xin[tt % NXB], in_=x[s, tt * 128:(tt + 1) * 128, :], writes=[txin[tt % NXB]])
                else:
                    S.dma("sp", out=xin[tt % NXB], in_=XBF[s - 1, tt * 128:(tt + 1) * 128, :], reads=[tXC[(s, tt)]], writes=[txin[tt % NXB]])

            for tt in range(NXB):
                load_x_tile(tt)
            WCG = [(0, 416), (416, 928), (928, 1440), (1440, 1960)]
            w_in3 = w_in.rearrange("(k p) c -> p k c", p=128)
            def load_win_group(gi):
                c0_, c1_ = WCG[gi]
                if s == 0:
                    S.dma("pool", out=win[:, :, c0_:c1_], in_=w_in3[:, :, c0_:c1_], writes=[tWinL[gi]])
                else:
                    S.dma("sp", out=win[:, :, c0_:c1_], in_=WINB[:, c0_:c1_].rearrange("(k p) c -> p k c", p=128),
                          reads=[tWC[gi]], writes=[tWinL[gi]])

            load_win_group(0)
            S.dma("pool", out=wkr, in_=wkr_sw.rearrange("(k p) c -> p k c", p=128), writes=[tWinL[8]])
            for gi in range(1, 4):
                load_win_group(gi)
            S.dma("pool", out=wq_s, in_=wq_d.rearrange("(k p) c -> p k c", p=128), writes=[tWs])
            S.dma("pool", out=wq_sw, in_=wqsw_d.rearrange("(k p) c -> p k c", p=128), writes=[tWs])
            S.dma("pool", out=wkv_s, in_=wkv_d[:, :], writes=[tWs])
            for r in range(2):
                S.op("dve", [tWs, tC], [tWs], lambda r=r: nc.vector.tensor_scalar_mul(out=wq_s[:, r, :], in0=wq_s[:, r, :], scalar1=qn[:, r:r + 1]))
                S.op("dve", [tWs, tC], [tWs], lambda r=r: nc.vector.tensor_scalar_mul(out=wq_sw[:, r, :], in0=wq_sw[:, r, :], scalar1=qn[:, r:r + 1]))
            S.op("dve", [tWs, tC], [tWs], lambda: nc.vector.tensor_scalar_mul(out=wkv_s, in0=wkv_s, scalar1=kvn[:, 0:1]))
            for tt in range(NT):
                transpose_to(xin[tt % NXB], txin[tt % NXB], actT, tt, tXT[tt])
                if tt + NXB < NT:
                    load_x_tile(tt + NXB)
            if s == 0:
                tDW = {n: [T() for _ in range(8)] for n in DWB}
                for n, src in (("wout", wout_d), ("xwk", xwk_d), ("xwv", xwv_d), ("xwq", xwq_d), ("xwo", xwo_d)):
                    for k in range(8):
                        S.dma("pool", out=DWB[n][k * 128:(k + 1) * 128, :], in_=src[k * 128:(k + 1) * 128, :], writes=[tDW[n][k]], slots="cast")
            if s == 0 and n_seq > 1:
                tXC = {}
                tWC = [T() for _ in range(4)]
                for gi in range(4):
                    S.dma("pool", out=WINB[:, WCG[gi][0]:WCG[gi][1]].rearrange("(k p) c -> p k c", p=128),
                          in_=w_in3[:, :, WCG[gi][0]:WCG[gi][1]], writes=[tWC[gi]], slots="cast")
                for s2 in range(1, n_seq):
                    for tt in range(NT):
                        tXC[(s2, tt)] = T()
                        S.dma("pool", out=XBF[s2 - 1, tt * 128:(tt + 1) * 128, :], in_=x[s2, tt * 128:(tt + 1) * 128, :],
                              writes=[tXC[(s2, tt)]], slots="cast")
            if s == 0 and stop_after is None:
                for e in range(NEXP):
                    rows = slice(e * 128, (e + 1) * 128)
                    S.dma("pool", out=WGB[rows, :].rearrange("p (k f) -> p k f", k=8), in_=wg_d[e].rearrange("(k p) f -> p k f", p=128), writes=[T()], slots="cast")
                    S.dma("pool", out=WUB[rows, :].rearrange("p (k f) -> p k f", k=8), in_=wu_d[e].rearrange("(k p) f -> p k f", p=128), writes=[T()], slots="cast")
                    S.dma("pool", out=WDB[rows, :].rearrange("p (k f) -> p k f", k=2), in_=wd_d[e].rearrange("(k p) f -> p k f", p=128), writes=[T()], slots="cast")
            if stop_after == "P1":
                dump("xT", actT[:, 0, :], tXT)
                break

            zs = v3(Hb(0, 4096), SEQ)
            xsT = v3(Hb(4096, 8192), SEQ)
            BT = v3(Hb(8192, 10240), SEQ)
            CT = v3(Hb(10240, 12288), SEQ)
            cqT = v3(Hb(12288, 14336), SEQ)
            ckvT = Hb(14336, 15360)
            kpe = Hb(15360, 16384)
            tZ, tXs, tB, tCt, tCq, tCkv, tKpe = T(), T(), T(), T(), T(), T(), T()
            pre = [Kf(2048 + i * 1040, 2048 + i * 1040 + 1030) for i in range(2)]
            acc = [Kf(4128 + i * 1024, 4128 + (i + 1) * 1024) for i in range(2)]
            sqb = K[:, 6176:6688]
            rsb = Kf(6688, 7712)
            tPre, tAcc, tSq, tRs = [T(), T()], [T(), T()], T(), T()
            tDt = T()

            def proj_mm(bank, M, lhs_fn, tg, wt=0):
                S.op("pe", list(tXT[tg * 4:tg * 4 + 4]) + [tWinL[wt]], bank.toks, lambda: [nc.tensor.matmul(
                    bank.ap[0:M, :], lhs_fn(k), actT[:, k, tg * 512:(tg + 1) * 512], start=(k == 0), stop=(k == 7)) for k in range(8)])

            for tg in range(NG):
                cols = slice(tg * 512, (tg + 1) * 512)
                bq = [ps1(), ps1()]
                for r in range(2):
                    proj_mm(bq[r], 128, lambda k, r=r: win[:, k, r * 128:(r + 1) * 128], tg)
                bs = ps1()
                for r in range(2):
                    S.op("act", bq[r].toks, [tSq], lambda r=r: nc.scalar.activation(out=sqb, in_=bq[r].ap, func=AF.Square))
                    S.op("pe", [tSq, tC], bs.toks, lambda r=r: nc.tensor.matmul(bs.ap, ones_bf[:], sqb, start=(r == 0), stop=(r == 1)))
                rstd_from(rsb, bs.ap, 1.0 / 256, bs.toks, [tRs])
                for r in range(2):
                    S.op("dve", bq[r].toks + [tRs], [tCq], lambda r=r: nc.vector.tensor_tensor(out=cqT[:, r, cols], in0=bq[r].ap, in1=rsb, op=ALU.mult))
                bk = ps1()
                proj_mm(bk, 128, lambda k: win[:, k, 256:384], tg)
                bs = ps1()
                S.op("act", bk.toks, [tSq], lambda: nc.scalar.activation(out=sqb, in_=bk.ap, func=AF.Square))
                S.op("pe", [tSq, tC], bs.toks, lambda: nc.tensor.matmul(bs.ap, ones_bf[:], sqb, start=True, stop=True))
                rstd_from(rsb, bs.ap, 1.0 / 128, bs.toks, [tRs])
                S.op("dve", bk.toks + [tRs], [tCkv], lambda: nc.vector.tensor_tensor(out=ckvT[:, cols], in0=bk.ap, in1=rsb, op=ALU.mult))
                ba, bb = ps1(), ps1()
                proj_mm(ba, 96, lambda k: win[:, k, 320:416], tg)
                proj_mm(bb, 96, lambda k: wkr[:, k, :], tg, 8)
                t1 = acc[0]
                t2 = acc[1]
                S.op("dve", ba.toks + [tC], [tAcc[0]], lambda: nc.vector.tensor_tensor(out=t1[64:96, :], in0=ba.ap[64:96, :], in1=cc[64:96, cols], op=ALU.mult))
                S.op("dve", bb.toks + [tC], [tAcc[1]], lambda: nc.vector.tensor_tensor(out=t2[64:96, :], in0=bb.ap[64:96, :], in1=ss[64:96, cols], op=ALU.mult))
                S.op("dve", [tAcc[0], tAcc[1]], [tKpe], lambda: nc.vector.tensor_tensor(out=kpe[64:96, cols], in0=t1[64:96, :], in1=t2[64:96, :], op=ALU.add))
                for j in range(4):
                    bz = ps1()
                    proj_mm(bz, 128, lambda k, j=j: win[:, k, 416 + j * 128:416 + (j + 1) * 128], tg, 1)
                    S.op("act", bz.toks, [tZ], lambda j=j, bz=bz: nc.scalar.activation(out=zs[:, j, cols], in_=bz.ap, func=AF.Silu))
            for c in range(8):
                if c < 4:
                    dst, tdst = xsT[:, c, :], tXs
                elif c < 6:
                    dst, tdst = BT[:, c - 4, :], tB
                else:
                    dst, tdst = CT[:, c - 6, :], tCt
                S.op("dve", [], [tPre[0]], lambda: nc.vector.memset(pre[0][:, 0:3], 0.0))
                for tg in range(NG):
                    p_, a_ = pre[tg % 2], acc[tg % 2]
                    tp, ta = tPre[tg % 2], tAcc[tg % 2]
                    bx = ps1()
                    proj_mm(bx, 128, lambda k, c=c: win[:, k, 928 + c * 128:928 + (c + 1) * 128], tg, 2 if c < 4 else 3)
                    S.op("act", bx.toks, [tp], lambda: nc.scalar.copy(out=p_[:, 3:515], in_=bx.ap))
                    if tg < NG - 1:
                        S.op("act", [tp], [tPre[(tg + 1) % 2]], lambda: nc.scalar.copy(out=pre[(tg + 1) % 2][:, 0:3], in_=p_[:, 512:515]))
                    S.op("dve", [tp, tC], [ta], lambda: nc.vector.tensor_scalar_mul(out=a_, in0=p_[:, 0:512], scalar1=convw[:, c * 4:c * 4 + 1]))
                    for j in range(1, 4):
                        S.op("dve", [tp, tC], [ta], lambda j=j: nc.vector.scalar_tensor_tensor(
                            out=a_, in0=p_[:, j:j + 512], scalar=convw[:, c * 4 + j:c * 4 + j + 1], in1=a_, op0=ALU.mult, op1=ALU.add))
                    S.op("act", [ta, tC], [tdst], lambda: nc.scalar.activation(
                        out=dst[:, tg * 512:(tg + 1) * 512], in_=a_, func=AF.Silu, bias=convb[:, c:c + 1]))
            bd = ps1()
            for tt in range(NT):
                S.op("pe", [tXT[tt], tWinL[3]], bd.toks, lambda tt=tt: [nc.tensor.matmul(
                    bd.ap[:, tt * 8:(tt + 1) * 8], actT[:, k, tt * 128:(tt + 1) * 128], win[:, k, 1952:1960],
                    start=(k == 0), stop=(k == 7)) for k in range(8)])
            S.op("dve", bd.toks + [tC], [tDt], lambda: nc.vector.tensor_tensor(out=dt_tok[:], in0=bd.ap[:, 0:128], in1=dtb[:], op=ALU.add))
            S.op("act", [tDt], [tDt], lambda: nc.scalar.activation(out=dt_tok[:], in_=dt_tok[:], func=AF.Exp))
            S.op("act", [tDt], [tDt], lambda: nc.scalar.activation(out=dt_tok[:], in_=dt_tok[:], func=AF.Ln, bias=1.0))
            if stop_after == "P2":
                dump("cqT", cqT[:, 0, :], [tCq])
                dump("ckvT", ckvT, [tCkv])
                dump("kpe", kpe, [tKpe])
                dump("zs", zs[:, 0, :], [tZ])
                dump("xsT", xsT[:, 0, :], [tXs])
                dump("BT", BT[:, 0, :], [tB])
                dump("dt", dt_tok[:], [tDt])
                break
            S.barrier(engines=("act", "dve", "pool", "sp"))
            qT = [A[:, i * 2048:(i + 1) * 2048] for i in range(2)]
            kT = [A[:, (2 + i) * 2048:(3 + i) * 2048] for i in range(2)]
            Vaug = [v3(A[:, (4 + i) * 2048:(5 + i) * 2048], 128) for i in range(2)]
            PTb = [A[:, 12288 + i * 512:12288 + (i + 1) * 512] for i in range(3)]
            rdb = A[:, 13824:14848].bitcast(F32)
            t1 = [Wf(0, 1024), A[:, 14848:15872].bitcast(F32)]
            t2 = [Wf(1024, 2048), Wf(13568, 14592)]
            tQ, tK, tV, tPT = [T(), T()], [T(), T()], [T(), T()], [T(), T(), T()]
            tT1, tT2, tRd = [T(), T()], [T(), T()], T()
            tCat = [T() for _ in range(8)]
            S.op("dve", [], [tV[0]], lambda: nc.vector.memset(Vaug[0][:, :, 64:128], 1.0))
            S.op("dve", [], [tV[1]], lambda: nc.vector.memset(Vaug[1][:, :, 0:64], 1.0))
            if s == 0 and stop_after is None:
                ztile = W[:, 14592:15616]
                tZ = T()
                S.op("dve", [], [tZ], lambda: nc.vector.memset(ztile, 0.0))
                for j in range(NTI * TB):
                    S.dma("sp", out=XS[j * 128:(j + 1) * 128, :], in_=ztile, reads=[tZ])
            SCALE = 96.0 ** -0.5
            pstate_pt = {"i": 0}

            def qkv_head(h):
                hp = h % 2
                q_h, k_h, V_h = qT[hp], kT[hp], Vaug[hp]
                tq, tk, tv = tQ[hp], tK[hp], tV[hp]
                for tg in range(NG):
                    cols = slice(tg * 512, (tg + 1) * 512)
                    S.op("dve", [tKpe], [tk], lambda: nc.vector.tensor_copy(out=k_h[64:96, cols], in_=kpe[64:96, cols]))
                    yield
                    ba, bb = ps1(), ps1()
                    S.op("pe", [tWs, tCq], ba.toks, lambda: [nc.tensor.matmul(
                        ba.ap[0:96, :], wq_s[:, r, h * 96:(h + 1) * 96], cqT[:, r, cols], start=(r == 0), stop=(r == 1)) for r in range(2)])
                    S.op("pe", [tWs, tCq], bb.toks, lambda: [nc.tensor.matmul(
                        bb.ap[0:96, :], wq_sw[:, r, h * 96:(h + 1) * 96], cqT[:, r, cols], start=(r == 0), stop=(r == 1)) for r in range(2)])
                    bk = ps1()
                    S.op("pe", [tWs, tCkv], bk.toks, lambda: nc.tensor.matmul(
                        bk.ap[0:64, :], wkv_s[:, h * 128:h * 128 + 64], ckvT[:, cols], start=True, stop=True))
                    tt1, tt2 = tT1[tg % 2], tT2[tg % 2]
                    u1, u2 = t1[tg % 2], t2[tg % 2]
                    S.op("dve", ba.toks + [tC], [tt1], lambda: nc.vector.tensor_tensor(out=u1[0:96, :], in0=ba.ap[0:96, :], in1=cc[0:96, cols], op=ALU.mult))
                    yield
                    S.op("dve", bb.toks + [tC], [tt2], lambda: nc.vector.tensor_tensor(out=u2[0:96, :], in0=bb.ap[0:96, :], in1=ss[0:96, cols], op=ALU.mult))
                    yield
                    S.op("dve", [tt1, tt2], [tq], lambda: nc.vector.tensor_tensor(out=q_h[0:96, cols], in0=u1[0:96, :], in1=u2[0:96, :], op=ALU.add))
                    yield
                    S.op("dve", bk.toks, [tk], lambda: nc.vector.tensor_copy(out=k_h[0:64, cols], in_=bk.ap[0:64, :]))
                    yield
                vo = 0 if hp == 0 else 64
                for half in range(2):
                    bv = ps1()
                    S.op("pe", [tWs, tCkv], bv.toks, lambda: [nc.tensor.matmul(
                        bv.ap[:, j * 64:(j + 1) * 64], ckvT[:, (half * 8 + j) * 128:(half * 8 + j + 1) * 128],
                        wkv_s[:, h * 128 + 64:(h + 1) * 128], start=True, stop=True) for j in range(8)])
                    S.op("dve", bv.toks, [tv], lambda: nc.vector.tensor_copy(out=V_h[:, half * 8:(half + 1) * 8, vo:vo + 64], in_=v3(bv.ap, 64)))
                    yield

            def attn_head(h):
                hp = h % 2
                q_h, k_h, V_h = qT[hp], kT[hp], Vaug[hp]
                tq, tk, tv = tQ[hp], tK[hp], tV[hp]
                orow = slice(0, 64) if hp == 0 else slice(64, 128)
                drow = slice(64, 128) if hp == 0 else slice(0, 64)
                items = [(g, kj) for g in range(NG) for kj in range(4 * g + 4)]
                sbank = {}
                bo_of = {}

                def emit_S(i):
                    g, kj = items[i]
                    c0 = max(0, kj - 4 * g) * 128
                    if g not in bo_of:
                        bo_of[g] = ps1(hold=True)
                    b_ = ps1()
                    sbank[i] = b_
                    S.op("pe", [tq, tk], b_.toks, lambda: nc.tensor.matmul(
                        b_.ap[:, c0:512], k_h[0:96, kj * 128:(kj + 1) * 128], q_h[0:96, g * 512 + c0:(g + 1) * 512], start=True, stop=True))

                LOOK = 2
                for i in range(min(LOOK, len(items))):
                    emit_S(i)
                for i, (g, kj) in enumerate(items):
                    nk = 4 * g + 4
                    c0 = max(0, kj - 4 * g) * 128
                    b_ = sbank.pop(i)
                    pt, tpt = PTb[pstate_pt["i"] % 3], tPT[pstate_pt["i"] % 3]
                    pstate_pt["i"] += 1
                    S.op("act", b_.toks, [tpt], lambda: nc.scalar.activation(out=pt[:, c0:512], in_=b_.ap[:, c0:512], func=AF.Exp, scale=SCALE))
                    if kj >= 4 * g:
                        S.op("dve", [], [tpt], lambda: nc.vector.memset(pt[64:128, c0:c0 + 64], 0.0))
                    if i + LOOK < len(items):
                        emit_S(i + LOOK)
                    bo = bo_of[g]
                    S.op("pe", [tpt, tv], bo.toks, lambda: nc.tensor.matmul(
                        bo.ap[:, c0:512], V_h[:, kj, :], pt[:, c0:512], start=(kj == 0), stop=(kj == nk - 1)))
                    if kj == nk - 1:
                        S.op("dve", bo.toks, [tRd], lambda: nc.vector.reciprocal(out=rdb[drow, :], in_=bo.ap[drow, :]))
                        S.op("dve", bo.toks + [tRd], [tCat[h // 2]], lambda: nc.vector.tensor_tensor(
                            out=catT[orow, h // 2, g * 512:(g + 1) * 512], in0=bo.ap[orow, :], in1=rdb[drow, :], op=ALU.mult))
                        psrel(bo)
                    yield

            def p4_gen():
                for _ in qkv_head(0):
                    pass
                for h in range(8):
                    nxt = qkv_head(h + 1) if h + 1 < 8 else None
                    for n_, _ in enumerate(attn_head(h)):
                        if nxt is not None and n_ % 2 == 1:
                            if next(nxt, "done") == "done":
                                nxt = None
                        yield
                    if nxt is not None:
                        for _ in nxt:
                            pass

            if stop_after == "P4":
                for _ in p4_gen():
                    pass
                dump("cat_attn", catT[:, 0:4, :], tCat)
                break
            def ssd_set(i):
                b0 = 2048 if i == 0 else 15616
                so = 16 if i == 0 else 64
                d = dict(
                    adt=small[:, so:so + 8], acs_sb=small[:, so + 8:so + 16], dte=small[:, so + 16:so + 24],
                    cdk=small[:, so + 24:so + 32], dd=small[:, so + 32:so + 40], nacs=small[:, so + 40:so + 48],
                    R=Wf(b0, b0 + 2048), E=Wf(b0 + 2048, b0 + 3072), DEC=Wf(b0 + 3072, b0 + 4096),
                    Mfin=[W[:, b0 + 4096:b0 + 4608], W[:, b0 + 4608:b0 + 5120]],
                    CexpT=[W[:, b0 + 5120:b0 + 5632], W[:, b0 + 5632:b0 + 6144]],
                    xdt=W[:, b0 + 6144:b0 + 6656], xdte=W[:, b0 + 6656:b0 + 7168], Btok=W[:, b0 + 7168:b0 + 7424],
                    cbm=[Wf(b0 + 7424, b0 + 7680), Wf(b0 + 7680, b0 + 7936)],
                    tAdt=T(), tSm2=T(), tR=T(), tE=T(), tDec=T(), tXdt=T(), tXdte=T(), tBtok=T(), tCbm=T(),
                    tMf=[T(), T()], tCe=[T(), T()])
                return d

            SB = [ssd_set(0), ssd_set(1)]
            prev_f = Wf(9984, 11008)
            prev_bf = W[:, 11008:11520]
            yz = Wf(11520, 12544)
            sq = W[:, 12544:13056]
            rstd_g = Wf(13056, 13312)
            yv = Wf(13312, 13568)
            tPrevF, tPrevB, tYv, tYz, tSq2, tRg = T(), T(), T(), T(), T(), T()
            S.op("dve", [], [tWs], lambda: nc.vector.memset(small[:, 250:251], 0.0))
            S.op("act", [], [tWs], lambda: nc.scalar.copy(out=small[:, 251:252], in_=small[:, 250:251]))
            S.op("dve", [], [tPrevF], lambda: nc.vector.memset(prev_f, 0.0))
            S.op("dve", [], [tPrevB], lambda: nc.vector.memset(prev_bf, 0.0))

            def ssd_stage1(c):
                B_ = SB[c % 2]
                adt, acs_sb, dte, cdk, dd, nacs = B_["adt"], B_["acs_sb"], B_["dte"], B_["cdk"], B_["dd"], B_["nacs"]
                R, E, DEC, Mfin, CexpT, xdt, xdte, Btok, cbm = (B_[k] for k in ("R", "E", "DEC", "Mfin", "CexpT", "xdt", "xdte", "Btok", "cbm"))
                tAdt, tSm2, tR, tE, tDec, tXdt, tXdte, tBtok, tCbm, tMf, tCe = (B_[k] for k in (
                    "tAdt", "tSm2", "tR", "tE", "tDec", "tXdt", "tXdte", "tBtok", "tCbm", "tMf", "tCe"))
                tsl = slice(c * 128, (c + 1) * 128)
                dtc = dt_tok[:, c * 8:(c + 1) * 8]
                S.op("dve", [tDt, tC], [tAdt], lambda: nc.vector.tensor_tensor(out=adt, in0=dtc, in1=a_bc[:], op=ALU.mult))
                bx = ps1()
                S.op("pe", [tXs, tC], bx.toks, lambda: [nc.tensor.transpose(
                    bx.bf[:, j * 128:(j + 1) * 128], xsT[:, j, tsl], ident_bf[:]) for j in range(4)])
                S.op("dve", bx.toks + [tDt], [tXdt], lambda: nc.vector.tensor_tensor(
                    out=v3(xdt, 64), in0=v3(bx.bf[:, 0:512], 64), in1=dtc.unsqueeze(2).to_broadcast([128, 8, 64]), op=ALU.mult))
                bb_ = ps1()
                S.op("pe", [tB, tC], bb_.toks, lambda: [nc.tensor.transpose(
                    bb_.bf[:, g * 128:(g + 1) * 128], BT[:, g, tsl], ident_bf[:]) for g in range(2)])
                S.op("act", bb_.toks, [tBtok], lambda: nc.scalar.copy(out=Btok, in_=bb_.bf[:, 0:256]))
                yield
                ba_ = ps1()
                S.op("pe", [tAdt, tC], ba_.toks, lambda: [
                    nc.tensor.matmul(ba_.ap[:, 0:8], tri_f[:], adt, start=True, stop=True),
                    nc.tensor.matmul(ba_.ap[:, 8:16], ones_f[:], adt, start=True, stop=True)])
                S.op("act", ba_.toks, [tSm2], lambda: nc.scalar.copy(out=acs_sb, in_=ba_.ap[:, 0:8]))
                S.op("dve", ba_.toks + [tSm2], [tSm2], lambda: nc.vector.tensor_tensor(out=dd, in0=ba_.ap[:, 8:16], in1=acs_sb, op=ALU.subtract))
                S.op("act", [tSm2], [tSm2], lambda: nc.scalar.activation(out=dte, in_=dd, func=AF.Exp))
                S.op("act", ba_.toks, [tSm2], lambda: nc.scalar.activation(out=cdk, in_=ba_.ap[:, 8:16], func=AF.Exp))
                S.op("dve", [tSm2], [tSm2], lambda: nc.vector.tensor_scalar_mul(out=nacs, in0=acs_sb, scalar1=-1.0))
                yield
                S.op("dve", [tXdt, tSm2], [tXdte], lambda: nc.vector.tensor_tensor(
                    out=v3(xdte, 64), in0=v3(xdt, 64), in1=dte.unsqueeze(2).to_broadcast([128, 8, 64]), op=ALU.mult))
                S.op("dve", [tAdt, tC], [tR], lambda: nc.vector.tensor_tensor(
                    out=v3(R, 128), in0=tri_f[:].unsqueeze(1).to_broadcast([128, 8, 128]),
                    in1=adt.unsqueeze(2).to_broadcast([128, 8, 128]), op=ALU.mult))
                yield
                for g in range(2):
                    bc = ps1()
                    S.op("pe", [tB, tCt], bc.toks, lambda: nc.tensor.matmul(bc.ap[:, 0:128], BT[:, g, tsl], CT[:, g, tsl], start=True, stop=True))
                    S.op("act", bc.toks, [tCbm], lambda: nc.scalar.copy(out=cbm[g], in_=bc.ap[:, 0:128]))
                    bA = ps1()
                    S.op("pe", [tR, tC], bA.toks, lambda: nc.tensor.matmul(bA.ap, ones_f[:], R[:, g * 512:(g + 1) * 512], start=True, stop=True))
                    yield
                    S.op("act", bA.toks, [tDec], lambda: nc.scalar.activation(out=DEC, in_=bA.ap, func=AF.Exp))
                    for hh in range(4):
                        S.op("dve", bA.toks + [tC, tDec, tSm2], [tE], lambda: nc.vector.scalar_tensor_tensor(
                            out=E[:, hh * 128:(hh + 1) * 128], in0=bA.ap[:, hh * 128:(hh + 1) * 128],
                            scalar=nacs[:, g * 4 + hh:g * 4 + hh + 1], in1=negmask[:, 0:128], op0=ALU.add, op1=ALU.add))
                    S.op("act", [tE], [tE], lambda: nc.scalar.activation(out=E, in_=E, func=AF.Exp))
                    yield
                    S.op("dve", [tE, tCbm], [tMf[g]], lambda: nc.vector.tensor_tensor(
                        out=v3(Mfin[g], 128), in0=v3(E, 128), in1=cbm[g].unsqueeze(1).to_broadcast([128, 4, 128]), op=ALU.mult))
                    S.op("dve", [tDec, tCt], [tCe[g]], lambda: nc.vector.tensor_tensor(
                        out=v3(CexpT[g], 128), in0=v3(DEC, 128), in1=CT[:, g, tsl].unsqueeze(1).to_broadcast([128, 4, 128]), op=ALU.mult))
                    yield

            def ssd_stage2(c):
                B_ = SB[c % 2]
                cdk, Mfin, CexpT, xdt, xdte, Btok = (B_[k] for k in ("cdk", "Mfin", "CexpT", "xdt", "xdte", "Btok"))
                tSm2, tXdt, tXdte, tBtok, tMf, tCe = (B_[k] for k in ("tSm2", "tXdt", "tXdte", "tBtok", "tMf", "tCe"))
                tsl = slice(c * 128, (c + 1) * 128)
                for pair in range(4):
                    g = pair // 2
                    by = ps1()
                    for hp in range(2):
                        h = pair * 2 + hp
                        hh = h % 4
                        rows = slice(hp * 64, hp * 64 + 64)
                        tp_ = None if hp == 0 else (0, 64)
                        S.op("pe", [tXdt, tMf[g], tPrevB, tCe[g]], by.toks, lambda: [
                            nc.tensor.matmul(by.ap[rows, 0:128], xdt[:, h * 64:(h + 1) * 64], Mfin[g][:, hh * 128:(hh + 1) * 128],
                                             start=True, stop=False, tile_position=tp_),
                            nc.tensor.matmul(by.ap[rows, 0:128], prev_bf[:, h * 64:(h + 1) * 64], CexpT[g][:, hh * 128:(hh + 1) * 128],
                                             start=False, stop=True, tile_position=tp_)])
                    S.op("dve", by.toks + [tXs, tC], [tYv], lambda: nc.vector.scalar_tensor_tensor(
                        out=yv, in0=xsT[:, pair, tsl], scalar=dcol[:, pair:pair + 1], in1=by.ap[:, 0:128], op0=ALU.mult, op1=ALU.add))
                    S.op("dve", [tYv, tZ], [tYz], lambda: nc.vector.tensor_tensor(
                        out=yz[:, pair * 128:(pair + 1) * 128], in0=yv, in1=zs[:, pair, tsl], op=ALU.mult))
                    S.op("act", [tYz], [tSq2], lambda: nc.scalar.activation(
                        out=sq[:, pair * 128:(pair + 1) * 128], in_=yz[:, pair * 128:(pair + 1) * 128], func=AF.Square))
                    yield
                    if pair % 2 == 1:
                        bn_ = ps1()
                        S.op("pe", [tSq2, tC], bn_.toks, lambda: [nc.tensor.matmul(
                            bn_.ap[:, 0:128], ones_bf[:], sq[:, (pair - 1 + i) * 128:(pair + i) * 128], start=(i == 0), stop=(i == 1)) for i in range(2)])
                        rstd_from(rstd_g, bn_.ap[:, 0:128], 1.0 / 256, bn_.toks, [tRg])
                        for pp in (pair - 1, pair):
                            S.op("dve", [tYz, tRg, tC], [tCat[4 + pp]], lambda: nc.vector.scalar_tensor_tensor(
                                out=catT[:, 4 + pp, tsl], in0=yz[:, pp * 128:(pp + 1) * 128], scalar=ncol[:, pp:pp + 1], in1=rstd_g,
                                op0=ALU.mult, op1=ALU.mult))
                        yield
                bst = ps1()
                S.op("pe", [tBtok, tXdte], bst.toks, lambda: [nc.tensor.matmul(
                    bst.ap[:, g * 256:(g + 1) * 256], Btok[:, g * 128:(g + 1) * 128], xdte[:, g * 256:(g + 1) * 256],
                    start=True, stop=True) for g in range(2)])
                S.op("dve", [tSm2], [tPrevF], lambda: nc.vector.tensor_tensor(
                    out=v3(prev_f, 64), in0=v3(prev_f, 64), in1=cdk.unsqueeze(2).to_broadcast([128, 8, 64]), op=ALU.mult))
                S.op("dve", bst.toks, [tPrevF], lambda: nc.vector.tensor_tensor(out=prev_f, in0=prev_f, in1=bst.ap, op=ALU.add))
                S.op("act", [tPrevF], [tPrevB], lambda: nc.scalar.copy(out=prev_bf, in_=prev_f))
                yield

            def p5_gen():
                yield from ssd_stage1(0)
                for c in range(NT):
                    ga = ssd_stage1(c + 1) if c + 1 < NT else iter(())
                    gb = ssd_stage2(c)
                    a_alive = b_alive = True
                    while a_alive or b_alive:
                        if a_alive and next(ga, "done") == "done":
                            a_alive = False
                        if b_alive and next(gb, "done") == "done":
                            b_alive = False
                        yield

            g4, g5 = p4_gen(), p5_gen()
            alive = {"4": True, "5": True}

            def step(gen, key, n):
                for _ in range(n):
                    if alive[key]:
                        try:
                            next(gen)
                        except StopIteration:
                            alive[key] = False

            while alive["4"]:
                step(g4, "4", 64)
            while alive["5"]:
                step(g5, "5", 64)

            if stop_after == "P5":
                dump("cat_ssd", catT[:, 4:8, :], tCat)
                break
            S.barrier(engines=("act", "dve", "pool", "sp"))
            wout = v3(W[:, 0:8192], 1024)
            tWoL = [T() for _ in range(8)]
            for k in range(8):
                S.dma("sp", out=wout[:, k, :], in_=DWB["wout"][k * 128:(k + 1) * 128, :], reads=[tDW["wout"][k]], writes=[tWoL[k]])
            load_ln(0)
            xres = [Wf(8192, 10240), Wf(10240, 12288)]
            hbW = [W[:, 12288:13312], W[:, 13312:14336]]
            tXr, tHb = [T(), T()], [T(), T()]
            tH = [T() for _ in range(NT)]
            tHT = [T() for _ in range(NT)]
            def p6_mm(tt):
                tok = slice(tt * 128, (tt + 1) * 128)
                xr = xres[tt % 2]
                S.dma("sp", out=xr, in_=x[s, tok, :], writes=[tXr[tt % 2]])
                bm = ps2()
                S.op("pe", tCat + tWoL, bm.toks, lambda: [nc.tensor.matmul(
                    bm.ap[:, hf * 512:(hf + 1) * 512], catT[:, c, tok], wout[:, c, hf * 512:(hf + 1) * 512],
                    start=(c == 0), stop=(c == 7)) for hf in range(2) for c in range(8)])
                return bm

            bms = {0: p6_mm(0)}
            for tt in range(NT):
                if tt + 1 < NT:
                    bms[tt + 1] = p6_mm(tt + 1)
                bm = bms.pop(tt)
                xr = xres[tt % 2]
                hap = hview[:, tt, :]
                S.op("dve", bm.toks + [tXr[tt % 2]], [tH[tt]], lambda: nc.vector.scalar_tensor_tensor(
                    out=hap, in0=xr, scalar=ALPHA, in1=bm.ap, op0=ALU.mult, op1=ALU.add))
                layer_norm_tile(hap, tH[tt], 0, s)
                S.op("act", [tH[tt]], [tHb[tt % 2]], lambda: nc.scalar.copy(out=hbW[tt % 2], in_=hap))
                transpose_to(hbW[tt % 2], tHb[tt % 2], actT, tt, tHT[tt])
            if stop_after == "P6":
                dump("h1", hview, tH)
                break
            S.barrier(engines=("act", "dve", "pool", "sp"))
            xo = v3(K[:, 0:4096], 512)
            KxT = v3(K[:, 4096:6144], 256)
            Vx = v3(K[:, 6144:8192], 1024)
            memT = v3(K[:, 8192:10240], 256)
            membf = v3(K[:, 10240:12288], 1024)
            hbK = [K[:, 12288:13312], K[:, 13312:14336]]
            QxT = [K[:, 14336:14848], K[:, 14848:15360]]
            PTx = [K[:, 15360:15872], K[:, 15872:16384]]
            wk = v3(W[:, 0:8192], 1024)
            wv = v3(W[:, 8192:16384], 1024)
            wq = v3(W[:, 16384:24576], 1024)
            tMem, tMemT, tKx, tVx, tXo, tRdx = T(), T(), T(), T(), T(), T()
            tWk, tWv, tWq = [T() for _ in range(8)], [T() for _ in range(8)], [T() for _ in range(8)]
            tQx, tPx, tRt = [T(), T()], [T(), T()], T()
            for k in range(8):
                S.dma("sp", out=wk[:, k, :], in_=DWB["xwk"][k * 128:(k + 1) * 128, :], reads=[tDW["xwk"][k]], writes=[tWk[k]])
            S.dma("pool", out=membf, in_=mem[s].rearrange("(t p) d -> p t d", p=128), writes=[tMem])
            for k in range(8):
                S.dma("sp", out=wv[:, k, :], in_=DWB["xwv"][k * 128:(k + 1) * 128, :], reads=[tDW["xwv"][k]], writes=[tWv[k]])
            for k in range(8):
                S.dma("sp", out=wq[:, k, :], in_=DWB["xwq"][k * 128:(k + 1) * 128, :], reads=[tDW["xwq"][k]], writes=[tWq[k]])
            load_ln(1)
            for mt in range(2):
                pb = ps1()
                S.op("pe", [tMem, tC], pb.toks, lambda: [nc.tensor.transpose(
                    pb.bf[:, k * 128:(k + 1) * 128], membf[:, mt, k * 128:(k + 1) * 128], ident_bf[:]) for k in range(8)])
                S.op("act", pb.toks, [tMemT], lambda: nc.scalar.copy(out=memT[:, :, mt * 128:(mt + 1) * 128], in_=v3(pb.bf[:, 0:1024], 128)))
            for c in range(8):
                b_ = ps1()
                S.op("pe", [tMemT] + tWk, b_.toks, lambda: [nc.tensor.matmul(
                    b_.ap[:, 0:256], wk[:, k, c * 128:(c + 1) * 128], memT[:, k, :], start=(k == 0), stop=(k == 7)) for k in range(8)])
                S.op("act", b_.toks, [tKx], lambda: nc.scalar.copy(out=KxT[:, c, :], in_=b_.ap[:, 0:256]))
            for mt in range(2):
                b2 = ps2()
                S.op("pe", [tMemT] + tWv, b2.toks, lambda: [nc.tensor.matmul(
                    b2.ap[:, hf * 512:(hf + 1) * 512], memT[:, k, mt * 128:(mt + 1) * 128], wv[:, k, hf * 512:(hf + 1) * 512],
                    start=(k == 0), stop=(k == 7)) for hf in range(2) for k in range(8)])
                S.op("act", b2.toks, [tVx], lambda: nc.scalar.copy(out=Vx[:, mt, :], in_=b2.ap))
            wo = wk
            for k in range(8):
                S.dma("sp", out=wo[:, k, :], in_=DWB["xwo"][k * 128:(k + 1) * 128, :], reads=[tDW["xwo"][k]], writes=[tWk[k]])
            XSC = 256.0 ** -0.5
            lg = small[:, 64:100]
            gmax, ngmax, gsum, ggate = small[:, 100:101], small[:, 101:102], small[:, 102:103], small[:, 103:104]
            gone, ge, pen = small[:, 104:108], small[:, 108:112], small[:, 112:116]
            me, one1, me2, one2 = small[:, 116:148], small[:, 148:180], small[:, 180:212], small[:, 212:244]
            m1, m2, d21, e21, g1, g2 = (small[:, 244 + i:245 + i] for i in range(6))
            QxTg = [[K[:, 14336 + (i * 2 + dc) * 512:14336 + (i * 2 + dc + 1) * 512] for dc in range(2)] for i in range(2)]
            PTxg = [[K[:, 10240 + (i * 2 + mt) * 512:10240 + (i * 2 + mt + 1) * 512] for mt in range(2)] for i in range(2)]
            tQxg = [[T(), T()], [T(), T()]]
            tPxg = [[T(), T()], [T(), T()]]
            tRdxg = [T(), T()]
            units = [(tg, h) for tg in range(NG) for h in range(4)]

            def xa_A(i):
                tg, h = units[i]
                cols = slice(tg * 512, (tg + 1) * 512)
                for dc in range(2):
                    c = h * 2 + dc
                    bq_ = ps1()
                    S.op("pe", tHT[tg * 4:tg * 4 + 4] + tWq, bq_.toks, lambda: [nc.tensor.matmul(
                        bq_.ap, wq[:, k, c * 128:(c + 1) * 128], actT[:, k, cols], start=(k == 0), stop=(k == 7)) for k in range(8)])
                    S.op("act", bq_.toks, [tQxg[i % 2][dc]], lambda: nc.scalar.copy(out=QxTg[i % 2][dc], in_=bq_.ap))

            def xa_B(i):
                tg, h = units[i]
                for mt in range(2):
                    bs_ = ps1()
                    S.op("pe", tQxg[i % 2] + [tKx, tMem], bs_.toks, lambda: [nc.tensor.matmul(
                        bs_.ap, KxT[:, h * 2 + dc, mt * 128:(mt + 1) * 128], QxTg[i % 2][dc], start=(dc == 0), stop=(dc == 1)) for dc in range(2)])
                    S.op("act", bs_.toks, [tPxg[i % 2][mt]], lambda: nc.scalar.activation(out=PTxg[i % 2][mt], in_=bs_.ap, func=AF.Exp, scale=XSC))

            def xa_C(i):
                tg, h = units[i]
                ptx = PTxg[i % 2]
                rd_ = rdx[:, (i % 2) * 512:(i % 2 + 1) * 512]
                bd_ = ps1()
                S.op("pe", tPxg[i % 2] + [tC], bd_.toks, lambda: [nc.tensor.matmul(
                    bd_.ap, ones_bf[:], ptx[mt], start=(mt == 0), stop=(mt == 1)) for mt in range(2)])
                S.op("dve", bd_.toks, [tRdxg[i % 2]], lambda: nc.vector.reciprocal(out=rd_, in_=bd_.ap))
                for dc in range(2):
                    c = h * 2 + dc
                    bo_ = ps1()
                    S.op("pe", tPxg[i % 2] + [tVx], bo_.toks, lambda: [nc.tensor.matmul(
                        bo_.ap, Vx[:, mt, c * 128:(c + 1) * 128], ptx[mt], start=(mt == 0), stop=(mt == 1)) for mt in range(2)])
                    S.op("dve", bo_.toks + [tRdxg[i % 2]], [tXo], lambda: nc.vector.tensor_tensor(out=xo[:, c, :], in0=bo_.ap, in1=rd_, op=ALU.mult))

            def xa_mm(tt):
                j = tt % 4
                bm = ps2()
                S.op("pe", [tXo] + tWk, bm.toks, lambda: [nc.tensor.matmul(
                    bm.ap[:, hf * 512:(hf + 1) * 512], xo[:, c, j * 128:(j + 1) * 128], wo[:, c, hf * 512:(hf + 1) * 512],
                    start=(c == 0), stop=(c == 7)) for hf in range(2) for c in range(8)])
                return bm

            V_ = nc.vector
            AXX_ = mybir.AxisListType.X
            tRtS = [T(), T()]

            def xa_tail(tt, bm, si):
                rb0 = 64 + si * 256
                lg = small[:, rb0:rb0 + 36]
                gmax, ngmax, gsum, ggate = (small[:, rb0 + 36 + i:rb0 + 37 + i] for i in range(4))
                gone, ge, pen = small[:, rb0 + 40:rb0 + 44], small[:, rb0 + 44:rb0 + 48], small[:, rb0 + 48:rb0 + 52]
                me, me2 = small[:, rb0 + 52:rb0 + 84], small[:, rb0 + 84:rb0 + 116]
                m1, m2, d21, e21, g1 = (small[:, rb0 + 116 + i:rb0 + 117 + i] for i in range(5))
                tRt = tRtS[si]
                rt = lambda f: S.op("dve", [tRt], [tRt], f)
                hap = hview[:, tt, :]
                tHt = tH[tt]
                gt = s * NT + tt
                S.op("dve", bm.toks, [tHt], lambda: nc.vector.scalar_tensor_tensor(
                    out=hap, in0=hap, scalar=ALPHA, in1=bm.ap, op0=ALU.mult, op1=ALU.add))
                j_ = lnstate["i"] % 4
                lnstate["i"] += 1
                tS = tLNS[j_]
                base = j_ * 16
                st, mv = lnst[:, base:base + 12], lnst[:, base + 12:base + 14]
                rs, nm = lnst[:, base + 14:base + 15], lnst[:, base + 15:base + 16]
                S.op("dve", [tHt], [tS], lambda: nc.vector.bn_stats(out=st[:, 0:6], in_=hap[:, 0:512]))
                S.op("dve", [tHt], [tS], lambda: nc.vector.bn_stats(out=st[:, 6:12], in_=hap[:, 512:1024]))
                S.op("dve", [], [tS], lambda: nc.vector.bn_aggr(out=mv, in_=st))
                yield
                rstd_from(rs, mv[:, 1:2], 1.0, [tS], [tS])
                yield
                S.op("dve", [tS], [tS], lambda: nc.vector.scalar_tensor_tensor(
                    out=nm, in0=mv[:, 0:1], scalar=-1.0, in1=rs, op0=ALU.mult, op1=ALU.mult))
                yield
                S.op("act", [tS], [tHt], lambda: nc.scalar.activation(out=hap, in_=hap, func=AF.Identity, scale=rs, bias=nm))
                yield
                S.op("dve", [tLN], [tHt], lambda: nc.vector.tensor_tensor(out=hap, in0=hap, in1=lng[:], op=ALU.mult))
                yield
                S.op("dve", [tLN], [tHt], lambda: nc.vector.tensor_tensor(out=hap, in0=hap, in1=lnb[:], op=ALU.add))
                yield
                S.op("act", [tHt], [tHb[tt % 2]], lambda: nc.scalar.copy(out=hbK[tt % 2], in_=hap))
                yield
                transpose_to(hbK[tt % 2], tHb[tt % 2], actT, tt, tHT[tt])
                yield
                bl = ps1()
                S.op("pe", [tHT[tt], tC], bl.toks, lambda: [nc.tensor.matmul(
                    bl.ap[:, 0:36], actT[:, k, tt * 128:(tt + 1) * 128], v3(rw[:], 36)[:, k, :], start=(k == 0), stop=(k == 7)) for k in range(8)])
                yield
                S.op("dve", bl.toks + [tC], [tRt], lambda: V_.tensor_tensor(out=lg, in0=bl.ap[:, 0:36], in1=rb[:], op=ALU.add))
                rt(lambda: V_.reduce_max(out=gmax, in_=lg[:, 0:4], axis=AXX_))
                rt(lambda: V_.tensor_scalar(out=gone, in0=lg[:, 0:4], scalar1=gmax, scalar2=None, op0=ALU.is_equal))
                rt(lambda: V_.tensor_scalar_mul(out=ngmax, in0=gmax, scalar1=-1.0))
                yield
                S.op("act", [tRt], [tRt], lambda: nc.scalar.activation(out=ge, in_=lg[:, 0:4], func=AF.Exp, bias=ngmax))
                yield
                rt(lambda: V_.reduce_sum(out=gsum, in_=ge, axis=AXX_))
                rt(lambda: V_.reciprocal(out=ggate, in_=gsum))
                rt(lambda: V_.tensor_scalar(out=pen, in0=gone, scalar1=-1.0, scalar2=1e9, op0=ALU.add, op1=ALU.mult))
                rt(lambda: V_.tensor_tensor(out=v3(me, 8), in0=v3(lg[:, 4:36], 8), in1=pen.unsqueeze(2).to_broadcast([128, 4, 8]), op=ALU.add))
                rt(lambda: V_.reduce_max(out=m1, in_=me, axis=AXX_))
                o1 = ONE[:, (gt * 2) * 32:(gt * 2 + 1) * 32]
                o2 = ONE[:, (gt * 2 + 1) * 32:(gt * 2 + 2) * 32]
                S.op("dve", [tRt], [tRt, tRk], lambda: V_.tensor_scalar(out=o1, in0=me, scalar1=m1, scalar2=None, op0=ALU.is_equal))
                rt(lambda: V_.scalar_tensor_tensor(out=me2, in0=o1, scalar=-1e9, in1=me, op0=ALU.mult, op1=ALU.add))
                rt(lambda: V_.reduce_max(out=m2, in_=me2, axis=AXX_))
                S.op("dve", [tRt], [tRt, tRk], lambda: V_.tensor_scalar(out=o2, in0=me2, scalar1=m2, scalar2=None, op0=ALU.is_equal))
                rt(lambda: V_.tensor_tensor(out=d21, in0=m2, in1=m1, op=ALU.subtract))
                yield
                S.op("act", [tRt], [tRt], lambda: nc.scalar.activation(out=e21, in_=d21, func=AF.Exp))
                yield
                rt(lambda: V_.tensor_scalar_add(out=g1, in0=e21, scalar1=1.0))
                rt(lambda: V_.reciprocal(out=g1, in_=g1))
                S.op("dve", [tRt], [tRt, tRk], lambda: V_.tensor_tensor(out=G12[:, gt * 2:gt * 2 + 1], in0=g1, in1=ggate, op=ALU.mult))
                S.op("dve", [tRt], [tRt, tRk], lambda: V_.tensor_tensor(out=G12[:, gt * 2 + 1:gt * 2 + 2], in0=G12[:, gt * 2:gt * 2 + 1], in1=e21, op=ALU.mult))
                bR = ps1()
                S.op("pe", [tRk, tC], bR.toks, lambda: [
                    nc.tensor.matmul(bR.ap[:, 0:64], su_bf[:], ONE[:, gt * 64:(gt + 1) * 64], start=True, stop=True),
                    nc.tensor.matmul(bR.ap[:, 64:128], ones_bf[:], ONE[:, gt * 64:(gt + 1) * 64], start=True, stop=True)])
                yield
                ta, tb = me, me2
                S.op("dve", bR.toks + [tRk, tRt], [tRt], lambda: V_.tensor_tensor(out=ta, in0=bR.ap[:, 0:32], in1=Crun[:], op=ALU.add))
                rt(lambda: V_.tensor_tensor(out=tb, in0=ta, in1=o1, op=ALU.mult))
                S.op("dve", [tRt], [tRt, tRk], lambda: V_.reduce_sum(out=RK[:, gt * 2:gt * 2 + 1], in_=tb, axis=AXX_))
                S.op("dve", bR.toks + [tRk, tRt], [tRt], lambda: V_.tensor_tensor(out=ta, in0=bR.ap[:, 32:64], in1=Crun[:], op=ALU.add))
                S.op("dve", bR.toks + [tRt], [tRt], lambda: V_.tensor_tensor(out=ta, in0=bR.ap[:, 64:96], in1=ta, op=ALU.add))
                rt(lambda: V_.tensor_tensor(out=tb, in0=ta, in1=o2, op=ALU.mult))
                S.op("dve", [tRt], [tRt, tRk], lambda: V_.reduce_sum(out=RK[:, gt * 2 + 1:gt * 2 + 2], in_=tb, axis=AXX_))
                S.op("dve", bR.toks + [tRt], [tRk], lambda: V_.tensor_tensor(out=Crun[:], in0=bR.ap[:, 64:96], in1=Crun[:], op=ALU.add))
                S.op("dve", bR.toks + [tRt], [tRk], lambda: V_.tensor_tensor(out=Crun[:], in0=bR.ap[:, 96:128], in1=Crun[:], op=ALU.add))
                S.dma("sp", out=XB[gt * 128:(gt + 1) * 128, :], in_=hbK[tt % 2], reads=[tHb[tt % 2]])
                S.dma("sp", out=H2D[gt * 128:(gt + 1) * 128, :], in_=hap, reads=[tHt])
                yield

            xa_A(0)
            for ui in range(len(units)):
                xa_B(ui)
                if ui + 1 < len(units):
                    xa_A(ui + 1)
                xa_C(ui)
                tg, h = units[ui]
                if h != 3:
                    continue
                for pr in ((0, 1), (2, 3)):
                    gens = []
                    for si, j in enumerate(pr):
                        tt = tg * 4 + j
                        gens.append(xa_tail(tt, xa_mm(tt), si))
                    alive_ = [True, True]
                    while any(alive_):
                        for gi in range(2):
                            if alive_[gi] and next(gens[gi], "done") == "done":
                                alive_[gi] = False
            if stop_after == "P7":
                dump("h2", hview, tH)
                break
            S.barrier(engines=("act", "dve", "pool", "sp"))
        if stop_after is None:
            S.barrier(full=True)
            V_ = nc.vector
            AXX = mybir.AxisListType.X
            tM = T()
            padc, pA, pB, basev = small[:, 116:148], small[:, 148:180], small[:, 180:212], small[:, 212:244]
            cmpb = H[:, 0:1024]
            mo = lambda f, extra=(): S.op("dve", [tM, tRk, tC] + list(extra), [tM], f)
            mo(lambda: V_.tensor_tensor(out=v3(cmpb, 32), in0=Crun[:].unsqueeze(2).to_broadcast([128, 32, 32]),
                                        in1=thr[:].unsqueeze(1).to_broadcast([128, 32, 32]), op=ALU.is_gt))
            mo(lambda: V_.reduce_sum(out=padc, in_=v3(cmpb, 32), axis=AXX))
            mo(lambda: V_.tensor_scalar_mul(out=padc, in0=padc, scalar1=float(TSZ)))
            cur, nxt = padc, pA
            for sft in (1, 2, 4, 8, 16):
                mo(lambda: V_.tensor_copy(out=nxt[:, 0:sft], in_=cur[:, 0:sft]))
                mo(lambda: V_.tensor_tensor(out=nxt[:, sft:32], in0=cur[:, sft:32], in1=cur[:, 0:32 - sft], op=ALU.add))
                cur, nxt = nxt, (pB if nxt is pA else pA)
            endv = cur
            mo(lambda: V_.tensor_tensor(out=basev, in0=endv, in1=padc, op=ALU.subtract))
            cmpt = H[:, 2048:2048 + NTI * 32]
            tef = mo_f[:, 0:NTI]
            widx = mo_i[:, 0:NTI]
            mo(lambda: V_.tensor_tensor(out=v3(cmpt, 32), in0=endv.unsqueeze(1).to_broadcast([128, NTI, 32]),
                                        in1=tstart[:, 0:NTI].unsqueeze(2).to_broadcast([128, NTI, 32]), op=ALU.is_le))
            mo(lambda: V_.reduce_sum(out=tef, in_=v3(cmpt, 32), axis=AXX))
            mo(lambda: V_.tensor_scalar(out=tef, in0=tef, scalar1=128.0, scalar2=pcol[:, 0:1], op0=ALU.mult, op1=ALU.add))
            mo(lambda: V_.tensor_copy(out=widx, in_=tef))
            tmp3 = H[:, 4096:4096 + GT * 64]
            posf = mo_f[:, 64:64 + GT * 2]
            posI = mo_i[:, 64:64 + GT * 2]
            mo(lambda: V_.tensor_tensor(out=v3(tmp3, 32), in0=v3(ONE[:], 32), in1=basev.unsqueeze(1).to_broadcast([128, GT * 2, 32]), op=ALU.mult))
            mo(lambda: V_.reduce_sum(out=posf, in_=v3(tmp3, 32), axis=AXX))
            mo(lambda: V_.tensor_tensor(out=posf, in0=posf, in1=RK[:], op=ALU.add))
            mo(lambda: V_.tensor_copy(out=posI, in_=posf))
            xbt = [A[:, i * 1024:(i + 1) * 1024] for i in range(2)]
            tXb = [T(), T()]
            for gt in range(GT):
                S.dma("sp", out=xbt[gt % 2], in_=XB[gt * 128:(gt + 1) * 128, :], writes=[tXb[gt % 2]])
                for k in range(2):
                    S.dma("pool", out=XS[:, :], in_=xbt[gt % 2], out_off=posI[:, gt * 2 + k:gt * 2 + k + 1], bound=NTI * TSZ - 1,
                          reads=[tXb[gt % 2], tM])
            S.barrier()
            xsb = [v3(A[:, 2048 + i * 4096:2048 + i * 4096 + TB * 1024], 1024) for i in range(2)]
            xst = [v3(K[:, 4096 + i * 4096:4096 + i * 4096 + 8 * TSZ], TSZ) for i in range(2)]
            Ssb = [Kf(0, 2 * TSZ), Kf(1024, 1024 + 2 * TSZ)]
            HD = [v3(K[:, 2048:2048 + 2 * TSZ], TSZ), v3(K[:, 3072:3072 + 2 * TSZ], TSZ)]
            Ysb = [K[:, 12288:13312], K[:, 13312:14336]]
            tXsb, tXst, tSs, tHD, tY = [T(), T()], [T(), T()], [T(), T()], [T(), T()], [T(), T()]
            NWB = 3
            Eb = [W[:, i * 6144:(i + 1) * 6144] for i in range(NWB)]
            tEw = [[T(), T(), T()] for _ in range(NWB)]

            def moe_prefetch_w(ti):
                eb = Eb[ti % NWB]
                for part, src in enumerate((WGB, WUB, WDB)):
                    S.dma("pool", out=eb[:, part * 2048:(part + 1) * 2048], in_=src[:, :], in_off=widx[:, ti:ti + 1], bound=NEXP * 128 - 1,
                          reads=[tM], writes=[tEw[ti % NWB][part]])

            def moe_prefetch_x(ti):
                S.dma("sp", out=xsb[ti % 2], in_=XS[ti * TSZ:(ti + 1) * TSZ, :].rearrange("(j p) d -> p j d", p=128), writes=[tXsb[ti % 2]])

            def moe_TR(ti):
                xs_, txs = xsb[ti % 2], tXsb[ti % 2]
                xt_, txt = xst[ti % 2], tXst[ti % 2]
                for j in range(TB):
                    pb = ps1()
                    S.op("pe", [txs, tC], pb.toks, lambda: [nc.tensor.transpose(
                        pb.bf[:, k * 128:(k + 1) * 128], xs_[:, j, k * 128:(k + 1) * 128], ident_bf[:]) for k in range(8)])
                    if j % 2 == 0:
                        S.op("act", pb.toks, [txt], lambda: nc.scalar.copy(out=xt_[:, :, j * 128:(j + 1) * 128], in_=v3(pb.bf[:, 0:1024], 128)))
                    else:
                        S.op("dve", pb.toks, [txt], lambda: nc.vector.tensor_copy(out=xt_[:, :, j * 128:(j + 1) * 128], in_=v3(pb.bf[:, 0:1024], 128)))

            def moe_GU(ti):
                eb = Eb[ti % NWB]
                tE3 = tEw[ti % NWB]
                wg = v3(eb[:, 0:2048], 256)
                wu = v3(eb[:, 2048:4096], 256)
                xt_, txt = xst[ti % 2], tXst[ti % 2]
                hd, thd = HD[ti % 2], tHD[ti % 2]
                for fc in range(2):
                    bg, bu = ps1(), ps1()
                    S.op("pe", [txt, tE3[0]], bg.toks, lambda: [nc.tensor.matmul(
                        bg.ap[:, 0:TSZ], wg[:, k, fc * 128:(fc + 1) * 128], xt_[:, k, :], start=(k == 0), stop=(k == 7)) for k in range(8)])
                    S.op("pe", [txt, tE3[1]], bu.toks, lambda: [nc.tensor.matmul(
                        bu.ap[:, 0:TSZ], wu[:, k, fc * 128:(fc + 1) * 128], xt_[:, k, :], start=(k == 0), stop=(k == 7)) for k in range(8)])
                    S.op("act", bg.toks, [tSs[fc]], lambda: nc.scalar.activation(out=Ssb[fc], in_=bg.ap[:, 0:TSZ], func=AF.Silu))
                    S.op("dve", bu.toks + [tSs[fc]], [thd], lambda: nc.vector.tensor_tensor(out=hd[:, fc, :], in0=Ssb[fc], in1=bu.ap[:, 0:TSZ], op=ALU.mult))

            def moe_D(ti):
                eb = Eb[ti % NWB]
                tE3 = tEw[ti % NWB]
                wd = v3(eb[:, 4096:6144], 1024)
                hd, thd = HD[ti % 2], tHD[ti % 2]
                for j in range(TB):
                    bd2 = ps2()
                    S.op("pe", [thd, tE3[2]], bd2.toks, lambda: [nc.tensor.matmul(
                        bd2.ap[:, hf * 512:(hf + 1) * 512], hd[:, fc, j * 128:(j + 1) * 128], wd[:, fc, hf * 512:(hf + 1) * 512],
                        start=(fc == 0), stop=(fc == 1)) for hf in range(2) for fc in range(2)])
                    yb, ty = Ysb[moe_state["yi"] % 2], tY[moe_state["yi"] % 2]
                    moe_state["yi"] += 1
                    S.op("act", bd2.toks, [ty], lambda: nc.scalar.copy(out=yb, in_=bd2.ap))
                    S.dma("sp", out=YS[ti * TSZ + j * 128:ti * TSZ + (j + 1) * 128, :], in_=yb, reads=[ty])

            moe_state = {"yi": 0}
            moe_prefetch_w(0)
            moe_prefetch_x(0)
            if NTI > 1:
                moe_prefetch_w(1)
                moe_prefetch_x(1)
            moe_TR(0)
            moe_GU(0)
            for ti in range(NTI):
                if ti + 2 < NTI:
                    moe_prefetch_w(ti + 2)
                if ti + 1 < NTI:
                    moe_TR(ti + 1)
                if ti + 2 < NTI:
                    moe_prefetch_x(ti + 2)
                moe_D(ti)
                if ti + 1 < NTI:
                    moe_GU(ti + 1)
            S.barrier()
            load_ln(2)
            NYB = 4
            ybuf = [[H[:, i * 3072:i * 3072 + 512].bitcast(BF16), H[:, i * 3072 + 512:i * 3072 + 1024].bitcast(BF16),
                     H[:, i * 3072 + 1024:i * 3072 + 2048], H[:, i * 3072 + 2048:i * 3072 + 3072]] for i in range(NYB)]
            tYb = [[T(), T(), T(), T()] for _ in range(NYB)]

            def comb_prefetch(gt):
                y1, y2, ytmp, hh = ybuf[gt % NYB]
                t1_, t2_, tt_, th = tYb[gt % NYB]
                S.dma("pool", out=y1, in_=YS[:, :], in_off=posI[:, gt * 2:gt * 2 + 1], bound=NTI * TSZ - 1, reads=[tM], writes=[t1_])
                S.dma("pool", out=y2, in_=YS[:, :], in_off=posI[:, gt * 2 + 1:gt * 2 + 2], bound=NTI * TSZ - 1, reads=[tM], writes=[t2_])
                S.dma("sp", out=hh, in_=H2D[gt * 128:(gt + 1) * 128, :], writes=[th])

            def comb_gen(gt):
                y1, y2, ytmp, hh = ybuf[gt % NYB]
                t1_, t2_, tt_, th = tYb[gt % NYB]
                S.op("act", [t1_, tRk], [tt_], lambda: nc.scalar.activation(out=ytmp, in_=y1, func=AF.Copy, scale=G12[:, gt * 2:gt * 2 + 1]))
                yield
                S.op("dve", [t2_, tRk, tt_], [tt_], lambda: V_.scalar_tensor_tensor(
                    out=ytmp, in0=y2, scalar=G12[:, gt * 2 + 1:gt * 2 + 2], in1=ytmp, op0=ALU.mult, op1=ALU.add))
                S.op("dve", [tt_], [th], lambda: V_.scalar_tensor_tensor(out=hh, in0=hh, scalar=ALPHA, in1=ytmp, op0=ALU.mult, op1=ALU.add))
                j_ = lnstate["i"] % 4
                lnstate["i"] += 1
                tS = tLNS[j_]
                base = j_ * 16
                st, mv = lnst[:, base:base + 12], lnst[:, base + 12:base + 14]
                rs, nm = lnst[:, base + 14:base + 15], lnst[:, base + 15:base + 16]
                S.op("dve", [th], [tS], lambda: nc.vector.bn_stats(out=st[:, 0:6], in_=hh[:, 0:512]))
                S.op("dve", [th], [tS], lambda: nc.vector.bn_stats(out=st[:, 6:12], in_=hh[:, 512:1024]))
                S.op("dve", [], [tS], lambda: nc.vector.bn_aggr(out=mv, in_=st))
                yield
                rstd_from(rs, mv[:, 1:2], 1.0, [tS], [tS])
                yield
                S.op("dve", [tS], [tS], lambda: nc.vector.scalar_tensor_tensor(
                    out=nm, in0=mv[:, 0:1], scalar=-1.0, in1=rs, op0=ALU.mult, op1=ALU.mult))
                yield
                S.op("act", [tS], [th], lambda: nc.scalar.activation(out=hh, in_=hh, func=AF.Identity, scale=rs, bias=nm))
                yield
                S.op("dve", [tLN], [th], lambda: nc.vector.tensor_tensor(out=hh, in0=hh, in1=lng[:], op=ALU.mult))
                yield
                S.op("dve", [tLN], [th], lambda: nc.vector.tensor_tensor(out=hh, in0=hh, in1=lnb[:], op=ALU.add))
                sq_, tq_ = gt // NT, gt % NT
                S.dma("sp", out=out_d[sq_, tq_ * 128:(tq_ + 1) * 128, :], in_=hh, reads=[th])
                yield

            comb_prefetch(0)
            comb_prefetch(1)
            for g0 in range(0, GT, 2):
                for gn in (g0 + 2, g0 + 3):
                    if gn < GT:
                        comb_prefetch(gn)
                gens = [comb_gen(g0), comb_gen(g0 + 1)]
                alive_ = [True, True]
                while any(alive_):
                    for gi in range(2):
                        if alive_[gi] and next(gens[gi], "done") == "done":
                            alive_[gi] = False
        S.barrier(engines=("sp",), full=True)
    return nc


def _rope_tables():
    pos = np.arange(SEQ, dtype=np.float32)
    inv_freq = (np.float32(10000.0) ** (-(np.arange(0, 32, 2, dtype=np.float32)) / np.float32(32))).astype(np.float32)
    ang = (pos[:, None] * inv_freq[None, :]).astype(np.float32)
    cos = np.cos(ang).astype(np.float32)
    sin = np.sin(ang).astype(np.float32)
    cc = np.zeros((128, SEQ), np.float32)
    ss = np.zeros((128, SEQ), np.float32)
    cc[0:64] = 1.0
    cc[64:80] = cos.T
    cc[80:96] = cos.T
    ss[64:80] = -sin.T
    ss[80:96] = sin.T
    return cc, ss


def prep_shared(inp):
    f = np.float32
    g = lambda k: np.asarray(inp[k], dtype=f)[0]
    w_in = g("w_in")
    wkr = np.zeros((D, 96), f)
    wkr[:, 64:80] = w_in[:, 400:416]
    wkr[:, 80:96] = w_in[:, 384:400]
    wq = g("w_q_up")
    perm = np.arange(768)
    for h in range(8):
        for j in range(32):
            perm[h * 96 + 64 + j] = h * 96 + 64 + (j + 16) % 32
    wq_sw = wq[:, perm]
    convw = g("ssd_conv_w").reshape(4, 8, 128).transpose(2, 1, 0).reshape(128, 32)
    convb = g("ssd_conv_b").reshape(8, 128).T
    dtb = np.broadcast_to(np.tile(g("ssd_dt_bias"), 16)[None, :], (128, 128))
    alog = np.broadcast_to(g("ssd_a_log")[None, :], (128, 8))
    sd = g("ssd_d")
    dcol = np.stack([sd[pair * 2 + (np.arange(128) // 64)] for pair in range(4)], axis=1)
    ncol = g("ssd_norm").reshape(4, 128).T
    lnp = np.stack([np.broadcast_to(g(k)[None, :], (128, D)) for k in ("ln1_g", "ln1_b", "ln2_g", "ln2_b", "ln3_g", "ln3_b")])
    rw = np.concatenate([g("router_group_w"), g("router_expert_w")], axis=1)
    rb = np.broadcast_to(np.concatenate([g("router_group_b"), g("router_expert_b")])[None, :], (128, 36))
    tri = np.triu(np.ones((128, 128), f))
    nm = np.where(np.arange(128)[None, :] >= np.arange(128)[:, None], 0.0, -30000.0).astype(f)
    cc, ss = _rope_tables()
    su = np.triu(np.ones((128, 128), f), k=1)
    thr = np.broadcast_to((np.arange(32, dtype=f) * 384.0)[None, :], (128, 32))
    tstart = np.broadcast_to((np.arange(64, dtype=f) * 384.0)[None, :], (128, 64))
    pcol = np.arange(128, dtype=f).reshape(128, 1)
    sh = {
        "su": su, "thr": thr, "tstart": tstart, "pcol": pcol,
        "w_in": w_in, "wkr_sw": wkr, "wq": wq, "wq_sw": wq_sw, "qn": g("mla_q_norm").reshape(2, 128).T,
        "wkv": g("w_kv_up"), "kvn": g("mla_kv_norm").reshape(128, 1), "convw": convw, "convb": convb,
        "dtb": dtb, "alog": alog, "dcol": dcol, "ncol": ncol, "wout": g("w_out"), "xwq": g("xa_wq"),
        "xwk": g("xa_wk"), "xwv": g("xa_wv"), "xwo": g("xa_wo"), "lnp": lnp, "rw": rw, "rb": rb,
        "wg": g("expert_w_gate"), "wu": g("expert_w_up"), "wd": g("expert_w_down"),
        "ident": np.eye(128, dtype=f), "tri": tri, "negmask": np.tile(nm, (1, 4)), "cc": cc, "ss": ss,
    }
    return {k: np.ascontiguousarray(v, dtype=f) for k, v in sh.items()}


def kernel(**inputs):
    sh = prep_shared(inputs)
    x = np.asarray(inputs["x"], dtype=np.float32)
    mem = np.asarray(inputs["mem"], dtype=np.float32)
    nc = build(n_seq=2)
    in_maps = []
    for c in range(N_CORES):
        m = dict(sh)
        m["x"] = np.ascontiguousarray(x[2 * c:2 * c + 2])
        m["mem"] = np.ascontiguousarray(mem[2 * c:2 * c + 2])
        in_maps.append(m)
    res = run_bass_kernel_spmd(nc, in_maps, core_ids=list(range(N_CORES)))
    return np.concatenate([r["out"] for r in res.results], axis=0)
```

```python
import numpy as np
from contextlib import ExitStack
import concourse.bass as bass
import concourse.mybir as mybir
from concourse.bass_utils import run_bass_kernel_spmd

F32 = mybir.dt.float32
BF16 = mybir.dt.bfloat16
AF = mybir.ActivationFunctionType
ALU = mybir.AluOpType

N_CORES = 8
SEQ = 2048
D = 1024
NT = SEQ // 128
NG = SEQ // 512
ALPHA = 2.0 ** 0.25
EPS = 1e-5
NEXP = 32


class T:
    __slots__ = ("w", "r")

    def __init__(self):
        self.w = None
        self.r = {}


class Sched:
    def __init__(self, nc, es, n_dma=80):
        self.nc = nc
        self.eng = {"pe": nc.tensor, "act": nc.scalar, "dve": nc.vector, "pool": nc.gpsimd, "sp": nc.sync}
        self.sem = {k: es.enter_context(nc.semaphore("s_" + k)) for k in ("pe", "act", "dve", "pool")}
        self.cnt = {k: 0 for k in self.sem}
        self.dsem = [es.enter_context(nc.semaphore("d%d" % i)) for i in range(n_dma)]
        self.dcnt = [0] * n_dma
        n_cast = 16
        self.qslots = {"sp": list(range(0, (n_dma - n_cast) // 2)), "pool": list(range((n_dma - n_cast) // 2, n_dma - n_cast)),
                       "cast": list(range(n_dma - n_cast, n_dma))}
        self.qnext = {"sp": 0, "pool": 0, "cast": 0}
        self.seen = {}
        self.bregs = {}

    def _semof(self, key):
        return self.sem[key] if isinstance(key, str) else self.dsem[key]

    def _need(self, eng, reads, writes):
        need = {}

        def add(k, v):
            if k == eng:
                if eng == "pe":
                    return
                if self.cnt[eng] - v >= 4:
                    return
            if self.seen.get((eng, k), 0) >= v:
                return
            if need.get(k, 0) < v:
                need[k] = v

        for t in reads:
            if t.w is not None:
                add(*t.w)
        for t in writes:
            if t.w is not None:
                add(*t.w)
            for k, v in t.r.items():
                add(k, v)
        return need

    def _wait(self, eng, key, val):
        if self.seen.get((eng, key), 0) >= val:
            return
        self.eng[eng].wait_ge(self._semof(key), val)
        self.seen[(eng, key)] = val

    def _waits(self, eng, reads, writes):
        for k, v in self._need(eng, reads, writes).items():
            self._wait(eng, k, v)

    def _commit(self, ticket, reads, writes):
        k, v = ticket
        for t in reads:
            if t.r.get(k, 0) < v:
                t.r[k] = v
        for t in writes:
            t.w = ticket
            t.r = {}

    def op(self, eng, reads, writes, emit):
        need = self._need(eng, reads, writes)
        keys = list(need)
        attach = keys[-1] if keys else None
        for k in keys[:-1]:
            self._wait(eng, k, need[k])
        r = emit()
        first, last = (r[0], r[-1]) if isinstance(r, list) else (r, r)
        if attach is not None:
            first._wait_ge(self._semof(attach), need[attach])
            self.seen[(eng, attach)] = need[attach]
        self.cnt[eng] += 1
        last.then_inc(self.sem[eng], 1)
        self._commit((eng, self.cnt[eng]), reads, writes)

    def dma(self, q, out, in_, reads=(), writes=(), out_off=None, in_off=None, bound=None, slots=None):
        self._waits(q, reads, writes)
        sp_ = slots or q
        sl = self.qslots[sp_]
        slot = sl[self.qnext[sp_]]
        self.qnext[sp_] = (self.qnext[sp_] + 1) % len(sl)
        if self.dcnt[slot] > 0:
            self._wait(q, slot, self.dcnt[slot])
        if out_off is None and in_off is None:
            inst = self.eng[q].dma_start(out=out, in_=in_)
        else:
            if bound not in self.bregs:
                self.bregs[bound] = self.nc.gpsimd.to_reg(bound)
            bound = self.bregs[bound]
            inst = self.nc.gpsimd.indirect_dma_start(
                out=out, out_offset=None if out_off is None else bass.IndirectOffsetOnAxis(ap=out_off, axis=0),
                in_=in_, in_offset=None if in_off is None else bass.IndirectOffsetOnAxis(ap=in_off, axis=0),
                bounds_check=bound, oob_is_err=False)
        inst.then_inc(self.dsem[slot], 16)
        self.dcnt[slot] += 16
        self._commit((slot, self.dcnt[slot]), reads, writes)

    def barrier(self, engines=("pe", "act", "dve", "pool", "sp"), full=False):
        skip = () if full else set(self.qslots["cast"])
        for e in engines:
            for k in self.sem:
                if self.cnt[k] > 0:
                    self._wait(e, k, self.cnt[k])
            for s in range(len(self.dsem)):
                if self.dcnt[s] > 0 and s not in skip:
                    self._wait(e, s, self.dcnt[s])


def v3(ap, b):
    return ap.rearrange("p (a b) -> p a b", b=b)


def build(n_seq=2, stop_after=None, dumps=()):
    nc = bass.Bass("TRN2", target_bir_lowering=False)

    def din(name, shape):
        return nc.dram_tensor(name, list(shape), F32, kind="ExternalInput").ap()

    x = din("x", [n_seq, SEQ, D])
    mem = din("mem", [n_seq, 256, D])
    w_in = din("w_in", [D, 1960])
    wkr_sw = din("wkr_sw", [D, 96])
    wq_d = din("wq", [256, 768])
    wqsw_d = din("wq_sw", [256, 768])
    qn_d = din("qn", [128, 2])
    wkv_d = din("wkv", [128, 1024])
    kvn_d = din("kvn", [128, 1])
    convw_d = din("convw", [128, 32])
    convb_d = din("convb", [128, 8])
    dtb_d = din("dtb", [128, 128])
    alog_d = din("alog", [128, 8])
    dcol_d = din("dcol", [128, 4])
    ncol_d = din("ncol", [128, 4])
    wout_d = din("wout", [D, D])
    xwq_d = din("xwq", [D, D])
    xwk_d = din("xwk", [D, D])
    xwv_d = din("xwv", [D, D])
    xwo_d = din("xwo", [D, D])
    lnp_d = din("lnp", [6, 128, D])
    rw_d = din("rw", [D, 36])
    rb_d = din("rb", [128, 36])
    wg_d = din("wg", [NEXP, D, 256])
    wu_d = din("wu", [NEXP, D, 256])
    wd_d = din("wd", [NEXP, 256, D])
    ident_d = din("ident", [128, 128])
    tri_d = din("tri", [128, 128])
    negmask_d = din("negmask", [128, 512])
    cc_d = din("cc", [128, SEQ])
    ss_d = din("ss", [128, SEQ])
    su_d = din("su", [128, 128])
    thr_d = din("thr", [128, 32])
    pcol_d = din("pcol", [128, 1])
    GT = n_seq * NT
    NTOK = n_seq * SEQ
    TB = 3
    TSZ = 128 * TB
    NTI = -(-(2 * NTOK) // TSZ) + 31
    tstart_d = din("tstart", [128, 64])
    out_d = nc.dram_tensor("out", [n_seq, SEQ, D], F32, kind="ExternalOutput").ap()
    XB = nc.dram_tensor("XB", [NTOK, D], BF16, kind="Internal").ap()
    H2D = nc.dram_tensor("H2D", [NTOK, D], F32, kind="Internal").ap()
    XS = nc.dram_tensor("XS", [NTI * TSZ, D], BF16, kind="Internal").ap()
    YS = nc.dram_tensor("YS", [NTI * TSZ, D], BF16, kind="Internal").ap()
    DWB = {n: nc.dram_tensor("DWB_" + n, [D, D], BF16, kind="Internal").ap() for n in ("wout", "xwq", "xwk", "xwv", "xwo")}
    XBF = nc.dram_tensor("XBF", [max(n_seq - 1, 1), SEQ, D], BF16, kind="Internal").ap()
    WINB = nc.dram_tensor("WINB", [D, 1960], BF16, kind="Internal").ap()
    WGB = nc.dram_tensor("WGB", [NEXP * 128, 2048], BF16, kind="Internal").ap()
    WUB = nc.dram_tensor("WUB", [NEXP * 128, 2048], BF16, kind="Internal").ap()
    WDB = nc.dram_tensor("WDB", [NEXP * 128, 2048], BF16, kind="Internal").ap()
    dump_d = {}
    for name, shape in dumps:
        dump_d[name] = nc.dram_tensor("dbg_" + name, list(shape), F32, kind="ExternalOutput").ap()

    es = ExitStack()
    with es:
        S = Sched(nc, es)

        def sb(name, shape, dt):
            return es.enter_context(nc.sbuf_tensor(name, list(shape), dt))

        ident_bf = sb("ident_bf", [128, 128], BF16)
        ones_bf = sb("ones_bf", [128, 128], BF16)
        ones_f = sb("ones_f", [128, 128], F32)
        tri_f = sb("tri_f", [128, 128], F32)
        ident_f = sb("ident_f", [128, 128], F32)
        negmask = sb("negmask_s", [128, 512], F32)
        cc = sb("cc_s", [128, SEQ], BF16)
        ss = sb("ss_s", [128, SEQ], BF16)
        lng = sb("lng", [128, D], F32)
        lnb = sb("lnb", [128, D], F32)
        qn = sb("qn_s", [128, 2], F32)
        kvn = sb("kvn_s", [128, 1], F32)
        convw = sb("convw_s", [128, 32], F32)
        convb = sb("convb_s", [128, 8], F32)
        dtb = sb("dtb_s", [128, 128], F32)
        a_bc = sb("a_bc", [128, 8], F32)
        dcol = sb("dcol_s", [128, 4], F32)
        ncol = sb("ncol_s", [128, 4], F32)
        rb = sb("rb_s", [128, 36], F32)
        rw = sb("rw_s", [128, 8 * 36], BF16)
        small = sb("small", [128, 512], F32)
        su_bf = sb("su_bf", [128, 128], BF16)
        thr = sb("thr_s", [128, 32], F32)
        pcol = sb("pcol_s", [128, 1], F32)
        tstart = sb("tstart_s", [128, 64], F32)
        ONE = sb("ONE", [128, GT * 64], BF16)
        RK = sb("RK", [128, GT * 2], F32)
        G12 = sb("G12", [128, GT * 2], F32)
        Crun = sb("Crun", [128, 32], F32)
        mo_f = sb("mo_f", [128, 128], F32)
        mo_i = sb("mo_i", [128, 128], mybir.dt.int32)
        tRk = T()
        rdx = cc[:].bitcast(F32)
        dt_tok = sb("dt_tok", [128, 128], F32)
        tC = T()
        tLN = T()
        tSmall = T()

        A = sb("arenaA", [128, 16384], BF16)
        H = sb("arenaH", [128, 16384], F32)
        K = sb("arenaK", [128, 16384], BF16)
        W = sb("arenaW", [128, 24576], BF16)
        lnst = sb("lnst", [128, 64], F32)
        tLNS = [T() for _ in range(4)]
        lnstate = {"i": 0}
        PS = [es.enter_context(nc.psum_tensor("ps%d" % i, [128, 1024], F32)) for i in range(4)]
        PT_ = [T() for _ in range(8)]
        pstate = {"b": 0}

        class Bank:
            def __init__(self, ap, toks):
                self.ap = ap
                self.toks = toks

            @property
            def bf(self):
                return self.ap.bitcast(BF16)

        busy = set()

        def ps1(hold=False):
            b = pstate["b"]
            while b in busy:
                b = (b + 1) % 8
            pstate["b"] = (b + 1) % 8
            bk_ = Bank(PS[b // 2][:, (b % 2) * 512:(b % 2) * 512 + 512], [PT_[b]])
            bk_.idx = [b]
            if hold:
                busy.add(b)
            return bk_

        def ps2(hold=False):
            b = pstate["b"]
            if b % 2:
                b = (b + 1) % 8
            while b in busy or (b + 1) in busy:
                b = (b + 2) % 8
            pstate["b"] = (b + 2) % 8
            bk_ = Bank(PS[b // 2][:, :], [PT_[b], PT_[b + 1]])
            bk_.idx = [b, b + 1]
            if hold:
                busy.update(bk_.idx)
            return bk_

        def psrel(bk_):
            for b in bk_.idx:
                busy.discard(b)

        def Wf(lo, hi):
            return W[:, lo:hi].bitcast(F32)

        def Kf(lo, hi):
            return K[:, lo:hi].bitcast(F32)

        def Hb(lo, hi):
            return H[:, lo:hi].bitcast(BF16)

        def dump(name, ap, toks):
            if name in dump_d:
                S.dma("pool", out=dump_d[name], in_=ap, reads=toks)

        def ld(q, dst, src):
            S.dma(q, out=dst, in_=src, writes=[tC])

        ld("pool", ident_bf[:], ident_d[:, :])
        ld("sp", ident_f[:], ident_d[:, :])
        ld("sp", tri_f[:], tri_d[:, :])
        ld("sp", negmask[:], negmask_d[:, :])
        ld("pool", su_bf[:], su_d[:, :])
        ld("sp", thr[:], thr_d[:, :])
        ld("sp", pcol[:], pcol_d[:, :])
        ld("sp", tstart[:], tstart_d[:, :])
        S.op("dve", [], [tRk], lambda: nc.vector.memset(Crun[:], 0.0))
        ld("pool", cc[:], cc_d[:, :])
        ld("pool", ss[:], ss_d[:, :])
        for dst, src in ((qn, qn_d), (kvn, kvn_d), (convw, convw_d), (convb, convb_d), (dtb, dtb_d),
                         (a_bc, alog_d), (dcol, dcol_d), (ncol, ncol_d), (rb, rb_d)):
            ld("sp", dst[:], src[:, :])
        ld("pool", v3(rw[:], 36), rw_d.rearrange("(k p) c -> p k c", p=128))
        S.op("dve", [], [tC], lambda: nc.vector.memset(ones_f[:], 1.0))
        S.op("dve", [], [tC], lambda: nc.vector.memset(ones_bf[:], 1.0))
        S.op("act", [tC], [tC], lambda: nc.scalar.activation(out=a_bc[:], in_=a_bc[:], func=AF.Exp))
        S.op("dve", [tC], [tC], lambda: nc.vector.tensor_scalar_mul(out=a_bc[:], in0=a_bc[:], scalar1=-1.0))
        S.barrier()

        def rstd_from(dst, src, scale, reads, writes):
            S.op("act", reads, writes, lambda: nc.scalar.activation(out=dst, in_=src, func=AF.Ln, scale=scale, bias=EPS))
            S.op("act", [], writes, lambda: nc.scalar.activation(out=dst, in_=dst, func=AF.Exp, scale=-0.5))

        def layer_norm_tile(hap, tH, li, s):
            j = lnstate["i"] % 4
            lnstate["i"] += 1
            tS = tLNS[j]
            base = j * 16
            st = lnst[:, base:base + 12]
            mv = lnst[:, base + 12:base + 14]
            rs = lnst[:, base + 14:base + 15]
            nm = lnst[:, base + 15:base + 16]
            S.op("dve", [tH], [tS], lambda: nc.vector.bn_stats(out=st[:, 0:6], in_=hap[:, 0:512]))
            S.op("dve", [tH], [tS], lambda: nc.vector.bn_stats(out=st[:, 6:12], in_=hap[:, 512:1024]))
            S.op("dve", [], [tS], lambda: nc.vector.bn_aggr(out=mv, in_=st))
            rstd_from(rs, mv[:, 1:2], 1.0, [tS], [tS])
            S.op("dve", [tS], [tS], lambda: nc.vector.scalar_tensor_tensor(
                out=nm, in0=mv[:, 0:1], scalar=-1.0, in1=rs, op0=ALU.mult, op1=ALU.mult))
            S.op("act", [tS], [tH], lambda: nc.scalar.activation(out=hap, in_=hap, func=AF.Identity, scale=rs, bias=nm))
            S.op("dve", [tLN], [tH], lambda: nc.vector.tensor_tensor(out=hap, in0=hap, in1=lng[:], op=ALU.mult))
            S.op("dve", [tLN], [tH], lambda: nc.vector.tensor_tensor(out=hap, in0=hap, in1=lnb[:], op=ALU.add))

        def load_ln(li):
            S.dma("sp", out=lng[:], in_=lnp_d[2 * li, :, :], writes=[tLN])
            S.dma("sp", out=lnb[:], in_=lnp_d[2 * li + 1, :, :], writes=[tLN])

        def transpose_to(hb, tHB, dstT, tt, tDst):
            pb = ps1()
            S.op("pe", [tHB, tC], pb.toks, lambda: [nc.tensor.transpose(
                pb.bf[:, k * 128:(k + 1) * 128], hb[:, k * 128:(k + 1) * 128], ident_bf[:]) for k in range(8)])
            S.op("act", pb.toks, [tDst], lambda: nc.scalar.copy(
                out=dstT[:, :, tt * 128:(tt + 1) * 128], in_=v3(pb.bf[:, 0:1024], 128)))

        actT = v3(A[:, :], SEQ)
        catT = v3(K[:, :], SEQ)
        hview = v3(H[:, :], D)

        for s in range(n_seq):
            tXT = [T() for _ in range(NT)]
            if s > 0:
                S.dma("pool", out=cc[:], in_=cc_d[:, :], writes=[tC])
            NXB = 4
            xin = [K[:, 0:1024], K[:, 1024:2048], K[:, 8192:9216], K[:, 9216:10240]]
            txin = [T() for _ in range(NXB)]
            win = v3(W[:, 0:15680], 1960)
            wkr = v3(W[:, 15680:16448], 96)
            wq_s = v3(W[:, 16448:17984], 768)
            wq_sw = v3(W[:, 17984:19520], 768)
            wkv_s = W[:, 19520:20544]
            tWinL, tWs = [T() for _ in range(9)], T()
            def load_x_tile(tt):
                if s == 0:
                    S.dma("pool", out=xin[tt % NXB], in_=x[s, tt * 128:(tt + 1) * 128, :], writes=[txin[tt % NXB]])
                else:
                    S.dma("sp", out=xin[tt % NXB], in_=XBF[s - 1, tt * 128:(tt + 1) * 128, :], reads=[tXC[(s, tt)]], writes=[txin[tt % NXB]])

            for tt in range(NXB):
                load_x_tile(tt)
            WCG = [(0, 416), (416, 928), (928, 1440), (1440, 1960)]
            w_in3 = w_in.rearrange("(k p) c -> p k c", p=128)
            def load_win_group(gi):
                c0_, c1_ = WCG[gi]
                if s == 0:
                    S.dma("pool", out=win[:, :, c0_:c1_], in_=w_in3[:, :, c0_:c1_], writes=[tWinL[gi]])
                else:
                    S.dma("sp", out=win[:, :, c0_:c1_], in_=WINB[:, c0_:c1_].rearrange("(k p) c -> p k c", p=128),
                          reads=[tWC[gi]], writes=[tWinL[gi]])

            load_win_group(0)
            S.dma("pool", out=wkr, in_=wkr_sw.rearrange("(k p) c -> p k c", p=128), writes=[tWinL[8]])
            for gi in range(1, 4):
                load_win_group(gi)
            S.dma("pool", out=wq_s, in_=wq_d.rearrange("(k p) c -> p k c", p=128), writes=[tWs])
            S.dma("pool", out=wq_sw, in_=wqsw_d.rearrange("(k p) c -> p k c", p=128), writes=[tWs])
            S.dma("pool", out=wkv_s, in_=wkv_d[:, :], writes=[tWs])
            for r in range(2):
                S.op("dve", [tWs, tC], [tWs], lambda r=r: nc.vector.tensor_scalar_mul(out=wq_s[:, r, :], in0=wq_s[:, r, :], scalar1=qn[:, r:r + 1]))
                S.op("dve", [tWs, tC], [tWs], lambda r=r: nc.vector.tensor_scalar_mul(out=wq_sw[:, r, :], in0=wq_sw[:, r, :], scalar1=qn[:, r:r + 1]))
            S.op("dve", [tWs, tC], [tWs], lambda: nc.vector.tensor_scalar_mul(out=wkv_s, in0=wkv_s, scalar1=kvn[:, 0:1]))
            for tt in range(NT):
                transpose_to(xin[tt % NXB], txin[tt % NXB], actT, tt, tXT[tt])
                if tt + NXB < NT:
                    load_x_tile(tt + NXB)
            if s == 0:
                tDW = {n: [T() for _ in range(8)] for n in DWB}
                for n, src in (("wout", wout_d), ("xwk", xwk_d), ("xwv", xwv_d), ("xwq", xwq_d), ("xwo", xwo_d)):
                    for k in range(8):
                        S.dma("pool", out=DWB[n][k * 128:(k + 1) * 128, :], in_=src[k * 128:(k + 1) * 128, :], writes=[tDW[n][k]], slots="cast")
            if s == 0 and n_seq > 1:
                tXC = {}
                tWC = [T() for _ in range(4)]
                for gi in range(4):
                    S.dma("pool", out=WINB[:, WCG[gi][0]:WCG[gi][1]].rearrange("(k p) c -> p k c", p=128),
                          in_=w_in3[:, :, WCG[gi][0]:WCG[gi][1]], writes=[tWC[gi]], slots="cast")
                for s2 in range(1, n_seq):
                    for tt in range(NT):
                        tXC[(s2, tt)] = T()
                        S.dma("pool", out=XBF[s2 - 1, tt * 128:(tt + 1) * 128, :], in_=x[s2, tt * 128:(tt + 1) * 128, :],
                              writes=[tXC[(s2, tt)]], slots="cast")
            if s == 0 and stop_after is None:
                for e in range(NEXP):
                    rows = slice(e * 128, (e + 1) * 128)
                    S.dma("pool", out=WGB[rows, :].rearrange("p (k f) -> p k f", k=8), in_=wg_d[e].rearrange("(k p) f -> p k f", p=128), writes=[T()], slots="cast")
                    S.dma("pool", out=WUB[rows, :].rearrange("p (k f) -> p k f", k=8), in_=wu_d[e].rearrange("(k p) f -> p k f", p=128), writes=[T()], slots="cast")
                    S.dma("pool", out=WDB[rows, :].rearrange("p (k f) -> p k f", k=2), in_=wd_d[e].rearrange("(k p) f -> p k f", p=128), writes=[T()], slots="cast")
            if stop_after == "P1":
                dump("xT", actT[:, 0, :], tXT)
                break

            zs = v3(Hb(0, 4096), SEQ)
            xsT = v3(Hb(4096, 8192), SEQ)
            BT = v3(Hb(8192, 10240), SEQ)
            CT = v3(Hb(10240, 12288), SEQ)
            cqT = v3(Hb(12288, 14336), SEQ)
            ckvT = Hb(14336, 15360)
            kpe = Hb(15360, 16384)
            tZ, tXs, tB, tCt, tCq, tCkv, tKpe = T(), T(), T(), T(), T(), T(), T()
            pre = [Kf(2048 + i * 1040, 2048 + i * 1040 + 1030) for i in range(2)]
            acc = [Kf(4128 + i * 1024, 4128 + (i + 1) * 1024) for i in range(2)]
            sqb = K[:, 6176:6688]
            rsb = Kf(6688, 7712)
            tPre, tAcc, tSq, tRs = [T(), T()], [T(), T()], T(), T()
            tDt = T()

            def proj_mm(bank, M, lhs_fn, tg, wt=0):
                S.op("pe", list(tXT[tg * 4:tg * 4 + 4]) + [tWinL[wt]], bank.toks, lambda: [nc.tensor.matmul(
                    bank.ap[0:M, :], lhs_fn(k), actT[:, k, tg * 512:(tg + 1) * 512], start=(k == 0), stop=(k == 7)) for k in range(8)])

            for tg in range(NG):
                cols = slice(tg * 512, (tg + 1) * 512)
                bq = [ps1(), ps1()]
                for r in range(2):
                    proj_mm(bq[r], 128, lambda k, r=r: win[:, k, r * 128:(r + 1) * 128], tg)
                bs = ps1()
                for r in range(2):
                    S.op("act", bq[r].toks, [tSq], lambda r=r: nc.scalar.activation(out=sqb, in_=bq[r].ap, func=AF.Square))
                    S.op("pe", [tSq, tC], bs.toks, lambda r=r: nc.tensor.matmul(bs.ap, ones_bf[:], sqb, start=(r == 0), stop=(r == 1)))
                rstd_from(rsb, bs.ap, 1.0 / 256, bs.toks, [tRs])
                for r in range(2):
                    S.op("dve", bq[r].toks + [tRs], [tCq], lambda r=r: nc.vector.tensor_tensor(out=cqT[:, r, cols], in0=bq[r].ap, in1=rsb, op=ALU.mult))
                bk = ps1()
                proj_mm(bk, 128, lambda k: win[:, k, 256:384], tg)
                bs = ps1()
                S.op("act", bk.toks, [tSq], lambda: nc.scalar.activation(out=sqb, in_=bk.ap, func=AF.Square))
                S.op("pe", [tSq, tC], bs.toks, lambda: nc.tensor.matmul(bs.ap, ones_bf[:], sqb, start=True, stop=True))
                rstd_from(rsb, bs.ap, 1.0 / 128, bs.toks, [tRs])
                S.op("dve", bk.toks + [tRs], [tCkv], lambda: nc.vector.tensor_tensor(out=ckvT[:, cols], in0=bk.ap, in1=rsb, op=ALU.mult))
                ba, bb = ps1(), ps1()
                proj_mm(ba, 96, lambda k: win[:, k, 320:416], tg)
                proj_mm(bb, 96, lambda k: wkr[:, k, :], tg, 8)
                t1 = acc[0]
                t2 = acc[1]
                S.op("dve", ba.toks + [tC], [tAcc[0]], lambda: nc.vector.tensor_tensor(out=t1[64:96, :], in0=ba.ap[64:96, :], in1=cc[64:96, cols], op=ALU.mult))
                S.op("dve", bb.toks + [tC], [tAcc[1]], lambda: nc.vector.tensor_tensor(out=t2[64:96, :], in0=bb.ap[64:96, :], in1=ss[64:96, cols], op=ALU.mult))
                S.op("dve", [tAcc[0], tAcc[1]], [tKpe], lambda: nc.vector.tensor_tensor(out=kpe[64:96, cols], in0=t1[64:96, :], in1=t2[64:96, :], op=ALU.add))
                for j in range(4):
                    bz = ps1()
                    proj_mm(bz, 128, lambda k, j=j: win[:, k, 416 + j * 128:416 + (j + 1) * 128], tg, 1)
                    S.op("act", bz.toks, [tZ], lambda j=j, bz=bz: nc.scalar.activation(out=zs[:, j, cols], in_=bz.ap, func=AF.Silu))
            for c in range(8):
                if c < 4:
                    dst, tdst = xsT[:, c, :], tXs
                elif c < 6:
                    dst, tdst = BT[:, c - 4, :], tB
                else:
                    dst, tdst = CT[:, c - 6, :], tCt
                S.op("dve", [], [tPre[0]], lambda: nc.vector.memset(pre[0][:, 0:3], 0.0))
                for tg in range(NG):
                    p_, a_ = pre[tg % 2], acc[tg % 2]
                    tp, ta = tPre[tg % 2], tAcc[tg % 2]
                    bx = ps1()
                    proj_mm(bx, 128, lambda k, c=c: win[:, k, 928 + c * 128:928 + (c + 1) * 128], tg, 2 if c < 4 else 3)
                    S.op("act", bx.toks, [tp], lambda: nc.scalar.copy(out=p_[:, 3:515], in_=bx.ap))
                    if tg < NG - 1:
                        S.op("act", [tp], [tPre[(tg + 1) % 2]], lambda: nc.scalar.copy(out=pre[(tg + 1) % 2][:, 0:3], in_=p_[:, 512:515]))
                    S.op("dve", [tp, tC], [ta], lambda: nc.vector.tensor_scalar_mul(out=a_, in0=p_[:, 0:512], scalar1=convw[:, c * 4:c * 4 + 1]))
                    for j in range(1, 4):
                        S.op("dve", [tp, tC], [ta], lambda j=j: nc.vector.scalar_tensor_tensor(
                            out=a_, in0=p_[:, j:j + 512], scalar=convw[:, c * 4 + j:c * 4 + j + 1], in1=a_, op0=ALU.mult, op1=ALU.add))
                    S.op("act", [ta, tC], [tdst], lambda: nc.scalar.activation(
                        out=dst[:, tg * 512:(tg + 1) * 512], in_=a_, func=AF.Silu, bias=convb[:, c:c + 1]))
            bd = ps1()
            for tt in range(NT):
                S.op("pe", [tXT[tt], tWinL[3]], bd.toks, lambda tt=tt: [nc.tensor.matmul(
                    bd.ap[:, tt * 8:(tt + 1) * 8], actT[:, k, tt * 128:(tt + 1) * 128], win[:, k, 1952:1960],
                    start=(k == 0), stop=(k == 7)) for k in range(8)])
            S.op("dve", bd.toks + [tC], [tDt], lambda: nc.vector.tensor_tensor(out=dt_tok[:], in0=bd.ap[:, 0:128], in1=dtb[:], op=ALU.add))
            S.op("act", [tDt], [tDt], lambda: nc.scalar.activation(out=dt_tok[:], in_=dt_tok[:], func=AF.Exp))
            S.op("act", [tDt], [tDt], lambda: nc.scalar.activation(out=dt_tok[:], in_=dt_tok[:], func=AF.Ln, bias=1.0))
            if stop_after == "P2":
                dump("cqT", cqT[:, 0, :], [tCq])
                dump("ckvT", ckvT, [tCkv])
                dump("kpe", kpe, [tKpe])
                dump("zs", zs[:, 0, :], [tZ])
                dump("xsT", xsT[:, 0, :], [tXs])
                dump("BT", BT[:, 0, :], [tB])
                dump("dt", dt_tok[:], [tDt])
                break
            S.barrier(engines=("act", "dve", "pool", "sp"))
            qT = [A[:, i * 2048:(i + 1) * 2048] for i in range(2)]
            kT = [A[:, (2 + i) * 2048:(3 + i) * 2048] for i in range(2)]
            Vaug = [v3(A[:, (4 + i) * 2048:(5 + i) * 2048], 128) for i in range(2)]
            PTb = [A[:, 12288 + i * 512:12288 + (i + 1) * 512] for i in range(3)]
            rdb = A[:, 13824:14848].bitcast(F32)
            t1 = [Wf(0, 1024), A[:, 14848:15872].bitcast(F32)]
            t2 = [Wf(1024, 2048), Wf(13568, 14592)]
            tQ, tK, tV, tPT = [T(), T()], [T(), T()], [T(), T()], [T(), T(), T()]
            tT1, tT2, tRd = [T(), T()], [T(), T()], T()
            tCat = [T() for _ in range(8)]
            S.op("dve", [], [tV[0]], lambda: nc.vector.memset(Vaug[0][:, :, 64:128], 1.0))
            S.op("dve", [], [tV[1]], lambda: nc.vector.memset(Vaug[1][:, :, 0:64], 1.0))
            if s == 0 and stop_after is None:
                ztile = W[:, 14592:15616]
                tZ = T()
                S.op("dve", [], [tZ], lambda: nc.vector.memset(ztile, 0.0))
                for j in range(NTI * TB):
                    S.dma("sp", out=XS[j * 128:(j + 1) * 128, :], in_=ztile, reads=[tZ])
            SCALE = 96.0 ** -0.5
            pstate_pt = {"i": 0}

            def qkv_head(h):
                hp = h % 2
                q_h, k_h, V_h = qT[hp], kT[hp], Vaug[hp]
                tq, tk, tv = tQ[hp], tK[hp], tV[hp]
                for tg in range(NG):
                    cols = slice(tg * 512, (tg + 1) * 512)
                    S.op("dve", [tKpe], [tk], lambda: nc.vector.tensor_copy(out=k_h[64:96, cols], in_=kpe[64:96, cols]))
                    yield
                    ba, bb = ps1(), ps1()
                    S.op("pe", [tWs, tCq], ba.toks, lambda: [nc.tensor.matmul(
                        ba.ap[0:96, :], wq_s[:, r, h * 96:(h + 1) * 96], cqT[:, r, cols], start=(r == 0), stop=(r == 1)) for r in range(2)])
                    S.op("pe", [tWs, tCq], bb.toks, lambda: [nc.tensor.matmul(
                        bb.ap[0:96, :], wq_sw[:, r, h * 96:(h + 1) * 96], cqT[:, r, cols], start=(r == 0), stop=(r == 1)) for r in range(2)])
                    bk = ps1()
                    S.op("pe", [tWs, tCkv], bk.toks, lambda: nc.tensor.matmul(
                        bk.ap[0:64, :], wkv_s[:, h * 128:h * 128 + 64], ckvT[:, cols], start=True, stop=True))
                    tt1, tt2 = tT1[tg % 2], tT2[tg % 2]
                    u1, u2 = t1[tg % 2], t2[tg % 2]
                    S.op("dve", ba.toks + [tC], [tt1], lambda: nc.vector.tensor_tensor(out=u1[0:96, :], in0=ba.ap[0:96, :], in1=cc[0:96, cols], op=ALU.mult))
                    yield
                    S.op("dve", bb.toks + [tC], [tt2], lambda: nc.vector.tensor_tensor(out=u2[0:96, :], in0=bb.ap[0:96, :], in1=ss[0:96, cols], op=ALU.mult))
                    yield
                    S.op("dve", [tt1, tt2], [tq], lambda: nc.vector.tensor_tensor(out=q_h[0:96, cols], in0=u1[0:96, :], in1=u2[0:96, :], op=ALU.add))
                    yield
                    S.op("dve", bk.toks, [tk], lambda: nc.vector.tensor_copy(out=k_h[0:64, cols], in_=bk.ap[0:64, :]))
                    yield
                vo = 0 if hp == 0 else 64
                for half in range(2):
                    bv = ps1()
                    S.op("pe", [tWs, tCkv], bv.toks, lambda: [nc.tensor.matmul(
                        bv.ap[:, j * 64:(j + 1) * 64], ckvT[:, (half * 8 + j) * 128:(half * 8 + j + 1) * 128],
                        wkv_s[:, h * 128 + 64:(h + 1) * 128], start=True, stop=True) for j in range(8)])
                    S.op("dve", bv.toks, [tv], lambda: nc.vector.tensor_copy(out=V_h[:, half * 8:(half + 1) * 8, vo:vo + 64], in_=v3(bv.ap, 64)))
                    yield

            def attn_head(h):
                hp = h % 2
                q_h, k_h, V_h = qT[hp], kT[hp], Vaug[hp]
                tq, tk, tv = tQ[hp], tK[hp], tV[hp]
                orow = slice(0, 64) if hp == 0 else slice(64, 128)
                drow = slice(64, 128) if hp == 0 else slice(0, 64)
                items = [(g, kj) for g in range(NG) for kj in range(4 * g + 4)]
                sbank = {}
                bo_of = {}

                def emit_S(i):
                    g, kj = items[i]
                    c0 = max(0, kj - 4 * g) * 128
                    if g not in bo_of:
                        bo_of[g] = ps1(hold=True)
                    b_ = ps1()
                    sbank[i] = b_
                    S.op("pe", [tq, tk], b_.toks, lambda: nc.tensor.matmul(
                        b_.ap[:, c0:512], k_h[0:96, kj * 128:(kj + 1) * 128], q_h[0:96, g * 512 + c0:(g + 1) * 512], start=True, stop=True))

                LOOK = 2
                for i in range(min(LOOK, len(items))):
                    emit_S(i)
                for i, (g, kj) in enumerate(items):
                    nk = 4 * g + 4
                    c0 = max(0, kj - 4 * g) * 128
                    b_ = sbank.pop(i)
                    pt, tpt = PTb[pstate_pt["i"] % 3], tPT[pstate_pt["i"] % 3]
                    pstate_pt["i"] += 1
                    S.op("act", b_.toks, [tpt], lambda: nc.scalar.activation(out=pt[:, c0:512], in_=b_.ap[:, c0:512], func=AF.Exp, scale=SCALE))
                    if kj >= 4 * g:
                        S.op("dve", [], [tpt], lambda: nc.vector.memset(pt[64:128, c0:c0 + 64], 0.0))
                    if i + LOOK < len(items):
                        emit_S(i + LOOK)
                    bo = bo_of[g]
                    S.op("pe", [tpt, tv], bo.toks, lambda: nc.tensor.matmul(
                        bo.ap[:, c0:512], V_h[:, kj, :], pt[:, c0:512], start=(kj == 0), stop=(kj == nk - 1)))
                    if kj == nk - 1:
                        S.op("dve", bo.toks, [tRd], lambda: nc.vector.reciprocal(out=rdb[drow, :], in_=bo.ap[drow, :]))
                        S.op("dve", bo.toks + [tRd], [tCat[h // 2]], lambda: nc.vector.tensor_tensor(
                            out=catT[orow, h // 2, g * 512:(g + 1) * 512], in0=bo.ap[orow, :], in1=rdb[drow, :], op=ALU.mult))
                        psrel(bo)
                    yield

            def p4_gen():
                for _ in qkv_head(0):
                    pass
                for h in range(8):
                    nxt = qkv_head(h + 1) if h + 1 < 8 else None
                    for n_, _ in enumerate(attn_head(h)):
                        if nxt is not None and n_ % 2 == 1:
                            if next(nxt, "done") == "done":
                                nxt = None
                        yield
                    if nxt is not None:
                        for _ in nxt:
                            pass

            if stop_after == "P4":
                for _ in p4_gen():
                    pass
                dump("cat_attn", catT[:, 0:4, :], tCat)
                break
            def ssd_set(i):
                b0 = 2048 if i == 0 else 15616
                so = 16 if i == 0 else 64
                d = dict(
                    adt=small[:, so:so + 8], acs_sb=small[:, so + 8:so + 16], dte=small[:, so + 16:so + 24],
                    cdk=small[:, so + 24:so + 32], dd=small[:, so + 32:so + 40], nacs=small[:, so + 40:so + 48],
                    R=Wf(b0, b0 + 2048), E=Wf(b0 + 2048, b0 + 3072), DEC=Wf(b0 + 3072, b0 + 4096),
                    Mfin=[W[:, b0 + 4096:b0 + 4608], W[:, b0 + 4608:b0 + 5120]],
                    CexpT=[W[:, b0 + 5120:b0 + 5632], W[:, b0 + 5632:b0 + 6144]],
                    xdt=W[:, b0 + 6144:b0 + 6656], xdte=W[:, b0 + 6656:b0 + 7168], Btok=W[:, b0 + 7168:b0 + 7424],
                    cbm=[Wf(b0 + 7424, b0 + 7680), Wf(b0 + 7680, b0 + 7936)],
                    tAdt=T(), tSm2=T(), tR=T(), tE=T(), tDec=T(), tXdt=T(), tXdte=T(), tBtok=T(), tCbm=T(),
                    tMf=[T(), T()], tCe=[T(), T()])
                return d

            SB = [ssd_set(0), ssd_set(1)]
            prev_f = Wf(9984, 11008)
            prev_bf = W[:, 11008:11520]
            yz = Wf(11520, 12544)
            sq = W[:, 12544:13056]
            rstd_g = Wf(13056, 13312)
            yv = Wf(13312, 13568)
            tPrevF, tPrevB, tYv, tYz, tSq2, tRg = T(), T(), T(), T(), T(), T()
            S.op("dve", [], [tWs], lambda: nc.vector.memset(small[:, 250:251], 0.0))
            S.op("act", [], [tWs], lambda: nc.scalar.copy(out=small[:, 251:252], in_=small[:, 250:251]))
            S.op("dve", [], [tPrevF], lambda: nc.vector.memset(prev_f, 0.0))
            S.op("dve", [], [tPrevB], lambda: nc.vector.memset(prev_bf, 0.0))

            def ssd_stage1(c):
                B_ = SB[c % 2]
                adt, acs_sb, dte, cdk, dd, nacs = B_["adt"], B_["acs_sb"], B_["dte"], B_["cdk"], B_["dd"], B_["nacs"]
                R, E, DEC, Mfin, CexpT, xdt, xdte, Btok, cbm = (B_[k] for k in ("R", "E", "DEC", "Mfin", "CexpT", "xdt", "xdte", "Btok", "cbm"))
                tAdt, tSm2, tR, tE, tDec, tXdt, tXdte, tBtok, tCbm, tMf, tCe = (B_[k] for k in (
                    "tAdt", "tSm2", "tR", "tE", "tDec", "tXdt", "tXdte", "tBtok", "tCbm", "tMf", "tCe"))
                tsl = slice(c * 128, (c + 1) * 128)
                dtc = dt_tok[:, c * 8:(c + 1) * 8]
                S.op("dve", [tDt, tC], [tAdt], lambda: nc.vector.tensor_tensor(out=adt, in0=dtc, in1=a_bc[:], op=ALU.mult))
                bx = ps1()
                S.op("pe", [tXs, tC], bx.toks, lambda: [nc.tensor.transpose(
                    bx.bf[:, j * 128:(j + 1) * 128], xsT[:, j, tsl], ident_bf[:]) for j in range(4)])
                S.op("dve", bx.toks + [tDt], [tXdt], lambda: nc.vector.tensor_tensor(
                    out=v3(xdt, 64), in0=v3(bx.bf[:, 0:512], 64), in1=dtc.unsqueeze(2).to_broadcast([128, 8, 64]), op=ALU.mult))
                bb_ = ps1()
                S.op("pe", [tB, tC], bb_.toks, lambda: [nc.tensor.transpose(
                    bb_.bf[:, g * 128:(g + 1) * 128], BT[:, g, tsl], ident_bf[:]) for g in range(2)])
                S.op("act", bb_.toks, [tBtok], lambda: nc.scalar.copy(out=Btok, in_=bb_.bf[:, 0:256]))
                yield
                ba_ = ps1()
                S.op("pe", [tAdt, tC], ba_.toks, lambda: [
                    nc.tensor.matmul(ba_.ap[:, 0:8], tri_f[:], adt, start=True, stop=True),
                    nc.tensor.matmul(ba_.ap[:, 8:16], ones_f[:], adt, start=True, stop=True)])
                S.op("act", ba_.toks, [tSm2], lambda: nc.scalar.copy(out=acs_sb, in_=ba_.ap[:, 0:8]))
                S.op("dve", ba_.toks + [tSm2], [tSm2], lambda: nc.vector.tensor_tensor(out=dd, in0=ba_.ap[:, 8:16], in1=acs_sb, op=ALU.subtract))
                S.op("act", [tSm2], [tSm2], lambda: nc.scalar.activation(out=dte, in_=dd, func=AF.Exp))
                S.op("act", ba_.toks, [tSm2], lambda: nc.scalar.activation(out=cdk, in_=ba_.ap[:, 8:16], func=AF.Exp))
                S.op("dve", [tSm2], [tSm2], lambda: nc.vector.tensor_scalar_mul(out=nacs, in0=acs_sb, scalar1=-1.0))
                yield
                S.op("dve", [tXdt, tSm2], [tXdte], lambda: nc.vector.tensor_tensor(
                    out=v3(xdte, 64), in0=v3(xdt, 64), in1=dte.unsqueeze(2).to_broadcast([128, 8, 64]), op=ALU.mult))
                S.op("dve", [tAdt, tC], [tR], lambda: nc.vector.tensor_tensor(
                    out=v3(R, 128), in0=tri_f[:].unsqueeze(1).to_broadcast([128, 8, 128]),
                    in1=adt.unsqueeze(2).to_broadcast([128, 8, 128]), op=ALU.mult))
                yield
                for g in range(2):
                    bc = ps1()
                    S.op("pe", [tB, tCt], bc.toks, lambda: nc.tensor.matmul(bc.ap[:, 0:128], BT[:, g, tsl], CT[:, g, tsl], start=True, stop=True))
                    S.op("act", bc.toks, [tCbm], lambda: nc.scalar.copy(out=cbm[g], in_=bc.ap[:, 0:128]))
                    bA = ps1()
                    S.op("pe", [tR, tC], bA.toks, lambda: nc.tensor.matmul(bA.ap, ones_f[:], R[:, g * 512:(g + 1) * 512], start=True, stop=True))
                    yield
                    S.op("act", bA.toks, [tDec], lambda: nc.scalar.activation(out=DEC, in_=bA.ap, func=AF.Exp))
                    for hh in range(4):
                        S.op("dve", bA.toks + [tC, tDec, tSm2], [tE], lambda: nc.vector.scalar_tensor_tensor(
                            out=E[:, hh * 128:(hh + 1) * 128], in0=bA.ap[:, hh * 128:(hh + 1) * 128],
                            scalar=nacs[:, g * 4 + hh:g * 4 + hh + 1], in1=negmask[:, 0:128], op0=ALU.add, op1=ALU.add))
                    S.op("act", [tE], [tE], lambda: nc.scalar.activation(out=E, in_=E, func=AF.Exp))
                    yield
                    S.op("dve", [tE, tCbm], [tMf[g]], lambda: nc.vector.tensor_tensor(
                        out=v3(Mfin[g], 128), in0=v3(E, 128), in1=cbm[g].unsqueeze(1).to_broadcast([128, 4, 128]), op=ALU.mult))
                    S.op("dve", [tDec, tCt], [tCe[g]], lambda: nc.vector.tensor_tensor(
                        out=v3(CexpT[g], 128), in0=v3(DEC, 128), in1=CT[:, g, tsl].unsqueeze(1).to_broadcast([128, 4, 128]), op=ALU.mult))
                    yield

            def ssd_stage2(c):
                B_ = SB[c % 2]
                cdk, Mfin, CexpT, xdt, xdte, Btok = (B_[k] for k in ("cdk", "Mfin", "CexpT", "xdt", "xdte", "Btok"))
                tSm2, tXdt, tXdte, tBtok, tMf, tCe = (B_[k] for k in ("tSm2", "tXdt", "tXdte", "tBtok", "tMf", "tCe"))
                tsl = slice(c * 128, (c + 1) * 128)
                for pair in range(4):
                    g = pair // 2
                    by = ps1()
                    for hp in range(2):
                        h = pair * 2 + hp
                        hh = h % 4
                        rows = slice(hp * 64, hp * 64 + 64)
                        tp_ = None if hp == 0 else (0, 64)
                        S.op("pe", [tXdt, tMf[g], tPrevB, tCe[g]], by.toks, lambda: [
                            nc.tensor.matmul(by.ap[rows, 0:128], xdt[:, h * 64:(h + 1) * 64], Mfin[g][:, hh * 128:(hh + 1) * 128],
                                             start=True, stop=False, tile_position=tp_),
                            nc.tensor.matmul(by.ap[rows, 0:128], prev_bf[:, h * 64:(h + 1) * 64], CexpT[g][:, hh * 128:(hh + 1) * 128],
                                             start=False, stop=True, tile_position=tp_)])
                    S.op("dve", by.toks + [tXs, tC], [tYv], lambda: nc.vector.scalar_tensor_tensor(
                        out=yv, in0=xsT[:, pair, tsl], scalar=dcol[:, pair:pair + 1], in1=by.ap[:, 0:128], op0=ALU.mult, op1=ALU.add))
                    S.op("dve", [tYv, tZ], [tYz], lambda: nc.vector.tensor_tensor(
                        out=yz[:, pair * 128:(pair + 1) * 128], in0=yv, in1=zs[:, pair, tsl], op=ALU.mult))
                    S.op("act", [tYz], [tSq2], lambda: nc.scalar.activation(
                        out=sq[:, pair * 128:(pair + 1) * 128], in_=yz[:, pair * 128:(pair + 1) * 128], func=AF.Square))
                    yield
                    if pair % 2 == 1:
                        bn_ = ps1()
                        S.op("pe", [tSq2, tC], bn_.toks, lambda: [nc.tensor.matmul(
                            bn_.ap[:, 0:128], ones_bf[:], sq[:, (pair - 1 + i) * 128:(pair + i) * 128], start=(i == 0), stop=(i == 1)) for i in range(2)])
                        rstd_from(rstd_g, bn_.ap[:, 0:128], 1.0 / 256, bn_.toks, [tRg])
                        for pp in (pair - 1, pair):
                            S.op("dve", [tYz, tRg, tC], [tCat[4 + pp]], lambda: nc.vector.scalar_tensor_tensor(
                                out=catT[:, 4 + pp, tsl], in0=yz[:, pp * 128:(pp + 1) * 128], scalar=ncol[:, pp:pp + 1], in1=rstd_g,
                                op0=ALU.mult, op1=ALU.mult))
                        yield
                bst = ps1()
                S.op("pe", [tBtok, tXdte], bst.toks, lambda: [nc.tensor.matmul(
                    bst.ap[:, g * 256:(g + 1) * 256], Btok[:, g * 128:(g + 1) * 128], xdte[:, g * 256:(g + 1) * 256],
                    start=True, stop=True) for g in range(2)])
                S.op("dve", [tSm2], [tPrevF], lambda: nc.vector.tensor_tensor(
                    out=v3(prev_f, 64), in0=v3(prev_f, 64), in1=cdk.unsqueeze(2).to_broadcast([128, 8, 64]), op=ALU.mult))
                S.op("dve", bst.toks, [tPrevF], lambda: nc.vector.tensor_tensor(out=prev_f, in0=prev_f, in1=bst.ap, op=ALU.add))
                S.op("act", [tPrevF], [tPrevB], lambda: nc.scalar.copy(out=prev_bf, in_=prev_f))
                yield

            def p5_gen():
                yield from ssd_stage1(0)
                for c in range(NT):
                    ga = ssd_stage1(c + 1) if c + 1 < NT else iter(())
                    gb = ssd_stage2(c)
                    a_alive = b_alive = True
                    while a_alive or b_alive:
                        if a_alive and next(ga, "done") == "done":
                            a_alive = False
                        if b_alive and next(gb, "done") == "done":
                            b_alive = False
                        yield

            g4, g5 = p4_gen(), p5_gen()
            alive = {"4": True, "5": True}

            def step(gen, key, n):
                for _ in range(n):
                    if alive[key]:
                        try:
                            next(gen)
                        except StopIteration:
                            alive[key] = False

            while alive["4"]:
                step(g4, "4", 64)
            while alive["5"]:
                step(g5, "5", 64)

            if stop_after == "P5":
                dump("cat_ssd", catT[:, 4:8, :], tCat)
                break
            S.barrier(engines=("act", "dve", "pool", "sp"))
            wout = v3(W[:, 0:8192], 1024)
            tWoL = [T() for _ in range(8)]
            for k in range(8):
                S.dma("sp", out=wout[:, k, :], in_=DWB["wout"][k * 128:(k + 1) * 128, :], reads=[tDW["wout"][k]], writes=[tWoL[k]])
            load_ln(0)
            xres = [Wf(8192, 10240), Wf(10240, 12288)]
            hbW = [W[:, 12288:13312], W[:, 13312:14336]]
            tXr, tHb = [T(), T()], [T(), T()]
            tH = [T() for _ in range(NT)]
            tHT = [T() for _ in range(NT)]
            def p6_mm(tt):
                tok = slice(tt * 128, (tt + 1) * 128)
                xr = xres[tt % 2]
                S.dma("sp", out=xr, in_=x[s, tok, :], writes=[tXr[tt % 2]])
                bm = ps2()
                S.op("pe", tCat + tWoL, bm.toks, lambda: [nc.tensor.matmul(
                    bm.ap[:, hf * 512:(hf + 1) * 512], catT[:, c, tok], wout[:, c, hf * 512:(hf + 1) * 512],
                    start=(c == 0), stop=(c == 7)) for hf in range(2) for c in range(8)])
                return bm

            bms = {0: p6_mm(0)}
            for tt in range(NT):
                if tt + 1 < NT:
                    bms[tt + 1] = p6_mm(tt + 1)
                bm = bms.pop(tt)
                xr = xres[tt % 2]
                hap = hview[:, tt, :]
                S.op("dve", bm.toks + [tXr[tt % 2]], [tH[tt]], lambda: nc.vector.scalar_tensor_tensor(
                    out=hap, in0=xr, scalar=ALPHA, in1=bm.ap, op0=ALU.mult, op1=ALU.add))
                layer_norm_tile(hap, tH[tt], 0, s)
                S.op("act", [tH[tt]], [tHb[tt % 2]], lambda: nc.scalar.copy(out=hbW[tt % 2], in_=hap))
                transpose_to(hbW[tt % 2], tHb[tt % 2], actT, tt, tHT[tt])
            if stop_after == "P6":
                dump("h1", hview, tH)
                break
            S.barrier(engines=("act", "dve", "pool", "sp"))
            xo = v3(K[:, 0:4096], 512)
            KxT = v3(K[:, 4096:6144], 256)
            Vx = v3(K[:, 6144:8192], 1024)
            memT = v3(K[:, 8192:10240], 256)
            membf = v3(K[:, 10240:12288], 1024)
            hbK = [K[:, 12288:13312], K[:, 13312:14336]]
            QxT = [K[:, 14336:14848], K[:, 14848:15360]]
            PTx = [K[:, 15360:15872], K[:, 15872:16384]]
            wk = v3(W[:, 0:8192], 1024)
            wv = v3(W[:, 8192:16384], 1024)
            wq = v3(W[:, 16384:24576], 1024)
            tMem, tMemT, tKx, tVx, tXo, tRdx = T(), T(), T(), T(), T(), T()
            tWk, tWv, tWq = [T() for _ in range(8)], [T() for _ in range(8)], [T() for _ in range(8)]
            tQx, tPx, tRt = [T(), T()], [T(), T()], T()
            for k in range(8):
                S.dma("sp", out=wk[:, k, :], in_=DWB["xwk"][k * 128:(k + 1) * 128, :], reads=[tDW["xwk"][k]], writes=[tWk[k]])
            S.dma("pool", out=membf, in_=mem[s].rearrange("(t p) d -> p t d", p=128), writes=[tMem])
            for k in range(8):
                S.dma("sp", out=wv[:, k, :], in_=DWB["xwv"][k * 128:(k + 1) * 128, :], reads=[tDW["xwv"][k]], writes=[tWv[k]])
            for k in range(8):
                S.dma("sp", out=wq[:, k, :], in_=DWB["xwq"][k * 128:(k + 1) * 128, :], reads=[tDW["xwq"][k]], writes=[tWq[k]])
            load_ln(1)
            for mt in range(2):
                pb = ps1()
                S.op("pe", [tMem, tC], pb.toks, lambda: [nc.tensor.transpose(
                    pb.bf[:, k * 128:(k + 1) * 128], membf[:, mt, k * 128:(k + 1) * 128], ident_bf[:]) for k in range(8)])
                S.op("act", pb.toks, [tMemT], lambda: nc.scalar.copy(out=memT[:, :, mt * 128:(mt + 1) * 128], in_=v3(pb.bf[:, 0:1024], 128)))
            for c in range(8):
                b_ = ps1()
                S.op("pe", [tMemT] + tWk, b_.toks, lambda: [nc.tensor.matmul(
                    b_.ap[:, 0:256], wk[:, k, c * 128:(c + 1) * 128], memT[:, k, :], start=(k == 0), stop=(k == 7)) for k in range(8)])
                S.op("act", b_.toks, [tKx], lambda: nc.scalar.copy(out=KxT[:, c, :], in_=b_.ap[:, 0:256]))
            for mt in range(2):
                b2 = ps2()
                S.op("pe", [tMemT] + tWv, b2.toks, lambda: [nc.tensor.matmul(
                    b2.ap[:, hf * 512:(hf + 1) * 512], memT[:, k, mt * 128:(mt + 1) * 128], wv[:, k, hf * 512:(hf + 1) * 512],
                    start=(k == 0), stop=(k == 7)) for hf in range(2) for k in range(8)])
                S.op("act", b2.toks, [tVx], lambda: nc.scalar.copy(out=Vx[:, mt, :], in_=b2.ap))
            wo = wk
            for k in range(8):
                S.dma("sp", out=wo[:, k, :], in_=DWB["xwo"][k * 128:(k + 1) * 128, :], reads=[tDW["xwo"][k]], writes=[tWk[k]])
            XSC = 256.0 ** -0.5
            lg = small[:, 64:100]
            gmax, ngmax, gsum, ggate = small[:, 100:101], small[:, 101:102], small[:, 102:103], small[:, 103:104]
            gone, ge, pen = small[:, 104:108], small[:, 108:112], small[:, 112:116]
            me, one1, me2, one2 = small[:, 116:148], small[:, 148:180], small[:, 180:212], small[:, 212:244]
            m1, m2, d21, e21, g1, g2 = (small[:, 244 + i:245 + i] for i in range(6))
            QxTg = [[K[:, 14336 + (i * 2 + dc) * 512:14336 + (i * 2 + dc + 1) * 512] for dc in range(2)] for i in range(2)]
            PTxg = [[K[:, 10240 + (i * 2 + mt) * 512:10240 + (i * 2 + mt + 1) * 512] for mt in range(2)] for i in range(2)]
            tQxg = [[T(), T()], [T(), T()]]
            tPxg = [[T(), T()], [T(), T()]]
            tRdxg = [T(), T()]
            units = [(tg, h) for tg in range(NG) for h in range(4)]

            def xa_A(i):
                tg, h = units[i]
                cols = slice(tg * 512, (tg + 1) * 512)
                for dc in range(2):
                    c = h * 2 + dc
                    bq_ = ps1()
                    S.op("pe", tHT[tg * 4:tg * 4 + 4] + tWq, bq_.toks, lambda: [nc.tensor.matmul(
                        bq_.ap, wq[:, k, c * 128:(c + 1) * 128], actT[:, k, cols], start=(k == 0), stop=(k == 7)) for k in range(8)])
                    S.op("act", bq_.toks, [tQxg[i % 2][dc]], lambda: nc.scalar.copy(out=QxTg[i % 2][dc], in_=bq_.ap))

            def xa_B(i):
                tg, h = units[i]
                for mt in range(2):
                    bs_ = ps1()
                    S.op("pe", tQxg[i % 2] + [tKx, tMem], bs_.toks, lambda: [nc.tensor.matmul(
                        bs_.ap, KxT[:, h * 2 + dc, mt * 128:(mt + 1) * 128], QxTg[i % 2][dc], start=(dc == 0), stop=(dc == 1)) for dc in range(2)])
                    S.op("act", bs_.toks, [tPxg[i % 2][mt]], lambda: nc.scalar.activation(out=PTxg[i % 2][mt], in_=bs_.ap, func=AF.Exp, scale=XSC))

            def xa_C(i):
                tg, h = units[i]
                ptx = PTxg[i % 2]
                rd_ = rdx[:, (i % 2) * 512:(i % 2 + 1) * 512]
                bd_ = ps1()
                S.op("pe", tPxg[i % 2] + [tC], bd_.toks, lambda: [nc.tensor.matmul(
                    bd_.ap, ones_bf[:], ptx[mt], start=(mt == 0), stop=(mt == 1)) for mt in range(2)])
                S.op("dve", bd_.toks, [tRdxg[i % 2]], lambda: nc.vector.reciprocal(out=rd_, in_=bd_.ap))
                for dc in range(2):
                    c = h * 2 + dc
                    bo_ = ps1()
                    S.op("pe", tPxg[i % 2] + [tVx], bo_.toks, lambda: [nc.tensor.matmul(
                        bo_.ap, Vx[:, mt, c * 128:(c + 1) * 128], ptx[mt], start=(mt == 0), stop=(mt == 1)) for mt in range(2)])
                    S.op("dve", bo_.toks + [tRdxg[i % 2]], [tXo], lambda: nc.vector.tensor_tensor(out=xo[:, c, :], in0=bo_.ap, in1=rd_, op=ALU.mult))

            def xa_mm(tt):
                j = tt % 4
                bm = ps2()
                S.op("pe", [tXo] + tWk, bm.toks, lambda: [nc.tensor.matmul(
                    bm.ap[:, hf * 512:(hf + 1) * 512], xo[:, c, j * 128:(j + 1) * 128], wo[:, c, hf * 512:(hf + 1) * 512],
                    start=(c == 0), stop=(c == 7)) for hf in range(2) for c in range(8)])
                return bm

            V_ = nc.vector
            AXX_ = mybir.AxisListType.X
            tRtS = [T(), T()]

            def xa_tail(tt, bm, si):
                rb0 = 64 + si * 256
                lg = small[:, rb0:rb0 + 36]
                gmax, ngmax, gsum, ggate = (small[:, rb0 + 36 + i:rb0 + 37 + i] for i in range(4))
                gone, ge, pen = small[:, rb0 + 40:rb0 + 44], small[:, rb0 + 44:rb0 + 48], small[:, rb0 + 48:rb0 + 52]
                me, me2 = small[:, rb0 + 52:rb0 + 84], small[:, rb0 + 84:rb0 + 116]
                m1, m2, d21, e21, g1 = (small[:, rb0 + 116 + i:rb0 + 117 + i] for i in range(5))
                tRt = tRtS[si]
                rt = lambda f: S.op("dve", [tRt], [tRt], f)
                hap = hview[:, tt, :]
                tHt = tH[tt]
                gt = s * NT + tt
                S.op("dve", bm.toks, [tHt], lambda: nc.vector.scalar_tensor_tensor(
                    out=hap, in0=hap, scalar=ALPHA, in1=bm.ap, op0=ALU.mult, op1=ALU.add))
                j_ = lnstate["i"] % 4
                lnstate["i"] += 1
                tS = tLNS[j_]
                base = j_ * 16
                st, mv = lnst[:, base:base + 12], lnst[:, base + 12:base + 14]
                rs, nm = lnst[:, base + 14:base + 15], lnst[:, base + 15:base + 16]
                S.op("dve", [tHt], [tS], lambda: nc.vector.bn_stats(out=st[:, 0:6], in_=hap[:, 0:512]))
                S.op("dve", [tHt], [tS], lambda: nc.vector.bn_stats(out=st[:, 6:12], in_=hap[:, 512:1024]))
                S.op("dve", [], [tS], lambda: nc.vector.bn_aggr(out=mv, in_=st))
                yield
                rstd_from(rs, mv[:, 1:2], 1.0, [tS], [tS])
                yield
                S.op("dve", [tS], [tS], lambda: nc.vector.scalar_tensor_tensor(
                    out=nm, in0=mv[:, 0:1], scalar=-1.0, in1=rs, op0=ALU.mult, op1=ALU.mult))
                yield
                S.op("act", [tS], [tHt], lambda: nc.scalar.activation(out=hap, in_=hap, func=AF.Identity, scale=rs, bias=nm))
                yield
                S.op("dve", [tLN], [tHt], lambda: nc.vector.tensor_tensor(out=hap, in0=hap, in1=lng[:], op=ALU.mult))
                yield
                S.op("dve", [tLN], [tHt], lambda: nc.vector.tensor_tensor(out=hap, in0=hap, in1=lnb[:], op=ALU.add))
                yield
                S.op("act", [tHt], [tHb[tt % 2]], lambda: nc.scalar.copy(out=hbK[tt % 2], in_=hap))
                yield
                transpose_to(hbK[tt % 2], tHb[tt % 2], actT, tt, tHT[tt])
                yield
                bl = ps1()
                S.op("pe", [tHT[tt], tC], bl.toks, lambda: [nc.tensor.matmul(
                    bl.ap[:, 0:36], actT[:, k, tt * 128:(tt + 1) * 128], v3(rw[:], 36)[:, k, :], start=(k == 0), stop=(k == 7)) for k in range(8)])
                yield
                S.op("dve", bl.toks + [tC], [tRt], lambda: V_.tensor_tensor(out=lg, in0=bl.ap[:, 0:36], in1=rb[:], op=ALU.add))
                rt(lambda: V_.reduce_max(out=gmax, in_=lg[:, 0:4], axis=AXX_))
                rt(lambda: V_.tensor_scalar(out=gone, in0=lg[:, 0:4], scalar1=gmax, scalar2=None, op0=ALU.is_equal))
                rt(lambda: V_.tensor_scalar_mul(out=ngmax, in0=gmax, scalar1=-1.0))
                yield
                S.op("act", [tRt], [tRt], lambda: nc.scalar.activation(out=ge, in_=lg[:, 0:4], func=AF.Exp, bias=ngmax))
                yield
                rt(lambda: V_.reduce_sum(out=gsum, in_=ge, axis=AXX_))
                rt(lambda: V_.reciprocal(out=ggate, in_=gsum))
                rt(lambda: V_.tensor_scalar(out=pen, in0=gone, scalar1=-1.0, scalar2=1e9, op0=ALU.add, op1=ALU.mult))
                rt(lambda: V_.tensor_tensor(out=v3(me, 8), in0=v3(lg[:, 4:36], 8), in1=pen.unsqueeze(2).to_broadcast([128, 4, 8]), op=ALU.add))
                rt(lambda: V_.reduce_max(out=m1, in_=me, axis=AXX_))
                o1 = ONE[:, (gt * 2) * 32:(gt * 2 + 1) * 32]
                o2 = ONE[:, (gt * 2 + 1) * 32:(gt * 2 + 2) * 32]
                S.op("dve", [tRt], [tRt, tRk], lambda: V_.tensor_scalar(out=o1, in0=me, scalar1=m1, scalar2=None, op0=ALU.is_equal))
                rt(lambda: V_.scalar_tensor_tensor(out=me2, in0=o1, scalar=-1e9, in1=me, op0=ALU.mult, op1=ALU.add))
                rt(lambda: V_.reduce_max(out=m2, in_=me2, axis=AXX_))
                S.op("dve", [tRt], [tRt, tRk], lambda: V_.tensor_scalar(out=o2, in0=me2, scalar1=m2, scalar2=None, op0=ALU.is_equal))
                rt(lambda: V_.tensor_tensor(out=d21, in0=m2, in1=m1, op=ALU.subtract))
                yield
                S.op("act", [tRt], [tRt], lambda: nc.scalar.activation(out=e21, in_=d21, func=AF.Exp))
                yield
                rt(lambda: V_.tensor_scalar_add(out=g1, in0=e21, scalar1=1.0))
                rt(lambda: V_.reciprocal(out=g1, in_=g1))
                S.op("dve", [tRt], [tRt, tRk], lambda: V_.tensor_tensor(out=G12[:, gt * 2:gt * 2 + 1], in0=g1, in1=ggate, op=ALU.mult))
                S.op("dve", [tRt], [tRt, tRk], lambda: V_.tensor_tensor(out=G12[:, gt * 2 + 1:gt * 2 + 2], in0=G12[:, gt * 2:gt * 2 + 1], in1=e21, op=ALU.mult))
                bR = ps1()
                S.op("pe", [tRk, tC], bR.toks, lambda: [
                    nc.tensor.matmul(bR.ap[:, 0:64], su_bf[:], ONE[:, gt * 64:(gt + 1) * 64], start=True, stop=True),
                    nc.tensor.matmul(bR.ap[:, 64:128], ones_bf[:], ONE[:, gt * 64:(gt + 1) * 64], start=True, stop=True)])
                yield
                ta, tb = me, me2
                S.op("dve", bR.toks + [tRk, tRt], [tRt], lambda: V_.tensor_tensor(out=ta, in0=bR.ap[:, 0:32], in1=Crun[:], op=ALU.add))
                rt(lambda: V_.tensor_tensor(out=tb, in0=ta, in1=o1, op=ALU.mult))
                S.op("dve", [tRt], [tRt, tRk], lambda: V_.reduce_sum(out=RK[:, gt * 2:gt * 2 + 1], in_=tb, axis=AXX_))
                S.op("dve", bR.toks + [tRk, tRt], [tRt], lambda: V_.tensor_tensor(out=ta, in0=bR.ap[:, 32:64], in1=Crun[:], op=ALU.add))
                S.op("dve", bR.toks + [tRt], [tRt], lambda: V_.tensor_tensor(out=ta, in0=bR.ap[:, 64:96], in1=ta, op=ALU.add))
                rt(lambda: V_.tensor_tensor(out=tb, in0=ta, in1=o2, op=ALU.mult))
                S.op("dve", [tRt], [tRt, tRk], lambda: V_.reduce_sum(out=RK[:, gt * 2 + 1:gt * 2 + 2], in_=tb, axis=AXX_))
                S.op("dve", bR.toks + [tRt], [tRk], lambda: V_.tensor_tensor(out=Crun[:], in0=bR.ap[:, 64:96], in1=Crun[:], op=ALU.add))
                S.op("dve", bR.toks + [tRt], [tRk], lambda: V_.tensor_tensor(out=Crun[:], in0=bR.ap[:, 96:128], in1=Crun[:], op=ALU.add))
                S.dma("sp", out=XB[gt * 128:(gt + 1) * 128, :], in_=hbK[tt % 2], reads=[tHb[tt % 2]])
                S.dma("sp", out=H2D[gt * 128:(gt + 1) * 128, :], in_=hap, reads=[tHt])
                yield

            xa_A(0)
            for ui in range(len(units)):
                xa_B(ui)
                if ui + 1 < len(units):
                    xa_A(ui + 1)
                xa_C(ui)
                tg, h = units[ui]
                if h != 3:
                    continue
                for pr in ((0, 1), (2, 3)):
                    gens = []
                    for si, j in enumerate(pr):
                        tt = tg * 4 + j
                        gens.append(xa_tail(tt, xa_mm(tt), si))
                    alive_ = [True, True]
                    while any(alive_):
                        for gi in range(2):
                            if alive_[gi] and next(gens[gi], "done") == "done":
                                alive_[gi] = False
            if stop_after == "P7":
                dump("h2", hview, tH)
                break
            S.barrier(engines=("act", "dve", "pool", "sp"))
        if stop_after is None:
            S.barrier(full=True)
            V_ = nc.vector
            AXX = mybir.AxisListType.X
            tM = T()
            padc, pA, pB, basev = small[:, 116:148], small[:, 148:180], small[:, 180:212], small[:, 212:244]
            cmpb = H[:, 0:1024]
            mo = lambda f, extra=(): S.op("dve", [tM, tRk, tC] + list(extra), [tM], f)
            mo(lambda: V_.tensor_tensor(out=v3(cmpb, 32), in0=Crun[:].unsqueeze(2).to_broadcast([128, 32, 32]),
                                        in1=thr[:].unsqueeze(1).to_broadcast([128, 32, 32]), op=ALU.is_gt))
            mo(lambda: V_.reduce_sum(out=padc, in_=v3(cmpb, 32), axis=AXX))
            mo(lambda: V_.tensor_scalar_mul(out=padc, in0=padc, scalar1=float(TSZ)))
            cur, nxt = padc, pA
            for sft in (1, 2, 4, 8, 16):
                mo(lambda: V_.tensor_copy(out=nxt[:, 0:sft], in_=cur[:, 0:sft]))
                mo(lambda: V_.tensor_tensor(out=nxt[:, sft:32], in0=cur[:, sft:32], in1=cur[:, 0:32 - sft], op=ALU.add))
                cur, nxt = nxt, (pB if nxt is pA else pA)
            endv = cur
            mo(lambda: V_.tensor_tensor(out=basev, in0=endv, in1=padc, op=ALU.subtract))
            cmpt = H[:, 2048:2048 + NTI * 32]
            tef = mo_f[:, 0:NTI]
            widx = mo_i[:, 0:NTI]
            mo(lambda: V_.tensor_tensor(out=v3(cmpt, 32), in0=endv.unsqueeze(1).to_broadcast([128, NTI, 32]),
                                        in1=tstart[:, 0:NTI].unsqueeze(2).to_broadcast([128, NTI, 32]), op=ALU.is_le))
            mo(lambda: V_.reduce_sum(out=tef, in_=v3(cmpt, 32), axis=AXX))
            mo(lambda: V_.tensor_scalar(out=tef, in0=tef, scalar1=128.0, scalar2=pcol[:, 0:1], op0=ALU.mult, op1=ALU.add))
            mo(lambda: V_.tensor_copy(out=widx, in_=tef))
            tmp3 = H[:, 4096:4096 + GT * 64]
            posf = mo_f[:, 64:64 + GT * 2]
            posI = mo_i[:, 64:64 + GT * 2]
            mo(lambda: V_.tensor_tensor(out=v3(tmp3, 32), in0=v3(ONE[:], 32), in1=basev.unsqueeze(1).to_broadcast([128, GT * 2, 32]), op=ALU.mult))
            mo(lambda: V_.reduce_sum(out=posf, in_=v3(tmp3, 32), axis=AXX))
            mo(lambda: V_.tensor_tensor(out=posf, in0=posf, in1=RK[:], op=ALU.add))
            mo(lambda: V_.tensor_copy(out=posI, in_=posf))
            xbt = [A[:, i * 1024:(i + 1) * 1024] for i in range(2)]
            tXb = [T(), T()]
            for gt in range(GT):
                S.dma("sp", out=xbt[gt % 2], in_=XB[gt * 128:(gt + 1) * 128, :], writes=[tXb[gt % 2]])
                for k in range(2):
                    S.dma("pool", out=XS[:, :], in_=xbt[gt % 2], out_off=posI[:, gt * 2 + k:gt * 2 + k + 1], bound=NTI * TSZ - 1,
                          reads=[tXb[gt % 2], tM])
            S.barrier()
            load_ln(2)
            xsb = [v3(A[:, 2048 + i * 4096:2048 + i * 4096 + TB * 1024], 1024) for i in range(2)]
            xst = [v3(K[:, 4096 + i * 4096:4096 + i * 4096 + 8 * TSZ], TSZ) for i in range(2)]
            Ssb = [Kf(0, 2 * TSZ), Kf(1024, 1024 + 2 * TSZ)]
            HD = [v3(K[:, 2048:2048 + 2 * TSZ], TSZ), v3(K[:, 3072:3072 + 2 * TSZ], TSZ)]
            Ysb = [K[:, 12288:13312], K[:, 13312:14336]]
            tXsb, tXst, tSs, tHD, tY = [T(), T()], [T(), T()], [T(), T()], [T(), T()], [T(), T()]
            NWB = 3
            Eb = [W[:, i * 6144:(i + 1) * 6144] for i in range(NWB)]
            tEw = [[T(), T(), T()] for _ in range(NWB)]

            def moe_prefetch_w(ti):
                eb = Eb[ti % NWB]
                for part, src in enumerate((WGB, WUB, WDB)):
                    S.dma("pool", out=eb[:, part * 2048:(part + 1) * 2048], in_=src[:, :], in_off=widx[:, ti:ti + 1], bound=NEXP * 128 - 1,
                          reads=[tM], writes=[tEw[ti % NWB][part]])

            def moe_prefetch_x(ti):
                S.dma("sp", out=xsb[ti % 2], in_=XS[ti * TSZ:(ti + 1) * TSZ, :].rearrange("(j p) d -> p j d", p=128), writes=[tXsb[ti % 2]])

            def moe_TR(ti):
                xs_, txs = xsb[ti % 2], tXsb[ti % 2]
                xt_, txt = xst[ti % 2], tXst[ti % 2]
                for j in range(TB):
                    pb = ps1()
                    S.op("pe", [txs, tC], pb.toks, lambda: [nc.tensor.transpose(
                        pb.bf[:, k * 128:(k + 1) * 128], xs_[:, j, k * 128:(k + 1) * 128], ident_bf[:]) for k in range(8)])
                    if j % 2 == 0:
                        S.op("act", pb.toks, [txt], lambda: nc.scalar.copy(out=xt_[:, :, j * 128:(j + 1) * 128], in_=v3(pb.bf[:, 0:1024], 128)))
                    else:
                        S.op("dve", pb.toks, [txt], lambda: nc.vector.tensor_copy(out=xt_[:, :, j * 128:(j + 1) * 128], in_=v3(pb.bf[:, 0:1024], 128)))

            def moe_GU(ti):
                eb = Eb[ti % NWB]
                tE3 = tEw[ti % NWB]
                wg = v3(eb[:, 0:2048], 256)
                wu = v3(eb[:, 2048:4096], 256)
                xt_, txt = xst[ti % 2], tXst[ti % 2]
                hd, thd = HD[ti % 2], tHD[ti % 2]
                for fc in range(2):
                    bg, bu = ps1(), ps1()
                    S.op("pe", [txt, tE3[0]], bg.toks, lambda: [nc.tensor.matmul(
                        bg.ap[:, 0:TSZ], wg[:, k, fc * 128:(fc + 1) * 128], xt_[:, k, :], start=(k == 0), stop=(k == 7)) for k in range(8)])
                    S.op("pe", [txt, tE3[1]], bu.toks, lambda: [nc.tensor.matmul(
                        bu.ap[:, 0:TSZ], wu[:, k, fc * 128:(fc + 1) * 128], xt_[:, k, :], start=(k == 0), stop=(k == 7)) for k in range(8)])
                    S.op("act", bg.toks, [tSs[fc]], lambda: nc.scalar.activation(out=Ssb[fc], in_=bg.ap[:, 0:TSZ], func=AF.Silu))
                    S.op("dve", bu.toks + [tSs[fc]], [thd], lambda: nc.vector.tensor_tensor(out=hd[:, fc, :], in0=Ssb[fc], in1=bu.ap[:, 0:TSZ], op=ALU.mult))

            def moe_D(ti):
                eb = Eb[ti % NWB]
                tE3 = tEw[ti % NWB]
                wd = v3(eb[:, 4096:6144], 1024)
                hd, thd = HD[ti % 2], tHD[ti % 2]
                for j in range(TB):
                    bd2 = ps2()
                    S.op("pe", [thd, tE3[2]], bd2.toks, lambda: [nc.tensor.matmul(
                        bd2.ap[:, hf * 512:(hf + 1) * 512], hd[:, fc, j * 128:(j + 1) * 128], wd[:, fc, hf * 512:(hf + 1) * 512],
                        start=(fc == 0), stop=(fc == 1)) for hf in range(2) for fc in range(2)])
                    yb, ty = Ysb[moe_state["yi"] % 2], tY[moe_state["yi"] % 2]
                    moe_state["yi"] += 1
                    S.op("act", bd2.toks, [ty], lambda: nc.scalar.copy(out=yb, in_=bd2.ap))
                    S.dma("sp", out=YS[ti * TSZ + j * 128:ti * TSZ + (j + 1) * 128, :], in_=yb, reads=[ty])

            moe_state = {"yi": 0}
            moe_prefetch_w(0)
            moe_prefetch_x(0)
            if NTI > 1:
                moe_prefetch_w(1)
                moe_prefetch_x(1)
            moe_TR(0)
            moe_GU(0)
            for ti in range(NTI):
                if ti + 2 < NTI:
                    moe_prefetch_w(ti + 2)
                if ti + 1 < NTI:
                    moe_TR(ti + 1)
                if ti + 2 < NTI:
                    moe_prefetch_x(ti + 2)
                moe_D(ti)
                if ti + 1 < NTI:
                    moe_GU(ti + 1)
            S.barrier()
            NYB = 4
            ybuf = [[H[:, i * 3072:i * 3072 + 512].bitcast(BF16), H[:, i * 3072 + 512:i * 3072 + 1024].bitcast(BF16),
                     H[:, i * 3072 + 1024:i * 3072 + 2048], H[:, i * 3072 + 2048:i * 3072 + 3072]] for i in range(NYB)]
            tYb = [[T(), T(), T(), T()] for _ in range(NYB)]

            def comb_prefetch(gt):
                y1, y2, ytmp, hh = ybuf[gt % NYB]
                t1_, t2_, tt_, th = tYb[gt % NYB]
                S.dma("pool", out=y1, in_=YS[:, :], in_off=posI[:, gt * 2:gt * 2 + 1], bound=NTI * TSZ - 1, reads=[tM], writes=[t1_])
                S.dma("pool", out=y2, in_=YS[:, :], in_off=posI[:, gt * 2 + 1:gt * 2 + 2], bound=NTI * TSZ - 1, reads=[tM], writes=[t2_])
                S.dma("sp", out=hh, in_=H2D[gt * 128:(gt + 1) * 128, :], writes=[th])

            def comb_gen(gt):
                y1, y2, ytmp, hh = ybuf[gt % NYB]
                t1_, t2_, tt_, th = tYb[gt % NYB]
                S.op("act", [t1_, tRk], [tt_], lambda: nc.scalar.activation(out=ytmp, in_=y1, func=AF.Copy, scale=G12[:, gt * 2:gt * 2 + 1]))
                yield
                S.op("dve", [t2_, tRk, tt_], [tt_], lambda: V_.scalar_tensor_tensor(
                    out=ytmp, in0=y2, scalar=G12[:, gt * 2 + 1:gt * 2 + 2], in1=ytmp, op0=ALU.mult, op1=ALU.add))
                S.op("dve", [tt_], [th], lambda: V_.scalar_tensor_tensor(out=hh, in0=hh, scalar=ALPHA, in1=ytmp, op0=ALU.mult, op1=ALU.add))
                j_ = lnstate["i"] % 4
                lnstate["i"] += 1
                tS = tLNS[j_]
                base = j_ * 16
                st, mv = lnst[:, base:base + 12], lnst[:, base + 12:base + 14]
                rs, nm = lnst[:, base + 14:base + 15], lnst[:, base + 15:base + 16]
                S.op("dve", [th], [tS], lambda: nc.vector.bn_stats(out=st[:, 0:6], in_=hh[:, 0:512]))
                S.op("dve", [th], [tS], lambda: nc.vector.bn_stats(out=st[:, 6:12], in_=hh[:, 512:1024]))
                S.op("dve", [], [tS], lambda: nc.vector.bn_aggr(out=mv, in_=st))
                yield
                rstd_from(rs, mv[:, 1:2], 1.0, [tS], [tS])
                yield
                S.op("dve", [tS], [tS], lambda: nc.vector.scalar_tensor_tensor(
                    out=nm, in0=mv[:, 0:1], scalar=-1.0, in1=rs, op0=ALU.mult, op1=ALU.mult))
                yield
                S.op("act", [tS], [th], lambda: nc.scalar.activation(out=hh, in_=hh, func=AF.Identity, scale=rs, bias=nm))
                yield
                S.op("dve", [tLN], [th], lambda: nc.vector.tensor_tensor(out=hh, in0=hh, in1=lng[:], op=ALU.mult))
                yield
                S.op("dve", [tLN], [th], lambda: nc.vector.tensor_tensor(out=hh, in0=hh, in1=lnb[:], op=ALU.add))
                sq_, tq_ = gt // NT, gt % NT
                S.dma("sp", out=out_d[sq_, tq_ * 128:(tq_ + 1) * 128, :], in_=hh, reads=[th])
                yield

            comb_prefetch(0)
            comb_prefetch(1)
            for g0 in range(0, GT, 2):
                for gn in (g0 + 2, g0 + 3):
                    if gn < GT:
                        comb_prefetch(gn)
                gens = [comb_gen(g0), comb_gen(g0 + 1)]
                alive_ = [True, True]
                while any(alive_):
                    for gi in range(2):
                        if alive_[gi] and next(gens[gi], "done") == "done":
                            alive_[gi] = False
        S.barrier(engines=("sp",), full=True)
    return nc


def _rope_tables():
    pos = np.arange(SEQ, dtype=np.float32)
    inv_freq = (np.float32(10000.0) ** (-(np.arange(0, 32, 2, dtype=np.float32)) / np.float32(32))).astype(np.float32)
    ang = (pos[:, None] * inv_freq[None, :]).astype(np.float32)
    cos = np.cos(ang).astype(np.float32)
    sin = np.sin(ang).astype(np.float32)
    cc = np.zeros((128, SEQ), np.float32)
    ss = np.zeros((128, SEQ), np.float32)
    cc[0:64] = 1.0
    cc[64:80] = cos.T
    cc[80:96] = cos.T
    ss[64:80] = -sin.T
    ss[80:96] = sin.T
    return cc, ss


def prep_shared(inp):
    f = np.float32
    g = lambda k: np.asarray(inp[k], dtype=f)[0]
    w_in = g("w_in")
    wkr = np.zeros((D, 96), f)
    wkr[:, 64:80] = w_in[:, 400:416]
    wkr[:, 80:96] = w_in[:, 384:400]
    wq = g("w_q_up")
    perm = np.arange(768)
    for h in range(8):
        for j in range(32):
            perm[h * 96 + 64 + j] = h * 96 + 64 + (j + 16) % 32
    wq_sw = wq[:, perm]
    convw = g("ssd_conv_w").reshape(4, 8, 128).transpose(2, 1, 0).reshape(128, 32)
    convb = g("ssd_conv_b").reshape(8, 128).T
    dtb = np.broadcast_to(np.tile(g("ssd_dt_bias"), 16)[None, :], (128, 128))
    alog = np.broadcast_to(g("ssd_a_log")[None, :], (128, 8))
    sd = g("ssd_d")
    dcol = np.stack([sd[pair * 2 + (np.arange(128) // 64)] for pair in range(4)], axis=1)
    ncol = g("ssd_norm").reshape(4, 128).T
    lnp = np.stack([np.broadcast_to(g(k)[None, :], (128, D)) for k in ("ln1_g", "ln1_b", "ln2_g", "ln2_b", "ln3_g", "ln3_b")])
    rw = np.concatenate([g("router_group_w"), g("router_expert_w")], axis=1)
    rb = np.broadcast_to(np.concatenate([g("router_group_b"), g("router_expert_b")])[None, :], (128, 36))
    tri = np.triu(np.ones((128, 128), f))
    nm = np.where(np.arange(128)[None, :] >= np.arange(128)[:, None], 0.0, -30000.0).astype(f)
    cc, ss = _rope_tables()
    su = np.triu(np.ones((128, 128), f), k=1)
    thr = np.broadcast_to((np.arange(32, dtype=f) * 384.0)[None, :], (128, 32))
    tstart = np.broadcast_to((np.arange(64, dtype=f) * 384.0)[None, :], (128, 64))
    pcol = np.arange(128, dtype=f).reshape(128, 1)
    sh = {
        "su": su, "thr": thr, "tstart": tstart, "pcol": pcol,
        "w_in": w_in, "wkr_sw": wkr, "wq": wq, "wq_sw": wq_sw, "qn": g("mla_q_norm").reshape(2, 128).T,
        "wkv": g("w_kv_up"), "kvn": g("mla_kv_norm").reshape(128, 1), "convw": convw, "convb": convb,
        "dtb": dtb, "alog": alog, "dcol": dcol, "ncol": ncol, "wout": g("w_out"), "xwq": g("xa_wq"),
        "xwk": g("xa_wk"), "xwv": g("xa_wv"), "xwo": g("xa_wo"), "lnp": lnp, "rw": rw, "rb": rb,
        "wg": g("expert_w_gate"), "wu": g("expert_w_up"), "wd": g("expert_w_down"),
        "ident": np.eye(128, dtype=f), "tri": tri, "negmask": np.tile(nm, (1, 4)), "cc": cc, "ss": ss,
    }
    return {k: np.ascontiguousarray(v, dtype=f) for k, v in sh.items()}


def kernel(**inputs):
    sh = prep_shared(inputs)
    x = np.asarray(inputs["x"], dtype=np.float32)
    mem = np.asarray(inputs["mem"], dtype=np.float32)
    nc = build(n_seq=2)
    in_maps = []
    for c in range(N_CORES):
        m = dict(sh)
        m["x"] = np.ascontiguousarray(x[2 * c:2 * c + 2])
        m["mem"] = np.ascontiguousarray(mem[2 * c:2 * c + 2])
        in_maps.append(m)
    res = run_bass_kernel_spmd(nc, in_maps, core_ids=list(range(N_CORES)))
    return np.concatenate([r["out"] for r in res.results], axis=0)
```

```python
import numpy as np
from contextlib import ExitStack
import concourse.bass as bass
import concourse.mybir as mybir
from concourse.bass_utils import run_bass_kernel_spmd

F32 = mybir.dt.float32
BF16 = mybir.dt.bfloat16
AF = mybir.ActivationFunctionType
ALU = mybir.AluOpType

N_CORES = 8
SEQ = 2048
D = 1024
NT = SEQ // 128
NG = SEQ // 512
ALPHA = 2.0 ** 0.25
EPS = 1e-5
NEXP = 32


class T:
    __slots__ = ("w", "r")

    def __init__(self):
        self.w = None
        self.r = {}


class Sched:
    def __init__(self, nc, es, n_dma=80):
        self.nc = nc
        self.eng = {"pe": nc.tensor, "act": nc.scalar, "dve": nc.vector, "pool": nc.gpsimd, "sp": nc.sync}
        self.sem = {k: es.enter_context(nc.semaphore("s_" + k)) for k in ("pe", "act", "dve", "pool")}
        self.cnt = {k: 0 for k in self.sem}
        self.dsem = [es.enter_context(nc.semaphore("d%d" % i)) for i in range(n_dma)]
        self.dcnt = [0] * n_dma
        n_cast = 16
        self.qslots = {"sp": list(range(0, (n_dma - n_cast) // 2)), "pool": list(range((n_dma - n_cast) // 2, n_dma - n_cast)),
                       "cast": list(range(n_dma - n_cast, n_dma))}
        self.qnext = {"sp": 0, "pool": 0, "cast": 0}
        self.seen = {}
        self.bregs = {}

    def _semof(self, key):
        return self.sem[key] if isinstance(key, str) else self.dsem[key]

    def _need(self, eng, reads, writes):
        need = {}

        def add(k, v):
            if k == eng:
                if eng == "pe":
                    return
                if self.cnt[eng] - v >= 4:
                    return
            if self.seen.get((eng, k), 0) >= v:
                return
            if need.get(k, 0) < v:
                need[k] = v

        for t in reads:
            if t.w is not None:
                add(*t.w)
        for t in writes:
            if t.w is not None:
                add(*t.w)
            for k, v in t.r.items():
                add(k, v)
        return need

    def _wait(self, eng, key, val):
        if self.seen.get((eng, key), 0) >= val:
            return
        self.eng[eng].wait_ge(self._semof(key), val)
        self.seen[(eng, key)] = val

    def _waits(self, eng, reads, writes):
        for k, v in self._need(eng, reads, writes).items():
            self._wait(eng, k, v)

    def _commit(self, ticket, reads, writes):
        k, v = ticket
        for t in reads:
            if t.r.get(k, 0) < v:
                t.r[k] = v
        for t in writes:
            t.w = ticket
            t.r = {}

    def op(self, eng, reads, writes, emit):
        need = self._need(eng, reads, writes)
        keys = list(need)
        attach = keys[-1] if keys else None
        for k in keys[:-1]:
            self._wait(eng, k, need[k])
        r = emit()
        first, last = (r[0], r[-1]) if isinstance(r, list) else (r, r)
        if attach is not None:
            first._wait_ge(self._semof(attach), need[attach])
            self.seen[(eng, attach)] = need[attach]
        self.cnt[eng] += 1
        last.then_inc(self.sem[eng], 1)
        self._commit((eng, self.cnt[eng]), reads, writes)

    def dma(self, q, out, in_, reads=(), writes=(), out_off=None, in_off=None, bound=None, slots=None):
        self._waits(q, reads, writes)
        sp_ = slots or q
        sl = self.qslots[sp_]
        slot = sl[self.qnext[sp_]]
        self.qnext[sp_] = (self.qnext[sp_] + 1) % len(sl)
        if self.dcnt[slot] > 0:
            self._wait(q, slot, self.dcnt[slot])
        if out_off is None and in_off is None:
            inst = self.eng[q].dma_start(out=out, in_=in_)
        else:
            if bound not in self.bregs:
                self.bregs[bound] = self.nc.gpsimd.to_reg(bound)
            bound = self.bregs[bound]
            inst = self.nc.gpsimd.indirect_dma_start(
                out=out, out_offset=None if out_off is None else bass.IndirectOffsetOnAxis(ap=out_off, axis=0),
                in_=in_, in_offset=None if in_off is None else bass.IndirectOffsetOnAxis(ap=in_off, axis=0),
                bounds_check=bound, oob_is_err=False)
        inst.then_inc(self.dsem[slot], 16)
        self.dcnt[slot] += 16
        self._commit((slot, self.dcnt[slot]), reads, writes)

    def barrier(self, engines=("pe", "act", "dve", "pool", "sp"), full=False):
        skip = () if full else set(self.qslots["cast"])
        for e in engines:
            for k in self.sem:
                if self.cnt[k] > 0:
                    self._wait(e, k, self.cnt[k])
            for s in range(len(self.dsem)):
                if self.dcnt[s] > 0 and s not in skip:
                    self._wait(e, s, self.dcnt[s])


def v3(ap, b):
    return ap.rearrange("p (a b) -> p a b", b=b)


def build(n_seq=2, stop_after=None, dumps=()):
    nc = bass.Bass("TRN2", target_bir_lowering=False)

    def din(name, shape):
        return nc.dram_tensor(name, list(shape), F32, kind="ExternalInput").ap()

    x = din("x", [n_seq, SEQ, D])
    mem = din("mem", [n_seq, 256, D])
    w_in = din("w_in", [D, 1960])
    wkr_sw = din("wkr_sw", [D, 96])
    wq_d = din("wq", [256, 768])
    wqsw_d = din("wq_sw", [256, 768])
    qn_d = din("qn", [128, 2])
    wkv_d = din("wkv", [128, 1024])
    kvn_d = din("kvn", [128, 1])
    convw_d = din("convw", [128, 32])
    convb_d = din("convb", [128, 8])
    dtb_d = din("dtb", [128, 128])
    alog_d = din("alog", [128, 8])
    dcol_d = din("dcol", [128, 4])
    ncol_d = din("ncol", [128, 4])
    wout_d = din("wout", [D, D])
    xwq_d = din("xwq", [D, D])
    xwk_d = din("xwk", [D, D])
    xwv_d = din("xwv", [D, D])
    xwo_d = din("xwo", [D, D])
    lnp_d = din("lnp", [6, 128, D])
    rw_d = din("rw", [D, 36])
    rb_d = din("rb", [128, 36])
    wg_d = din("wg", [NEXP, D, 256])
    wu_d = din("wu", [NEXP, D, 256])
    wd_d = din("wd", [NEXP, 256, D])
    ident_d = din("ident", [128, 128])
    tri_d = din("tri", [128, 128])
    negmask_d = din("negmask", [128, 512])
    cc_d = din("cc", [128, SEQ])
    ss_d = din("ss", [128, SEQ])
    su_d = din("su", [128, 128])
    thr_d = din("thr", [128, 32])
    pcol_d = din("pcol", [128, 1])
    GT = n_seq * NT
    NTOK = n_seq * SEQ
    TB = 3
    TSZ = 128 * TB
    NTI = -(-(2 * NTOK) // TSZ) + 31
    tstart_d = din("tstart", [128, 64])
    out_d = nc.dram_tensor("out", [n_seq, SEQ, D], F32, kind="ExternalOutput").ap()
    XB = nc.dram_tensor("XB", [NTOK, D], BF16, kind="Internal").ap()
    H2D = nc.dram_tensor("H2D", [NTOK, D], F32, kind="Internal").ap()
    XS = nc.dram_tensor("XS", [NTI * TSZ, D], BF16, kind="Internal").ap()
    YS = nc.dram_tensor("YS", [NTI * TSZ, D], BF16, kind="Internal").ap()
    DWB = {n: nc.dram_tensor("DWB_" + n, [D, D], BF16, kind="Internal").ap() for n in ("wout", "xwq", "xwk", "xwv", "xwo")}
    XBF = nc.dram_tensor("XBF", [max(n_seq - 1, 1), SEQ, D], BF16, kind="Internal").ap()
    WINB = nc.dram_tensor("WINB", [D, 1960], BF16, kind="Internal").ap()
    WGB = nc.dram_tensor("WGB", [NEXP * 128, 2048], BF16, kind="Internal").ap()
    WUB = nc.dram_tensor("WUB", [NEXP * 128, 2048], BF16, kind="Internal").ap()
    WDB = nc.dram_tensor("WDB", [NEXP * 128, 2048], BF16, kind="Internal").ap()
    dump_d = {}
    for name, shape in dumps:
        dump_d[name] = nc.dram_tensor("dbg_" + name, list(shape), F32, kind="ExternalOutput").ap()

    es = ExitStack()
    with es:
        S = Sched(nc, es)

        def sb(name, shape, dt):
            return es.enter_context(nc.sbuf_tensor(name, list(shape), dt))

        ident_bf = sb("ident_bf", [128, 128], BF16)
        ones_bf = sb("ones_bf", [128, 128], BF16)
        ones_f = sb("ones_f", [128, 128], F32)
        tri_f = sb("tri_f", [128, 128], F32)
        ident_f = sb("ident_f", [128, 128], F32)
        negmask = sb("negmask_s", [128, 512], F32)
        cc = sb("cc_s", [128, SEQ], BF16)
        ss = sb("ss_s", [128, SEQ], BF16)
        lng = sb("lng", [128, D], F32)
        lnb = sb("lnb", [128, D], F32)
        qn = sb("qn_s", [128, 2], F32)
        kvn = sb("kvn_s", [128, 1], F32)
        convw = sb("convw_s", [128, 32], F32)
        convb = sb("convb_s", [128, 8], F32)
        dtb = sb("dtb_s", [128, 128], F32)
        a_bc = sb("a_bc", [128, 8], F32)
        dcol = sb("dcol_s", [128, 4], F32)
        ncol = sb("ncol_s", [128, 4], F32)
        rb = sb("rb_s", [128, 36], F32)
        rw = sb("rw_s", [128, 8 * 36], BF16)
        small = sb("small", [128, 512], F32)
        su_bf = sb("su_bf", [128, 128], BF16)
        thr = sb("thr_s", [128, 32], F32)
        pcol = sb("pcol_s", [128, 1], F32)
        tstart = sb("tstart_s", [128, 64], F32)
        ONE = sb("ONE", [128, GT * 64], BF16)
        RK = sb("RK", [128, GT * 2], F32)
        G12 = sb("G12", [128, GT * 2], F32)
        Crun = sb("Crun", [128, 32], F32)
        mo_f = sb("mo_f", [128, 128], F32)
        mo_i = sb("mo_i", [128, 128], mybir.dt.int32)
        tRk = T()
        rdx = cc[:].bitcast(F32)
        dt_tok = sb("dt_tok", [128, 128], F32)
        tC = T()
        tLN = T()
        tSmall = T()

        A = sb("arenaA", [128, 16384], BF16)
        H = sb("arenaH", [128, 16384], F32)
        K = sb("arenaK", [128, 16384], BF16)
        W = sb("arenaW", [128, 24576], BF16)
        lnst = sb("lnst", [128, 64], F32)
        tLNS = [T() for _ in range(4)]
        lnstate = {"i": 0}
        PS = [es.enter_context(nc.psum_tensor("ps%d" % i, [128, 1024], F32)) for i in range(4)]
        PT_ = [T() for _ in range(8)]
        pstate = {"b": 0}

        class Bank:
            def __init__(self, ap, toks):
                self.ap = ap
                self.toks = toks

            @property
            def bf(self):
                return self.ap.bitcast(BF16)

        busy = set()

        def ps1(hold=False):
            b = pstate["b"]
            while b in busy:
                b = (b + 1) % 8
            pstate["b"] = (b + 1) % 8
            bk_ = Bank(PS[b // 2][:, (b % 2) * 512:(b % 2) * 512 + 512], [PT_[b]])
            bk_.idx = [b]
            if hold:
                busy.add(b)
            return bk_

        def ps2(hold=False):
            b = pstate["b"]
            if b % 2:
                b = (b + 1) % 8
            while b in busy or (b + 1) in busy:
                b = (b + 2) % 8
            pstate["b"] = (b + 2) % 8
            bk_ = Bank(PS[b // 2][:, :], [PT_[b], PT_[b + 1]])
            bk_.idx = [b, b + 1]
            if hold:
                busy.update(bk_.idx)
            return bk_

        def psrel(bk_):
            for b in bk_.idx:
                busy.discard(b)

        def Wf(lo, hi):
            return W[:, lo:hi].bitcast(F32)

        def Kf(lo, hi):
            return K[:, lo:hi].bitcast(F32)

        def Hb(lo, hi):
            return H[:, lo:hi].bitcast(BF16)

        def dump(name, ap, toks):
            if name in dump_d:
                S.dma("pool", out=dump_d[name], in_=ap, reads=toks)

        def ld(q, dst, src):
            S.dma(q, out=dst, in_=src, writes=[tC])

        ld("pool", ident_bf[:], ident_d[:, :])
        ld("sp", ident_f[:], ident_d[:, :])
        ld("sp", tri_f[:], tri_d[:, :])
        ld("sp", negmask[:], negmask_d[:, :])
        ld("pool", su_bf[:], su_d[:, :])
        ld("sp", thr[:], thr_d[:, :])
        ld("sp", pcol[:], pcol_d[:, :])
        ld("sp", tstart[:], tstart_d[:, :])
        S.op("dve", [], [tRk], lambda: nc.vector.memset(Crun[:], 0.0))
        ld("pool", cc[:], cc_d[:, :])
        ld("pool", ss[:], ss_d[:, :])
        for dst, src in ((qn, qn_d), (kvn, kvn_d), (convw, convw_d), (convb, convb_d), (dtb, dtb_d),
                         (a_bc, alog_d), (dcol, dcol_d), (ncol, ncol_d), (rb, rb_d)):
            ld("sp", dst[:], src[:, :])
        ld("pool", v3(rw[:], 36), rw_d.rearrange("(k p) c -> p k c", p=128))
        S.op("dve", [], [tC], lambda: nc.vector.memset(ones_f[:], 1.0))
        S.op("dve", [], [tC], lambda: nc.vector.memset(ones_bf[:], 1.0))
        S.op("act", [tC], [tC], lambda: nc.scalar.activation(out=a_bc[:], in_=a_bc[:], func=AF.Exp))
        S.op("dve", [tC], [tC], lambda: nc.vector.tensor_scalar_mul(out=a_bc[:], in0=a_bc[:], scalar1=-1.0))
        S.barrier()

        def rstd_from(dst, src, scale, reads, writes):
            S.op("act", reads, writes, lambda: nc.scalar.activation(out=dst, in_=src, func=AF.Ln, scale=scale, bias=EPS))
            S.op("act", [], writes, lambda: nc.scalar.activation(out=dst, in_=dst, func=AF.Exp, scale=-0.5))

        def layer_norm_tile(hap, tH, li, s):
            j = lnstate["i"] % 4
            lnstate["i"] += 1
            tS = tLNS[j]
            base = j * 16
            st = lnst[:, base:base + 12]
            mv = lnst[:, base + 12:base + 14]
            rs = lnst[:, base + 14:base + 15]
            nm = lnst[:, base + 15:base + 16]
            S.op("dve", [tH], [tS], lambda: nc.vector.bn_stats(out=st[:, 0:6], in_=hap[:, 0:512]))
            S.op("dve", [tH], [tS], lambda: nc.vector.bn_stats(out=st[:, 6:12], in_=hap[:, 512:1024]))
            S.op("dve", [], [tS], lambda: nc.vector.bn_aggr(out=mv, in_=st))
            rstd_from(rs, mv[:, 1:2], 1.0, [tS], [tS])
            S.op("dve", [tS], [tS], lambda: nc.vector.scalar_tensor_tensor(
                out=nm, in0=mv[:, 0:1], scalar=-1.0, in1=rs, op0=ALU.mult, op1=ALU.mult))
            S.op("act", [tS], [tH], lambda: nc.scalar.activation(out=hap, in_=hap, func=AF.Identity, scale=rs, bias=nm))
            S.op("dve", [tLN], [tH], lambda: nc.vector.tensor_tensor(out=hap, in0=hap, in1=lng[:], op=ALU.mult))
            S.op("dve", [tLN], [tH], lambda: nc.vector.tensor_tensor(out=hap, in0=hap, in1=lnb[:], op=ALU.add))

        def load_ln(li):
            S.dma("sp", out=lng[:], in_=lnp_d[2 * li, :, :], writes=[tLN])
            S.dma("sp", out=lnb[:], in_=lnp_d[2 * li + 1, :, :], writes=[tLN])

        def transpose_to(hb, tHB, dstT, tt, tDst):
            pb = ps1()
            S.op("pe", [tHB, tC], pb.toks, lambda: [nc.tensor.transpose(
                pb.bf[:, k * 128:(k + 1) * 128], hb[:, k * 128:(k + 1) * 128], ident_bf[:]) for k in range(8)])
            S.op("act", pb.toks, [tDst], lambda: nc.scalar.copy(
                out=dstT[:, :, tt * 128:(tt + 1) * 128], in_=v3(pb.bf[:, 0:1024], 128)))

        actT = v3(A[:, :], SEQ)
        catT = v3(K[:, :], SEQ)
        hview = v3(H[:, :], D)

        for s in range(n_seq):
            tXT = [T() for _ in range(NT)]
            if s > 0:
                S.dma("pool", out=cc[:], in_=cc_d[:, :], writes=[tC])
            NXB = 4
            xin = [K[:, 0:1024], K[:, 1024:2048], K[:, 8192:9216], K[:, 9216:10240]]
            txin = [T() for _ in range(NXB)]
            win = v3(W[:, 0:15680], 1960)
            wkr = v3(W[:, 15680:16448], 96)
            wq_s = v3(W[:, 16448:17984], 768)
            wq_sw = v3(W[:, 17984:19520], 768)
            wkv_s = W[:, 19520:20544]
            tWinL, tWs = [T() for _ in range(9)], T()
            def load_x_tile(tt):
                if s == 0:
                    S.dma("pool", out=xin[tt % NXB], in_=x[s, tt * 128:(tt + 1) * 128, :], writes=[txin[tt % NXB]])
                else:
                    S.dma("sp", out=xin[tt % NXB], in_=XBF[s - 1, tt * 128:(tt + 1) * 128, :], reads=[tXC[(s, tt)]], writes=[txin[tt % NXB]])

            for tt in range(NXB):
                load_x_tile(tt)
            WCG = [(0, 416), (416, 928), (928, 1440), (1440, 1960)]
            w_in3 = w_in.rearrange("(k p) c -> p k c", p=128)
            def load_win_group(gi):
                c0_, c1_ = WCG[gi]
                if s == 0:
                    S.dma("pool", out=win[:, :, c0_:c1_], in_=w_in3[:, :, c0_:c1_], writes=[tWinL[gi]])
                else:
                    S.dma("sp", out=win[:, :, c0_:c1_], in_=WINB[:, c0_:c1_].rearrange("(k p) c -> p k c", p=128),
                          reads=[tWC[gi]], writes=[tWinL[gi]])

            load_win_group(0)
            S.dma("pool", out=wkr, in_=wkr_sw.rearrange("(k p) c -> p k c", p=128), writes=[tWinL[8]])
            for gi in range(1, 4):
                load_win_group(gi)
            S.dma("pool", out=wq_s, in_=wq_d.rearrange("(k p) c -> p k c", p=128), writes=[tWs])
            S.dma("pool", out=wq_sw, in_=wqsw_d.rearrange("(k p) c -> p k c", p=128), writes=[tWs])
            S.dma("pool", out=wkv_s, in_=wkv_d[:, :], writes=[tWs])
            for r in range(2):
                S.op("dve", [tWs, tC], [tWs], lambda r=r: nc.vector.tensor_scalar_mul(out=wq_s[:, r, :], in0=wq_s[:, r, :], scalar1=qn[:, r:r + 1]))
                S.op("dve", [tWs, tC], [tWs], lambda r=r: nc.vector.tensor_scalar_mul(out=wq_sw[:, r, :], in0=wq_sw[:, r, :], scalar1=qn[:, r:r + 1]))
            S.op("dve", [tWs, tC], [tWs], lambda: nc.vector.tensor_scalar_mul(out=wkv_s, in0=wkv_s, scalar1=kvn[:, 0:1]))
            for tt in range(NT):
                transpose_to(xin[tt % NXB], txin[tt % NXB], actT, tt, tXT[tt])
                if tt + NXB < NT:
                    load_x_tile(tt + NXB)
            if s == 0:
                tDW = {n: [T() for _ in range(8)] for n in DWB}
                for n, src in (("wout", wout_d), ("xwk", xwk_d), ("xwv", xwv_d), ("xwq", xwq_d), ("xwo", xwo_d)):
                    for k in range(8):
                        S.dma("pool", out=DWB[n][k * 128:(k + 1) * 128, :], in_=src[k * 128:(k + 1) * 128, :], writes=[tDW[n][k]], slots="cast")
            if s == 0 and n_seq > 1:
                tXC = {}
                tWC = [T() for _ in range(4)]
                for gi in range(4):
                    S.dma("pool", out=WINB[:, WCG[gi][0]:WCG[gi][1]].rearrange("(k p) c -> p k c", p=128),
                          in_=w_in3[:, :, WCG[gi][0]:WCG[gi][1]], writes=[tWC[gi]], slots="cast")
                for s2 in range(1, n_seq):
                    for tt in range(NT):
                        tXC[(s2, tt)] = T()
                        S.dma("pool", out=XBF[s2 - 1, tt * 128:(tt + 1) * 128, :], in_=x[s2, tt * 128:(tt + 1) * 128, :],
                              writes=[tXC[(s2, tt)]], slots="cast")
            if s == 0 and stop_after is None:
                for e in range(NEXP):
                    rows = slice(e * 128, (e + 1) * 128)
                    S.dma("pool", out=WGB[rows, :].rearrange("p (k f) -> p k f", k=8), in_=wg_d[e].rearrange("(k p) f -> p k f", p=128), writes=[T()], slots="cast")
                    S.dma("pool", out=WUB[rows, :].rearrange("p (k f) -> p k f", k=8), in_=wu_d[e].rearrange("(k p) f -> p k f", p=128), writes=[T()], slots="cast")
                    S.dma("pool", out=WDB[rows, :].rearrange("p (k f) -> p k f", k=2), in_=wd_d[e].rearrange("(k p) f -> p k f", p=128), writes=[T()], slots="cast")
            if stop_after == "P1":
                dump("xT", actT[:, 0, :], tXT)
                break

            zs = v3(Hb(0, 4096), SEQ)
            xsT = v3(Hb(4096, 8192), SEQ)
            BT = v3(Hb(8192, 10240), SEQ)
            CT = v3(Hb(10240, 12288), SEQ)
            cqT = v3(Hb(12288, 14336), SEQ)
            ckvT = Hb(14336, 15360)
            kpe = Hb(15360, 16384)
            tZ, tXs, tB, tCt, tCq, tCkv, tKpe = T(), T(), T(), T(), T(), T(), T()
            pre = [Kf(2048 + i * 1040, 2048 + i * 1040 + 1030) for i in range(2)]
            acc = [Kf(4128 + i * 1024, 4128 + (i + 1) * 1024) for i in range(2)]
            sqb = K[:, 6176:6688]
            rsb = Kf(6688, 7712)
            tPre, tAcc, tSq, tRs = [T(), T()], [T(), T()], T(), T()
            tDt = T()

            def proj_mm(bank, M, lhs_fn, tg, wt=0):
                S.op("pe", list(tXT[tg * 4:tg * 4 + 4]) + [tWinL[wt]], bank.toks, lambda: [nc.tensor.matmul(
                    bank.ap[0:M, :], lhs_fn(k), actT[:, k, tg * 512:(tg + 1) * 512], start=(k == 0), stop=(k == 7)) for k in range(8)])

            for tg in range(NG):
                cols = slice(tg * 512, (tg + 1) * 512)
                bq = [ps1(), ps1()]
                for r in range(2):
                    proj_mm(bq[r], 128, lambda k, r=r: win[:, k, r * 128:(r + 1) * 128], tg)
                bs = ps1()
                for r in range(2):
                    S.op("act", bq[r].toks, [tSq], lambda r=r: nc.scalar.activation(out=sqb, in_=bq[r].ap, func=AF.Square))
                    S.op("pe", [tSq, tC], bs.toks, lambda r=r: nc.tensor.matmul(bs.ap, ones_bf[:], sqb, start=(r == 0), stop=(r == 1)))
                rstd_from(rsb, bs.ap, 1.0 / 256, bs.toks, [tRs])
                for r in range(2):
                    S.op("dve", bq[r].toks + [tRs], [tCq], lambda r=r: nc.vector.tensor_tensor(out=cqT[:, r, cols], in0=bq[r].ap, in1=rsb, op=ALU.mult))
                bk = ps1()
                proj_mm(bk, 128, lambda k: win[:, k, 256:384], tg)
                bs = ps1()
                S.op("act", bk.toks, [tSq], lambda: nc.scalar.activation(out=sqb, in_=bk.ap, func=AF.Square))
                S.op("pe", [tSq, tC], bs.toks, lambda: nc.tensor.matmul(bs.ap, ones_bf[:], sqb, start=True, stop=True))
                rstd_from(rsb, bs.ap, 1.0 / 128, bs.toks, [tRs])
                S.op("dve", bk.toks + [tRs], [tCkv], lambda: nc.vector.tensor_tensor(out=ckvT[:, cols], in0=bk.ap, in1=rsb, op=ALU.mult))
                ba, bb = ps1(), ps1()
                proj_mm(ba, 96, lambda k: win[:, k, 320:416], tg)
                proj_mm(bb, 96, lambda k: wkr[:, k, :], tg, 8)
                t1 = acc[0]
                t2 = acc[1]
                S.op("dve", ba.toks + [tC], [tAcc[0]], lambda: nc.vector.tensor_tensor(out=t1[64:96, :], in0=ba.ap[64:96, :], in1=cc[64:96, cols], op=ALU.mult))
                S.op("dve", bb.toks + [tC], [tAcc[1]], lambda: nc.vector.tensor_tensor(out=t2[64:96, :], in0=bb.ap[64:96, :], in1=ss[64:96, cols], op=ALU.mult))
                S.op("dve", [tAcc[0], tAcc[1]], [tKpe], lambda: nc.vector.tensor_tensor(out=kpe[64:96, cols], in0=t1[64:96, :], in1=t2[64:96, :], op=ALU.add))
                for j in range(4):
                    bz = ps1()
                    proj_mm(bz, 128, lambda k, j=j: win[:, k, 416 + j * 128:416 + (j + 1) * 128], tg, 1)
                    S.op("act", bz.toks, [tZ], lambda j=j, bz=bz: nc.scalar.activation(out=zs[:, j, cols], in_=bz.ap, func=AF.Silu))
            for c in range(8):
                if c < 4:
                    dst, tdst = xsT[:, c, :], tXs
                elif c < 6:
                    dst, tdst = BT[:, c - 4, :], tB
                else:
                    dst, tdst = CT[:, c - 6, :], tCt
                S.op("dve", [], [tPre[0]], lambda: nc.vector.memset(pre[0][:, 0:3], 0.0))
                for tg in range(NG):
                    p_, a_ = pre[tg % 2], acc[tg % 2]
                    tp, ta = tPre[tg % 2], tAcc[tg % 2]
                    bx = ps1()
                    proj_mm(bx, 128, lambda k, c=c: win[:, k, 928 + c * 128:928 + (c + 1) * 128], tg, 2 if c < 4 else 3)
                    S.op("act", bx.toks, [tp], lambda: nc.scalar.copy(out=p_[:, 3:515], in_=bx.ap))
                    if tg < NG - 1:
                        S.op("act", [tp], [tPre[(tg + 1) % 2]], lambda: nc.scalar.copy(out=pre[(tg + 1) % 2][:, 0:3], in_=p_[:, 512:515]))
                    S.op("dve", [tp, tC], [ta], lambda: nc.vector.tensor_scalar_mul(out=a_, in0=p_[:, 0:512], scalar1=convw[:, c * 4:c * 4 + 1]))
                    for j in range(1, 4):
                        S.op("dve", [tp, tC], [ta], lambda j=j: nc.vector.scalar_tensor_tensor(
                            out=a_, in0=p_[:, j:j + 512], scalar=convw[:, c * 4 + j:c * 4 + j + 1], in1=a_, op0=ALU.mult, op1=ALU.add))
                    S.op("act", [ta, tC], [tdst], lambda: nc.scalar.activation(
                        out=dst[:, tg * 512:(tg + 1) * 512], in_=a_, func=AF.Silu, bias=convb[:, c:c + 1]))
            bd = ps1()
            for tt in range(NT):
                S.op("pe", [tXT[tt], tWinL[3]], bd.toks, lambda tt=tt: [nc.tensor.matmul(
                    bd.ap[:, tt * 8:(tt + 1) * 8], actT[:, k, tt * 128:(tt + 1) * 128], win[:, k, 1952:1960],
                    start=(k == 0), stop=(k == 7)) for k in range(8)])
            S.op("dve", bd.toks + [tC], [tDt], lambda: nc.vector.tensor_tensor(out=dt_tok[:], in0=bd.ap[:, 0:128], in1=dtb[:], op=ALU.add))
            S.op("act", [tDt], [tDt], lambda: nc.scalar.activation(out=dt_tok[:], in_=dt_tok[:], func=AF.Exp))
            S.op("act", [tDt], [tDt], lambda: nc.scalar.activation(out=dt_tok[:], in_=dt_tok[:], func=AF.Ln, bias=1.0))
            if stop_after == "P2":
                dump("cqT", cqT[:, 0, :], [tCq])
                dump("ckvT", ckvT, [tCkv])
                dump("kpe", kpe, [tKpe])
                dump("zs", zs[:, 0, :], [tZ])
                dump("xsT", xsT[:, 0, :], [tXs])
                dump("BT", BT[:, 0, :], [tB])
                dump("dt", dt_tok[:], [tDt])
                break
            S.barrier(engines=("act", "dve", "pool", "sp"))
            qT = [A[:, i * 2048:(i + 1) * 2048] for i in range(2)]
            kT = [A[:, (2 + i) * 2048:(3 + i) * 2048] for i in range(2)]
            Vaug = [v3(A[:, (4 + i) * 2048:(5 + i) * 2048], 128) for i in range(2)]
            PTb = [A[:, 12288 + i * 512:12288 + (i + 1) * 512] for i in range(3)]
            rdb = A[:, 13824:14848].bitcast(F32)
            t1 = [Wf(0, 1024), A[:, 14848:15872].bitcast(F32)]
            t2 = [Wf(1024, 2048), Wf(13568, 14592)]
            tQ, tK, tV, tPT = [T(), T()], [T(), T()], [T(), T()], [T(), T(), T()]
            tT1, tT2, tRd = [T(), T()], [T(), T()], T()
            tCat = [T() for _ in range(8)]
            S.op("dve", [], [tV[0]], lambda: nc.vector.memset(Vaug[0][:, :, 64:128], 1.0))
            S.op("dve", [], [tV[1]], lambda: nc.vector.memset(Vaug[1][:, :, 0:64], 1.0))
            if s == 0 and stop_after is None:
                ztile = W[:, 14592:15616]
                tZ = T()
                S.op("dve", [], [tZ], lambda: nc.vector.memset(ztile, 0.0))
                for j in range(NTI * TB):
                    S.dma("sp", out=XS[j * 128:(j + 1) * 128, :], in_=ztile, reads=[tZ])
            SCALE = 96.0 ** -0.5
            pstate_pt = {"i": 0}

            def qkv_head(h):
                hp = h % 2
                q_h, k_h, V_h = qT[hp], kT[hp], Vaug[hp]
                tq, tk, tv = tQ[hp], tK[hp], tV[hp]
                for tg in range(NG):
                    cols = slice(tg * 512, (tg + 1) * 512)
                    S.op("dve", [tKpe], [tk], lambda: nc.vector.tensor_copy(out=k_h[64:96, cols], in_=kpe[64:96, cols]))
                    yield
                    ba, bb = ps1(), ps1()
                    S.op("pe", [tWs, tCq], ba.toks, lambda: [nc.tensor.matmul(
                        ba.ap[0:96, :], wq_s[:, r, h * 96:(h + 1) * 96], cqT[:, r, cols], start=(r == 0), stop=(r == 1)) for r in range(2)])
                    S.op("pe", [tWs, tCq], bb.toks, lambda: [nc.tensor.matmul(
                        bb.ap[0:96, :], wq_sw[:, r, h * 96:(h + 1) * 96], cqT[:, r, cols], start=(r == 0), stop=(r == 1)) for r in range(2)])
                    bk = ps1()
                    S.op("pe", [tWs, tCkv], bk.toks, lambda: nc.tensor.matmul(
                        bk.ap[0:64, :], wkv_s[:, h * 128:h * 128 + 64], ckvT[:, cols], start=True, stop=True))
                    tt1, tt2 = tT1[tg % 2], tT2[tg % 2]
                    u1, u2 = t1[tg % 2], t2[tg % 2]
                    S.op("dve", ba.toks + [tC], [tt1], lambda: nc.vector.tensor_tensor(out=u1[0:96, :], in0=ba.ap[0:96, :], in1=cc[0:96, cols], op=ALU.mult))
                    yield
                    S.op("dve", bb.toks + [tC], [tt2], lambda: nc.vector.tensor_tensor(out=u2[0:96, :], in0=bb.ap[0:96, :], in1=ss[0:96, cols], op=ALU.mult))
                    yield
                    S.op("dve", [tt1, tt2], [tq], lambda: nc.vector.tensor_tensor(out=q_h[0:96, cols], in0=u1[0:96, :], in1=u2[0:96, :], op=ALU.add))
                    yield
                    S.op("dve", bk.toks, [tk], lambda: nc.vector.tensor_copy(out=k_h[0:64, cols], in_=bk.ap[0:64, :]))
                    yield
                vo = 0 if hp == 0 else 64
                for half in range(2):
                    bv = ps1()
                    S.op("pe", [tWs, tCkv], bv.toks, lambda: [nc.tensor.matmul(
                        bv.ap[:, j * 64:(j + 1) * 64], ckvT[:, (half * 8 + j) * 128:(half * 8 + j + 1) * 128],
                        wkv_s[:, h * 128 + 64:(h + 1) * 128], start=True, stop=True) for j in range(8)])
                    S.op("dve", bv.toks, [tv], lambda: nc.vector.tensor_copy(out=V_h[:, half * 8:(half + 1) * 8, vo:vo + 64], in_=v3(bv.ap, 64)))
                    yield

            def attn_head(h):
                hp = h % 2
                q_h, k_h, V_h = qT[hp], kT[hp], Vaug[hp]
                tq, tk, tv = tQ[hp], tK[hp], tV[hp]
                orow = slice(0, 64) if hp == 0 else slice(64, 128)
                drow = slice(64, 128) if hp == 0 else slice(0, 64)
                items = [(g, kj) for g in range(NG) for kj in range(4 * g + 4)]
                sbank = {}
                bo_of = {}

                def emit_S(i):
                    g, kj = items[i]
                    c0 = max(0, kj - 4 * g) * 128
                    if g not in bo_of:
                        bo_of[g] = ps1(hold=True)
                    b_ = ps1()
                    sbank[i] = b_
                    S.op("pe", [tq, tk], b_.toks, lambda: nc.tensor.matmul(
                        b_.ap[:, c0:512], k_h[0:96, kj * 128:(kj + 1) * 128], q_h[0:96, g * 512 + c0:(g + 1) * 512], start=True, stop=True))

                LOOK = 2
                for i in range(min(LOOK, len(items))):
                    emit_S(i)
                for i, (g, kj) in enumerate(items):
                    nk = 4 * g + 4
                    c0 = max(0, kj - 4 * g) * 128
                    b_ = sbank.pop(i)
                    pt, tpt = PTb[pstate_pt["i"] % 3], tPT[pstate_pt["i"] % 3]
                    pstate_pt["i"] += 1
                    S.op("act", b_.toks, [tpt], lambda: nc.scalar.activation(out=pt[:, c0:512], in_=b_.ap[:, c0:512], func=AF.Exp, scale=SCALE))
                    if kj >= 4 * g:
                        S.op("dve", [], [tpt], lambda: nc.vector.memset(pt[64:128, c0:c0 + 64], 0.0))
                    if i + LOOK < len(items):
                        emit_S(i + LOOK)
                    bo = bo_of[g]
                    S.op("pe", [tpt, tv], bo.toks, lambda: nc.tensor.matmul(
                        bo.ap[:, c0:512], V_h[:, kj, :], pt[:, c0:512], start=(kj == 0), stop=(kj == nk - 1)))
                    if kj == nk - 1:
                        S.op("dve", bo.toks, [tRd], lambda: nc.vector.reciprocal(out=rdb[drow, :], in_=bo.ap[drow, :]))
                        S.op("dve", bo.toks + [tRd], [tCat[h // 2]], lambda: nc.vector.tensor_tensor(
                            out=catT[orow, h // 2, g * 512:(g + 1) * 512], in0=bo.ap[orow, :], in1=rdb[drow, :], op=ALU.mult))
                        psrel(bo)
                    yield

            def p4_gen():
                for _ in qkv_head(0):
                    pass
                for h in range(8):
                    nxt = qkv_head(h + 1) if h + 1 < 8 else None
                    for n_, _ in enumerate(attn_head(h)):
                        if nxt is not None and n_ % 2 == 1:
                            if next(nxt, "done") == "done":
                                nxt = None
                        yield
                    if nxt is not None:
                        for _ in nxt:
                            pass

            if stop_after == "P4":
                for _ in p4_gen():
                    pass
                dump("cat_attn", catT[:, 0:4, :], tCat)
                break
            def ssd_set(i):
                b0 = 2048 if i == 0 else 15616
                so = 16 if i == 0 else 64
                d = dict(
                    adt=small[:, so:so + 8], acs_sb=small[:, so + 8:so + 16], dte=small[:, so + 16:so + 24],
                    cdk=small[:, so + 24:so + 32], dd=small[:, so + 32:so + 40], nacs=small[:, so + 40:so + 48],
                    R=Wf(b0, b0 + 2048), E=Wf(b0 + 2048, b0 + 3072), DEC=Wf(b0 + 3072, b0 + 4096),
                    Mfin=[W[:, b0 + 4096:b0 + 4608], W[:, b0 + 4608:b0 + 5120]],
                    CexpT=[W[:, b0 + 5120:b0 + 5632], W[:, b0 + 5632:b0 + 6144]],
                    xdt=W[:, b0 + 6144:b0 + 6656], xdte=W[:, b0 + 6656:b0 + 7168], Btok=W[:, b0 + 7168:b0 + 7424],
                    cbm=[Wf(b0 + 7424, b0 + 7680), Wf(b0 + 7680, b0 + 7936)],
                    tAdt=T(), tSm2=T(), tR=T(), tE=T(), tDec=T(), tXdt=T(), tXdte=T(), tBtok=T(), tCbm=T(),
                    tMf=[T(), T()], tCe=[T(), T()])
                return d

            SB = [ssd_set(0), ssd_set(1)]
            prev_f = Wf(9984, 11008)
            prev_bf = W[:, 11008:11520]
            yz = Wf(11520, 12544)
            sq = W[:, 12544:13056]
            rstd_g = Wf(13056, 13312)
            yv = Wf(13312, 13568)
            tPrevF, tPrevB, tYv, tYz, tSq2, tRg = T(), T(), T(), T(), T(), T()
            S.op("dve", [], [tWs], lambda: nc.vector.memset(small[:, 250:251], 0.0))
            S.op("act", [], [tWs], lambda: nc.scalar.copy(out=small[:, 251:252], in_=small[:, 250:251]))
            S.op("dve", [], [tPrevF], lambda: nc.vector.memset(prev_f, 0.0))
            S.op("dve", [], [tPrevB], lambda: nc.vector.memset(prev_bf, 0.0))

            def ssd_stage1(c):
                B_ = SB[c % 2]
                adt, acs_sb, dte, cdk, dd, nacs = B_["adt"], B_["acs_sb"], B_["dte"], B_["cdk"], B_["dd"], B_["nacs"]
                R, E, DEC, Mfin, CexpT, xdt, xdte, Btok, cbm = (B_[k] for k in ("R", "E", "DEC", "Mfin", "CexpT", "xdt", "xdte", "Btok", "cbm"))
                tAdt, tSm2, tR, tE, tDec, tXdt, tXdte, tBtok, tCbm, tMf, tCe = (B_[k] for k in (
                    "tAdt", "tSm2", "tR", "tE", "tDec", "tXdt", "tXdte", "tBtok", "tCbm", "tMf", "tCe"))
                tsl = slice(c * 128, (c + 1) * 128)
                dtc = dt_tok[:, c * 8:(c + 1) * 8]
                S.op("dve", [tDt, tC], [tAdt], lambda: nc.vector.tensor_tensor(out=adt, in0=dtc, in1=a_bc[:], op=ALU.mult))
                bx = ps1()
                S.op("pe", [tXs, tC], bx.toks, lambda: [nc.tensor.transpose(
                    bx.bf[:, j * 128:(j + 1) * 128], xsT[:, j, tsl], ident_bf[:]) for j in range(4)])
                S.op("dve", bx.toks + [tDt], [tXdt], lambda: nc.vector.tensor_tensor(
                    out=v3(xdt, 64), in0=v3(bx.bf[:, 0:512], 64), in1=dtc.unsqueeze(2).to_broadcast([128, 8, 64]), op=ALU.mult))
                bb_ = ps1()
                S.op("pe", [tB, tC], bb_.toks, lambda: [nc.tensor.transpose(
                    bb_.bf[:, g * 128:(g + 1) * 128], BT[:, g, tsl], ident_bf[:]) for g in range(2)])
                S.op("act", bb_.toks, [tBtok], lambda: nc.scalar.copy(out=Btok, in_=bb_.bf[:, 0:256]))
                yield
                ba_ = ps1()
                S.op("pe", [tAdt, tC], ba_.toks, lambda: [
                    nc.tensor.matmul(ba_.ap[:, 0:8], tri_f[:], adt, start=True, stop=True),
                    nc.tensor.matmul(ba_.ap[:, 8:16], ones_f[:], adt, start=True, stop=True)])
                S.op("act", ba_.toks, [tSm2], lambda: nc.scalar.copy(out=acs_sb, in_=ba_.ap[:, 0:8]))
                S.op("dve", ba_.toks + [tSm2], [tSm2], lambda: nc.vector.tensor_tensor(out=dd, in0=ba_.ap[:, 8:16], in1=acs_sb, op=ALU.subtract))
                S.op("act", [tSm2], [tSm2], lambda: nc.scalar.activation(out=dte, in_=dd, func=AF.Exp))
                S.op("act", ba_.toks, [tSm2], lambda: nc.scalar.activation(out=cdk, in_=ba_.ap[:, 8:16], func=AF.Exp))
                S.op("dve", [tSm2], [tSm2], lambda: nc.vector.tensor_scalar_mul(out=nacs, in0=acs_sb, scalar1=-1.0))
                yield
                S.op("dve", [tXdt, tSm2], [tXdte], lambda: nc.vector.tensor_tensor(
                    out=v3(xdte, 64), in0=v3(xdt, 64), in1=dte.unsqueeze(2).to_broadcast([128, 8, 64]), op=ALU.mult))
                S.op("dve", [tAdt, tC], [tR], lambda: nc.vector.tensor_tensor(
                    out=v3(R, 128), in0=tri_f[:].unsqueeze(1).to_broadcast([128, 8, 128]),
                    in1=adt.unsqueeze(2).to_broadcast([128, 8, 128]), op=ALU.mult))
                yield
                for g in range(2):
                    bc = ps1()
                    S.op("pe", [tB, tCt], bc.toks, lambda: nc.tensor.matmul(bc.ap[:, 0:128], BT[:, g, tsl], CT[:, g, tsl], start=True, stop=True))
                    S.op("act", bc.toks, [tCbm], lambda: nc.scalar.copy(out=cbm[g], in_=bc.ap[:, 0:128]))
                    bA = ps1()
                    S.op("pe", [tR, tC], bA.toks, lambda: nc.tensor.matmul(bA.ap, ones_f[:], R[:, g * 512:(g + 1) * 512], start=True, stop=True))
                    yield
                    S.op("act", bA.toks, [tDec], lambda: nc.scalar.activation(out=DEC, in_=bA.ap, func=AF.Exp))
                    for hh in range(4):
                        S.op("dve", bA.toks + [tC, tDec, tSm2], [tE], lambda: nc.vector.scalar_tensor_tensor(
                            out=E[:, hh * 128:(hh + 1) * 128], in0=bA.ap[:, hh * 128:(hh + 1) * 128],
                            scalar=nacs[:, g * 4 + hh:g * 4 + hh + 1], in1=negmask[:, 0:128], op0=ALU.add, op1=ALU.add))
                    S.op("act", [tE], [tE], lambda: nc.scalar.activation(out=E, in_=E, func=AF.Exp))
                    yield
                    S.op("dve", [tE, tCbm], [tMf[g]], lambda: nc.vector.tensor_tensor(
                        out=v3(Mfin[g], 128), in0=v3(E, 128), in1=cbm[g].unsqueeze(1).to_broadcast([128, 4, 128]), op=ALU.mult))
                    S.op("dve", [tDec, tCt], [tCe[g]], lambda: nc.vector.tensor_tensor(
                        out=v3(CexpT[g], 128), in0=v3(DEC, 128), in1=CT[:, g, tsl].unsqueeze(1).to_broadcast([128, 4, 128]), op=ALU.mult))
                    yield

            def ssd_stage2(c):
                B_ = SB[c % 2]
                cdk, Mfin, CexpT, xdt, xdte, Btok = (B_[k] for k in ("cdk", "Mfin", "CexpT", "xdt", "xdte", "Btok"))
                tSm2, tXdt, tXdte, tBtok, tMf, tCe = (B_[k] for k in ("tSm2", "tXdt", "tXdte", "tBtok", "tMf", "tCe"))
                tsl = slice(c * 128, (c + 1) * 128)
                for pair in range(4):
                    g = pair // 2
                    by = ps1()
                    for hp in range(2):
                        h = pair * 2 + hp
                        hh = h % 4
                        rows = slice(hp * 64, hp * 64 + 64)
                        tp_ = None if hp == 0 else (0, 64)
                        S.op("pe", [tXdt, tMf[g], tPrevB, tCe[g]], by.toks, lambda: [
                            nc.tensor.matmul(by.ap[rows, 0:128], xdt[:, h * 64:(h + 1) * 64], Mfin[g][:, hh * 128:(hh + 1) * 128],
                                             start=True, stop=False, tile_position=tp_),
                            nc.tensor.matmul(by.ap[rows, 0:128], prev_bf[:, h * 64:(h + 1) * 64], CexpT[g][:, hh * 128:(hh + 1) * 128],
                                             start=False, stop=True, tile_position=tp_)])
                    S.op("dve", by.toks + [tXs, tC], [tYv], lambda: nc.vector.scalar_tensor_tensor(
                        out=yv, in0=xsT[:, pair, tsl], scalar=dcol[:, pair:pair + 1], in1=by.ap[:, 0:128], op0=ALU.mult, op1=ALU.add))
                    S.op("dve", [tYv, tZ], [tYz], lambda: nc.vector.tensor_tensor(
                        out=yz[:, pair * 128:(pair + 1) * 128], in0=yv, in1=zs[:, pair, tsl], op=ALU.mult))
                    S.op("act", [tYz], [tSq2], lambda: nc.scalar.activation(
                        out=sq[:, pair * 128:(pair + 1) * 128], in_=yz[:, pair * 128:(pair + 1) * 128], func=AF.Square))
                    yield
                    if pair % 2 == 1:
                        bn_ = ps1()
                        S.op("pe", [tSq2, tC], bn_.toks, lambda: [nc.tensor.matmul(
                            bn_.ap[:, 0:128], ones_bf[:], sq[:, (pair - 1 + i) * 128:(pair + i) * 128], start=(i == 0), stop=(i == 1)) for i in range(2)])
                        rstd_from(rstd_g, bn_.ap[:, 0:128], 1.0 / 256, bn_.toks, [tRg])
                        for pp in (pair - 1, pair):
                            S.op("dve", [tYz, tRg, tC], [tCat[4 + pp]], lambda: nc.vector.scalar_tensor_tensor(
                                out=catT[:, 4 + pp, tsl], in0=yz[:, pp * 128:(pp + 1) * 128], scalar=ncol[:, pp:pp + 1], in1=rstd_g,
                                op0=ALU.mult, op1=ALU.mult))
                        yield
                bst = ps1()
                S.op("pe", [tBtok, tXdte], bst.toks, lambda: [nc.tensor.matmul(
                    bst.ap[:, g * 256:(g + 1) * 256], Btok[:, g * 128:(g + 1) * 128], xdte[:, g * 256:(g + 1) * 256],
                    start=True, stop=True) for g in range(2)])
                S.op("dve", [tSm2], [tPrevF], lambda: nc.vector.tensor_tensor(
                    out=v3(prev_f, 64), in0=v3(prev_f, 64), in1=cdk.unsqueeze(2).to_broadcast([128, 8, 64]), op=ALU.mult))
                S.op("dve", bst.toks, [tPrevF], lambda: nc.vector.tensor_tensor(out=prev_f, in0=prev_f, in1=bst.ap, op=ALU.add))
                S.op("act", [tPrevF], [tPrevB], lambda: nc.scalar.copy(out=prev_bf, in_=prev_f))
                yield

            def p5_gen():
                yield from ssd_stage1(0)
                for c in range(NT):
                    ga = ssd_stage1(c + 1) if c + 1 < NT else iter(())
                    gb = ssd_stage2(c)
                    a_alive = b_alive = True
                    while a_alive or b_alive:
                        if a_alive and next(ga, "done") == "done":
                            a_alive = False
                        if b_alive and next(gb, "done") == "done":
                            b_alive = False
                        yield

            g4, g5 = p4_gen(), p5_gen()
            alive = {"4": True, "5": True}

            def step(gen, key, n):
                for _ in range(n):
                    if alive[key]:
                        try:
                            next(gen)
                        except StopIteration:
                            alive[key] = False

            while alive["4"]:
                step(g4, "4", 64)
            while alive["5"]:
                step(g5, "5", 64)

            if stop_after == "P5":
                dump("cat_ssd", catT[:, 4:8, :], tCat)
                break
            S.barrier(engines=("act", "dve", "pool", "sp"))
            wout = v3(W[:, 0:8192], 1024)
            tWoL = [T() for _ in range(8)]
            for k in range(8):
                S.dma("sp", out=wout[:, k, :], in_=DWB["wout"][k * 128:(k + 1) * 128, :], reads=[tDW["wout"][k]], writes=[tWoL[k]])
            load_ln(0)
            xres = [Wf(8192, 10240), Wf(10240, 12288)]
            hbW = [W[:, 12288:13312], W[:, 13312:14336]]
            tXr, tHb = [T(), T()], [T(), T()]
            tH = [T() for _ in range(NT)]
            tHT = [T() for _ in range(NT)]
            def p6_mm(tt):
                tok = slice(tt * 128, (tt + 1) * 128)
                xr = xres[tt % 2]
                S.dma("sp", out=xr, in_=x[s, tok, :], writes=[tXr[tt % 2]])
                bm = ps2()
                S.op("pe", tCat + tWoL, bm.toks, lambda: [nc.tensor.matmul(
                    bm.ap[:, hf * 512:(hf + 1) * 512], catT[:, c, tok], wout[:, c, hf * 512:(hf + 1) * 512],
                    start=(c == 0), stop=(c == 7)) for hf in range(2) for c in range(8)])
                return bm

            bms = {0: p6_mm(0)}
            for tt in range(NT):
                if tt + 1 < NT:
                    bms[tt + 1] = p6_mm(tt + 1)
                bm = bms.pop(tt)
                xr = xres[tt % 2]
                hap = hview[:, tt, :]
                S.op("dve", bm.toks + [tXr[tt % 2]], [tH[tt]], lambda: nc.vector.scalar_tensor_tensor(
                    out=hap, in0=xr, scalar=ALPHA, in1=bm.ap, op0=ALU.mult, op1=ALU.add))
                layer_norm_tile(hap, tH[tt], 0, s)
                S.op("act", [tH[tt]], [tHb[tt % 2]], lambda: nc.scalar.copy(out=hbW[tt % 2], in_=hap))
                transpose_to(hbW[tt % 2], tHb[tt % 2], actT, tt, tHT[tt])
            if stop_after == "P6":
                dump("h1", hview, tH)
                break
            S.barrier(engines=("act", "dve", "pool", "sp"))
            xo = v3(K[:, 0:4096], 512)
            KxT = v3(K[:, 4096:6144], 256)
            Vx = v3(K[:, 6144:8192], 1024)
            memT = v3(K[:, 8192:10240], 256)
            membf = v3(K[:, 10240:12288], 1024)
            hbK = [K[:, 12288:13312], K[:, 13312:14336]]
            QxT = [K[:, 14336:14848], K[:, 14848:15360]]
            PTx = [K[:, 15360:15872], K[:, 15872:16384]]
            wk = v3(W[:, 0:8192], 1024)
            wv = v3(W[:, 8192:16384], 1024)
            wq = v3(W[:, 16384:24576], 1024)
            tMem, tMemT, tKx, tVx, tXo, tRdx = T(), T(), T(), T(), T(), T()
            tWk, tWv, tWq = [T() for _ in range(8)], [T() for _ in range(8)], [T() for _ in range(8)]
            tQx, tPx, tRt = [T(), T()], [T(), T()], T()
            for k in range(8):
                S.dma("sp", out=wk[:, k, :], in_=DWB["xwk"][k * 128:(k + 1) * 128, :], reads=[tDW["xwk"][k]], writes=[tWk[k]])
            S.dma("pool", out=membf, in_=mem[s].rearrange("(t p) d -> p t d", p=128), writes=[tMem])
            for k in range(8):
                S.dma("sp", out=wv[:, k, :], in_=DWB["xwv"][k * 128:(k + 1) * 128, :], reads=[tDW["xwv"][k]], writes=[tWv[k]])
            for k in range(8):
                S.dma("sp", out=wq[:, k, :], in_=DWB["xwq"][k * 128:(k + 1) * 128, :], reads=[tDW["xwq"][k]], writes=[tWq[k]])
            load_ln(1)
            for mt in range(2):
                pb = ps1()
                S.op("pe", [tMem, tC], pb.toks, lambda: [nc.tensor.transpose(
                    pb.bf[:, k * 128:(k + 1) * 128], membf[:, mt, k * 128:(k + 1) * 128], ident_bf[:]) for k in range(8)])
                S.op("act", pb.toks, [tMemT], lambda: nc.scalar.copy(out=memT[:, :, mt * 128:(mt + 1) * 128], in_=v3(pb.bf[:, 0:1024], 128)))
            for c in range(8):
                b_ = ps1()
                S.op("pe", [tMemT] + tWk, b_.toks, lambda: [nc.tensor.matmul(
                    b_.ap[:, 0:256], wk[:, k, c * 128:(c + 1) * 128], memT[:, k, :], start=(k == 0), stop=(k == 7)) for k in range(8)])
                S.op("act", b_.toks, [tKx], lambda: nc.scalar.copy(out=KxT[:, c, :], in_=b_.ap[:, 0:256]))
            for mt in range(2):
                b2 = ps2()
                S.op("pe", [tMemT] + tWv, b2.toks, lambda: [nc.tensor.matmul(
                    b2.ap[:, hf * 512:(hf + 1) * 512], memT[:, k, mt * 128:(mt + 1) * 128], wv[:, k, hf * 512:(hf + 1) * 512],
                    start=(k == 0), stop=(k == 7)) for hf in range(2) for k in range(8)])
                S.op("act", b2.toks, [tVx], lambda: nc.scalar.copy(out=Vx[:, mt, :], in_=b2.ap))
            wo = wk
            for k in range(8):
                S.dma("sp", out=wo[:, k, :], in_=DWB["xwo"][k * 128:(k + 1) * 128, :], reads=[tDW["xwo"][k]], writes=[tWk[k]])
            XSC = 256.0 ** -0.5
            lg = small[:, 64:100]
            gmax, ngmax, gsum, ggate = small[:, 100:101], small[:, 101:102], small[:, 102:103], small[:, 103:104]
            gone, ge, pen = small[:, 104:108], small[:, 108:112], small[:, 112:116]
            me, one1, me2, one2 = small[:, 116:148], small[:, 148:180], small[:, 180:212], small[:, 212:244]
            m1, m2, d21, e21, g1, g2 = (small[:, 244 + i:245 + i] for i in range(6))
            QxTg = [[K[:, 14336 + (i * 2 + dc) * 512:14336 + (i * 2 + dc + 1) * 512] for dc in range(2)] for i in range(2)]
            PTxg = [[K[:, 10240 + (i * 2 + mt) * 512:10240 + (i * 2 + mt + 1) * 512] for mt in range(2)] for i in range(2)]
            tQxg = [[T(), T()], [T(), T()]]
            tPxg = [[T(), T()], [T(), T()]]
            tRdxg = [T(), T()]
            units = [(tg, h) for tg in range(NG) for h in range(4)]

            def xa_A(i):
                tg, h = units[i]
                cols = slice(tg * 512, (tg + 1) * 512)
                for dc in range(2):
                    c = h * 2 + dc
                    bq_ = ps1()
                    S.op("pe", tHT[tg * 4:tg * 4 + 4] + tWq, bq_.toks, lambda: [nc.tensor.matmul(
                        bq_.ap, wq[:, k, c * 128:(c + 1) * 128], actT[:, k, cols], start=(k == 0), stop=(k == 7)) for k in range(8)])
                    S.op("act", bq_.toks, [tQxg[i % 2][dc]], lambda: nc.scalar.copy(out=QxTg[i % 2][dc], in_=bq_.ap))

            def xa_B(i):
                tg, h = units[i]
                for mt in range(2):
                    bs_ = ps1()
                    S.op("pe", tQxg[i % 2] + [tKx, tMem], bs_.toks, lambda: [nc.tensor.matmul(
                        bs_.ap, KxT[:, h * 2 + dc, mt * 128:(mt + 1) * 128], QxTg[i % 2][dc], start=(dc == 0), stop=(dc == 1)) for dc in range(2)])
                    S.op("act", bs_.toks, [tPxg[i % 2][mt]], lambda: nc.scalar.activation(out=PTxg[i % 2][mt], in_=bs_.ap, func=AF.Exp, scale=XSC))

            def xa_C(i):
                tg, h = units[i]
                ptx = PTxg[i % 2]
                rd_ = rdx[:, (i % 2) * 512:(i % 2 + 1) * 512]
                bd_ = ps1()
                S.op("pe", tPxg[i % 2] + [tC], bd_.toks, lambda: [nc.tensor.matmul(
                    bd_.ap, ones_bf[:], ptx[mt], start=(mt == 0), stop=(mt == 1)) for mt in range(2)])
                S.op("dve", bd_.toks, [tRdxg[i % 2]], lambda: nc.vector.reciprocal(out=rd_, in_=bd_.ap))
                for dc in range(2):
                    c = h * 2 + dc
                    bo_ = ps1()
                    S.op("pe", tPxg[i % 2] + [tVx], bo_.toks, lambda: [nc.tensor.matmul(
                        bo_.ap, Vx[:, mt, c * 128:(c + 1) * 128], ptx[mt], start=(mt == 0), stop=(mt == 1)) for mt in range(2)])
                    S.op("dve", bo_.toks + [tRdxg[i % 2]], [tXo], lambda: nc.vector.tensor_tensor(out=xo[:, c, :], in0=bo_.ap, in1=rd_, op=ALU.mult))

            def xa_mm(tt):
                j = tt % 4
                bm = ps2()
                S.op("pe", [tXo] + tWk, bm.toks, lambda: [nc.tensor.matmul(
                    bm.ap[:, hf * 512:(hf + 1) * 512], xo[:, c, j * 128:(j + 1) * 128], wo[:, c, hf * 512:(hf + 1) * 512],
                    start=(c == 0), stop=(c == 7)) for hf in range(2) for c in range(8)])
                return bm

            V_ = nc.vector
            AXX_ = mybir.AxisListType.X
            tRtS = [T(), T()]

            def xa_tail(tt, bm, si):
                rb0 = 64 + si * 256
                lg = small[:, rb0:rb0 + 36]
                gmax, ngmax, gsum, ggate = (small[:, rb0 + 36 + i:rb0 + 37 + i] for i in range(4))
                gone, ge, pen = small[:, rb0 + 40:rb0 + 44], small[:, rb0 + 44:rb0 + 48], small[:, rb0 + 48:rb0 + 52]
                me, me2 = small[:, rb0 + 52:rb0 + 84], small[:, rb0 + 84:rb0 + 116]
                m1, m2, d21, e21, g1 = (small[:, rb0 + 116 + i:rb0 + 117 + i] for i in range(5))
                tRt = tRtS[si]
                rt = lambda f: S.op("dve", [tRt], [tRt], f)
                hap = hview[:, tt, :]
                tHt = tH[tt]
                gt = s * NT + tt
                S.op("dve", bm.toks, [tHt], lambda: nc.vector.scalar_tensor_tensor(
                    out=hap, in0=hap, scalar=ALPHA, in1=bm.ap, op0=ALU.mult, op1=ALU.add))
                j_ = lnstate["i"] % 4
                lnstate["i"] += 1
                tS = tLNS[j_]
                base = j_ * 16
                st, mv = lnst[:, base:base + 12], lnst[:, base + 12:base + 14]
                rs, nm = lnst[:, base + 14:base + 15], lnst[:, base + 15:base + 16]
                S.op("dve", [tHt], [tS], lambda: nc.vector.bn_stats(out=st[:, 0:6], in_=hap[:, 0:512]))
                S.op("dve", [tHt], [tS], lambda: nc.vector.bn_stats(out=st[:, 6:12], in_=hap[:, 512:1024]))
                S.op("dve", [], [tS], lambda: nc.vector.bn_aggr(out=mv, in_=st))
                yield
                rstd_from(rs, mv[:, 1:2], 1.0, [tS], [tS])
                yield
                S.op("dve", [tS], [tS], lambda: nc.vector.scalar_tensor_tensor(
                    out=nm, in0=mv[:, 0:1], scalar=-1.0, in1=rs, op0=ALU.mult, op1=ALU.mult))
                yield
                S.op("act", [tS], [tHt], lambda: nc.scalar.activation(out=hap, in_=hap, func=AF.Identity, scale=rs, bias=nm))
                yield
                S.op("dve", [tLN], [tHt], lambda: nc.vector.tensor_tensor(out=hap, in0=hap, in1=lng[:], op=ALU.mult))
                yield
                S.op("dve", [tLN], [tHt], lambda: nc.vector.tensor_tensor(out=hap, in0=hap, in1=lnb[:], op=ALU.add))
                yield
                S.op("act", [tHt], [tHb[tt % 2]], lambda: nc.scalar.copy(out=hbK[tt % 2], in_=hap))
                yield
                transpose_to(hbK[tt % 2], tHb[tt % 2], actT, tt, tHT[tt])
                yield
                bl = ps1()
                S.op("pe", [tHT[tt], tC], bl.toks, lambda: [nc.tensor.matmul(
                    bl.ap[:, 0:36], actT[:, k, tt * 128:(tt + 1) * 128], v3(rw[:], 36)[:, k, :], start=(k == 0), stop=(k == 7)) for k in range(8)])
                yield
                S.op("dve", bl.toks + [tC], [tRt], lambda: V_.tensor_tensor(out=lg, in0=bl.ap[:, 0:36], in1=rb[:], op=ALU.add))
                rt(lambda: V_.reduce_max(out=gmax, in_=lg[:, 0:4], axis=AXX_))
                rt(lambda: V_.tensor_scalar(out=gone, in0=lg[:, 0:4], scalar1=gmax, scalar2=None, op0=ALU.is_equal))
                rt(lambda: V_.tensor_scalar_mul(out=ngmax, in0=gmax, scalar1=-1.0))
                yield
                S.op("act", [tRt], [tRt], lambda: nc.scalar.activation(out=ge, in_=lg[:, 0:4], func=AF.Exp, bias=ngmax))
                yield
                rt(lambda: V_.reduce_sum(out=gsum, in_=ge, axis=AXX_))
                rt(lambda: V_.reciprocal(out=ggate, in_=gsum))
                rt(lambda: V_.tensor_scalar(out=pen, in0=gone, scalar1=-1.0, scalar2=1e9, op0=ALU.add, op1=ALU.mult))
                rt(lambda: V_.tensor_tensor(out=v3(me, 8), in0=v3(lg[:, 4:36], 8), in1=pen.unsqueeze(2).to_broadcast([128, 4, 8]), op=ALU.add))
                rt(lambda: V_.reduce_max(out=m1, in_=me, axis=AXX_))
                o1 = ONE[:, (gt * 2) * 32:(gt * 2 + 1) * 32]
                o2 = ONE[:, (gt * 2 + 1) * 32:(gt * 2 + 2) * 32]
                S.op("dve", [tRt], [tRt, tRk], lambda: V_.tensor_scalar(out=o1, in0=me, scalar1=m1, scalar2=None, op0=ALU.is_equal))
                rt(lambda: V_.scalar_tensor_tensor(out=me2, in0=o1, scalar=-1e9, in1=me, op0=ALU.mult, op1=ALU.add))
                rt(lambda: V_.reduce_max(out=m2, in_=me2, axis=AXX_))
                S.op("dve", [tRt], [tRt, tRk], lambda: V_.tensor_scalar(out=o2, in0=me2, scalar1=m2, scalar2=None, op0=ALU.is_equal))
                rt(lambda: V_.tensor_tensor(out=d21, in0=m2, in1=m1, op=ALU.subtract))
                yield
                S.op("act", [tRt], [tRt], lambda: nc.scalar.activation(out=e21, in_=d21, func=AF.Exp))
                yield
                rt(lambda: V_.tensor_scalar_add(out=g1, in0=e21, scalar1=1.0))
                rt(lambda: V_.reciprocal(out=g1, in_=g1))
                S.op("dve", [tRt], [tRt, tRk], lambda: V_.tensor_tensor(out=G12[:, gt * 2:gt * 2 + 1], in0=g1, in1=ggate, op=ALU.mult))
                S.op("dve", [tRt], [tRt, tRk], lambda: V_.tensor_tensor(out=G12[:, gt * 2 + 1:gt * 2 + 2], in0=G12[:, gt * 2:gt * 2 + 1], in1=e21, op=ALU.mult))
                bR = ps1()
                S.op("pe", [tRk, tC], bR.toks, lambda: [
                    nc.tensor.matmul(bR.ap[:, 0:64], su_bf[:], ONE[:, gt * 64:(gt + 1) * 64], start=True, stop=True),
                    nc.tensor.matmul(bR.ap[:, 64:128], ones_bf[:], ONE[:, gt * 64:(gt + 1) * 64], start=True, stop=True)])
                yield
                ta, tb = me, me2
                S.op("dve", bR.toks + [tRk, tRt], [tRt], lambda: V_.tensor_tensor(out=ta, in0=bR.ap[:, 0:32], in1=Crun[:], op=ALU.add))
                rt(lambda: V_.tensor_tensor(out=tb, in0=ta, in1=o1, op=ALU.mult))
                S.op("dve", [tRt], [tRt, tRk], lambda: V_.reduce_sum(out=RK[:, gt * 2:gt * 2 + 1], in_=tb, axis=AXX_))
                S.op("dve", bR.toks + [tRk, tRt], [tRt], lambda: V_.tensor_tensor(out=ta, in0=bR.ap[:, 32:64], in1=Crun[:], op=ALU.add))
                S.op("dve", bR.toks + [tRt], [tRt], lambda: V_.tensor_tensor(out=ta, in0=bR.ap[:, 64:96], in1=ta, op=ALU.add))
                rt(lambda: V_.tensor_tensor(out=tb, in0=ta, in1=o2, op=ALU.mult))
                S.op("dve", [tRt], [tRt, tRk], lambda: V_.reduce_sum(out=RK[:, gt * 2 + 1:gt * 2 + 2], in_=tb, axis=AXX_))
                S.op("dve", bR.toks + [tRt], [tRk], lambda: V_.tensor_tensor(out=Crun[:], in0=bR.ap[:, 64:96], in1=Crun[:], op=ALU.add))
                S.op("dve", bR.toks + [tRt], [tRk], lambda: V_.tensor_tensor(out=Crun[:], in0=bR.ap[:, 96:128], in1=Crun[:], op=ALU.add))
                S.dma("sp", out=XB[gt * 128:(gt + 1) * 128, :], in_=hbK[tt % 2], reads=[tHb[tt % 2]])
                S.dma("sp", out=H2D[gt * 128:(gt + 1) * 128, :], in_=hap, reads=[tHt])
                yield

            xa_A(0)
            for ui in range(len(units)):
                xa_B(ui)
                if ui + 1 < len(units):
                    xa_A(ui + 1)
                xa_C(ui)
                tg, h = units[ui]
                if h != 3:
                    continue
                for pr in ((0, 1), (2, 3)):
                    gens = []
                    for si, j in enumerate(pr):
                        tt = tg * 4 + j
                        gens.append(xa_tail(tt, xa_mm(tt), si))
                    alive_ = [True, True]
                    while any(alive_):
                        for gi in range(2):
                            if alive_[gi] and next(gens[gi], "done") == "done":
                                alive_[gi] = False
            if stop_after == "P7":
                dump("h2", hview, tH)
                break
            S.barrier(engines=("act", "dve", "pool", "sp"))
        if stop_after is None:
            S.barrier(full=True)
            V_ = nc.vector
            AXX = mybir.AxisListType.X
            tM = T()
            padc, pA, pB, basev = small[:, 116:148], small[:, 148:180], small[:, 180:212], small[:, 212:244]
            cmpb = H[:, 0:1024]
            mo = lambda f, extra=(): S.op("dve", [tM, tRk, tC] + list(extra), [tM], f)
            mo(lambda: V_.tensor_tensor(out=v3(cmpb, 32), in0=Crun[:].unsqueeze(2).to_broadcast([128, 32, 32]),
                                        in1=thr[:].unsqueeze(1).to_broadcast([128, 32, 32]), op=ALU.is_gt))
            mo(lambda: V_.reduce_sum(out=padc, in_=v3(cmpb, 32), axis=AXX))
            mo(lambda: V_.tensor_scalar_mul(out=padc, in0=padc, scalar1=float(TSZ)))
            cur, nxt = padc, pA
            for sft in (1, 2, 4, 8, 16):
                mo(lambda: V_.tensor_copy(out=nxt[:, 0:sft], in_=cur[:, 0:sft]))
                mo(lambda: V_.tensor_tensor(out=nxt[:, sft:32], in0=cur[:, sft:32], in1=cur[:, 0:32 - sft], op=ALU.add))
                cur, nxt = nxt, (pB if nxt is pA else pA)
            endv = cur
            mo(lambda: V_.tensor_tensor(out=basev, in0=endv, in1=padc, op=ALU.subtract))
            cmpt = H[:, 2048:2048 + NTI * 32]
            tef = mo_f[:, 0:NTI]
            widx = mo_i[:, 0:NTI]
            mo(lambda: V_.tensor_tensor(out=v3(cmpt, 32), in0=endv.unsqueeze(1).to_broadcast([128, NTI, 32]),
                                        in1=tstart[:, 0:NTI].unsqueeze(2).to_broadcast([128, NTI, 32]), op=ALU.is_le))
            mo(lambda: V_.reduce_sum(out=tef, in_=v3(cmpt, 32), axis=AXX))
            mo(lambda: V_.tensor_scalar(out=tef, in0=tef, scalar1=128.0, scalar2=pcol[:, 0:1], op0=ALU.mult, op1=ALU.add))
            mo(lambda: V_.tensor_copy(out=widx, in_=tef))
            tmp3 = H[:, 4096:4096 + GT * 64]
            posf = mo_f[:, 64:64 + GT * 2]
            posI = mo_i[:, 64:64 + GT * 2]
            mo(lambda: V_.tensor_tensor(out=v3(tmp3, 32), in0=v3(ONE[:], 32), in1=basev.unsqueeze(1).to_broadcast([128, GT * 2, 32]), op=ALU.mult))
            mo(lambda: V_.reduce_sum(out=posf, in_=v3(tmp3, 32), axis=AXX))
            mo(lambda: V_.tensor_tensor(out=posf, in0=posf, in1=RK[:], op=ALU.add))
            mo(lambda: V_.tensor_copy(out=posI, in_=posf))
            NDB = 8
            xbt = [A[:, i * 1024:(i + 1) * 1024] for i in range(NDB)]
            tXb = [T() for _ in range(NDB)]
            for gt in range(min(NDB, GT)):
                S.dma("sp", out=xbt[gt], in_=XB[gt * 128:(gt + 1) * 128, :], writes=[tXb[gt]])
            for gt in range(GT):
                for k in range(2):
                    S.dma("pool", out=XS[:, :], in_=xbt[gt % NDB], out_off=posI[:, gt * 2 + k:gt * 2 + k + 1], bound=NTI * TSZ - 1,
                          reads=[tXb[gt % NDB], tM])
                if gt + NDB < GT:
                    S.dma("sp", out=xbt[gt % NDB], in_=XB[(gt + NDB) * 128:(gt + NDB + 1) * 128, :], writes=[tXb[gt % NDB]])
            S.barrier()
            load_ln(2)
            xsb = [v3(A[:, 2048 + i * 4096:2048 + i * 4096 + TB * 1024], 1024) for i in range(2)]
            xst = [v3(K[:, 4096 + i * 4096:4096 + i * 4096 + 8 * TSZ], TSZ) for i in range(2)]
            Ssb = [Kf(0, 2 * TSZ), Kf(1024, 1024 + 2 * TSZ)]
            HD = [v3(K[:, 2048:2048 + 2 * TSZ], TSZ), v3(K[:, 3072:3072 + 2 * TSZ], TSZ)]
            Ysb = [K[:, 12288:13312], K[:, 13312:14336]]
            tXsb, tXst, tSs, tHD, tY = [T(), T()], [T(), T()], [T(), T()], [T(), T()], [T(), T()]
            NWB = 3
            Eb = [W[:, i * 6144:(i + 1) * 6144] for i in range(NWB)]
            tEw = [[T(), T(), T()] for _ in range(NWB)]

            def moe_prefetch_w(ti):
                eb = Eb[ti % NWB]
                for part, src in enumerate((WGB, WUB, WDB)):
                    S.dma("pool", out=eb[:, part * 2048:(part + 1) * 2048], in_=src[:, :], in_off=widx[:, ti:ti + 1], bound=NEXP * 128 - 1,
                          reads=[tM], writes=[tEw[ti % NWB][part]])

            def moe_prefetch_x(ti):
                S.dma("sp", out=xsb[ti % 2], in_=XS[ti * TSZ:(ti + 1) * TSZ, :].rearrange("(j p) d -> p j d", p=128), writes=[tXsb[ti % 2]])

            def moe_TR(ti):
                xs_, txs = xsb[ti % 2], tXsb[ti % 2]
                xt_, txt = xst[ti % 2], tXst[ti % 2]
                for j in range(TB):
                    pb = ps1()
                    S.op("pe", [txs, tC], pb.toks, lambda: [nc.tensor.transpose(
                        pb.bf[:, k * 128:(k + 1) * 128], xs_[:, j, k * 128:(k + 1) * 128], ident_bf[:]) for k in range(8)])
                    if j % 2 == 0:
                        S.op("act", pb.toks, [txt], lambda: nc.scalar.copy(out=xt_[:, :, j * 128:(j + 1) * 128], in_=v3(pb.bf[:, 0:1024], 128)))
                    else:
                        S.op("dve", pb.toks, [txt], lambda: nc.vector.tensor_copy(out=xt_[:, :, j * 128:(j + 1) * 128], in_=v3(pb.bf[:, 0:1024], 128)))

            def moe_GU(ti):
                eb = Eb[ti % NWB]
                tE3 = tEw[ti % NWB]
                wg = v3(eb[:, 0:2048], 256)
                wu = v3(eb[:, 2048:4096], 256)
                xt_, txt = xst[ti % 2], tXst[ti % 2]
                hd, thd = HD[ti % 2], tHD[ti % 2]
                for fc in range(2):
                    bg, bu = ps1(), ps1()
                    S.op("pe", [txt, tE3[0]], bg.toks, lambda: [nc.tensor.matmul(
                        bg.ap[:, 0:TSZ], wg[:, k, fc * 128:(fc + 1) * 128], xt_[:, k, :], start=(k == 0), stop=(k == 7)) for k in range(8)])
                    S.op("pe", [txt, tE3[1]], bu.toks, lambda: [nc.tensor.matmul(
                        bu.ap[:, 0:TSZ], wu[:, k, fc * 128:(fc + 1) * 128], xt_[:, k, :], start=(k == 0), stop=(k == 7)) for k in range(8)])
                    S.op("act", bg.toks, [tSs[fc]], lambda: nc.scalar.activation(out=Ssb[fc], in_=bg.ap[:, 0:TSZ], func=AF.Silu))
                    S.op("dve", bu.toks + [tSs[fc]], [thd], lambda: nc.vector.tensor_tensor(out=hd[:, fc, :], in0=Ssb[fc], in1=bu.ap[:, 0:TSZ], op=ALU.mult))

            def moe_D(ti):
                eb = Eb[ti % NWB]
                tE3 = tEw[ti % NWB]
                wd = v3(eb[:, 4096:6144], 1024)
                hd, thd = HD[ti % 2], tHD[ti % 2]
                for j in range(TB):
                    bd2 = ps2()
                    S.op("pe", [thd, tE3[2]], bd2.toks, lambda: [nc.tensor.matmul(
                        bd2.ap[:, hf * 512:(hf + 1) * 512], hd[:, fc, j * 128:(j + 1) * 128], wd[:, fc, hf * 512:(hf + 1) * 512],
                        start=(fc == 0), stop=(fc == 1)) for hf in range(2) for fc in range(2)])
                    yb, ty = Ysb[moe_state["yi"] % 2], tY[moe_state["yi"] % 2]
                    moe_state["yi"] += 1
                    S.op("act", bd2.toks, [ty], lambda: nc.scalar.copy(out=yb, in_=bd2.ap))
                    S.dma("sp", out=YS[ti * TSZ + j * 128:ti * TSZ + (j + 1) * 128, :], in_=yb, reads=[ty])

            moe_state = {"yi": 0}
            moe_prefetch_w(0)
            moe_prefetch_x(0)
            if NTI > 1:
                moe_prefetch_w(1)
                moe_prefetch_x(1)
            moe_TR(0)
            moe_GU(0)
            for ti in range(NTI):
                if ti + 2 < NTI:
                    moe_prefetch_w(ti + 2)
                if ti + 1 < NTI:
                    moe_TR(ti + 1)
                if ti + 2 < NTI:
                    moe_prefetch_x(ti + 2)
                moe_D(ti)
                if ti + 1 < NTI:
                    moe_GU(ti + 1)
            S.barrier()
            NYB = 4
            ybuf = [[H[:, i * 3072:i * 3072 + 512].bitcast(BF16), H[:, i * 3072 + 512:i * 3072 + 1024].bitcast(BF16),
                     H[:, i * 3072 + 1024:i * 3072 + 2048], H[:, i * 3072 + 2048:i * 3072 + 3072]] for i in range(NYB)]
            tYb = [[T(), T(), T(), T()] for _ in range(NYB)]

            def comb_prefetch(gt):
                y1, y2, ytmp, hh = ybuf[gt % NYB]
                t1_, t2_, tt_, th = tYb[gt % NYB]
                S.dma("pool", out=y1, in_=YS[:, :], in_off=posI[:, gt * 2:gt * 2 + 1], bound=NTI * TSZ - 1, reads=[tM], writes=[t1_])
                S.dma("pool", out=y2, in_=YS[:, :], in_off=posI[:, gt * 2 + 1:gt * 2 + 2], bound=NTI * TSZ - 1, reads=[tM], writes=[t2_])
                S.dma("sp", out=hh, in_=H2D[gt * 128:(gt + 1) * 128, :], writes=[th])

            def comb_gen(gt):
                y1, y2, ytmp, hh = ybuf[gt % NYB]
                t1_, t2_, tt_, th = tYb[gt % NYB]
                S.op("act", [t1_, tRk], [tt_], lambda: nc.scalar.activation(out=ytmp, in_=y1, func=AF.Copy, scale=G12[:, gt * 2:gt * 2 + 1]))
                yield
                S.op("dve", [t2_, tRk, tt_], [tt_], lambda: V_.scalar_tensor_tensor(
                    out=ytmp, in0=y2, scalar=G12[:, gt * 2 + 1:gt * 2 + 2], in1=ytmp, op0=ALU.mult, op1=ALU.add))
                S.op("dve", [tt_], [th], lambda: V_.scalar_tensor_tensor(out=hh, in0=hh, scalar=ALPHA, in1=ytmp, op0=ALU.mult, op1=ALU.add))
                j_ = lnstate["i"] % 4
                lnstate["i"] += 1
                tS = tLNS[j_]
                base = j_ * 16
                st, mv = lnst[:, base:base + 12], lnst[:, base + 12:base + 14]
                rs, nm = lnst[:, base + 14:base + 15], lnst[:, base + 15:base + 16]
                S.op("dve", [th], [tS], lambda: nc.vector.bn_stats(out=st[:, 0:6], in_=hh[:, 0:512]))
                S.op("dve", [th], [tS], lambda: nc.vector.bn_stats(out=st[:, 6:12], in_=hh[:, 512:1024]))
                S.op("dve", [], [tS], lambda: nc.vector.bn_aggr(out=mv, in_=st))
                yield
                rstd_from(rs, mv[:, 1:2], 1.0, [tS], [tS])
                yield
                S.op("dve", [tS], [tS], lambda: nc.vector.scalar_tensor_tensor(
                    out=nm, in0=mv[:, 0:1], scalar=-1.0, in1=rs, op0=ALU.mult, op1=ALU.mult))
                yield
                S.op("act", [tS], [th], lambda: nc.scalar.activation(out=hh, in_=hh, func=AF.Identity, scale=rs, bias=nm))
                yield
                S.op("dve", [tLN], [th], lambda: nc.vector.tensor_tensor(out=hh, in0=hh, in1=lng[:], op=ALU.mult))
                yield
                S.op("dve", [tLN], [th], lambda: nc.vector.tensor_tensor(out=hh, in0=hh, in1=lnb[:], op=ALU.add))
                sq_, tq_ = gt // NT, gt % NT
                S.dma("sp", out=out_d[sq_, tq_ * 128:(tq_ + 1) * 128, :], in_=hh, reads=[th])
                yield

            comb_prefetch(0)
            comb_prefetch(1)
            for g0 in range(0, GT, 2):
                for gn in (g0 + 2, g0 + 3):
                    if gn < GT:
                        comb_prefetch(gn)
                gens = [comb_gen(g0), comb_gen(g0 + 1)]
                alive_ = [True, True]
                while any(alive_):
                    for gi in range(2):
                        if alive_[gi] and next(gens[gi], "done") == "done":
                            alive_[gi] = False
        S.barrier(engines=("sp",), full=True)
    return nc


def _rope_tables():
    pos = np.arange(SEQ, dtype=np.float32)
    inv_freq = (np.float32(10000.0) ** (-(np.arange(0, 32, 2, dtype=np.float32)) / np.float32(32))).astype(np.float32)
    ang = (pos[:, None] * inv_freq[None, :]).astype(np.float32)
    cos = np.cos(ang).astype(np.float32)
    sin = np.sin(ang).astype(np.float32)
    cc = np.zeros((128, SEQ), np.float32)
    ss = np.zeros((128, SEQ), np.float32)
    cc[0:64] = 1.0
    cc[64:80] = cos.T
    cc[80:96] = cos.T
    ss[64:80] = -sin.T
    ss[80:96] = sin.T
    return cc, ss


def prep_shared(inp):
    f = np.float32
    g = lambda k: np.asarray(inp[k], dtype=f)[0]
    w_in = g("w_in")
    wkr = np.zeros((D, 96), f)
    wkr[:, 64:80] = w_in[:, 400:416]
    wkr[:, 80:96] = w_in[:, 384:400]
    wq = g("w_q_up")
    perm = np.arange(768)
    for h in range(8):
        for j in range(32):
            perm[h * 96 + 64 + j] = h * 96 + 64 + (j + 16) % 32
    wq_sw = wq[:, perm]
    convw = g("ssd_conv_w").reshape(4, 8, 128).transpose(2, 1, 0).reshape(128, 32)
    convb = g("ssd_conv_b").reshape(8, 128).T
    dtb = np.broadcast_to(np.tile(g("ssd_dt_bias"), 16)[None, :], (128, 128))
    alog = np.broadcast_to(g("ssd_a_log")[None, :], (128, 8))
    sd = g("ssd_d")
    dcol = np.stack([sd[pair * 2 + (np.arange(128) // 64)] for pair in range(4)], axis=1)
    ncol = g("ssd_norm").reshape(4, 128).T
    lnp = np.stack([np.broadcast_to(g(k)[None, :], (128, D)) for k in ("ln1_g", "ln1_b", "ln2_g", "ln2_b", "ln3_g", "ln3_b")])
    rw = np.concatenate([g("router_group_w"), g("router_expert_w")], axis=1)
    rb = np.broadcast_to(np.concatenate([g("router_group_b"), g("router_expert_b")])[None, :], (128, 36))
    tri = np.triu(np.ones((128, 128), f))
    nm = np.where(np.arange(128)[None, :] >= np.arange(128)[:, None], 0.0, -30000.0).astype(f)
    cc, ss = _rope_tables()
    su = np.triu(np.ones((128, 128), f), k=1)
    thr = np.broadcast_to((np.arange(32, dtype=f) * 384.0)[None, :], (128, 32))
    tstart = np.broadcast_to((np.arange(64, dtype=f) * 384.0)[None, :], (128, 64))
    pcol = np.arange(128, dtype=f).reshape(128, 1)
    sh = {
        "su": su, "thr": thr, "tstart": tstart, "pcol": pcol,
        "w_in": w_in, "wkr_sw": wkr, "wq": wq, "wq_sw": wq_sw, "qn": g("mla_q_norm").reshape(2, 128).T,
        "wkv": g("w_kv_up"), "kvn": g("mla_kv_norm").reshape(128, 1), "convw": convw, "convb": convb,
        "dtb": dtb, "alog": alog, "dcol": dcol, "ncol": ncol, "wout": g("w_out"), "xwq": g("xa_wq"),
        "xwk": g("xa_wk"), "xwv": g("xa_wv"), "xwo": g("xa_wo"), "lnp": lnp, "rw": rw, "rb": rb,
        "wg": g("expert_w_gate"), "wu": g("expert_w_up"), "wd": g("expert_w_down"),
        "ident": np.eye(128, dtype=f), "tri": tri, "negmask": np.tile(nm, (1, 4)), "cc": cc, "ss": ss,
    }
    return {k: np.ascontiguousarray(v, dtype=f) for k, v in sh.items()}


def kernel(**inputs):
    sh = prep_shared(inputs)
    x = np.asarray(inputs["x"], dtype=np.float32)
    mem = np.asarray(inputs["mem"], dtype=np.float32)
    nc = build(n_seq=2)
    in_maps = []
    for c in range(N_CORES):
        m = dict(sh)
        m["x"] = np.ascontiguousarray(x[2 * c:2 * c + 2])
        m["mem"] = np.ascontiguousarray(mem[2 * c:2 * c + 2])
        in_maps.append(m)
    res = run_bass_kernel_spmd(nc, in_maps, core_ids=list(range(N_CORES)))
    return np.concatenate([r["out"] for r in res.results], axis=0)
```

```python
import numpy as np
from contextlib import ExitStack
import concourse.bass as bass
import concourse.mybir as mybir
from concourse.bass_utils import run_bass_kernel_spmd

F32 = mybir.dt.float32
BF16 = mybir.dt.bfloat16
AF = mybir.ActivationFunctionType
ALU = mybir.AluOpType

N_CORES = 8
SEQ = 2048
D = 1024
NT = SEQ // 128
NG = SEQ // 512
ALPHA = 2.0 ** 0.25
EPS = 1e-5
NEXP = 32


class T:
    __slots__ = ("w", "r")

    def __init__(self):
        self.w = None
        self.r = {}


class Sched:
    def __init__(self, nc, es, n_dma=80):
        self.nc = nc
        self.eng = {"pe": nc.tensor, "act": nc.scalar, "dve": nc.vector, "pool": nc.gpsimd, "sp": nc.sync}
        self.sem = {k: es.enter_context(nc.semaphore("s_" + k)) for k in ("pe", "act", "dve", "pool")}
        self.cnt = {k: 0 for k in self.sem}
        self.dsem = [es.enter_context(nc.semaphore("d%d" % i)) for i in range(n_dma)]
        self.dcnt = [0] * n_dma
        n_cast = 16
        self.qslots = {"sp": list(range(0, (n_dma - n_cast) // 2)), "pool": list(range((n_dma - n_cast) // 2, n_dma - n_cast)),
                       "cast": list(range(n_dma - n_cast, n_dma))}
        self.qnext = {"sp": 0, "pool": 0, "cast": 0}
        self.seen = {}
        self.bregs = {}

    def _semof(self, key):
        return self.sem[key] if isinstance(key, str) else self.dsem[key]

    def _need(self, eng, reads, writes):
        need = {}

        def add(k, v):
            if k == eng:
                if eng == "pe":
                    return
                if self.cnt[eng] - v >= 4:
                    return
            if self.seen.get((eng, k), 0) >= v:
                return
            if need.get(k, 0) < v:
                need[k] = v

        for t in reads:
            if t.w is not None:
                add(*t.w)
        for t in writes:
            if t.w is not None:
                add(*t.w)
            for k, v in t.r.items():
                add(k, v)
        return need

    def _wait(self, eng, key, val):
        if self.seen.get((eng, key), 0) >= val:
            return
        self.eng[eng].wait_ge(self._semof(key), val)
        self.seen[(eng, key)] = val

    def _waits(self, eng, reads, writes):
        for k, v in self._need(eng, reads, writes).items():
            self._wait(eng, k, v)

    def _commit(self, ticket, reads, writes):
        k, v = ticket
        for t in reads:
            if t.r.get(k, 0) < v:
                t.r[k] = v
        for t in writes:
            t.w = ticket
            t.r = {}

    def op(self, eng, reads, writes, emit):
        need = self._need(eng, reads, writes)
        keys = list(need)
        attach = keys[-1] if keys else None
        for k in keys[:-1]:
            self._wait(eng, k, need[k])
        r = emit()
        first, last = (r[0], r[-1]) if isinstance(r, list) else (r, r)
        if attach is not None:
            first._wait_ge(self._semof(attach), need[attach])
            self.seen[(eng, attach)] = need[attach]
        self.cnt[eng] += 1
        last.then_inc(self.sem[eng], 1)
        self._commit((eng, self.cnt[eng]), reads, writes)

    def dma(self, q, out, in_, reads=(), writes=(), out_off=None, in_off=None, bound=None, slots=None):
        self._waits(q, reads, writes)
        sp_ = slots or q
        sl = self.qslots[sp_]
        slot = sl[self.qnext[sp_]]
        self.qnext[sp_] = (self.qnext[sp_] + 1) % len(sl)
        if self.dcnt[slot] > 0:
            self._wait(q, slot, self.dcnt[slot])
        if out_off is None and in_off is None:
            inst = self.eng[q].dma_start(out=out, in_=in_)
        else:
            if bound not in self.bregs:
                self.bregs[bound] = self.nc.gpsimd.to_reg(bound)
            bound = self.bregs[bound]
            inst = self.nc.gpsimd.indirect_dma_start(
                out=out, out_offset=None if out_off is None else bass.IndirectOffsetOnAxis(ap=out_off, axis=0),
                in_=in_, in_offset=None if in_off is None else bass.IndirectOffsetOnAxis(ap=in_off, axis=0),
                bounds_check=bound, oob_is_err=False)
        inst.then_inc(self.dsem[slot], 16)
        self.dcnt[slot] += 16
        self._commit((slot, self.dcnt[slot]), reads, writes)

    def barrier(self, engines=("pe", "act", "dve", "pool", "sp"), full=False):
        skip = () if full else set(self.qslots["cast"])
        for e in engines:
            for k in self.sem:
                if self.cnt[k] > 0:
                    self._wait(e, k, self.cnt[k])
            for s in range(len(self.dsem)):
                if self.dcnt[s] > 0 and s not in skip:
                    self._wait(e, s, self.dcnt[s])


def v3(ap, b):
    return ap.rearrange("p (a b) -> p a b", b=b)


def build(n_seq=2, stop_after=None, dumps=()):
    nc = bass.Bass("TRN2", target_bir_lowering=False)

    def din(name, shape):
        return nc.dram_tensor(name, list(shape), F32, kind="ExternalInput").ap()

    x = din("x", [n_seq, SEQ, D])
    mem = din("mem", [n_seq, 256, D])
    w_in = din("w_in", [D, 1960])
    wkr_sw = din("wkr_sw", [D, 96])
    wq_d = din("wq", [256, 768])
    wqsw_d = din("wq_sw", [256, 768])
    qn_d = din("qn", [128, 2])
    wkv_d = din("wkv", [128, 1024])
    kvn_d = din("kvn", [128, 1])
    convw_d = din("convw", [128, 32])
    convb_d = din("convb", [128, 8])
    dtb_d = din("dtb", [128, 128])
    alog_d = din("alog", [128, 8])
    dcol_d = din("dcol", [128, 4])
    ncol_d = din("ncol", [128, 4])
    wout_d = din("wout", [D, D])
    xwq_d = din("xwq", [D, D])
    xwk_d = din("xwk", [D, D])
    xwv_d = din("xwv", [D, D])
    xwo_d = din("xwo", [D, D])
    lnp_d = din("lnp", [6, 128, D])
    rw_d = din("rw", [D, 36])
    rb_d = din("rb", [128, 36])
    wg_d = din("wg", [NEXP, D, 256])
    wu_d = din("wu", [NEXP, D, 256])
    wd_d = din("wd", [NEXP, 256, D])
    ident_d = din("ident", [128, 128])
    tri_d = din("tri", [128, 128])
    negmask_d = din("negmask", [128, 512])
    cc_d = din("cc", [128, SEQ])
    ss_d = din("ss", [128, SEQ])
    su_d = din("su", [128, 128])
    thr_d = din("thr", [128, 32])
    pcol_d = din("pcol", [128, 1])
    GT = n_seq * NT
    NTOK = n_seq * SEQ
    TB = 3
    TSZ = 128 * TB
    NTI = -(-(2 * NTOK) // TSZ) + 31
    tstart_d = din("tstart", [128, 64])
    out_d = nc.dram_tensor("out", [n_seq, SEQ, D], F32, kind="ExternalOutput").ap()
    XB = nc.dram_tensor("XB", [NTOK, D], BF16, kind="Internal").ap()
    H2D = nc.dram_tensor("H2D", [NTOK, D], F32, kind="Internal").ap()
    XS = nc.dram_tensor("XS", [NTI * TSZ, D], BF16, kind="Internal").ap()
    YS = nc.dram_tensor("YS", [NTI * TSZ, D], BF16, kind="Internal").ap()
    DWB = {n: nc.dram_tensor("DWB_" + n, [D, D], BF16, kind="Internal").ap() for n in ("wout", "xwq", "xwk", "xwv", "xwo")}
    XBF = nc.dram_tensor("XBF", [max(n_seq - 1, 1), SEQ, D], BF16, kind="Internal").ap()
    WINB = nc.dram_tensor("WINB", [D, 1960], BF16, kind="Internal").ap()
    WGB = nc.dram_tensor("WGB", [NEXP * 128, 2048], BF16, kind="Internal").ap()
    WUB = nc.dram_tensor("WUB", [NEXP * 128, 2048], BF16, kind="Internal").ap()
    WDB = nc.dram_tensor("WDB", [NEXP * 128, 2048], BF16, kind="Internal").ap()
    dump_d = {}
    for name, shape in dumps:
        dump_d[name] = nc.dram_tensor("dbg_" + name, list(shape), F32, kind="ExternalOutput").ap()

    es = ExitStack()
    with es:
        S = Sched(nc, es)

        def sb(name, shape, dt):
            return es.enter_context(nc.sbuf_tensor(name, list(shape), dt))

        ident_bf = sb("ident_bf", [128, 128], BF16)
        ones_bf = sb("ones_bf", [128, 128], BF16)
        ones_f = sb("ones_f", [128, 128], F32)
        tri_f = sb("tri_f", [128, 128], F32)
        ident_f = sb("ident_f", [128, 128], F32)
        negmask = sb("negmask_s", [128, 512], F32)
        cc = sb("cc_s", [128, SEQ], BF16)
        ss = sb("ss_s", [128, SEQ], BF16)
        lng = sb("lng", [128, D], F32)
        lnb = sb("lnb", [128, D], F32)
        qn = sb("qn_s", [128, 2], F32)
        kvn = sb("kvn_s", [128, 1], F32)
        convw = sb("convw_s", [128, 32], F32)
        convb = sb("convb_s", [128, 8], F32)
        dtb = sb("dtb_s", [128, 128], F32)
        a_bc = sb("a_bc", [128, 8], F32)
        dcol = sb("dcol_s", [128, 4], F32)
        ncol = sb("ncol_s", [128, 4], F32)
        rb = sb("rb_s", [128, 36], F32)
        rw = sb("rw_s", [128, 8 * 36], BF16)
        small = sb("small", [128, 512], F32)
        su_bf = sb("su_bf", [128, 128], BF16)
        thr = sb("thr_s", [128, 32], F32)
        pcol = sb("pcol_s", [128, 1], F32)
        tstart = sb("tstart_s", [128, 64], F32)
        ONE = sb("ONE", [128, GT * 64], BF16)
        RK = sb("RK", [128, GT * 2], F32)
        G12 = sb("G12", [128, GT * 2], F32)
        Crun = sb("Crun", [128, 32], F32)
        mo_f = sb("mo_f", [128, 128], F32)
        mo_i = sb("mo_i", [128, 128], mybir.dt.int32)
        tRk = T()
        rdx = cc[:].bitcast(F32)
        dt_tok = sb("dt_tok", [128, 128], F32)
        tC = T()
        tLN = T()
        tSmall = T()

        A = sb("arenaA", [128, 16384], BF16)
        H = sb("arenaH", [128, 16384], F32)
        K = sb("arenaK", [128, 16384], BF16)
        W = sb("arenaW", [128, 24576], BF16)
        lnst = sb("lnst", [128, 64], F32)
        tLNS = [T() for _ in range(4)]
        lnstate = {"i": 0}
        PS = [es.enter_context(nc.psum_tensor("ps%d" % i, [128, 1024], F32)) for i in range(4)]
        PT_ = [T() for _ in range(8)]
        pstate = {"b": 0}

        class Bank:
            def __init__(self, ap, toks):
                self.ap = ap
                self.toks = toks

            @property
            def bf(self):
                return self.ap.bitcast(BF16)

        busy = set()

        def ps1(hold=False):
            b = pstate["b"]
            while b in busy:
                b = (b + 1) % 8
            pstate["b"] = (b + 1) % 8
            bk_ = Bank(PS[b // 2][:, (b % 2) * 512:(b % 2) * 512 + 512], [PT_[b]])
            bk_.idx = [b]
            if hold:
                busy.add(b)
            return bk_

        def ps2(hold=False):
            b = pstate["b"]
            if b % 2:
                b = (b + 1) % 8
            while b in busy or (b + 1) in busy:
                b = (b + 2) % 8
            pstate["b"] = (b + 2) % 8
            bk_ = Bank(PS[b // 2][:, :], [PT_[b], PT_[b + 1]])
            bk_.idx = [b, b + 1]
            if hold:
                busy.update(bk_.idx)
            return bk_

        def psrel(bk_):
            for b in bk_.idx:
                busy.discard(b)

        def Wf(lo, hi):
            return W[:, lo:hi].bitcast(F32)

        def Kf(lo, hi):
            return K[:, lo:hi].bitcast(F32)

        def Hb(lo, hi):
            return H[:, lo:hi].bitcast(BF16)

        def dump(name, ap, toks):
            if name in dump_d:
                S.dma("pool", out=dump_d[name], in_=ap, reads=toks)

        def ld(q, dst, src):
            S.dma(q, out=dst, in_=src, writes=[T()])

        ld("pool", ident_bf[:], ident_d[:, :])
        ld("sp", ident_f[:], ident_d[:, :])
        ld("sp", tri_f[:], tri_d[:, :])
        ld("sp", negmask[:], negmask_d[:, :])
        ld("pool", su_bf[:], su_d[:, :])
        ld("sp", thr[:], thr_d[:, :])
        ld("sp", pcol[:], pcol_d[:, :])
        ld("sp", tstart[:], tstart_d[:, :])
        S.op("dve", [], [tRk], lambda: nc.vector.memset(Crun[:], 0.0))
        ld("pool", cc[:], cc_d[:, :])
        ld("pool", ss[:], ss_d[:, :])
        for dst, src in ((qn, qn_d), (kvn, kvn_d), (convw, convw_d), (convb, convb_d), (dtb, dtb_d),
                         (a_bc, alog_d), (dcol, dcol_d), (ncol, ncol_d), (rb, rb_d)):
            ld("sp", dst[:], src[:, :])
        ld("pool", v3(rw[:], 36), rw_d.rearrange("(k p) c -> p k c", p=128))
        S.barrier()
        S.op("dve", [], [tC], lambda: nc.vector.memset(ones_f[:], 1.0))
        S.op("dve", [], [tC], lambda: nc.vector.memset(ones_bf[:], 1.0))
        S.op("act", [tC], [tC], lambda: nc.scalar.activation(out=a_bc[:], in_=a_bc[:], func=AF.Exp))
        S.op("dve", [tC], [tC], lambda: nc.vector.tensor_scalar_mul(out=a_bc[:], in0=a_bc[:], scalar1=-1.0))
        S.barrier()

        def rstd_from(dst, src, scale, reads, writes):
            S.op("act", reads, writes, lambda: nc.scalar.activation(out=dst, in_=src, func=AF.Ln, scale=scale, bias=EPS))
            S.op("act", [], writes, lambda: nc.scalar.activation(out=dst, in_=dst, func=AF.Exp, scale=-0.5))

        def layer_norm_tile(hap, tH, li, s):
            j = lnstate["i"] % 4
            lnstate["i"] += 1
            tS = tLNS[j]
            base = j * 16
            st = lnst[:, base:base + 12]
            mv = lnst[:, base + 12:base + 14]
            rs = lnst[:, base + 14:base + 15]
            nm = lnst[:, base + 15:base + 16]
            S.op("dve", [tH], [tS], lambda: nc.vector.bn_stats(out=st[:, 0:6], in_=hap[:, 0:512]))
            S.op("dve", [tH], [tS], lambda: nc.vector.bn_stats(out=st[:, 6:12], in_=hap[:, 512:1024]))
            S.op("dve", [], [tS], lambda: nc.vector.bn_aggr(out=mv, in_=st))
            rstd_from(rs, mv[:, 1:2], 1.0, [tS], [tS])
            S.op("dve", [tS], [tS], lambda: nc.vector.scalar_tensor_tensor(
                out=nm, in0=mv[:, 0:1], scalar=-1.0, in1=rs, op0=ALU.mult, op1=ALU.mult))
            S.op("act", [tS], [tH], lambda: nc.scalar.activation(out=hap, in_=hap, func=AF.Identity, scale=rs, bias=nm))
            S.op("dve", [tLN], [tH], lambda: nc.vector.tensor_tensor(out=hap, in0=hap, in1=lng[:], op=ALU.mult))
            S.op("dve", [tLN], [tH], lambda: nc.vector.tensor_tensor(out=hap, in0=hap, in1=lnb[:], op=ALU.add))

        def load_ln(li):
            S.dma("sp", out=lng[:], in_=lnp_d[2 * li, :, :], writes=[tLN])
            S.dma("sp", out=lnb[:], in_=lnp_d[2 * li + 1, :, :], writes=[tLN])

        def transpose_to(hb, tHB, dstT, tt, tDst):
            pb = ps1()
            S.op("pe", [tHB, tC], pb.toks, lambda: [nc.tensor.transpose(
                pb.bf[:, k * 128:(k + 1) * 128], hb[:, k * 128:(k + 1) * 128], ident_bf[:]) for k in range(8)])
            S.op("act", pb.toks, [tDst], lambda: nc.scalar.copy(
                out=dstT[:, :, tt * 128:(tt + 1) * 128], in_=v3(pb.bf[:, 0:1024], 128)))

        actT = v3(A[:, :], SEQ)
        catT = v3(K[:, :], SEQ)
        hview = v3(H[:, :], D)

        for s in range(n_seq):
            tXT = [T() for _ in range(NT)]
            if s > 0:
                S.dma("pool", out=cc[:], in_=cc_d[:, :], writes=[tC])
            NXB = 4
            xin = [K[:, 0:1024], K[:, 1024:2048], K[:, 8192:9216], K[:, 9216:10240]]
            txin = [T() for _ in range(NXB)]
            win = v3(W[:, 0:15680], 1960)
            wkr = v3(W[:, 15680:16448], 96)
            wq_s = v3(W[:, 16448:17984], 768)
            wq_sw = v3(W[:, 17984:19520], 768)
            wkv_s = W[:, 19520:20544]
            tWinL, tWs = [T() for _ in range(9)], T()
            def load_x_tile(tt):
                if s == 0:
                    S.dma("pool", out=xin[tt % NXB], in_=x[s, tt * 128:(tt + 1) * 128, :], writes=[txin[tt % NXB]])
                else:
                    S.dma("sp", out=xin[tt % NXB], in_=XBF[s - 1, tt * 128:(tt + 1) * 128, :], reads=[tXC[(s, tt)]], writes=[txin[tt % NXB]])

            for tt in range(NXB):
                load_x_tile(tt)
            WCG = [(0, 416), (416, 928), (928, 1440), (1440, 1960)]
            w_in3 = w_in.rearrange("(k p) c -> p k c", p=128)
            def load_win_group(gi):
                c0_, c1_ = WCG[gi]
                if s == 0:
                    S.dma("pool", out=win[:, :, c0_:c1_], in_=w_in3[:, :, c0_:c1_], writes=[tWinL[gi]])
                else:
                    S.dma("sp", out=win[:, :, c0_:c1_], in_=WINB[:, c0_:c1_].rearrange("(k p) c -> p k c", p=128),
                          reads=[tWC[gi]], writes=[tWinL[gi]])

            load_win_group(0)
            S.dma("pool", out=wkr, in_=wkr_sw.rearrange("(k p) c -> p k c", p=128), writes=[tWinL[8]])
            for gi in range(1, 4):
                load_win_group(gi)
            S.dma("pool", out=wq_s, in_=wq_d.rearrange("(k p) c -> p k c", p=128), writes=[tWs])
            S.dma("pool", out=wq_sw, in_=wqsw_d.rearrange("(k p) c -> p k c", p=128), writes=[tWs])
            S.dma("pool", out=wkv_s, in_=wkv_d[:, :], writes=[tWs])
            for r in range(2):
                S.op("dve", [tWs, tC], [tWs], lambda r=r: nc.vector.tensor_scalar_mul(out=wq_s[:, r, :], in0=wq_s[:, r, :], scalar1=qn[:, r:r + 1]))
                S.op("dve", [tWs, tC], [tWs], lambda r=r: nc.vector.tensor_scalar_mul(out=wq_sw[:, r, :], in0=wq_sw[:, r, :], scalar1=qn[:, r:r + 1]))
            S.op("dve", [tWs, tC], [tWs], lambda: nc.vector.tensor_scalar_mul(out=wkv_s, in0=wkv_s, scalar1=kvn[:, 0:1]))
            for tt in range(NT):
                transpose_to(xin[tt % NXB], txin[tt % NXB], actT, tt, tXT[tt])
                if tt + NXB < NT:
                    load_x_tile(tt + NXB)
            if s == 0:
                tDW = {n: [T() for _ in range(8)] for n in DWB}
                for n, src in (("wout", wout_d), ("xwk", xwk_d), ("xwv", xwv_d), ("xwq", xwq_d), ("xwo", xwo_d)):
                    for k in range(8):
                        S.dma("pool", out=DWB[n][k * 128:(k + 1) * 128, :], in_=src[k * 128:(k + 1) * 128, :], writes=[tDW[n][k]], slots="cast")
            if s == 0 and n_seq > 1:
                tXC = {}
                tWC = [T() for _ in range(4)]
                for gi in range(4):
                    S.dma("pool", out=WINB[:, WCG[gi][0]:WCG[gi][1]].rearrange("(k p) c -> p k c", p=128),
                          in_=w_in3[:, :, WCG[gi][0]:WCG[gi][1]], writes=[tWC[gi]], slots="cast")
                for s2 in range(1, n_seq):
                    for tt in range(NT):
                        tXC[(s2, tt)] = T()
                        S.dma("pool", out=XBF[s2 - 1, tt * 128:(tt + 1) * 128, :], in_=x[s2, tt * 128:(tt + 1) * 128, :],
                              writes=[tXC[(s2, tt)]], slots="cast")
            if s == 0 and stop_after is None:
                for e in range(NEXP):
                    rows = slice(e * 128, (e + 1) * 128)
                    S.dma("pool", out=WGB[rows, :].rearrange("p (k f) -> p k f", k=8), in_=wg_d[e].rearrange("(k p) f -> p k f", p=128), writes=[T()], slots="cast")
                    S.dma("pool", out=WUB[rows, :].rearrange("p (k f) -> p k f", k=8), in_=wu_d[e].rearrange("(k p) f -> p k f", p=128), writes=[T()], slots="cast")
                    S.dma("pool", out=WDB[rows, :].rearrange("p (k f) -> p k f", k=2), in_=wd_d[e].rearrange("(k p) f -> p k f", p=128), writes=[T()], slots="cast")
            if stop_after == "P1":
                dump("xT", actT[:, 0, :], tXT)
                break

            zs = v3(Hb(0, 4096), SEQ)
            xsT = v3(Hb(4096, 8192), SEQ)
            BT = v3(Hb(8192, 10240), SEQ)
            CT = v3(Hb(10240, 12288), SEQ)
            cqT = v3(Hb(12288, 14336), SEQ)
            ckvT = Hb(14336, 15360)
            kpe = Hb(15360, 16384)
            tZ, tXs, tB, tCt, tCq, tCkv, tKpe = T(), T(), T(), T(), T(), T(), T()
            pre = [Kf(2048 + i * 1040, 2048 + i * 1040 + 1030) for i in range(2)]
            acc = [Kf(4128 + i * 1024, 4128 + (i + 1) * 1024) for i in range(2)]
            sqb = K[:, 6176:6688]
            rsb = Kf(6688, 7712)
            tPre, tAcc, tSq, tRs = [T(), T()], [T(), T()], T(), T()
            tDt = T()

            def proj_mm(bank, M, lhs_fn, tg, wt=0):
                S.op("pe", list(tXT[tg * 4:tg * 4 + 4]) + [tWinL[wt]], bank.toks, lambda: [nc.tensor.matmul(
                    bank.ap[0:M, :], lhs_fn(k), actT[:, k, tg * 512:(tg + 1) * 512], start=(k == 0), stop=(k == 7)) for k in range(8)])

            for tg in range(NG):
                cols = slice(tg * 512, (tg + 1) * 512)
                bq = [ps1(), ps1()]
                for r in range(2):
                    proj_mm(bq[r], 128, lambda k, r=r: win[:, k, r * 128:(r + 1) * 128], tg)
                bs = ps1()
                for r in range(2):
                    S.op("act", bq[r].toks, [tSq], lambda r=r: nc.scalar.activation(out=sqb, in_=bq[r].ap, func=AF.Square))
                    S.op("pe", [tSq, tC], bs.toks, lambda r=r: nc.tensor.matmul(bs.ap, ones_bf[:], sqb, start=(r == 0), stop=(r == 1)))
                rstd_from(rsb, bs.ap, 1.0 / 256, bs.toks, [tRs])
                for r in range(2):
                    S.op("dve", bq[r].toks + [tRs], [tCq], lambda r=r: nc.vector.tensor_tensor(out=cqT[:, r, cols], in0=bq[r].ap, in1=rsb, op=ALU.mult))
                bk = ps1()
                proj_mm(bk, 128, lambda k: win[:, k, 256:384], tg)
                bs = ps1()
                S.op("act", bk.toks, [tSq], lambda: nc.scalar.activation(out=sqb, in_=bk.ap, func=AF.Square))
                S.op("pe", [tSq, tC], bs.toks, lambda: nc.tensor.matmul(bs.ap, ones_bf[:], sqb, start=True, stop=True))
                rstd_from(rsb, bs.ap, 1.0 / 128, bs.toks, [tRs])
                S.op("dve", bk.toks + [tRs], [tCkv], lambda: nc.vector.tensor_tensor(out=ckvT[:, cols], in0=bk.ap, in1=rsb, op=ALU.mult))
                ba, bb = ps1(), ps1()
                proj_mm(ba, 96, lambda k: win[:, k, 320:416], tg)
                proj_mm(bb, 96, lambda k: wkr[:, k, :], tg, 8)
                t1 = acc[0]
                t2 = acc[1]
                S.op("dve", ba.toks + [tC], [tAcc[0]], lambda: nc.vector.tensor_tensor(out=t1[64:96, :], in0=ba.ap[64:96, :], in1=cc[64:96, cols], op=ALU.mult))
                S.op("dve", bb.toks + [tC], [tAcc[1]], lambda: nc.vector.tensor_tensor(out=t2[64:96, :], in0=bb.ap[64:96, :], in1=ss[64:96, cols], op=ALU.mult))
                S.op("dve", [tAcc[0], tAcc[1]], [tKpe], lambda: nc.vector.tensor_tensor(out=kpe[64:96, cols], in0=t1[64:96, :], in1=t2[64:96, :], op=ALU.add))
                for j in range(4):
                    bz = ps1()
                    proj_mm(bz, 128, lambda k, j=j: win[:, k, 416 + j * 128:416 + (j + 1) * 128], tg, 1)
                    S.op("act", bz.toks, [tZ], lambda j=j, bz=bz: nc.scalar.activation(out=zs[:, j, cols], in_=bz.ap, func=AF.Silu))
            for c in range(8):
                if c < 4:
                    dst, tdst = xsT[:, c, :], tXs
                elif c < 6:
                    dst, tdst = BT[:, c - 4, :], tB
                else:
                    dst, tdst = CT[:, c - 6, :], tCt
                S.op("dve", [], [tPre[0]], lambda: nc.vector.memset(pre[0][:, 0:3], 0.0))
                for tg in range(NG):
                    p_, a_ = pre[tg % 2], acc[tg % 2]
                    tp, ta = tPre[tg % 2], tAcc[tg % 2]
                    bx = ps1()
                    proj_mm(bx, 128, lambda k, c=c: win[:, k, 928 + c * 128:928 + (c + 1) * 128], tg, 2 if c < 4 else 3)
                    S.op("act", bx.toks, [tp], lambda: nc.scalar.copy(out=p_[:, 3:515], in_=bx.ap))
                    if tg < NG - 1:
                        S.op("act", [tp], [tPre[(tg + 1) % 2]], lambda: nc.scalar.copy(out=pre[(tg + 1) % 2][:, 0:3], in_=p_[:, 512:515]))
                    S.op("dve", [tp, tC], [ta], lambda: nc.vector.tensor_scalar_mul(out=a_, in0=p_[:, 0:512], scalar1=convw[:, c * 4:c * 4 + 1]))
                    for j in range(1, 4):
                        S.op("dve", [tp, tC], [ta], lambda j=j: nc.vector.scalar_tensor_tensor(
                            out=a_, in0=p_[:, j:j + 512], scalar=convw[:, c * 4 + j:c * 4 + j + 1], in1=a_, op0=ALU.mult, op1=ALU.add))
                    S.op("act", [ta, tC], [tdst], lambda: nc.scalar.activation(
                        out=dst[:, tg * 512:(tg + 1) * 512], in_=a_, func=AF.Silu, bias=convb[:, c:c + 1]))
            bd = ps1()
            for tt in range(NT):
                S.op("pe", [tXT[tt], tWinL[3]], bd.toks, lambda tt=tt: [nc.tensor.matmul(
                    bd.ap[:, tt * 8:(tt + 1) * 8], actT[:, k, tt * 128:(tt + 1) * 128], win[:, k, 1952:1960],
                    start=(k == 0), stop=(k == 7)) for k in range(8)])
            S.op("dve", bd.toks + [tC], [tDt], lambda: nc.vector.tensor_tensor(out=dt_tok[:], in0=bd.ap[:, 0:128], in1=dtb[:], op=ALU.add))
            S.op("act", [tDt], [tDt], lambda: nc.scalar.activation(out=dt_tok[:], in_=dt_tok[:], func=AF.Exp))
            S.op("act", [tDt], [tDt], lambda: nc.scalar.activation(out=dt_tok[:], in_=dt_tok[:], func=AF.Ln, bias=1.0))
            if stop_after == "P2":
                dump("cqT", cqT[:, 0, :], [tCq])
                dump("ckvT", ckvT, [tCkv])
                dump("kpe", kpe, [tKpe])
                dump("zs", zs[:, 0, :], [tZ])
                dump("xsT", xsT[:, 0, :], [tXs])
                dump("BT", BT[:, 0, :], [tB])
                dump("dt", dt_tok[:], [tDt])
                break
            S.barrier(engines=("act", "dve", "pool", "sp"))
            qT = [A[:, i * 2048:(i + 1) * 2048] for i in range(2)]
            kT = [A[:, (2 + i) * 2048:(3 + i) * 2048] for i in range(2)]
            Vaug = [v3(A[:, (4 + i) * 2048:(5 + i) * 2048], 128) for i in range(2)]
            PTb = [A[:, 12288 + i * 512:12288 + (i + 1) * 512] for i in range(3)]
            rdb = A[:, 13824:14848].bitcast(F32)
            t1 = [Wf(0, 1024), A[:, 14848:15872].bitcast(F32)]
            t2 = [Wf(1024, 2048), Wf(13568, 14592)]
            tQ, tK, tV, tPT = [T(), T()], [T(), T()], [T(), T()], [T(), T(), T()]
            tT1, tT2, tRd = [T(), T()], [T(), T()], T()
            tCat = [T() for _ in range(8)]
            S.op("dve", [], [tV[0]], lambda: nc.vector.memset(Vaug[0][:, :, 64:128], 1.0))
            S.op("dve", [], [tV[1]], lambda: nc.vector.memset(Vaug[1][:, :, 0:64], 1.0))
            if s == 0 and stop_after is None:
                ztile = W[:, 14592:15616]
                tZ = T()
                S.op("dve", [], [tZ], lambda: nc.vector.memset(ztile, 0.0))
                for j in range(NTI * TB):
                    S.dma("sp", out=XS[j * 128:(j + 1) * 128, :], in_=ztile, reads=[tZ])
            SCALE = 96.0 ** -0.5
            pstate_pt = {"i": 0}

            def qkv_head(h):
                hp = h % 2
                q_h, k_h, V_h = qT[hp], kT[hp], Vaug[hp]
                tq, tk, tv = tQ[hp], tK[hp], tV[hp]
                for tg in range(NG):
                    cols = slice(tg * 512, (tg + 1) * 512)
                    S.op("dve", [tKpe], [tk], lambda: nc.vector.tensor_copy(out=k_h[64:96, cols], in_=kpe[64:96, cols]))
                    yield
                    ba, bb = ps1(), ps1()
                    S.op("pe", [tWs, tCq], ba.toks, lambda: [nc.tensor.matmul(
                        ba.ap[0:96, :], wq_s[:, r, h * 96:(h + 1) * 96], cqT[:, r, cols], start=(r == 0), stop=(r == 1)) for r in range(2)])
                    S.op("pe", [tWs, tCq], bb.toks, lambda: [nc.tensor.matmul(
                        bb.ap[0:96, :], wq_sw[:, r, h * 96:(h + 1) * 96], cqT[:, r, cols], start=(r == 0), stop=(r == 1)) for r in range(2)])
                    bk = ps1()
                    S.op("pe", [tWs, tCkv], bk.toks, lambda: nc.tensor.matmul(
                        bk.ap[0:64, :], wkv_s[:, h * 128:h * 128 + 64], ckvT[:, cols], start=True, stop=True))
                    tt1, tt2 = tT1[tg % 2], tT2[tg % 2]
                    u1, u2 = t1[tg % 2], t2[tg % 2]
                    S.op("dve", ba.toks + [tC], [tt1], lambda: nc.vector.tensor_tensor(out=u1[0:96, :], in0=ba.ap[0:96, :], in1=cc[0:96, cols], op=ALU.mult))
                    yield
                    S.op("dve", bb.toks + [tC], [tt2], lambda: nc.vector.tensor_tensor(out=u2[0:96, :], in0=bb.ap[0:96, :], in1=ss[0:96, cols], op=ALU.mult))
                    yield
                    S.op("dve", [tt1, tt2], [tq], lambda: nc.vector.tensor_tensor(out=q_h[0:96, cols], in0=u1[0:96, :], in1=u2[0:96, :], op=ALU.add))
                    yield
                    S.op("dve", bk.toks, [tk], lambda: nc.vector.tensor_copy(out=k_h[0:64, cols], in_=bk.ap[0:64, :]))
                    yield
                vo = 0 if hp == 0 else 64
                for half in range(2):
                    bv = ps1()
                    S.op("pe", [tWs, tCkv], bv.toks, lambda: [nc.tensor.matmul(
                        bv.ap[:, j * 64:(j + 1) * 64], ckvT[:, (half * 8 + j) * 128:(half * 8 + j + 1) * 128],
                        wkv_s[:, h * 128 + 64:(h + 1) * 128], start=True, stop=True) for j in range(8)])
                    S.op("dve", bv.toks, [tv], lambda: nc.vector.tensor_copy(out=V_h[:, half * 8:(half + 1) * 8, vo:vo + 64], in_=v3(bv.ap, 64)))
                    yield

            def attn_head(h):
                hp = h % 2
                q_h, k_h, V_h = qT[hp], kT[hp], Vaug[hp]
                tq, tk, tv = tQ[hp], tK[hp], tV[hp]
                orow = slice(0, 64) if hp == 0 else slice(64, 128)
                drow = slice(64, 128) if hp == 0 else slice(0, 64)
                items = [(g, kj) for g in range(NG) for kj in range(4 * g + 4)]
                sbank = {}
                bo_of = {}

                def emit_S(i):
                    g, kj = items[i]
                    c0 = max(0, kj - 4 * g) * 128
                    if g not in bo_of:
                        bo_of[g] = ps1(hold=True)
                    b_ = ps1()
                    sbank[i] = b_
                    S.op("pe", [tq, tk], b_.toks, lambda: nc.tensor.matmul(
                        b_.ap[:, c0:512], k_h[0:96, kj * 128:(kj + 1) * 128], q_h[0:96, g * 512 + c0:(g + 1) * 512], start=True, stop=True))

                LOOK = 2
                for i in range(min(LOOK, len(items))):
                    emit_S(i)
                for i, (g, kj) in enumerate(items):
                    nk = 4 * g + 4
                    c0 = max(0, kj - 4 * g) * 128
                    b_ = sbank.pop(i)
                    pt, tpt = PTb[pstate_pt["i"] % 3], tPT[pstate_pt["i"] % 3]
                    pstate_pt["i"] += 1
                    S.op("act", b_.toks, [tpt], lambda: nc.scalar.activation(out=pt[:, c0:512], in_=b_.ap[:, c0:512], func=AF.Exp, scale=SCALE))
                    if kj >= 4 * g:
                        S.op("dve", [], [tpt], lambda: nc.vector.memset(pt[64:128, c0:c0 + 64], 0.0))
                    if i + LOOK < len(items):
                        emit_S(i + LOOK)
                    bo = bo_of[g]
                    S.op("pe", [tpt, tv], bo.toks, lambda: nc.tensor.matmul(
                        bo.ap[:, c0:512], V_h[:, kj, :], pt[:, c0:512], start=(kj == 0), stop=(kj == nk - 1)))
                    if kj == nk - 1:
                        S.op("dve", bo.toks, [tRd], lambda: nc.vector.reciprocal(out=rdb[drow, :], in_=bo.ap[drow, :]))
                        S.op("dve", bo.toks + [tRd], [tCat[h // 2]], lambda: nc.vector.tensor_tensor(
                            out=catT[orow, h // 2, g * 512:(g + 1) * 512], in0=bo.ap[orow, :], in1=rdb[drow, :], op=ALU.mult))
                        psrel(bo)
                    yield

            def p4_gen():
                for _ in qkv_head(0):
                    pass
                for h in range(8):
                    nxt = qkv_head(h + 1) if h + 1 < 8 else None
                    for n_, _ in enumerate(attn_head(h)):
                        if nxt is not None and n_ % 2 == 1:
                            if next(nxt, "done") == "done":
                                nxt = None
                        yield
                    if nxt is not None:
                        for _ in nxt:
                            pass

            if stop_after == "P4":
                for _ in p4_gen():
                    pass
                dump("cat_attn", catT[:, 0:4, :], tCat)
                break
            def ssd_set(i):
                b0 = 2048 if i == 0 else 15616
                so = 16 if i == 0 else 64
                d = dict(
                    adt=small[:, so:so + 8], acs_sb=small[:, so + 8:so + 16], dte=small[:, so + 16:so + 24],
                    cdk=small[:, so + 24:so + 32], dd=small[:, so + 32:so + 40], nacs=small[:, so + 40:so + 48],
                    R=Wf(b0, b0 + 2048), E=Wf(b0 + 2048, b0 + 3072), DEC=Wf(b0 + 3072, b0 + 4096),
                    Mfin=[W[:, b0 + 4096:b0 + 4608], W[:, b0 + 4608:b0 + 5120]],
                    CexpT=[W[:, b0 + 5120:b0 + 5632], W[:, b0 + 5632:b0 + 6144]],
                    xdt=W[:, b0 + 6144:b0 + 6656], xdte=W[:, b0 + 6656:b0 + 7168], Btok=W[:, b0 + 7168:b0 + 7424],
                    cbm=[Wf(b0 + 7424, b0 + 7680), Wf(b0 + 7680, b0 + 7936)],
                    tAdt=T(), tSm2=T(), tR=T(), tE=T(), tDec=T(), tXdt=T(), tXdte=T(), tBtok=T(), tCbm=T(),
                    tMf=[T(), T()], tCe=[T(), T()])
                return d

            SB = [ssd_set(0), ssd_set(1)]
            prev_f = Wf(9984, 11008)
            prev_bf = W[:, 11008:11520]
            yz = Wf(11520, 12544)
            sq = W[:, 12544:13056]
            rstd_g = Wf(13056, 13312)
            yv = Wf(13312, 13568)
            tPrevF, tPrevB, tYv, tYz, tSq2, tRg = T(), T(), T(), T(), T(), T()
            S.op("dve", [], [tWs], lambda: nc.vector.memset(small[:, 250:251], 0.0))
            S.op("act", [], [tWs], lambda: nc.scalar.copy(out=small[:, 251:252], in_=small[:, 250:251]))
            S.op("dve", [], [tPrevF], lambda: nc.vector.memset(prev_f, 0.0))
            S.op("dve", [], [tPrevB], lambda: nc.vector.memset(prev_bf, 0.0))

            def ssd_stage1(c):
                B_ = SB[c % 2]
                adt, acs_sb, dte, cdk, dd, nacs = B_["adt"], B_["acs_sb"], B_["dte"], B_["cdk"], B_["dd"], B_["nacs"]
                R, E, DEC, Mfin, CexpT, xdt, xdte, Btok, cbm = (B_[k] for k in ("R", "E", "DEC", "Mfin", "CexpT", "xdt", "xdte", "Btok", "cbm"))
                tAdt, tSm2, tR, tE, tDec, tXdt, tXdte, tBtok, tCbm, tMf, tCe = (B_[k] for k in (
                    "tAdt", "tSm2", "tR", "tE", "tDec", "tXdt", "tXdte", "tBtok", "tCbm", "tMf", "tCe"))
                tsl = slice(c * 128, (c + 1) * 128)
                dtc = dt_tok[:, c * 8:(c + 1) * 8]
                S.op("dve", [tDt, tC], [tAdt], lambda: nc.vector.tensor_tensor(out=adt, in0=dtc, in1=a_bc[:], op=ALU.mult))
                bx = ps1()
                S.op("pe", [tXs, tC], bx.toks, lambda: [nc.tensor.transpose(
                    bx.bf[:, j * 128:(j + 1) * 128], xsT[:, j, tsl], ident_bf[:]) for j in range(4)])
                S.op("dve", bx.toks + [tDt], [tXdt], lambda: nc.vector.tensor_tensor(
                    out=v3(xdt, 64), in0=v3(bx.bf[:, 0:512], 64), in1=dtc.unsqueeze(2).to_broadcast([128, 8, 64]), op=ALU.mult))
                bb_ = ps1()
                S.op("pe", [tB, tC], bb_.toks, lambda: [nc.tensor.transpose(
                    bb_.bf[:, g * 128:(g + 1) * 128], BT[:, g, tsl], ident_bf[:]) for g in range(2)])
                S.op("act", bb_.toks, [tBtok], lambda: nc.scalar.copy(out=Btok, in_=bb_.bf[:, 0:256]))
                yield
                ba_ = ps1()
                S.op("pe", [tAdt, tC], ba_.toks, lambda: [
                    nc.tensor.matmul(ba_.ap[:, 0:8], tri_f[:], adt, start=True, stop=True),
                    nc.tensor.matmul(ba_.ap[:, 8:16], ones_f[:], adt, start=True, stop=True)])
                S.op("act", ba_.toks, [tSm2], lambda: nc.scalar.copy(out=acs_sb, in_=ba_.ap[:, 0:8]))
                S.op("dve", ba_.toks + [tSm2], [tSm2], lambda: nc.vector.tensor_tensor(out=dd, in0=ba_.ap[:, 8:16], in1=acs_sb, op=ALU.subtract))
                S.op("act", [tSm2], [tSm2], lambda: nc.scalar.activation(out=dte, in_=dd, func=AF.Exp))
                S.op("act", ba_.toks, [tSm2], lambda: nc.scalar.activation(out=cdk, in_=ba_.ap[:, 8:16], func=AF.Exp))
                S.op("dve", [tSm2], [tSm2], lambda: nc.vector.tensor_scalar_mul(out=nacs, in0=acs_sb, scalar1=-1.0))
                yield
                S.op("dve", [tXdt, tSm2], [tXdte], lambda: nc.vector.tensor_tensor(
                    out=v3(xdte, 64), in0=v3(xdt, 64), in1=dte.unsqueeze(2).to_broadcast([128, 8, 64]), op=ALU.mult))
                S.op("dve", [tAdt, tC], [tR], lambda: nc.vector.tensor_tensor(
                    out=v3(R, 128), in0=tri_f[:].unsqueeze(1).to_broadcast([128, 8, 128]),
                    in1=adt.unsqueeze(2).to_broadcast([128, 8, 128]), op=ALU.mult))
                yield
                for g in range(2):
                    bc = ps1()
                    S.op("pe", [tB, tCt], bc.toks, lambda: nc.tensor.matmul(bc.ap[:, 0:128], BT[:, g, tsl], CT[:, g, tsl], start=True, stop=True))
                    S.op("act", bc.toks, [tCbm], lambda: nc.scalar.copy(out=cbm[g], in_=bc.ap[:, 0:128]))
                    bA = ps1()
                    S.op("pe", [tR, tC], bA.toks, lambda: nc.tensor.matmul(bA.ap, ones_f[:], R[:, g * 512:(g + 1) * 512], start=True, stop=True))
                    yield
                    S.op("act", bA.toks, [tDec], lambda: nc.scalar.activation(out=DEC, in_=bA.ap, func=AF.Exp))
                    for hh in range(4):
                        S.op("dve", bA.toks + [tC, tDec, tSm2], [tE], lambda: nc.vector.scalar_tensor_tensor(
                            out=E[:, hh * 128:(hh + 1) * 128], in0=bA.ap[:, hh * 128:(hh + 1) * 128],
                            scalar=nacs[:, g * 4 + hh:g * 4 + hh + 1], in1=negmask[:, 0:128], op0=ALU.add, op1=ALU.add))
                    S.op("act", [tE], [tE], lambda: nc.scalar.activation(out=E, in_=E, func=AF.Exp))
                    yield
                    S.op("dve", [tE, tCbm], [tMf[g]], lambda: nc.vector.tensor_tensor(
                        out=v3(Mfin[g], 128), in0=v3(E, 128), in1=cbm[g].unsqueeze(1).to_broadcast([128, 4, 128]), op=ALU.mult))
                    S.op("dve", [tDec, tCt], [tCe[g]], lambda: nc.vector.tensor_tensor(
                        out=v3(CexpT[g], 128), in0=v3(DEC, 128), in1=CT[:, g, tsl].unsqueeze(1).to_broadcast([128, 4, 128]), op=ALU.mult))
                    yield

            def ssd_stage2(c):
                B_ = SB[c % 2]
                cdk, Mfin, CexpT, xdt, xdte, Btok = (B_[k] for k in ("cdk", "Mfin", "CexpT", "xdt", "xdte", "Btok"))
                tSm2, tXdt, tXdte, tBtok, tMf, tCe = (B_[k] for k in ("tSm2", "tXdt", "tXdte", "tBtok", "tMf", "tCe"))
                tsl = slice(c * 128, (c + 1) * 128)
                for pair in range(4):
                    g = pair // 2
                    by = ps1()
                    for hp in range(2):
                        h = pair * 2 + hp
                        hh = h % 4
                        rows = slice(hp * 64, hp * 64 + 64)
                        tp_ = None if hp == 0 else (0, 64)
                        S.op("pe", [tXdt, tMf[g], tPrevB, tCe[g]], by.toks, lambda: [
                            nc.tensor.matmul(by.ap[rows, 0:128], xdt[:, h * 64:(h + 1) * 64], Mfin[g][:, hh * 128:(hh + 1) * 128],
                                             start=True, stop=False, tile_position=tp_),
                            nc.tensor.matmul(by.ap[rows, 0:128], prev_bf[:, h * 64:(h + 1) * 64], CexpT[g][:, hh * 128:(hh + 1) * 128],
                                             start=False, stop=True, tile_position=tp_)])
                    S.op("dve", by.toks + [tXs, tC], [tYv], lambda: nc.vector.scalar_tensor_tensor(
                        out=yv, in0=xsT[:, pair, tsl], scalar=dcol[:, pair:pair + 1], in1=by.ap[:, 0:128], op0=ALU.mult, op1=ALU.add))
                    S.op("dve", [tYv, tZ], [tYz], lambda: nc.vector.tensor_tensor(
                        out=yz[:, pair * 128:(pair + 1) * 128], in0=yv, in1=zs[:, pair, tsl], op=ALU.mult))
                    S.op("act", [tYz], [tSq2], lambda: nc.scalar.activation(
                        out=sq[:, pair * 128:(pair + 1) * 128], in_=yz[:, pair * 128:(pair + 1) * 128], func=AF.Square))
                    yield
                    if pair % 2 == 1:
                        bn_ = ps1()
                        S.op("pe", [tSq2, tC], bn_.toks, lambda: [nc.tensor.matmul(
                            bn_.ap[:, 0:128], ones_bf[:], sq[:, (pair - 1 + i) * 128:(pair + i) * 128], start=(i == 0), stop=(i == 1)) for i in range(2)])
                        rstd_from(rstd_g, bn_.ap[:, 0:128], 1.0 / 256, bn_.toks, [tRg])
                        for pp in (pair - 1, pair):
                            S.op("dve", [tYz, tRg, tC], [tCat[4 + pp]], lambda: nc.vector.scalar_tensor_tensor(
                                out=catT[:, 4 + pp, tsl], in0=yz[:, pp * 128:(pp + 1) * 128], scalar=ncol[:, pp:pp + 1], in1=rstd_g,
                                op0=ALU.mult, op1=ALU.mult))
                        yield
                bst = ps1()
                S.op("pe", [tBtok, tXdte], bst.toks, lambda: [nc.tensor.matmul(
                    bst.ap[:, g * 256:(g + 1) * 256], Btok[:, g * 128:(g + 1) * 128], xdte[:, g * 256:(g + 1) * 256],
                    start=True, stop=True) for g in range(2)])
                S.op("dve", [tSm2], [tPrevF], lambda: nc.vector.tensor_tensor(
                    out=v3(prev_f, 64), in0=v3(prev_f, 64), in1=cdk.unsqueeze(2).to_broadcast([128, 8, 64]), op=ALU.mult))
                S.op("dve", bst.toks, [tPrevF], lambda: nc.vector.tensor_tensor(out=prev_f, in0=prev_f, in1=bst.ap, op=ALU.add))
                S.op("act", [tPrevF], [tPrevB], lambda: nc.scalar.copy(out=prev_bf, in_=prev_f))
                yield

            def p5_gen():
                yield from ssd_stage1(0)
                for c in range(NT):
                    ga = ssd_stage1(c + 1) if c + 1 < NT else iter(())
                    gb = ssd_stage2(c)
                    a_alive = b_alive = True
                    while a_alive or b_alive:
                        if a_alive and next(ga, "done") == "done":
                            a_alive = False
                        if b_alive and next(gb, "done") == "done":
                            b_alive = False
                        yield

            g4, g5 = p4_gen(), p5_gen()
            alive = {"4": True, "5": True}

            def step(gen, key, n):
                for _ in range(n):
                    if alive[key]:
                        try:
                            next(gen)
                        except StopIteration:
                            alive[key] = False

            while alive["4"]:
                step(g4, "4", 64)
            while alive["5"]:
                step(g5, "5", 64)

            if stop_after == "P5":
                dump("cat_ssd", catT[:, 4:8, :], tCat)
                break
            S.barrier(engines=("act", "dve", "pool", "sp"))
            wout = v3(W[:, 0:8192], 1024)
            tWoL = [T() for _ in range(8)]
            for k in range(8):
                S.dma("sp", out=wout[:, k, :], in_=DWB["wout"][k * 128:(k + 1) * 128, :], reads=[tDW["wout"][k]], writes=[tWoL[k]])
            load_ln(0)
            xres = [Wf(8192, 10240), Wf(10240, 12288)]
            hbW = [W[:, 12288:13312], W[:, 13312:14336]]
            tXr, tHb = [T(), T()], [T(), T()]
            tH = [T() for _ in range(NT)]
            tHT = [T() for _ in range(NT)]
            def p6_mm(tt):
                tok = slice(tt * 128, (tt + 1) * 128)
                xr = xres[tt % 2]
                S.dma("sp", out=xr, in_=x[s, tok, :], writes=[tXr[tt % 2]])
                bm = ps2()
                S.op("pe", tCat + tWoL, bm.toks, lambda: [nc.tensor.matmul(
                    bm.ap[:, hf * 512:(hf + 1) * 512], catT[:, c, tok], wout[:, c, hf * 512:(hf + 1) * 512],
                    start=(c == 0), stop=(c == 7)) for hf in range(2) for c in range(8)])
                return bm

            bms = {0: p6_mm(0)}
            for tt in range(NT):
                if tt + 1 < NT:
                    bms[tt + 1] = p6_mm(tt + 1)
                bm = bms.pop(tt)
                xr = xres[tt % 2]
                hap = hview[:, tt, :]
                S.op("dve", bm.toks + [tXr[tt % 2]], [tH[tt]], lambda: nc.vector.scalar_tensor_tensor(
                    out=hap, in0=xr, scalar=ALPHA, in1=bm.ap, op0=ALU.mult, op1=ALU.add))
                layer_norm_tile(hap, tH[tt], 0, s)
                S.op("act", [tH[tt]], [tHb[tt % 2]], lambda: nc.scalar.copy(out=hbW[tt % 2], in_=hap))
                transpose_to(hbW[tt % 2], tHb[tt % 2], actT, tt, tHT[tt])
            if stop_after == "P6":
                dump("h1", hview, tH)
                break
            S.barrier(engines=("act", "dve", "pool", "sp"))
            xo = v3(K[:, 0:4096], 512)
            KxT = v3(K[:, 4096:6144], 256)
            Vx = v3(K[:, 6144:8192], 1024)
            memT = v3(K[:, 8192:10240], 256)
            membf = v3(K[:, 10240:12288], 1024)
            hbK = [K[:, 12288:13312], K[:, 13312:14336]]
            QxT = [K[:, 14336:14848], K[:, 14848:15360]]
            PTx = [K[:, 15360:15872], K[:, 15872:16384]]
            wk = v3(W[:, 0:8192], 1024)
            wv = v3(W[:, 8192:16384], 1024)
            wq = v3(W[:, 16384:24576], 1024)
            tMem, tMemT, tKx, tVx, tXo, tRdx = T(), T(), T(), T(), T(), T()
            tWk, tWv, tWq = [T() for _ in range(8)], [T() for _ in range(8)], [T() for _ in range(8)]
            tQx, tPx, tRt = [T(), T()], [T(), T()], T()
            for k in range(8):
                S.dma("sp", out=wk[:, k, :], in_=DWB["xwk"][k * 128:(k + 1) * 128, :], reads=[tDW["xwk"][k]], writes=[tWk[k]])
            S.dma("pool", out=membf, in_=mem[s].rearrange("(t p) d -> p t d", p=128), writes=[tMem])
            for k in range(8):
                S.dma("sp", out=wv[:, k, :], in_=DWB["xwv"][k * 128:(k + 1) * 128, :], reads=[tDW["xwv"][k]], writes=[tWv[k]])
            for k in range(8):
                S.dma("sp", out=wq[:, k, :], in_=DWB["xwq"][k * 128:(k + 1) * 128, :], reads=[tDW["xwq"][k]], writes=[tWq[k]])
            load_ln(1)
            for mt in range(2):
                pb = ps1()
                S.op("pe", [tMem, tC], pb.toks, lambda: [nc.tensor.transpose(
                    pb.bf[:, k * 128:(k + 1) * 128], membf[:, mt, k * 128:(k + 1) * 128], ident_bf[:]) for k in range(8)])
                S.op("act", pb.toks, [tMemT], lambda: nc.scalar.copy(out=memT[:, :, mt * 128:(mt + 1) * 128], in_=v3(pb.bf[:, 0:1024], 128)))
            for c in range(8):
                b_ = ps1()
                S.op("pe", [tMemT] + tWk, b_.toks, lambda: [nc.tensor.matmul(
                    b_.ap[:, 0:256], wk[:, k, c * 128:(c + 1) * 128], memT[:, k, :], start=(k == 0), stop=(k == 7)) for k in range(8)])
                S.op("act", b_.toks, [tKx], lambda: nc.scalar.copy(out=KxT[:, c, :], in_=b_.ap[:, 0:256]))
            for mt in range(2):
                b2 = ps2()
                S.op("pe", [tMemT] + tWv, b2.toks, lambda: [nc.tensor.matmul(
                    b2.ap[:, hf * 512:(hf + 1) * 512], memT[:, k, mt * 128:(mt + 1) * 128], wv[:, k, hf * 512:(hf + 1) * 512],
                    start=(k == 0), stop=(k == 7)) for hf in range(2) for k in range(8)])
                S.op("act", b2.toks, [tVx], lambda: nc.scalar.copy(out=Vx[:, mt, :], in_=b2.ap))
            wo = wk
            for k in range(8):
                S.dma("sp", out=wo[:, k, :], in_=DWB["xwo"][k * 128:(k + 1) * 128, :], reads=[tDW["xwo"][k]], writes=[tWk[k]])
            XSC = 256.0 ** -0.5
            lg = small[:, 64:100]
            gmax, ngmax, gsum, ggate = small[:, 100:101], small[:, 101:102], small[:, 102:103], small[:, 103:104]
            gone, ge, pen = small[:, 104:108], small[:, 108:112], small[:, 112:116]
            me, one1, me2, one2 = small[:, 116:148], small[:, 148:180], small[:, 180:212], small[:, 212:244]
            m1, m2, d21, e21, g1, g2 = (small[:, 244 + i:245 + i] for i in range(6))
            QxTg = [[K[:, 14336 + (i * 2 + dc) * 512:14336 + (i * 2 + dc + 1) * 512] for dc in range(2)] for i in range(2)]
            PTxg = [[K[:, 10240 + (i * 2 + mt) * 512:10240 + (i * 2 + mt + 1) * 512] for mt in range(2)] for i in range(2)]
            tQxg = [[T(), T()], [T(), T()]]
            tPxg = [[T(), T()], [T(), T()]]
            tRdxg = [T(), T()]
            units = [(tg, h) for tg in range(NG) for h in range(4)]

            def xa_A(i):
                tg, h = units[i]
                cols = slice(tg * 512, (tg + 1) * 512)
                for dc in range(2):
                    c = h * 2 + dc
                    bq_ = ps1()
                    S.op("pe", tHT[tg * 4:tg * 4 + 4] + tWq, bq_.toks, lambda: [nc.tensor.matmul(
                        bq_.ap, wq[:, k, c * 128:(c + 1) * 128], actT[:, k, cols], start=(k == 0), stop=(k == 7)) for k in range(8)])
                    S.op("act", bq_.toks, [tQxg[i % 2][dc]], lambda: nc.scalar.copy(out=QxTg[i % 2][dc], in_=bq_.ap))

            def xa_B(i):
                tg, h = units[i]
                for mt in range(2):
                    bs_ = ps1()
                    S.op("pe", tQxg[i % 2] + [tKx, tMem], bs_.toks, lambda: [nc.tensor.matmul(
                        bs_.ap, KxT[:, h * 2 + dc, mt * 128:(mt + 1) * 128], QxTg[i % 2][dc], start=(dc == 0), stop=(dc == 1)) for dc in range(2)])
                    S.op("act", bs_.toks, [tPxg[i % 2][mt]], lambda: nc.scalar.activation(out=PTxg[i % 2][mt], in_=bs_.ap, func=AF.Exp, scale=XSC))

            def xa_C(i):
                tg, h = units[i]
                ptx = PTxg[i % 2]
                rd_ = rdx[:, (i % 2) * 512:(i % 2 + 1) * 512]
                bd_ = ps1()
                S.op("pe", tPxg[i % 2] + [tC], bd_.toks, lambda: [nc.tensor.matmul(
                    bd_.ap, ones_bf[:], ptx[mt], start=(mt == 0), stop=(mt == 1)) for mt in range(2)])
                S.op("dve", bd_.toks, [tRdxg[i % 2]], lambda: nc.vector.reciprocal(out=rd_, in_=bd_.ap))
                for dc in range(2):
                    c = h * 2 + dc
                    bo_ = ps1()
                    S.op("pe", tPxg[i % 2] + [tVx], bo_.toks, lambda: [nc.tensor.matmul(
                        bo_.ap, Vx[:, mt, c * 128:(c + 1) * 128], ptx[mt], start=(mt == 0), stop=(mt == 1)) for mt in range(2)])
                    S.op("dve", bo_.toks + [tRdxg[i % 2]], [tXo], lambda: nc.vector.tensor_tensor(out=xo[:, c, :], in0=bo_.ap, in1=rd_, op=ALU.mult))

            def xa_mm(tt):
                j = tt % 4
                bm = ps2()
                S.op("pe", [tXo] + tWk, bm.toks, lambda: [nc.tensor.matmul(
                    bm.ap[:, hf * 512:(hf + 1) * 512], xo[:, c, j * 128:(j + 1) * 128], wo[:, c, hf * 512:(hf + 1) * 512],
                    start=(c == 0), stop=(c == 7)) for hf in range(2) for c in range(8)])
                return bm

            V_ = nc.vector
            AXX_ = mybir.AxisListType.X
            tRtS = [T(), T()]

            def xa_tail(tt, bm, si):
                rb0 = 64 + si * 256
                lg = small[:, rb0:rb0 + 36]
                gmax, ngmax, gsum, ggate = (small[:, rb0 + 36 + i:rb0 + 37 + i] for i in range(4))
                gone, ge, pen = small[:, rb0 + 40:rb0 + 44], small[:, rb0 + 44:rb0 + 48], small[:, rb0 + 48:rb0 + 52]
                me, me2 = small[:, rb0 + 52:rb0 + 84], small[:, rb0 + 84:rb0 + 116]
                m1, m2, d21, e21, g1 = (small[:, rb0 + 116 + i:rb0 + 117 + i] for i in range(5))
                tRt = tRtS[si]
                rt = lambda f: S.op("dve", [tRt], [tRt], f)
                hap = hview[:, tt, :]
                tHt = tH[tt]
                gt = s * NT + tt
                S.op("dve", bm.toks, [tHt], lambda: nc.vector.scalar_tensor_tensor(
                    out=hap, in0=hap, scalar=ALPHA, in1=bm.ap, op0=ALU.mult, op1=ALU.add))
                j_ = lnstate["i"] % 4
                lnstate["i"] += 1
                tS = tLNS[j_]
                base = j_ * 16
                st, mv = lnst[:, base:base + 12], lnst[:, base + 12:base + 14]
                rs, nm = lnst[:, base + 14:base + 15], lnst[:, base + 15:base + 16]
                S.op("dve", [tHt], [tS], lambda: nc.vector.bn_stats(out=st[:, 0:6], in_=hap[:, 0:512]))
                S.op("dve", [tHt], [tS], lambda: nc.vector.bn_stats(out=st[:, 6:12], in_=hap[:, 512:1024]))
                S.op("dve", [], [tS], lambda: nc.vector.bn_aggr(out=mv, in_=st))
                yield
                rstd_from(rs, mv[:, 1:2], 1.0, [tS], [tS])
                yield
                S.op("dve", [tS], [tS], lambda: nc.vector.scalar_tensor_tensor(
                    out=nm, in0=mv[:, 0:1], scalar=-1.0, in1=rs, op0=ALU.mult, op1=ALU.mult))
                yield
                S.op("act", [tS], [tHt], lambda: nc.scalar.activation(out=hap, in_=hap, func=AF.Identity, scale=rs, bias=nm))
                yield
                S.op("dve", [tLN], [tHt], lambda: nc.vector.tensor_tensor(out=hap, in0=hap, in1=lng[:], op=ALU.mult))
                yield
                S.op("dve", [tLN], [tHt], lambda: nc.vector.tensor_tensor(out=hap, in0=hap, in1=lnb[:], op=ALU.add))
                yield
                S.op("act", [tHt], [tHb[tt % 2]], lambda: nc.scalar.copy(out=hbK[tt % 2], in_=hap))
                yield
                transpose_to(hbK[tt % 2], tHb[tt % 2], actT, tt, tHT[tt])
                yield
                bl = ps1()
                S.op("pe", [tHT[tt], tC], bl.toks, lambda: [nc.tensor.matmul(
                    bl.ap[:, 0:36], actT[:, k, tt * 128:(tt + 1) * 128], v3(rw[:], 36)[:, k, :], start=(k == 0), stop=(k == 7)) for k in range(8)])
                yield
                S.op("dve", bl.toks + [tC], [tRt], lambda: V_.tensor_tensor(out=lg, in0=bl.ap[:, 0:36], in1=rb[:], op=ALU.add))
                rt(lambda: V_.reduce_max(out=gmax, in_=lg[:, 0:4], axis=AXX_))
                rt(lambda: V_.tensor_scalar(out=gone, in0=lg[:, 0:4], scalar1=gmax, scalar2=None, op0=ALU.is_equal))
                rt(lambda: V_.tensor_scalar_mul(out=ngmax, in0=gmax, scalar1=-1.0))
                yield
                S.op("act", [tRt], [tRt], lambda: nc.scalar.activation(out=ge, in_=lg[:, 0:4], func=AF.Exp, bias=ngmax))
                yield
                rt(lambda: V_.reduce_sum(out=gsum, in_=ge, axis=AXX_))
                rt(lambda: V_.reciprocal(out=ggate, in_=gsum))
                rt(lambda: V_.tensor_scalar(out=pen, in0=gone, scalar1=-1.0, scalar2=1e9, op0=ALU.add, op1=ALU.mult))
                rt(lambda: V_.tensor_tensor(out=v3(me, 8), in0=v3(lg[:, 4:36], 8), in1=pen.unsqueeze(2).to_broadcast([128, 4, 8]), op=ALU.add))
                rt(lambda: V_.reduce_max(out=m1, in_=me, axis=AXX_))
                o1 = ONE[:, (gt * 2) * 32:(gt * 2 + 1) * 32]
                o2 = ONE[:, (gt * 2 + 1) * 32:(gt * 2 + 2) * 32]
                S.op("dve", [tRt], [tRt, tRk], lambda: V_.tensor_scalar(out=o1, in0=me, scalar1=m1, scalar2=None, op0=ALU.is_equal))
                rt(lambda: V_.scalar_tensor_tensor(out=me2, in0=o1, scalar=-1e9, in1=me, op0=ALU.mult, op1=ALU.add))
                rt(lambda: V_.reduce_max(out=m2, in_=me2, axis=AXX_))
                S.op("dve", [tRt], [tRt, tRk], lambda: V_.tensor_scalar(out=o2, in0=me2, scalar1=m2, scalar2=None, op0=ALU.is_equal))
                rt(lambda: V_.tensor_tensor(out=d21, in0=m2, in1=m1, op=ALU.subtract))
                yield
                S.op("act", [tRt], [tRt], lambda: nc.scalar.activation(out=e21, in_=d21, func=AF.Exp))
                yield
                rt(lambda: V_.tensor_scalar_add(out=g1, in0=e21, scalar1=1.0))
                rt(lambda: V_.reciprocal(out=g1, in_=g1))
                S.op("dve", [tRt], [tRt, tRk], lambda: V_.tensor_tensor(out=G12[:, gt * 2:gt * 2 + 1], in0=g1, in1=ggate, op=ALU.mult))
                S.op("dve", [tRt], [tRt, tRk], lambda: V_.tensor_tensor(out=G12[:, gt * 2 + 1:gt * 2 + 2], in0=G12[:, gt * 2:gt * 2 + 1], in1=e21, op=ALU.mult))
                bR = ps1()
                S.op("pe", [tRk, tC], bR.toks, lambda: [
                    nc.tensor.matmul(bR.ap[:, 0:64], su_bf[:], ONE[:, gt * 64:(gt + 1) * 64], start=True, stop=True),
                    nc.tensor.matmul(bR.ap[:, 64:128], ones_bf[:], ONE[:, gt * 64:(gt + 1) * 64], start=True, stop=True)])
                yield
                ta, tb = me, me2
                S.op("dve", bR.toks + [tRk, tRt], [tRt], lambda: V_.tensor_tensor(out=ta, in0=bR.ap[:, 0:32], in1=Crun[:], op=ALU.add))
                rt(lambda: V_.tensor_tensor(out=tb, in0=ta, in1=o1, op=ALU.mult))
                S.op("dve", [tRt], [tRt, tRk], lambda: V_.reduce_sum(out=RK[:, gt * 2:gt * 2 + 1], in_=tb, axis=AXX_))
                S.op("dve", bR.toks + [tRk, tRt], [tRt], lambda: V_.tensor_tensor(out=ta, in0=bR.ap[:, 32:64], in1=Crun[:], op=ALU.add))
                S.op("dve", bR.toks + [tRt], [tRt], lambda: V_.tensor_tensor(out=ta, in0=bR.ap[:, 64:96], in1=ta, op=ALU.add))
                rt(lambda: V_.tensor_tensor(out=tb, in0=ta, in1=o2, op=ALU.mult))
                S.op("dve", [tRt], [tRt, tRk], lambda: V_.reduce_sum(out=RK[:, gt * 2 + 1:gt * 2 + 2], in_=tb, axis=AXX_))
                S.op("dve", bR.toks + [tRt], [tRk], lambda: V_.tensor_tensor(out=Crun[:], in0=bR.ap[:, 64:96], in1=Crun[:], op=ALU.add))
                S.op("dve", bR.toks + [tRt], [tRk], lambda: V_.tensor_tensor(out=Crun[:], in0=bR.ap[:, 96:128], in1=Crun[:], op=ALU.add))
                S.dma("sp", out=XB[gt * 128:(gt + 1) * 128, :], in_=hbK[tt % 2], reads=[tHb[tt % 2]])
                S.dma("sp", out=H2D[gt * 128:(gt + 1) * 128, :], in_=hap, reads=[tHt])
                yield

            xa_A(0)
            for ui in range(len(units)):
                xa_B(ui)
                if ui + 1 < len(units):
                    xa_A(ui + 1)
                xa_C(ui)
                tg, h = units[ui]
                if h != 3:
                    continue
                for pr in ((0, 1), (2, 3)):
                    gens = []
                    for si, j in enumerate(pr):
                        tt = tg * 4 + j
                        gens.append(xa_tail(tt, xa_mm(tt), si))
                    alive_ = [True, True]
                    while any(alive_):
                        for gi in range(2):
                            if alive_[gi] and next(gens[gi], "done") == "done":
                                alive_[gi] = False
            if stop_after == "P7":
                dump("h2", hview, tH)
                break
            S.barrier(engines=("act", "dve", "pool", "sp"))
        if stop_after is None:
            S.barrier(full=True)
            V_ = nc.vector
            AXX = mybir.AxisListType.X
            tM = T()
            padc, pA, pB, basev = small[:, 116:148], small[:, 148:180], small[:, 180:212], small[:, 212:244]
            cmpb = H[:, 0:1024]
            mo = lambda f, extra=(): S.op("dve", [tM, tRk, tC] + list(extra), [tM], f)
            mo(lambda: V_.tensor_tensor(out=v3(cmpb, 32), in0=Crun[:].unsqueeze(2).to_broadcast([128, 32, 32]),
                                        in1=thr[:].unsqueeze(1).to_broadcast([128, 32, 32]), op=ALU.is_gt))
            mo(lambda: V_.reduce_sum(out=padc, in_=v3(cmpb, 32), axis=AXX))
            mo(lambda: V_.tensor_scalar_mul(out=padc, in0=padc, scalar1=float(TSZ)))
            cur, nxt = padc, pA
            for sft in (1, 2, 4, 8, 16):
                mo(lambda: V_.tensor_copy(out=nxt[:, 0:sft], in_=cur[:, 0:sft]))
                mo(lambda: V_.tensor_tensor(out=nxt[:, sft:32], in0=cur[:, sft:32], in1=cur[:, 0:32 - sft], op=ALU.add))
                cur, nxt = nxt, (pB if nxt is pA else pA)
            endv = cur
            mo(lambda: V_.tensor_tensor(out=basev, in0=endv, in1=padc, op=ALU.subtract))
            cmpt = H[:, 2048:2048 + NTI * 32]
            tef = mo_f[:, 0:NTI]
            widx = mo_i[:, 0:NTI]
            mo(lambda: V_.tensor_tensor(out=v3(cmpt, 32), in0=endv.unsqueeze(1).to_broadcast([128, NTI, 32]),
                                        in1=tstart[:, 0:NTI].unsqueeze(2).to_broadcast([128, NTI, 32]), op=ALU.is_le))
            mo(lambda: V_.reduce_sum(out=tef, in_=v3(cmpt, 32), axis=AXX))
            mo(lambda: V_.tensor_scalar(out=tef, in0=tef, scalar1=128.0, scalar2=pcol[:, 0:1], op0=ALU.mult, op1=ALU.add))
            mo(lambda: V_.tensor_copy(out=widx, in_=tef))
            tmp3 = H[:, 4096:4096 + GT * 64]
            posf = mo_f[:, 64:64 + GT * 2]
            posI = mo_i[:, 64:64 + GT * 2]
            mo(lambda: V_.tensor_tensor(out=v3(tmp3, 32), in0=v3(ONE[:], 32), in1=basev.unsqueeze(1).to_broadcast([128, GT * 2, 32]), op=ALU.mult))
            mo(lambda: V_.reduce_sum(out=posf, in_=v3(tmp3, 32), axis=AXX))
            mo(lambda: V_.tensor_tensor(out=posf, in0=posf, in1=RK[:], op=ALU.add))
            mo(lambda: V_.tensor_copy(out=posI, in_=posf))
            NDB = 8
            xbt = [A[:, i * 1024:(i + 1) * 1024] for i in range(NDB)]
            tXb = [T() for _ in range(NDB)]
            for gt in range(min(NDB, GT)):
                S.dma("sp", out=xbt[gt], in_=XB[gt * 128:(gt + 1) * 128, :], writes=[tXb[gt]])
            for gt in range(GT):
                for k in range(2):
                    S.dma("pool", out=XS[:, :], in_=xbt[gt % NDB], out_off=posI[:, gt * 2 + k:gt * 2 + k + 1], bound=NTI * TSZ - 1,
                          reads=[tXb[gt % NDB], tM])
                if gt + NDB < GT:
                    S.dma("sp", out=xbt[gt % NDB], in_=XB[(gt + NDB) * 128:(gt + NDB + 1) * 128, :], writes=[tXb[gt % NDB]])
            S.barrier()
            load_ln(2)
            xsb = [v3(A[:, 2048 + i * 4096:2048 + i * 4096 + TB * 1024], 1024) for i in range(2)]
            xst = [v3(K[:, 4096 + i * 4096:4096 + i * 4096 + 8 * TSZ], TSZ) for i in range(2)]
            Ssb = [Kf(0, 2 * TSZ), Kf(1024, 1024 + 2 * TSZ)]
            HD = [v3(K[:, 2048:2048 + 2 * TSZ], TSZ), v3(K[:, 3072:3072 + 2 * TSZ], TSZ)]
            Ysb = [K[:, 12288:13312], K[:, 13312:14336]]
            tXsb, tXst, tSs, tHD, tY = [T(), T()], [T(), T()], [T(), T()], [T(), T()], [T(), T()]
            NWB = 3
            Eb = [W[:, i * 6144:(i + 1) * 6144] for i in range(NWB)]
            tEw = [[T(), T(), T()] for _ in range(NWB)]

            def moe_prefetch_w(ti):
                eb = Eb[ti % NWB]
                for part, src in enumerate((WGB, WUB, WDB)):
                    S.dma("pool", out=eb[:, part * 2048:(part + 1) * 2048], in_=src[:, :], in_off=widx[:, ti:ti + 1], bound=NEXP * 128 - 1,
                          reads=[tM], writes=[tEw[ti % NWB][part]])

            def moe_prefetch_x(ti):
                S.dma("sp", out=xsb[ti % 2], in_=XS[ti * TSZ:(ti + 1) * TSZ, :].rearrange("(j p) d -> p j d", p=128), writes=[tXsb[ti % 2]])

            def moe_TR(ti):
                xs_, txs = xsb[ti % 2], tXsb[ti % 2]
                xt_, txt = xst[ti % 2], tXst[ti % 2]
                for j in range(TB):
                    pb = ps1()
                    S.op("pe", [txs, tC], pb.toks, lambda: [nc.tensor.transpose(
                        pb.bf[:, k * 128:(k + 1) * 128], xs_[:, j, k * 128:(k + 1) * 128], ident_bf[:]) for k in range(8)])
                    if j % 2 == 0:
                        S.op("act", pb.toks, [txt], lambda: nc.scalar.copy(out=xt_[:, :, j * 128:(j + 1) * 128], in_=v3(pb.bf[:, 0:1024], 128)))
                    else:
                        S.op("dve", pb.toks, [txt], lambda: nc.vector.tensor_copy(out=xt_[:, :, j * 128:(j + 1) * 128], in_=v3(pb.bf[:, 0:1024], 128)))

            def moe_GU(ti):
                eb = Eb[ti % NWB]
                tE3 = tEw[ti % NWB]
                wg = v3(eb[:, 0:2048], 256)
                wu = v3(eb[:, 2048:4096], 256)
                xt_, txt = xst[ti % 2], tXst[ti % 2]
                hd, thd = HD[ti % 2], tHD[ti % 2]
                for fc in range(2):
                    bg, bu = ps1(), ps1()
                    S.op("pe", [txt, tE3[0]], bg.toks, lambda: [nc.tensor.matmul(
                        bg.ap[:, 0:TSZ], wg[:, k, fc * 128:(fc + 1) * 128], xt_[:, k, :], start=(k == 0), stop=(k == 7)) for k in range(8)])
                    S.op("pe", [txt, tE3[1]], bu.toks, lambda: [nc.tensor.matmul(
                        bu.ap[:, 0:TSZ], wu[:, k, fc * 128:(fc + 1) * 128], xt_[:, k, :], start=(k == 0), stop=(k == 7)) for k in range(8)])
                    S.op("act", bg.toks, [tSs[fc]], lambda: nc.scalar.activation(out=Ssb[fc], in_=bg.ap[:, 0:TSZ], func=AF.Silu))
                    S.op("dve", bu.toks + [tSs[fc]], [thd], lambda: nc.vector.tensor_tensor(out=hd[:, fc, :], in0=Ssb[fc], in1=bu.ap[:, 0:TSZ], op=ALU.mult))

            def moe_D(ti):
                eb = Eb[ti % NWB]
                tE3 = tEw[ti % NWB]
                wd = v3(eb[:, 4096:6144], 1024)
                hd, thd = HD[ti % 2], tHD[ti % 2]
                for j in range(TB):
                    bd2 = ps2()
                    S.op("pe", [thd, tE3[2]], bd2.toks, lambda: [nc.tensor.matmul(
                        bd2.ap[:, hf * 512:(hf + 1) * 512], hd[:, fc, j * 128:(j + 1) * 128], wd[:, fc, hf * 512:(hf + 1) * 512],
                        start=(fc == 0), stop=(fc == 1)) for hf in range(2) for fc in range(2)])
                    yb, ty = Ysb[moe_state["yi"] % 2], tY[moe_state["yi"] % 2]
                    moe_state["yi"] += 1
                    S.op("act", bd2.toks, [ty], lambda: nc.scalar.copy(out=yb, in_=bd2.ap))
                    S.dma("sp", out=YS[ti * TSZ + j * 128:ti * TSZ + (j + 1) * 128, :], in_=yb, reads=[ty])

            moe_state = {"yi": 0}
            moe_prefetch_w(0)
            moe_prefetch_x(0)
            if NTI > 1:
                moe_prefetch_w(1)
                moe_prefetch_x(1)
            moe_TR(0)
            moe_GU(0)
            for ti in range(NTI):
                if ti + 2 < NTI:
                    moe_prefetch_w(ti + 2)
                if ti + 1 < NTI:
                    moe_TR(ti + 1)
                if ti + 2 < NTI:
                    moe_prefetch_x(ti + 2)
                moe_D(ti)
                if ti + 1 < NTI:
                    moe_GU(ti + 1)
            S.barrier()
            NYB = 4
            ybuf = [[H[:, i * 3072:i * 3072 + 512].bitcast(BF16), H[:, i * 3072 + 512:i * 3072 + 1024].bitcast(BF16),
                     H[:, i * 3072 + 1024:i * 3072 + 2048], H[:, i * 3072 + 2048:i * 3072 + 3072]] for i in range(NYB)]
            tYb = [[T(), T(), T(), T()] for _ in range(NYB)]

            def comb_prefetch(gt):
                y1, y2, ytmp, hh = ybuf[gt % NYB]
                t1_, t2_, tt_, th = tYb[gt % NYB]
                S.dma("pool", out=y1, in_=YS[:, :], in_off=posI[:, gt * 2:gt * 2 + 1], bound=NTI * TSZ - 1, reads=[tM], writes=[t1_])
                S.dma("pool", out=y2, in_=YS[:, :], in_off=posI[:, gt * 2 + 1:gt * 2 + 2], bound=NTI * TSZ - 1, reads=[tM], writes=[t2_])
                S.dma("sp", out=hh, in_=H2D[gt * 128:(gt + 1) * 128, :], writes=[th])

            def comb_gen(gt):
                y1, y2, ytmp, hh = ybuf[gt % NYB]
                t1_, t2_, tt_, th = tYb[gt % NYB]
                S.op("act", [t1_, tRk], [tt_], lambda: nc.scalar.activation(out=ytmp, in_=y1, func=AF.Copy, scale=G12[:, gt * 2:gt * 2 + 1]))
                yield
                S.op("dve", [t2_, tRk, tt_], [tt_], lambda: V_.scalar_tensor_tensor(
                    out=ytmp, in0=y2, scalar=G12[:, gt * 2 + 1:gt * 2 + 2], in1=ytmp, op0=ALU.mult, op1=ALU.add))
                S.op("dve", [tt_], [th], lambda: V_.scalar_tensor_tensor(out=hh, in0=hh, scalar=ALPHA, in1=ytmp, op0=ALU.mult, op1=ALU.add))
                j_ = lnstate["i"] % 4
                lnstate["i"] += 1
                tS = tLNS[j_]
                base = j_ * 16
                st, mv = lnst[:, base:base + 12], lnst[:, base + 12:base + 14]
                rs, nm = lnst[:, base + 14:base + 15], lnst[:, base + 15:base + 16]
                S.op("dve", [th], [tS], lambda: nc.vector.bn_stats(out=st[:, 0:6], in_=hh[:, 0:512]))
                S.op("dve", [th], [tS], lambda: nc.vector.bn_stats(out=st[:, 6:12], in_=hh[:, 512:1024]))
                S.op("dve", [], [tS], lambda: nc.vector.bn_aggr(out=mv, in_=st))
                yield
                rstd_from(rs, mv[:, 1:2], 1.0, [tS], [tS])
                yield
                S.op("dve", [tS], [tS], lambda: nc.vector.scalar_tensor_tensor(
                    out=nm, in0=mv[:, 0:1], scalar=-1.0, in1=rs, op0=ALU.mult, op1=ALU.mult))
                yield
                S.op("act", [tS], [th], lambda: nc.scalar.activation(out=hh, in_=hh, func=AF.Identity, scale=rs, bias=nm))
                yield
                S.op("dve", [tLN], [th], lambda: nc.vector.tensor_tensor(out=hh, in0=hh, in1=lng[:], op=ALU.mult))
                yield
                S.op("dve", [tLN], [th], lambda: nc.vector.tensor_tensor(out=hh, in0=hh, in1=lnb[:], op=ALU.add))
                sq_, tq_ = gt // NT, gt % NT
                S.dma("sp", out=out_d[sq_, tq_ * 128:(tq_ + 1) * 128, :], in_=hh, reads=[th])
                yield

            comb_prefetch(0)
            comb_prefetch(1)
            for g0 in range(0, GT, 2):
                for gn in (g0 + 2, g0 + 3):
                    if gn < GT:
                        comb_prefetch(gn)
                gens = [comb_gen(g0), comb_gen(g0 + 1)]
                alive_ = [True, True]
                while any(alive_):
                    for gi in range(2):
                        if alive_[gi] and next(gens[gi], "done") == "done":
                            alive_[gi] = False
        S.barrier(engines=("sp",), full=True)
    return nc


def _rope_tables():
    pos = np.arange(SEQ, dtype=np.float32)
    inv_freq = (np.float32(10000.0) ** (-(np.arange(0, 32, 2, dtype=np.float32)) / np.float32(32))).astype(np.float32)
    ang = (pos[:, None] * inv_freq[None, :]).astype(np.float32)
    cos = np.cos(ang).astype(np.float32)
    sin = np.sin(ang).astype(np.float32)
    cc = np.zeros((128, SEQ), np.float32)
    ss = np.zeros((128, SEQ), np.float32)
    cc[0:64] = 1.0
    cc[64:80] = cos.T
    cc[80:96] = cos.T
    ss[64:80] = -sin.T
    ss[80:96] = sin.T
    return cc, ss


def prep_shared(inp):
    f = np.float32
    g = lambda k: np.asarray(inp[k], dtype=f)[0]
    w_in = g("w_in")
    wkr = np.zeros((D, 96), f)
    wkr[:, 64:80] = w_in[:, 400:416]
    wkr[:, 80:96] = w_in[:, 384:400]
    wq = g("w_q_up")
    perm = np.arange(768)
    for h in range(8):
        for j in range(32):
            perm[h * 96 + 64 + j] = h * 96 + 64 + (j + 16) % 32
    wq_sw = wq[:, perm]
    convw = g("ssd_conv_w").reshape(4, 8, 128).transpose(2, 1, 0).reshape(128, 32)
    convb = g("ssd_conv_b").reshape(8, 128).T
    dtb = np.broadcast_to(np.tile(g("ssd_dt_bias"), 16)[None, :], (128, 128))
    alog = np.broadcast_to(g("ssd_a_log")[None, :], (128, 8))
    sd = g("ssd_d")
    dcol = np.stack([sd[pair * 2 + (np.arange(128) // 64)] for pair in range(4)], axis=1)
    ncol = g("ssd_norm").reshape(4, 128).T
    lnp = np.stack([np.broadcast_to(g(k)[None, :], (128, D)) for k in ("ln1_g", "ln1_b", "ln2_g", "ln2_b", "ln3_g", "ln3_b")])
    rw = np.concatenate([g("router_group_w"), g("router_expert_w")], axis=1)
    rb = np.broadcast_to(np.concatenate([g("router_group_b"), g("router_expert_b")])[None, :], (128, 36))
    tri = np.triu(np.ones((128, 128), f))
    nm = np.where(np.arange(128)[None, :] >= np.arange(128)[:, None], 0.0, -30000.0).astype(f)
    cc, ss = _rope_tables()
    su = np.triu(np.ones((128, 128), f), k=1)
    thr = np.broadcast_to((np.arange(32, dtype=f) * 384.0)[None, :], (128, 32))
    tstart = np.broadcast_to((np.arange(64, dtype=f) * 384.0)[None, :], (128, 64))
    pcol = np.arange(128, dtype=f).reshape(128, 1)
    sh = {
        "su": su, "thr": thr, "tstart": tstart, "pcol": pcol,
        "w_in": w_in, "wkr_sw": wkr, "wq": wq, "wq_sw": wq_sw, "qn": g("mla_q_norm").reshape(2, 128).T,
        "wkv": g("w_kv_up"), "kvn": g("mla_kv_norm").reshape(128, 1), "convw": convw, "convb": convb,
        "dtb": dtb, "alog": alog, "dcol": dcol, "ncol": ncol, "wout": g("w_out"), "xwq": g("xa_wq"),
        "xwk": g("xa_wk"), "xwv": g("xa_wv"), "xwo": g("xa_wo"), "lnp": lnp, "rw": rw, "rb": rb,
        "wg": g("expert_w_gate"), "wu": g("expert_w_up"), "wd": g("expert_w_down"),
        "ident": np.eye(128, dtype=f), "tri": tri, "negmask": np.tile(nm, (1, 4)), "cc": cc, "ss": ss,
    }
    return {k: np.ascontiguousarray(v, dtype=f) for k, v in sh.items()}


def kernel(**inputs):
    sh = prep_shared(inputs)
    x = np.asarray(inputs["x"], dtype=np.float32)
    mem = np.asarray(inputs["mem"], dtype=np.float32)
    nc = build(n_seq=2)
    in_maps = []
    for c in range(N_CORES):
        m = dict(sh)
        m["x"] = np.ascontiguousarray(x[2 * c:2 * c + 2])
        m["mem"] = np.ascontiguousarray(mem[2 * c:2 * c + 2])
        in_maps.append(m)
    res = run_bass_kernel_spmd(nc, in_maps, core_ids=list(range(N_CORES)))
    return np.concatenate([r["out"] for r in res.results], axis=0)
```
